# Optimizing a Trainium2 kernel written in Bass

```python
import math
import jax
import jax.numpy as jnp
from jax import lax
import numpy as np

D_MODEL = 1024
BATCH = 16
SEQ = 2048
DEPTH = 1

MIX_WIDTH = 2 * D_MODEL
SSD_WIDTH = MIX_WIDTH // 2
SSD_HEAD_DIM = 64
SSD_HEADS = SSD_WIDTH // SSD_HEAD_DIM
SSD_GROUPS = 4
SSD_HPG = SSD_HEADS // SSD_GROUPS
SSD_STATE = 128
SSD_CONV = 4
SSD_CHUNK = 128
CONV_CH = SSD_WIDTH + 2 * SSD_GROUPS * SSD_STATE
S5_WIDTH = MIX_WIDTH - SSD_WIDTH
S5_GROUP_CH = 16
S5_GROUPS = S5_WIDTH // S5_GROUP_CH
S5_STATE = 64
DT_MIN = 1e-3
DT_MAX = 1e-1
IN_WIDTH = SSD_WIDTH + CONV_CH + SSD_HEADS + S5_WIDTH
PEER_HEADS = 8
PEER_NKEYS = 128
PEER_EXPERTS = PEER_NKEYS * PEER_NKEYS
PEER_TOPK = 16
PEER_KEY_DIM = 256
PEER_HALF = PEER_KEY_DIM // 2
PEER_TOK_BLOCK = 128
EPS = 1e-6

kernel_name = 'hybrid_ssd_s5_peer_adaln_block'


def rms_norm(x, g):
    xf = x.astype(jnp.float32)
    y = xf * lax.rsqrt(jnp.mean(xf * xf, axis=-1, keepdims=True) + EPS)
    return (y * g.astype(jnp.float32)).astype(x.dtype)


def ssd_mixer(z, xbc, dt_raw, conv_w, conv_b, dt_bias, a_log, d_ssd, norm_ssd_g):
    b, s, _ = xbc.shape
    nc = s // SSD_CHUNK
    f32 = jnp.float32
    xpad = jnp.pad(xbc, ((0, 0), (SSD_CONV - 1, 0), (0, 0)))
    conv = conv_b
    for k in range(SSD_CONV):
        conv = conv + xpad[:, k:k + s, :] * conv_w[k]
    xbc = jax.nn.silu(conv.astype(f32))
    xs, bm, cm = jnp.split(xbc, [SSD_WIDTH, SSD_WIDTH + SSD_GROUPS * SSD_STATE], axis=-1)
    dt = jax.nn.softplus(dt_raw.astype(f32) + dt_bias.astype(f32))
    a = -jnp.exp(a_log.astype(f32))
    xc = xs.reshape(b, nc, SSD_CHUNK, SSD_GROUPS, SSD_HPG, SSD_HEAD_DIM)
    bc = bm.reshape(b, nc, SSD_CHUNK, SSD_GROUPS, SSD_STATE)
    cc = cm.reshape(b, nc, SSD_CHUNK, SSD_GROUPS, SSD_STATE)
    dtc = dt.reshape(b, nc, SSD_CHUNK, SSD_GROUPS, SSD_HPG)
    xdt = xc * dtc[..., None]
    da_cs = jnp.cumsum(dtc * a.reshape(SSD_GROUPS, SSD_HPG), axis=2)
    seg = jnp.moveaxis(da_cs, 2, -1)
    diff = seg[..., :, None] - seg[..., None, :]
    causal = jnp.tril(jnp.ones((SSD_CHUNK, SSD_CHUNK), dtype=bool))
    lmat = jnp.where(causal, jnp.exp(jnp.where(causal, diff, 0.0)), 0.0)
    cb = jnp.einsum('bclgn,bcsgn->bcgls', cc, bc)
    y_diag = jnp.einsum('bcgls,bcgrls,bcsgrp->bclgrp', cb, lmat, xdt)
    decay_states = jnp.exp(da_cs[:, :, -1:] - da_cs)
    states = jnp.einsum('bclgn,bclgr,bclgrp->bcgrpn', bc, decay_states, xdt)
    chunk_decay = jnp.exp(da_cs[:, :, -1])

    def step(carry, inp):
        st, dec = inp
        return carry * dec[..., None, None] + st, carry

    init = jnp.zeros((b, SSD_GROUPS, SSD_HPG, SSD_HEAD_DIM, SSD_STATE), f32)
    _, prev = lax.scan(step, init, (jnp.moveaxis(states, 1, 0), jnp.moveaxis(chunk_decay, 1, 0)))
    prev = jnp.moveaxis(prev, 0, 1)
    y_off = jnp.einsum('bclgn,bcgrpn,bclgr->bclgrp', cc, prev, jnp.exp(da_cs))
    y = y_diag + y_off + d_ssd.astype(f32).reshape(SSD_GROUPS, SSD_HPG, 1) * xc
    y = y.reshape(b, s, SSD_WIDTH) * jax.nn.silu(z.astype(f32))
    yg = y.reshape(b, s, SSD_GROUPS, SSD_WIDTH // SSD_GROUPS)
    yg = yg * lax.rsqrt(jnp.mean(yg * yg, axis=-1, keepdims=True) + EPS)
    return (yg.reshape(b, s, SSD_WIDTH) * norm_ssd_g.astype(f32)).astype(z.dtype)


def s5_mixer(u, a_re, a_im, log_dt, b_re, b_im, c_re, c_im, d_s5, glu_w, glu_b, norm_s5_g):
    b, s, _ = u.shape
    f32 = jnp.float32
    uf = u.astype(f32).reshape(b, s, S5_GROUPS, S5_GROUP_CH)
    lr = a_re.astype(f32)
    li = a_im.astype(f32)
    dt = jnp.exp(log_dt.astype(f32))[:, None]
    mag = jnp.exp(lr * dt)
    lb_re = mag * jnp.cos(li * dt)
    lb_im = mag * jnp.sin(li * dt)
    den = lr * lr + li * li
    coef_re = ((lb_re - 1.0) * lr + lb_im * li) / den
    coef_im = (lb_im * lr - (lb_re - 1.0) * li) / den
    br = b_re.astype(f32)
    bi = b_im.astype(f32)
    bb_re = coef_re[..., None] * br - coef_im[..., None] * bi
    bb_im = coef_re[..., None] * bi + coef_im[..., None] * br
    bu_re = jnp.einsum('bsgh,gph->sbgp', uf, bb_re)
    bu_im = jnp.einsum('bsgh,gph->sbgp', uf, bb_im)
    a_re_t = jnp.broadcast_to(lb_re, (s, 1, S5_GROUPS, S5_STATE))
    a_im_t = jnp.broadcast_to(lb_im, (s, 1, S5_GROUPS, S5_STATE))

    def combine(e1, e2):
        a1r, a1i, b1r, b1i = e1
        a2r, a2i, b2r, b2i = e2
        return (a2r * a1r - a2i * a1i,
                a2r * a1i + a2i * a1r,
                a2r * b1r - a2i * b1i + b2r,
                a2r * b1i + a2i * b1r + b2i)

    _, _, xr, xi = lax.associative_scan(combine, (a_re_t, a_im_t, bu_re, bu_im), axis=0)
    y = (jnp.einsum('sbgp,ghp->bsgh', xr, c_re.astype(f32))
         - jnp.einsum('sbgp,ghp->bsgh', xi, c_im.astype(f32))
         + d_s5.astype(f32) * uf)
    v = jax.nn.gelu(y, approximate=False)
    out = v * jax.nn.sigmoid(jnp.einsum('bsgh,ghk->bsgk', v, glu_w.astype(f32)) + glu_b.astype(f32))
    out = out.reshape(b, s, S5_WIDTH)
    return rms_norm(out, norm_s5_g).astype(u.dtype)


def peer_ffn(h, w_query, sub_keys, expert_u, expert_v):
    b, s, d = h.shape
    f32 = jnp.float32
    q = (h @ w_query).reshape(b, s, PEER_HEADS, 2, PEER_HALF)
    scores = jnp.einsum('bshcd,hckd->bshck', q, sub_keys).astype(f32)
    s1, i1 = lax.top_k(scores[..., 0, :], PEER_TOPK)
    s2, i2 = lax.top_k(scores[..., 1, :], PEER_TOPK)
    cand = (s1[..., :, None] + s2[..., None, :]).reshape(b, s, PEER_HEADS, PEER_TOPK * PEER_TOPK)
    cand_id = (i1[..., :, None] * PEER_NKEYS + i2[..., None, :]).reshape(b, s, PEER_HEADS, PEER_TOPK * PEER_TOPK)
    top_s, pos = lax.top_k(cand, PEER_TOPK)
    eid = jnp.take_along_axis(cand_id, pos, axis=-1)
    gates = jax.nn.softmax(top_s, axis=-1)
    n_blocks = (b * s) // PEER_TOK_BLOCK
    hb = h.reshape(n_blocks, PEER_TOK_BLOCK, d)
    eb = eid.reshape(n_blocks, PEER_TOK_BLOCK, PEER_HEADS * PEER_TOPK)
    gb = gates.reshape(n_blocks, PEER_TOK_BLOCK, PEER_HEADS * PEER_TOPK)

    def block(args):
        hx, ex, gx = args
        u = jnp.take(expert_u, ex, axis=0)
        v = jnp.take(expert_v, ex, axis=0)
        act = jax.nn.gelu(jnp.einsum('td,ted->te', hx, u).astype(f32), approximate=False)
        return jnp.einsum('te,ted->td', (gx * act).astype(h.dtype), v).astype(h.dtype)

    out = lax.map(block, (hb, eb, gb))
    return out.reshape(b, s, d)


def setup_inputs(seed: int = 0) -> dict:
    key = jax.random.key(seed)
    ks = jax.random.split(key, 32)
    L = DEPTH
    f32 = jnp.float32

    def nrm(k, shape, scale):
        return jax.random.normal(k, shape, f32) * scale

    x = nrm(ks[0], (BATCH, SEQ, D_MODEL), 1.0)
    c = nrm(ks[1], (BATCH, D_MODEL), 1.0)
    w_ada = nrm(ks[2], (L, D_MODEL, 6 * D_MODEL), D_MODEL ** -0.5)
    b_ada = nrm(ks[3], (L, 6 * D_MODEL), 0.02)
    norm1_g = 1.0 + nrm(ks[4], (L, D_MODEL), 0.02)
    w_in = nrm(ks[5], (L, D_MODEL, IN_WIDTH), D_MODEL ** -0.5)
    conv_w = nrm(ks[6], (L, SSD_CONV, CONV_CH), SSD_CONV ** -0.5)
    conv_b = nrm(ks[7], (L, CONV_CH), 0.02)
    dt0 = jnp.exp(jax.random.uniform(ks[8], (L, SSD_HEADS), f32, math.log(DT_MIN), math.log(DT_MAX)))
    dt_bias = dt0 + jnp.log(-jnp.expm1(-dt0))
    a_log = jnp.log(jax.random.uniform(ks[9], (L, SSD_HEADS), f32, 1.0, 16.0))
    d_ssd = 1.0 + nrm(ks[10], (L, SSD_HEADS), 0.02)
    norm_ssd_g = 1.0 + nrm(ks[11], (L, SSD_WIDTH), 0.02)
    s5_a_re = -0.5 + nrm(ks[12], (L, S5_GROUPS, S5_STATE), 0.01)
    s5_a_im = jnp.pi * jnp.arange(S5_STATE, dtype=f32) + nrm(ks[13], (L, S5_GROUPS, S5_STATE), 0.01)
    s5_log_dt = jax.random.uniform(ks[14], (L, S5_GROUPS), f32, math.log(DT_MIN), math.log(DT_MAX))
    s5_b_re = nrm(ks[15], (L, S5_GROUPS, S5_STATE, S5_GROUP_CH), (2 * S5_GROUP_CH) ** -0.5)
    s5_b_im = nrm(ks[16], (L, S5_GROUPS, S5_STATE, S5_GROUP_CH), (2 * S5_GROUP_CH) ** -0.5)
    s5_c_re = nrm(ks[17], (L, S5_GROUPS, S5_GROUP_CH, S5_STATE), (2 * S5_STATE) ** -0.5)
    s5_c_im = nrm(ks[18], (L, S5_GROUPS, S5_GROUP_CH, S5_STATE), (2 * S5_STATE) ** -0.5)
    s5_d = nrm(ks[19], (L, S5_GROUPS, S5_GROUP_CH), 1.0)
    glu_w = nrm(ks[20], (L, S5_GROUPS, S5_GROUP_CH, S5_GROUP_CH), S5_GROUP_CH ** -0.5)
    glu_b = nrm(ks[21], (L, S5_GROUPS, S5_GROUP_CH), 0.02)
    norm_s5_g = 1.0 + nrm(ks[22], (L, S5_WIDTH), 0.02)
    w_out = nrm(ks[23], (L, MIX_WIDTH, D_MODEL), MIX_WIDTH ** -0.5)
    norm2_g = 1.0 + nrm(ks[24], (L, D_MODEL), 0.02)
    w_query = nrm(ks[25], (L, D_MODEL, PEER_HEADS * PEER_KEY_DIM), D_MODEL ** -0.5)
    sub_keys = nrm(ks[26], (L, PEER_HEADS, 2, PEER_NKEYS, PEER_HALF), PEER_HALF ** -0.5)
    expert_u = nrm(ks[27], (L, PEER_EXPERTS, D_MODEL), D_MODEL ** -0.5)
    expert_v = nrm(ks[28], (L, PEER_EXPERTS, D_MODEL), PEER_HEADS ** -0.5)
    norm_f_g = 1.0 + nrm(ks[29], (D_MODEL,), 0.02)
    return {'x': x, 'c': c, 'w_ada': w_ada, 'b_ada': b_ada, 'norm1_g': norm1_g, 'w_in': w_in,
            'conv_w': conv_w, 'conv_b': conv_b, 'dt_bias': dt_bias, 'a_log': a_log, 'd_ssd': d_ssd,
            'norm_ssd_g': norm_ssd_g, 's5_a_re': s5_a_re, 's5_a_im': s5_a_im, 's5_log_dt': s5_log_dt,
            's5_b_re': s5_b_re, 's5_b_im': s5_b_im, 's5_c_re': s5_c_re, 's5_c_im': s5_c_im, 's5_d': s5_d,
            'glu_w': glu_w, 'glu_b': glu_b, 'norm_s5_g': norm_s5_g, 'w_out': w_out, 'norm2_g': norm2_g,
            'w_query': w_query, 'sub_keys': sub_keys, 'expert_u': expert_u, 'expert_v': expert_v,
            'norm_f_g': norm_f_g}


def reference(x, c, w_ada, b_ada, norm1_g, w_in, conv_w, conv_b, dt_bias, a_log, d_ssd, norm_ssd_g,
              s5_a_re, s5_a_im, s5_log_dt, s5_b_re, s5_b_im, s5_c_re, s5_c_im, s5_d, glu_w, glu_b,
              norm_s5_g, w_out, norm2_g, w_query, sub_keys, expert_u, expert_v, norm_f_g):
    for l in range(DEPTH):
        mod = jax.nn.silu(c) @ w_ada[l] + b_ada[l]
        shift1, scale1, gate1, shift2, scale2, gate2 = jnp.split(mod[:, None, :], 6, axis=-1)
        h = rms_norm(x, norm1_g[l]) * (1.0 + scale1) + shift1
        proj = h @ w_in[l]
        z, xbc, dt_raw, u = jnp.split(
            proj, [SSD_WIDTH, SSD_WIDTH + CONV_CH, SSD_WIDTH + CONV_CH + SSD_HEADS], axis=-1)
        y_ssd = ssd_mixer(z, xbc, dt_raw, conv_w[l], conv_b[l], dt_bias[l], a_log[l], d_ssd[l], norm_ssd_g[l])
        y_s5 = s5_mixer(u, s5_a_re[l], s5_a_im[l], s5_log_dt[l], s5_b_re[l], s5_b_im[l], s5_c_re[l],
                        s5_c_im[l], s5_d[l], glu_w[l], glu_b[l], norm_s5_g[l])
        mix = jnp.concatenate([y_ssd, y_s5], axis=-1) @ w_out[l]
        x = x + gate1 * mix
        h = rms_norm(x, norm2_g[l]) * (1.0 + scale2) + shift2
        x = x + gate2 * peer_ffn(h, w_query[l], sub_keys[l], expert_u[l], expert_v[l])
    return rms_norm(x, norm_f_g)
```

```python
import os
from contextlib import ExitStack

import numpy as np
import concourse.bass as bass
import concourse.mybir as mybir
from concourse.bass_utils import run_bass_kernel_spmd

F32 = mybir.dt.float32
BF16 = mybir.dt.bfloat16
I32 = mybir.dt.int32
U32 = mybir.dt.uint32
AF = mybir.ActivationFunctionType
ALU = mybir.AluOpType
AX = mybir.AxisListType

NCORES = 8
D = 1024
NB = 2
SEQ = 2048
T = NB * SEQ
NT = T // 128
INW = 4112
EPS = 1e-6
MAGIC = 12582912.0
TWO_PI = 6.283185307179586


class _Op:
    __slots__ = ("eng", "fn", "dom", "order", "waits", "target", "val", "is_dma")

    def __init__(self, eng, fn, dom, order, is_dma):
        self.eng, self.fn, self.dom, self.order, self.is_dma = eng, fn, dom, order, is_dma
        self.waits = []
        self.target = is_dma
        self.val = None


class Sched:
    CE = ("pe", "act", "dve", "pool")

    def __init__(self, nc, stack):
        self.nc = nc
        self.stack = stack
        self.q = {k: [] for k in ("pe", "act", "dve", "pool", "sp")}
        self.sem = {k: stack.enter_context(nc.semaphore("c_" + k)) for k in self.CE}
        self.cnt = {k: 0 for k in self.CE}
        self.order = {k: 0 for k in self.CE}
        self.dsem = {}
        self.dcnt = {}
        self.dslot = {}
        self.dfree = []
        self.dorder = {}
        self.waited = {k: {} for k in self.q}
        self.lastw = {}
        self.readers = {}
        self.lastop = {}
        self.ninst = 0

    def _need(self, eng, p, out):
        if self.waited[eng].get(p.dom, 0) >= p.order:
            return
        cur = out.get(p.dom)
        if cur is None or cur.order < p.order:
            out[p.dom] = p

    def _deps(self, eng, r, w, is_dma=False):
        need = {}
        for b in r:
            p = self.lastw.get(b)
            if p is not None and (is_dma or not (p.eng == eng and eng == "pe" and not p.is_dma)):
                self._need(eng, p, need)
        for b in w:
            p = self.lastw.get(b)
            if p is not None and (is_dma or p.is_dma or p.eng != eng or eng != "pe"):
                self._need(eng, p, need)
            for p in self.readers.get(b, ()):
                if is_dma or p.is_dma or p.eng != eng or eng != "pe":
                    self._need(eng, p, need)
        return need

    def _add(self, op, need, r, w):
        for dom, p in need.items():
            self.waited[op.eng][dom] = p.order
            p.target = True
            op.waits.append(p)
        self.q[op.eng].append(op)
        self.lastop[op.dom] = op
        for b in r:
            self.readers.setdefault(b, []).append(op)
        for b in w:
            self.lastw[b] = op
            self.readers[b] = []
        self.ninst += 1

    def op(self, eng, fn, r=(), w=()):
        need = self._deps(eng, r, w)
        self.order[eng] += 1
        o = _Op(eng, fn, eng, self.order[eng], False)
        self._add(o, need, r, w)

    def dma(self, eng, out, in_, r=(), w=(), stream="d", **kw):
        if eng == "pool":
            key = "swd_%d" % len(self.dsem)
            self.dsem[key] = self.stack.enter_context(self.nc.semaphore(key))
            self.dcnt[key] = 0
            self.dorder[key] = 0
            self.dslot["__swd__" + key] = key
            stream = "__swd__" + key
        if stream not in self.dslot:
            if self.dfree:
                self.dslot[stream] = self.dfree.pop()
            else:
                k = "dma_%d" % len(self.dsem)
                self.dsem[k] = self.stack.enter_context(self.nc.semaphore(k))
                self.dcnt[k] = 0
                self.dorder[k] = 0
                self.dslot[stream] = k
        key = self.dslot[stream]
        need = self._deps(eng, r, w, is_dma=True)
        self.dorder[key] += 1
        fn = lambda e, out=out, in_=in_, kw=kw: e.dma_start(out=out, in_=in_, **kw)
        o = _Op(eng, fn, key, self.dorder[key], True)
        self._add(o, need, r, w)

    def barrier(self):
        lasts = list(self.lastop.values())
        for eng in self.q:
            need = {}
            for p in lasts:
                self._need(eng, p, need)
            if need:
                o = _Op(eng, None, None, 0, False)
                for dom, p in need.items():
                    self.waited[eng][dom] = p.order
                    p.target = True
                    o.waits.append(p)
                self.q[eng].append(o)
        self.lastw = {}
        self.readers = {}

    def flush(self):
        nc = self.nc
        self.barrier()
        q = self.q
        for eng in q:
            for o in q[eng]:
                if o.fn is None:
                    continue
                if o.is_dma:
                    self.dcnt[o.dom] += 16
                    o.val = self.dcnt[o.dom]
                elif o.target:
                    self.cnt[eng] += 1
                    o.val = self.cnt[eng]
        sem, dsem = self.sem, self.dsem

        def run(e, ops):
            for o in ops:
                for p in o.waits:
                    e.wait_ge(dsem[p.dom] if p.is_dma else sem[p.dom], p.val)
                if o.fn is None:
                    continue
                ins = o.fn(e)
                if o.is_dma:
                    ins.then_inc(dsem[o.dom], 16)
                elif o.target:
                    ins.then_inc(sem[o.eng], 1)

        with nc.Block() as block:
            @block.tensor
            def _(e):
                run(e, q["pe"])

            @block.scalar
            def _(e):
                run(e, q["act"])

            @block.vector
            def _(e):
                run(e, q["dve"])

            @block.gpsimd
            def _(e):
                run(e, q["pool"])

            @block.sync
            def _(e):
                run(e, q["sp"])
        for k in q:
            q[k] = []
        self.dfree.extend(v for k_, v in self.dslot.items() if not k_.startswith("__swd__"))
        self.dslot = {}
        self.lastop = {}


C_IDENT, C_TRIU, C_TRIS, C_ONES, C_IOTA, C_BD16, C_RM8, C_IOTA16, C_END = (
    0, 128, 256, 384, 512, 640, 768, 776, 792)


def make_consts():
    c = np.zeros((128, C_END), np.float32)
    k = np.arange(128)
    c[:, C_IDENT:C_IDENT + 128] = np.eye(128)
    c[:, C_TRIU:C_TRIU + 128] = (k[:, None] <= k[None, :])
    c[:, C_TRIS:C_TRIS + 128] = (k[:, None] > k[None, :])
    c[:, C_ONES:C_ONES + 128] = 1.0
    c[:, C_IOTA:C_IOTA + 128] = k[None, :]
    c[:, C_BD16:C_BD16 + 128] = (k[:, None] // 16 == k[None, :] // 16)
    c[:, C_RM8:C_RM8 + 8] = (k[:, None] // 16 == np.arange(8)[None, :])
    c[:, C_IOTA16:C_IOTA16 + 16] = np.arange(16)[None, :]
    return c


def skew_pipeline(stages, n_items):
    ns = len(stages)
    for k in range(n_items + ns - 1):
        for s_ in reversed(range(ns)):
            n = k - s_
            if 0 <= n < n_items:
                stages[s_](n)


IN_SPECS = [
    ("x", [T, D]), ("c", [NB, D]), ("w_ada", [D, 6 * D]), ("b_ada", [1, 6 * D]),
    ("norm1_g", [1, D]), ("w_in", [D, INW]), ("conv_w", [4, 2048]), ("conv_b", [1, 2048]),
    ("dt_bias", [1, 16]), ("a_log", [1, 16]), ("d_ssd", [1, 16]), ("norm_ssd_g", [1, D]),
    ("s5_a_re", [64, 64]), ("s5_a_im", [64, 64]), ("s5_log_dt", [1, 64]),
    ("s5_b_re", [64, 64, 16]), ("s5_b_im", [64, 64, 16]), ("s5_c_re", [64, 16, 64]),
    ("s5_c_im", [64, 16, 64]), ("s5_d", [64, 16]), ("glu_w", [64, 16, 16]), ("glu_b", [64, 16]),
    ("norm_s5_g", [1, D]), ("w_out", [2 * D, D]), ("norm2_g", [1, D]), ("w_query", [D, 2048]),
    ("sub_keys", [16, 128, 128]), ("expert_u", [16384, D]), ("expert_v", [16384, D]),
    ("norm_f_g", [1, D]), ("consts", [128, C_END]),
]


def build(debug=(), stop_after=None):
    nc = bass.Bass("TRN2", target_bir_lowering=False)
    I = {n: nc.dram_tensor(n, sh, F32, kind="ExternalInput").ap() for n, sh in IN_SPECS}
    out = nc.dram_tensor("out", [T, D], F32, kind="ExternalOutput").ap()

    def SCR(name, shape, dt):
        kind = "ExternalOutput" if name in debug else "Internal"
        return nc.dram_tensor(name, shape, dt, kind=kind).ap()

    MODs = SCR("MODs", [NB, 6 * D], F32)
    XCs = SCR("XCs", [2048, T], BF16)
    UTs = SCR("UTs", [1024, T], BF16)
    Zs = SCR("Zs", [T, D], BF16)
    DTs = SCR("DTs", [T, 16], F32)
    YCs = SCR("YCs", [2048, T], BF16)
    Y5s = SCR("Y5s", [1024, T], F32)
    X1s = SCR("X1s", [T, D], F32)
    H2Ts = SCR("H2Ts", [D, T], BF16)
    RTs = SCR("RTs", [128, 3, T], BF16)
    UTb = SCR("UTb", [128, 128, 1024], BF16)
    Vb = SCR("Vb", [16384, D], BF16)

    with ExitStack() as top:
        S = Sched(nc, top)

        def mm(out_, lhsT, rhs, start, stop, r, w):
            S.op("pe", lambda e: e.matmul(out_, lhsT=lhsT, rhs=rhs, start=start, stop=stop), r=r, w=w)

        def tr(out_, in_, ident, r, w):
            S.op("pe", lambda e: e.transpose(out=out_, in_=in_, identity=ident), r=r, w=w)

        def act(out_, in_, func, r, w, eng="act", **kw):
            S.op(eng, lambda e: e.activation(out=out_, in_=in_, func=func, **kw), r=r, w=w)

        def tt(eng, out_, in0, in1, op, r, w):
            S.op(eng, lambda e: e.tensor_tensor(out=out_, in0=in0, in1=in1, op=op), r=r, w=w)

        def ts(eng, out_, in0, s1, s2, op0, op1, r, w):
            if s2 is None:
                S.op(eng, lambda e: e.tensor_scalar(out=out_, in0=in0, scalar1=s1, scalar2=None, op0=op0), r=r, w=w)
            else:
                S.op(eng, lambda e: e.tensor_scalar(out=out_, in0=in0, scalar1=s1, scalar2=s2, op0=op0, op1=op1),
                     r=r, w=w)

        def stt(out_, in0, scalar, in1, op0, op1, r, w):
            S.op("dve", lambda e: e.scalar_tensor_tensor(out=out_, in0=in0, scalar=scalar, in1=in1, op0=op0, op1=op1),
                 r=r, w=w)

        def cp(eng, out_, in_, r, w):
            if eng == "act":
                act(out_, in_, AF.Copy, r, w)
            else:
                S.op(eng, lambda e: e.tensor_copy(out=out_, in_=in_), r=r, w=w)

        def rsqrt(col, n, r, w):
            ts("dve", col, col, 1.0 / n, EPS, ALU.mult, ALU.add, r, w)
            act(col, col, AF.Sqrt, w, w)
            S.op("dve", lambda e: e.reciprocal(out=col, in_=col), r=w, w=w)

        cst = top.enter_context(nc.sbuf_tensor("cst", [128, C_END], F32))
        cstb = top.enter_context(nc.sbuf_tensor("cstb", [128, C_END], BF16))
        S.dma("sp", cst[:], I["consts"][:, :], w=["cst"], stream="cst")
        cp("dve", cstb[:], cst[:], ["cst"], ["cstb"])
        ident_f = cst[:, C_IDENT:C_IDENT + 128]
        ident_b = cstb[:, C_IDENT:C_IDENT + 128]
        triu_f = cst[:, C_TRIU:C_TRIU + 128]
        tris_f = cst[:, C_TRIS:C_TRIS + 128]
        ones_f = cst[:, C_ONES:C_ONES + 128]
        ones_b = cstb[:, C_ONES:C_ONES + 128]

        with ExitStack() as ph:
            A = lambda n, sh, dt=F32: ph.enter_context(nc.sbuf_tensor(n, sh, dt))
            cT = A("cT", [128, 8, NB])
            rep = A("rep", [128, 8, NB, 128])
            bada = A("bada", [128, 6 * D])
            g1bc = A("g1bc", [128, D])
            g2bc = A("g2bc", [128, D])
            wa = [A("wa%d" % i, [128, 8, 512]) for i in range(2)]
            modbc = [A("modbc%d" % b, [128, 6 * D]) for b in range(NB)]
            pm = [ph.enter_context(nc.psum_tensor("pm%d" % i, [128, 512], F32)) for i in range(2)]
            for b in range(NB):
                S.dma("sp", cT[:, :, b], I["c"][b:b + 1, :].rearrange("o (kc p) -> p (o kc)", p=128), w=["cT"],
                      stream="p0", allow_slow_non_contiguous=True)
            S.dma("sp", bada[:], I["b_ada"][0:1, :].partition_broadcast(128), w=["bada"], stream="p0b")
            S.dma("sp", g1bc[:], I["norm1_g"][0:1, :].partition_broadcast(128), w=["g1bc"], stream="p0c")
            S.dma("sp", g2bc[:], I["norm2_g"][0:1, :].partition_broadcast(128), w=["g2bc"], stream="p0d")
            act(cT[:], cT[:], AF.Silu, ["cT"], ["cT"])
            cp("dve", rep[:], cT[:].unsqueeze(3).to_broadcast([128, 8, NB, 128]), ["cT"], ["rep"])
            wav = I["w_ada"].rearrange("(kc p) n -> p kc n", p=128)
            for n in range(12):
                wb = wa[n % 2]
                wk = "wa%d" % (n % 2)
                S.dma("sp", wb[:], wav[:, :, n * 512:(n + 1) * 512], w=[wk], stream=wk)
                for b in range(NB):
                    pk = "pm%d" % b
                    for kc in range(8):
                        mm(pm[b][:, :], rep[:, kc, b, :], wb[:, kc, :], kc == 0, kc == 7, ["rep", wk], [pk])
                    tt("dve", modbc[b][:, n * 512:(n + 1) * 512], pm[b][:, :], bada[:, n * 512:(n + 1) * 512],
                       ALU.add, [pk, "bada"], ["modbc%d" % b])
            for b in range(NB):
                mk = "modbc%d" % b
                stt(modbc[b][:, 1024:2048], modbc[b][:, 1024:2048], 1.0, g1bc[:], ALU.add, ALU.mult, [mk, "g1bc"], [mk])
                stt(modbc[b][:, 4096:5120], modbc[b][:, 4096:5120], 1.0, g2bc[:], ALU.add, ALU.mult, [mk, "g2bc"], [mk])
                S.dma("sp", MODs[b:b + 1, :], modbc[b][0:1, :], r=[mk], w=["MODs"], stream="p0s")
            S.flush()
        if stop_after == 0:
            return nc

        with ExitStack() as ph:
            A = lambda n, sh, dt=F32: ph.enter_context(nc.sbuf_tensor(n, sh, dt))
            P = lambda n, sh, dt=F32: ph.enter_context(nc.psum_tensor(n, sh, dt))
            win = A("win", [128, 8, INW], BF16)
            hT = A("hT", [128, 8, SEQ], BF16)
            G1 = A("G1", [128, D])
            SH1 = A("SH1", [128, D])
            xin = [A("xin%d" % i, [128, D]) for i in range(5)]
            t1 = [A("t1_%d" % i, [128, D]) for i in range(3)]
            junk = A("junk", [128, D], BF16)
            hb = [A("hb%d" % i, [128, D], BF16) for i in range(3)]
            ss = A("ss", [128, 16])
            zst = [A("zst%d" % i, [128, D], BF16) for i in range(2)]
            dts = [A("dts%d" % i, [128, 16]) for i in range(2)]
            xpad = [A("xpad%d" % i, [128, 3 + SEQ]) for i in range(2)]
            acc = [A("acc%d" % i, [128, SEQ]) for i in range(2)]
            xo = [A("xo%d" % i, [128, SEQ], BF16) for i in range(2)]
            cw = A("cw", [128, 16, 4])
            cb = A("cb", [128, 16])
            pT = [P("pT%d" % i, [128, 1024], BF16) for i in range(2)]
            pz = P("pz", [128, 1024])
            pdt = P("pdt", [128, 16])
            pc = [P("pc%d" % i, [128, 512]) for i in range(2)]

            winv = I["w_in"].rearrange("(kc p) n -> p kc n", p=128)
            for kc in range(8):
                S.dma("pool", win[:, kc, :], winv[:, kc, :], w=["win%d" % kc], stream="win")
            for k in range(4):
                S.dma("sp", cw[:, :, k], I["conv_w"][k:k + 1, :].rearrange("o (ct p) -> p (o ct)", p=128), w=["cw"],
                      stream="cw", allow_slow_non_contiguous=True)
            S.dma("sp", cb[:], I["conv_b"].rearrange("o (ct p) -> p (o ct)", p=128), w=["cb"], stream="cb",
                  allow_slow_non_contiguous=True)
            for i in range(2):
                S.op("dve", lambda e, i=i: e.memset(xpad[i][:, 0:3], 0.0), w=["xpad%d" % i])

            def tokgen(b, i):
                tok0 = b * SEQ + i * 128
                r3, r2 = i % 3, i % 2
                xk, tk_, hk, pk = "xin%d" % (i % 5), "t1_%d" % r3, "hb%d" % r3, "pT%d" % r2
                xt = xin[i % 5]
                col = ss[:, i:i + 1]
                S.dma("sp", xt[:], I["x"][tok0:tok0 + 128, :], w=[xk], stream=xk)
                yield
                act(junk[:], xt[:], AF.Square, [xk], ["junk", "ss%d" % i], accum_out=col)
                yield
                ts("dve", col, col, 1.0 / D, EPS, ALU.mult, ALU.add, ["ss%d" % i], ["ss%d" % i])
                yield
                act(col, col, AF.Sqrt, ["ss%d" % i], ["ss%d" % i])
                yield
                S.op("dve", lambda e: e.reciprocal(out=col, in_=col), r=["ss%d" % i], w=["ss%d" % i])
                stt(t1[r3][:], xt[:], col, G1[:], ALU.mult, ALU.mult, [xk, "ss%d" % i, "G1"], [tk_])
                yield
                tt("pool", hb[r3][:], t1[r3][:], SH1[:], ALU.add, [tk_, "SH1"], [hk])
                yield
                for kc in range(8):
                    tr(pT[r2][:, kc * 128:(kc + 1) * 128], hb[r3][:, kc * 128:(kc + 1) * 128], ident_b, [hk, "cstb"], [pk])
                yield
                cp("act", hT[:, :, i * 128:(i + 1) * 128], pT[r2][:, :].rearrange("p (k t) -> p k t", k=8), [pk],
                   ["hT%d" % i])
                yield
                for half in range(2):
                    for kc in range(8):
                        mm(pz[:, half * 512:(half + 1) * 512], hT[:, kc, i * 128:(i + 1) * 128],
                           win[:, kc, half * 512:(half + 1) * 512], kc == 0, kc == 7, ["hT%d" % i, "win%d" % kc], ["pz"])
                for kc in range(8):
                    mm(pdt[:, :], hT[:, kc, i * 128:(i + 1) * 128], win[:, kc, 3072:3088], kc == 0, kc == 7,
                       ["hT%d" % i, "win%d" % kc], ["pdt"])
                yield
                zk, dk = "zst%d" % r2, "dts%d" % r2
                cp("act", zst[r2][:], pz[:, :], ["pz"], [zk])
                cp("dve", dts[r2][:], pdt[:, :], ["pdt"], [dk])
                S.dma("sp", Zs[tok0:tok0 + 128, :], zst[r2][:], r=[zk], w=["Zs"], stream=zk)
                S.dma("sp", DTs[tok0:tok0 + 128, :], dts[r2][:], r=[dk], w=["DTs"], stream=dk)
                yield

            def chgen(b, ct):
                col0 = 1024 + ct * 128 if ct < 16 else 3088 + (ct - 16) * 128
                par = ct % 2
                xpk, xok, ak = "xpad%d" % par, "xo%d" % par, "acc%d" % par
                for blk in range(4):
                    pck = "pc%d" % (blk % 2)
                    for kc in range(8):
                        mm(pc[blk % 2][:, :], win[:, kc, col0:col0 + 128], hT[:, kc, blk * 512:(blk + 1) * 512],
                           kc == 0, kc == 7, ["win%d" % kc] + ["hT%d" % j for j in range(blk * 4, blk * 4 + 4)], [pck])
                    if ct < 16:
                        cp("act", xpad[par][:, 3 + blk * 512:3 + (blk + 1) * 512], pc[blk % 2][:, :], [pck], [xpk])
                    else:
                        cp("act", xo[par][:, blk * 512:(blk + 1) * 512], pc[blk % 2][:, :], [pck], [xok])
                    if blk % 2 == 1:
                        yield
                if ct < 16:
                    xp = xpad[par]
                    ts("dve", acc[par][:], xp[:, 3:3 + SEQ], cw[:, ct, 3:4], cb[:, ct:ct + 1], ALU.mult, ALU.add,
                       [xpk, "cw", "cb"], [ak])
                    yield
                    for k in (2, 1, 0):
                        stt(acc[par][:], xp[:, k:k + SEQ], cw[:, ct, k:k + 1], acc[par][:], ALU.mult, ALU.add,
                            [xpk, "cw", ak], [ak])
                        yield
                    act(xo[par][:], acc[par][:], AF.Silu, [ak], [xok])
                    yield
                    S.dma("sp", XCs[ct * 128:(ct + 1) * 128, b * SEQ:(b + 1) * SEQ], xo[par][:], r=[xok], w=["XCs"],
                          stream=xok)
                else:
                    S.dma("sp", UTs[(ct - 16) * 128:(ct - 15) * 128, b * SEQ:(b + 1) * SEQ], xo[par][:], r=[xok],
                          w=["UTs"], stream=xok)
                yield

            def run_skewed(gens, every):
                active = []
                k = 0
                while gens or active:
                    if gens and k % every == 0:
                        active.append(gens.pop(0))
                    for g_ in list(active):
                        try:
                            next(g_)
                        except StopIteration:
                            active.remove(g_)
                    k += 1

            for b in range(NB):
                S.dma("sp", G1[:], MODs[b:b + 1, 1024:2048].partition_broadcast(128), r=["MODs"], w=["G1"], stream="g1")
                S.dma("sp", SH1[:], MODs[b:b + 1, 0:1024].partition_broadcast(128), r=["MODs"], w=["SH1"], stream="sh1")
                run_skewed([tokgen(b, i) for i in range(16)], 1)
                run_skewed([chgen(b, ct) for ct in range(24)], 4)
            S.flush()
        if stop_after == 1:
            return nc

        with ExitStack() as ph:
            A = lambda n, sh, dt=F32: ph.enter_context(nc.sbuf_tensor("B_" + n, sh, dt))
            P = lambda n, sh, dt=F32: ph.enter_context(nc.psum_tensor("B_" + n, sh, dt))
            dtb = A("dtb", [128, 16]); abc = A("abc", [128, 16]); d16 = A("d16", [128, 16])
            dssd = A("dssd", [128, D]); normg = A("normg", [128, D])
            S32 = A("S32", [128, 16, 64]); Sbf = A("Sbf", [128, 16, 64], BF16)
            RC = 4
            xct = [A("xct%d" % i, [128, 16, 128], BF16) for i in range(RC)]
            zt = [A("zt%d" % i, [128, D], BF16) for i in range(RC)]
            dtr = [A("dtr%d" % i, [128, 16]) for i in range(RC)]
            dtt = [A("dtt%d" % i, [128, 16]) for i in range(RC)]
            da = [A("da%d" % i, [128, 16]) for i in range(RC)]
            c3 = [A("c3_%d" % i, [128, 48]) for i in range(RC)]
            e3 = [A("e3_%d" % i, [128, 48]) for i in range(RC)]
            xs = [A("xs%d" % i, [128, D], BF16) for i in range(RC)]
            Btok = [A("Btok%d" % i, [128, 512], BF16) for i in range(RC)]
            xdt = [A("xdt%d" % i, [128, D], BF16) for i in range(RC)]
            xdd = [A("xdd%d" % i, [128, D], BF16) for i in range(RC)]
            CBm = [A("CBm%d" % i, [128, 4, 128]) for i in range(RC)]
            RH = 3
            lD = [A("lD%d" % i, [128, 128]) for i in range(RH)]
            Lm = [A("Lm%d" % i, [128, 128]) for i in range(RH)]
            Mt = [A("Mt%d" % i, [128, 128], BF16) for i in range(RH)]
            yo = A("yo", [128, D]); y1 = A("y1", [128, D]); xD = A("xD", [128, D]); sz = A("sz", [128, D])
            yg = A("yg", [128, D]); junkb = A("junkb", [128, 256], BF16); ssq = A("ssq", [128, 4])
            yn = A("yn", [128, D]); ynb = A("ynb", [128, D], BF16)
            ynT = [A("ynT%d" % i, [128, 8, 128], BF16) for i in range(2)]
            pTr = P("pTr", [128, 1024], BF16)
            pDs = [P("pD%d" % i, [128, 512]) for i in range(2)]
            pSl = [P("pS%d" % i, [128, 512]) for i in range(2)]
            pPro = P("pPro", [128, 512])
            pY = P("pY", [128, 512])
            pYo = P("pYo", [128, 512])

            S.dma("sp", dtb[:], I["dt_bias"][0:1, :].partition_broadcast(128), w=["dtb"], stream="b0")
            S.dma("sp", abc[:], I["a_log"][0:1, :].partition_broadcast(128), w=["abc"], stream="b1")
            S.dma("sp", d16[:], I["d_ssd"][0:1, :].partition_broadcast(128), w=["d16"], stream="b2")
            S.dma("sp", normg[:], I["norm_ssd_g"][0:1, :].partition_broadcast(128), w=["normg"], stream="b3")
            act(abc[:], abc[:], AF.Exp, ["abc"], ["abc"])
            ts("dve", abc[:], abc[:], -1.0, None, ALU.mult, None, ["abc"], ["abc"])
            cp("dve", dssd[:, :].rearrange("p (h q) -> p h q", q=64), d16[:, :].unsqueeze(2).to_broadcast([128, 16, 64]),
               ["d16"], ["dssd"])
            XCv = XCs.rearrange("(ct p) t -> p ct t", p=128)
            YCv = YCs.rearrange("(ct p) t -> p ct t", p=128)
            v3 = lambda ap: ap.rearrange("p (h q) -> p h q", q=64)
            bc3 = lambda col: col.unsqueeze(2).to_broadcast([128, 16, 64])
            NCH = NB * 16

            def prologue(ci):
                r_ = ci % RC
                tok0 = ci * 128
                K_ = lambda nm: "%s%d" % (nm, r_)
                X = xct[r_]
                S.dma("sp", X[:], XCv[:, :, tok0:tok0 + 128], r=["XCs"], w=[K_("xct")], stream=K_("xct"))
                S.dma("sp", zt[r_][:], Zs[tok0:tok0 + 128, :], r=["Zs"], w=[K_("zt")], stream=K_("zt"))
                S.dma("sp", dtr[r_][:], DTs[tok0:tok0 + 128, :], r=["DTs"], w=[K_("dtr")], stream=K_("dtr"))
                yield
                tt("dve", dtt[r_][:], dtr[r_][:], dtb[:], ALU.add, [K_("dtr"), "dtb"], [K_("dtt")])
                yield
                act(dtt[r_][:], dtt[r_][:], AF.Exp, [K_("dtt")], [K_("dtt")])
                act(dtt[r_][:], dtt[r_][:], AF.Ln, [K_("dtt")], [K_("dtt")], bias=1.0)
                yield
                tt("dve", da[r_][:], dtt[r_][:], abc[:], ALU.mult, [K_("dtt"), "abc"], [K_("da")])
                yield
                mm(pPro[:, 0:16], triu_f, da[r_][:], True, True, ["cst", K_("da")], ["pm_cs"])
                mm(pPro[:, 16:32], ones_f, da[r_][:], True, True, ["cst", K_("da")], ["pm_cs"])
                cp("dve", c3[r_][:, 0:32], pPro[:, 0:32], ["pm_cs"], [K_("c3")])
                tt("dve", c3[r_][:, 32:48], c3[r_][:, 16:32], c3[r_][:, 0:16], ALU.subtract, [K_("c3")], [K_("c3")])
                yield
                act(e3[r_][:], c3[r_][:], AF.Exp, [K_("c3")], [K_("e3")])
                yield
                for j in range(8):
                    tr(pTr[:, j * 128:(j + 1) * 128], X[:, j, :], ident_b, [K_("xct"), "cstb"], ["pTr"])
                cp("act", xs[r_][:], pTr[:, :], ["pTr"], [K_("xs")])
                yield
                for g in range(4):
                    tr(pTr[:, g * 128:(g + 1) * 128], X[:, 8 + g, :], ident_b, [K_("xct"), "cstb"], ["pTr"])
                cp("act", Btok[r_][:], pTr[:, 0:512], ["pTr"], [K_("Btok")])
                yield
                tt("dve", v3(xdt[r_][:, :]), v3(xs[r_][:, :]), bc3(dtt[r_][:, :]), ALU.mult, [K_("xs"), K_("dtt")],
                   [K_("xdt")])
                tt("dve", v3(xdd[r_][:, :]), v3(xdt[r_][:, :]), bc3(e3[r_][:, 32:48]), ALU.mult, [K_("xdt"), K_("e3")],
                   [K_("xdd")])
                yield
                for g in range(4):
                    mm(pPro[:, 128:256], X[:, 8 + g, :], X[:, 12 + g, :], True, True, [K_("xct")], ["pm_cb"])
                    tt("dve", CBm[r_][:, g, :], pPro[:, 128:256], triu_f, ALU.mult, ["pm_cb", "cst"], [K_("CBm") + "_%d" % g])
                    yield

            def epilogue(ci):
                r_ = ci % RC
                tok0 = ci * 128
                c = ci % 16
                K_ = lambda nm: "%s%d" % (nm, r_)
                yield
                tt("pool", xD[:], xs[r_][:], dssd[:], ALU.mult, [K_("xs"), "dssd"], ["xD"])
                yield
                tt("dve", y1[:], y1[:], xD[:], ALU.add, ["y1", "xD"], ["y1"])
                act(sz[:], zt[r_][:], AF.Silu, [K_("zt")], ["sz"])
                yield
                tt("dve", yg[:], y1[:], sz[:], ALU.mult, ["y1", "sz"], ["yg"])
                yield
                for G_ in range(4):
                    act(junkb[:], yg[:, G_ * 256:(G_ + 1) * 256], AF.Square, ["yg"], ["junkb", "ssq"],
                        accum_out=ssq[:, G_:G_ + 1])
                yield
                ts("dve", ssq[:], ssq[:], 1.0 / 256, EPS, ALU.mult, ALU.add, ["ssq"], ["ssq"])
                yield
                act(ssq[:], ssq[:], AF.Sqrt, ["ssq"], ["ssq"])
                yield
                S.op("dve", lambda e: e.reciprocal(out=ssq[:], in_=ssq[:]), r=["ssq"], w=["ssq"])
                tt("dve", yn[:, :].rearrange("p (g q) -> p g q", q=256), yg[:, :].rearrange("p (g q) -> p g q", q=256),
                   ssq[:, :].unsqueeze(2).to_broadcast([128, 4, 256]), ALU.mult, ["yg", "ssq"], ["yn"])
                yield
                tt("pool", ynb[:], yn[:], normg[:], ALU.mult, ["yn", "normg"], ["ynb"])
                yield
                for j in range(8):
                    tr(pTr[:, j * 128:(j + 1) * 128], ynb[:, j * 128:(j + 1) * 128], ident_b, ["ynb", "cstb"], ["pTr"])
                yk = "ynT%d" % (ci % 2)
                cp("act", ynT[ci % 2][:, :, :], pTr[:, :].rearrange("p (k t) -> p k t", k=8), ["pTr"], [yk])
                S.dma("sp", YCv[:, 0:8, tok0:tok0 + 128], ynT[ci % 2][:, :, :], r=[yk], w=["YCs"], stream=yk)
                yield

            def e1_half(ci, hf):
                r_ = ci % RC
                cs_ = slice(hf * 512, (hf + 1) * 512)
                v3h = lambda ap: ap.rearrange("p (h q) -> p h q", q=64)
                if ci % 16 == 0:
                    cp("dve", y1[:, cs_], pY[:, :], ["pY"], ["y1"])
                else:
                    tt("dve", v3h(yo[:, cs_]), v3h(pYo[:, :]),
                       e3[r_][:, hf * 8:hf * 8 + 8].unsqueeze(2).to_broadcast([128, 8, 64]), ALU.mult,
                       ["pYo", "e3_%d" % r_], ["yo"])
                    tt("dve", y1[:, cs_], yo[:, cs_], pY[:, :], ALU.add, ["yo", "pY"], ["y1"])

            def hinfo(n):
                ci, h = n // 16, n % 16
                return ci, h, h // 4, ci % RC, n % RH, ci % 16

            def h_a(n):
                ci, h, g, r_, hr, c = hinfo(n)
                tt("pool", lD[hr][:], tris_f, da[r_][:, h:h + 1].to_broadcast([128, 128]), ALU.mult,
                   ["cst", "da%d" % r_], ["lD%d" % hr])

            def h_b(n):
                ci, h, g, r_, hr, c = hinfo(n)
                mm(pDs[n % 2][:, 0:128], lD[hr][:], triu_f, True, True, ["lD%d" % hr, "cst"], ["pD%d" % (n % 2)])

            def h_c(n):
                ci, h, g, r_, hr, c = hinfo(n)
                act(Lm[hr][:], pDs[n % 2][:, 0:128], AF.Exp, ["pD%d" % (n % 2)], ["Lm%d" % hr])

            def h_d(n):
                ci, h, g, r_, hr, c = hinfo(n)
                tt("dve", Mt[hr][:], Lm[hr][:], CBm[r_][:, g, :], ALU.mult, ["Lm%d" % hr, "CBm%d_%d" % (r_, g)],
                   ["Mt%d" % hr])

            def h_e(n):
                ci, h, g, r_, hr, c = hinfo(n)
                X = xct[r_]
                mm(pY[:, (h % 8) * 64:(h % 8 + 1) * 64], Mt[hr][:], xdt[r_][:, h * 64:(h + 1) * 64], True, True,
                   ["Mt%d" % hr, "xdt%d" % r_], ["pY"])
                if c != 0:
                    mm(pYo[:, (h % 8) * 64:(h % 8 + 1) * 64], X[:, 12 + g, :], Sbf[:, h, :], True, True,
                       ["xct%d" % r_, "Sbf%d" % h], ["pYo"])
                mm(pSl[n % 2][:, 0:64], Btok[r_][:, g * 128:(g + 1) * 128], xdd[r_][:, h * 64:(h + 1) * 64],
                   True, True, ["Btok%d" % r_, "xdd%d" % r_], ["pS%d" % (n % 2)])

            def h_f(n):
                ci, h, g, r_, hr, c = hinfo(n)
                if c == 0:
                    cp("dve", S32[:, h, :], pSl[n % 2][:, 0:64], ["pS%d" % (n % 2)], ["S32_%d" % h])
                else:
                    stt(S32[:, h, :], S32[:, h, :], e3[r_][:, 16 + h:17 + h], pSl[n % 2][:, 0:64],
                        ALU.mult, ALU.add, ["S32_%d" % h, "e3_%d" % r_, "pS%d" % (n % 2)], ["S32_%d" % h])

            def h_g(n):
                ci, h, g, r_, hr, c = hinfo(n)
                cp("pool", Sbf[:, h, :], S32[:, h, :], ["S32_%d" % h], ["Sbf%d" % h])

            stages = [h_a, h_b, h_c, h_d, h_e, h_f, h_g]
            NS = len(stages)
            for _ in prologue(0):
                pass
            for _ in prologue(1):
                pass
            side = []
            NI_ = NCH * 16
            for k in range(NI_ + NS - 1):
                for s_ in reversed(range(NS)):
                    n = k - s_
                    if 0 <= n < NI_:
                        stages[s_](n)
                if k >= 11 and (k - 11) % 16 == 0:
                    e1_half((k - 11) // 16, 0)
                if k >= 19 and (k - 19) % 16 == 0:
                    e1_half((k - 19) // 16, 1)
                    side.append(epilogue((k - 19) // 16))
                if k % 16 == 0 and k // 16 + 2 < NCH:
                    side.append(prologue(k // 16 + 2))
                for g_ in list(side):
                    try:
                        next(g_)
                    except StopIteration:
                        side.remove(g_)
            while side:
                for g_ in list(side):
                    try:
                        next(g_)
                    except StopIteration:
                        side.remove(g_)
            S.flush()
        if stop_after == 2:
            return nc

        with ExitStack() as ph:
            A = lambda n, sh, dt=F32: ph.enter_context(nc.sbuf_tensor(n, sh, dt))
            Blk1 = A("Blk1", [128, 64, 128], BF16)
            Blk2 = A("Blk2", [128, 64, 128], BF16)
            CL1 = A("CL1", [128, 64, 16], BF16)
            CL2 = A("CL2", [128, 64, 16], BF16)
            rcol = A("rcol", [128, 64])
            fcol = A("fcol", [128, 64])
            D5c = A("D5c", [128, 8])
            glub = A("glub", [128, 8])
            g5c = A("g5c", [128, 8])
            Wg = A("Wg", [128, 8, 128], BF16)
            rm8 = cst[:, C_RM8:C_RM8 + 8]
            INV2PI = 1.0 / TWO_PI

            def sin_turns(out_, f_ap, tk, tf, keys_in, kout, e1="dve", e2="dve"):
                ts(e1, tk, f_ap, MAGIC, MAGIC, ALU.add, ALU.subtract, keys_in, ["_tk"])
                tt(e2, tf, f_ap, tk, ALU.subtract, keys_in + ["_tk"], ["_tf"])
                act(out_, tf, AF.Sin, ["_tf"], [kout], scale=TWO_PI)

            with ExitStack() as p0:
                B_ = lambda n, sh, dt=F32: p0.enter_context(nc.sbuf_tensor(n, sh, dt))
                lrT = B_("lrT", [128, 64]); liT = B_("liT", [128, 64]); dtg = B_("dtg", [128, 64])
                ldt = B_("ldt", [128, 64]); f2 = B_("f2", [128, 64]); tk = B_("tk", [128, 64]); tf = B_("tf", [128, 64])
                sn = B_("sn", [128, 64]); cs_ = B_("cs_", [128, 64]); lbr = B_("lbr", [128, 64]); lbi = B_("lbi", [128, 64])
                den = B_("den", [128, 64]); t_a = B_("t_a", [128, 64]); t_b = B_("t_b", [128, 64])
                cre = B_("cre", [128, 64]); cim = B_("cim", [128, 64])
                br = B_("br", [64, 64, 16]); bi = B_("bi", [64, 64, 16])
                bbr = B_("bbr", [64, 64, 16]); bbi = B_("bbi", [64, 64, 16]); t_c = B_("t_c", [64, 64, 16])
                Dre = B_("Dre", [128, 8, 64]); Dim = B_("Dim", [128, 8, 64]); nDre = B_("nDre", [128, 8, 64])
                cc1 = B_("cc1", [128, 128]); cc2 = B_("cc2", [128, 128]); wrow = B_("wrow", [128, 8, 16])
                ptc = p0.enter_context(nc.psum_tensor("ptc", [128, 128], F32))
                for half in range(2):
                    S.dma("sp", lrT[half * 64:(half + 1) * 64, :], I["s5_a_re"].rearrange("g p -> p g"), w=["lrT"],
                          stream="c0a", allow_slow_non_contiguous=True)
                    S.dma("sp", liT[half * 64:(half + 1) * 64, :], I["s5_a_im"].rearrange("g p -> p g"), w=["liT"],
                          stream="c0b", allow_slow_non_contiguous=True)
                S.dma("sp", dtg[:], I["s5_log_dt"][0:1, :].partition_broadcast(128), w=["dtg"], stream="c0c")
                act(dtg[:], dtg[:], AF.Exp, ["dtg"], ["dtg"])
                tt("dve", ldt[:], lrT[:], dtg[:], ALU.mult, ["lrT", "dtg"], ["ldt"])
                act(rcol[:], ldt[:], AF.Exp, ["ldt"], ["rcol"])
                tt("dve", fcol[:], liT[:], dtg[:], ALU.mult, ["liT", "dtg"], ["fcol"])
                ts("dve", fcol[:], fcol[:], INV2PI, None, ALU.mult, None, ["fcol"], ["fcol"])
                sin_turns(sn[:], fcol[:], tk[:], tf[:], ["fcol"], "sn")
                ts("dve", f2[:], fcol[:], 0.25, None, ALU.add, None, ["fcol"], ["f2"])
                sin_turns(cs_[:], f2[:], tk[:], tf[:], ["f2"], "cs_")
                tt("dve", lbr[:], rcol[:], cs_[:], ALU.mult, ["rcol", "cs_"], ["lbr"])
                tt("dve", lbi[:], rcol[:], sn[:], ALU.mult, ["rcol", "sn"], ["lbi"])
                ts("dve", lbr[:], lbr[:], -1.0, None, ALU.add, None, ["lbr"], ["lbr"])
                tt("dve", den[:], lrT[:], lrT[:], ALU.mult, ["lrT"], ["den"])
                tt("dve", t_a[:], liT[:], liT[:], ALU.mult, ["liT"], ["t_a"])
                tt("dve", den[:], den[:], t_a[:], ALU.add, ["den", "t_a"], ["den"])
                S.op("dve", lambda e: e.reciprocal(out=den[:], in_=den[:]), r=["den"], w=["den"])
                tt("dve", t_a[:], lbr[:], lrT[:], ALU.mult, ["lbr", "lrT"], ["t_a"])
                tt("dve", t_b[:], lbi[:], liT[:], ALU.mult, ["lbi", "liT"], ["t_b"])
                tt("dve", t_a[:], t_a[:], t_b[:], ALU.add, ["t_a", "t_b"], ["t_a"])
                tt("dve", cre[:], t_a[:], den[:], ALU.mult, ["t_a", "den"], ["cre"])
                tt("dve", t_a[:], lbi[:], lrT[:], ALU.mult, ["lbi", "lrT"], ["t_a"])
                tt("dve", t_b[:], lbr[:], liT[:], ALU.mult, ["lbr", "liT"], ["t_b"])
                tt("dve", t_a[:], t_a[:], t_b[:], ALU.subtract, ["t_a", "t_b"], ["t_a"])
                tt("dve", cim[:], t_a[:], den[:], ALU.mult, ["t_a", "den"], ["cim"])
                for q4 in range(4):
                    gs = slice(q4 * 16, (q4 + 1) * 16)
                    S.dma("sp", br[:, gs, :], I["s5_b_re"][gs].rearrange("g p h -> p g h"), w=["br"], stream="c0d")
                    S.dma("sp", bi[:, gs, :], I["s5_b_im"][gs].rearrange("g p h -> p g h"), w=["bi"], stream="c0e")
                bcr = cre[0:64, :].unsqueeze(2).to_broadcast([64, 64, 16])
                bci = cim[0:64, :].unsqueeze(2).to_broadcast([64, 64, 16])
                tt("dve", bbr[:], br[:], bcr, ALU.mult, ["br", "cre"], ["bbr"])
                tt("dve", t_c[:], bi[:], bci, ALU.mult, ["bi", "cim"], ["t_c"])
                tt("dve", bbr[:], bbr[:], t_c[:], ALU.subtract, ["bbr", "t_c"], ["bbr"])
                tt("dve", bbi[:], bi[:], bcr, ALU.mult, ["bi", "cre"], ["bbi"])
                tt("dve", t_c[:], br[:], bci, ALU.mult, ["br", "cim"], ["t_c"])
                tt("dve", bbi[:], bbi[:], t_c[:], ALU.add, ["bbi", "t_c"], ["bbi"])
                for j in range(8):
                    tr(ptc[:, 0:64], bbr[:, j * 8:(j + 1) * 8, :].rearrange("p g h -> p (g h)"), ident_f[0:64, 0:64], ["bbr", "cst"], ["ptc"])
                    cp("dve", Dre[:, j, :], ptc[:, 0:64], ["ptc"], ["Dre"])
                    tr(ptc[:, 64:128], bbi[:, j * 8:(j + 1) * 8, :].rearrange("p g h -> p (g h)"), ident_f[0:64, 0:64], ["bbi", "cst"], ["ptc2"])
                    cp("dve", Dim[:, j, :], ptc[:, 64:128], ["ptc2"], ["Dim"])
                ts("dve", nDre[:], Dre[:], -1.0, None, ALU.mult, None, ["Dre"], ["nDre"])
                rmb = rm8.unsqueeze(2).to_broadcast([128, 8, 64])
                for j in range(8):
                    gs = slice(j * 8, (j + 1) * 8)
                    bcD = lambda t: t[:, j, :].unsqueeze(1).to_broadcast([128, 8, 64])
                    tt("dve", Blk1[:, gs, 0:64], bcD(Dre), rmb, ALU.mult, ["Dre", "cst"], ["Blk1"])
                    tt("dve", Blk1[:, gs, 64:128], bcD(Dim), rmb, ALU.mult, ["Dim", "cst"], ["Blk1"])
                    tt("dve", Blk2[:, gs, 0:64], bcD(Dim), rmb, ALU.mult, ["Dim", "cst"], ["Blk2"])
                    tt("dve", Blk2[:, gs, 64:128], bcD(nDre), rmb, ALU.mult, ["nDre", "cst"], ["Blk2"])
                crv = I["s5_c_re"].rearrange("g h p -> (g h) p")
                civ = I["s5_c_im"].rearrange("g h p -> (g h) p")
                for j in range(8):
                    rs_ = slice(j * 128, (j + 1) * 128)
                    S.dma("sp", cc1[:, 0:64], crv[rs_, :], w=["cc1"], stream="c0f")
                    S.dma("sp", cc1[:, 64:128], civ[rs_, :], w=["cc1"], stream="c0f")
                    S.dma("sp", cc2[:, 0:64], civ[rs_, :], w=["cc2"], stream="c0g")
                    S.dma("sp", cc2[:, 64:128], crv[rs_, :], w=["cc2"], stream="c0g")
                    tr(ptc[:, :], cc1[:], ident_f, ["cc1", "cst"], ["ptc", "ptc2"])
                    gs = slice(j * 8, (j + 1) * 8)
                    cp("dve", CL1[0:64, gs, :], ptc[0:64, :].rearrange("p (g h) -> p g h", h=16), ["ptc"], ["CL1"])
                    ts("dve", CL1[64:128, gs, :], ptc[64:128, :].rearrange("p (g h) -> p g h", h=16), -1.0, None,
                       ALU.mult, None, ["ptc"], ["CL1"])
                    tr(ptc[:, :], cc2[:], ident_f, ["cc2", "cst"], ["ptc", "ptc2"])
                    ts("dve", CL2[:, gs, :], ptc[:, :].rearrange("p (g h) -> p g h", h=16), -1.0, None, ALU.mult, None,
                       ["ptc"], ["CL2"])
                S.dma("sp", D5c[:], I["s5_d"].rearrange("(j gl) h -> (gl h) j", gl=8), w=["D5c"], stream="c0h",
                      allow_slow_non_contiguous=True)
                S.dma("sp", glub[:], I["glu_b"].rearrange("(j gl) h -> (gl h) j", gl=8), w=["glub"], stream="c0i",
                      allow_slow_non_contiguous=True)
                S.dma("sp", g5c[:], I["norm_s5_g"].rearrange("o (j p) -> p (o j)", p=128), w=["g5c"], stream="c0j",
                      allow_slow_non_contiguous=True)
                S.dma("sp", wrow[:], I["glu_w"].rearrange("(j gl) h k -> (gl h) j k", gl=8), w=["wrow"], stream="c0k")
                for j in range(8):
                    tt("dve", Wg[:, j, :].rearrange("p (g k) -> p g k", k=16),
                       wrow[:, j, :].unsqueeze(1).to_broadcast([128, 8, 16]),
                       rm8.unsqueeze(2).to_broadcast([128, 8, 16]), ALU.mult, ["wrow", "cst"], ["Wg"])
                S.flush()

            with ExitStack() as p1:
                B_ = lambda n, sh, dt=F32: p1.enter_context(nc.sbuf_tensor(n, sh, dt))
                Pp = lambda n, sh, dt=F32: p1.enter_context(nc.psum_tensor(n, sh, dt))
                iot = B_("iot", [128, SEQ])
                SIN = [B_("SIN%d" % i, [128, SEQ], BF16) for i in range(3)]
                COS = [B_("COS%d" % i, [128, SEQ], BF16) for i in range(3)]
                u1 = B_("u1", [128, SEQ]); k1 = B_("k1", [128, SEQ]); fr = B_("fr", [128, SEQ])
                uT = [B_("uT%d" % i, [128, T], BF16) for i in range(2)]
                R3 = 3
                p1b = [B_("p1b%d" % i, [128, 512], BF16) for i in range(R3)]
                p2b = [B_("p2b%d" % i, [128, 512], BF16) for i in range(R3)]
                w1 = [B_("w1_%d" % i, [128, 512], BF16) for i in range(R3)]
                w2 = [B_("w2_%d" % i, [128, 512], BF16) for i in range(R3)]
                ww = [B_("ww%d" % i, [128, 512]) for i in range(R3)]
                zz = [[B_("zz%d_%d" % (bb, i), [128, 512]) for i in range(R3)] for bb in range(NB)]
                zb = [B_("zb%d" % i, [128, 512], BF16) for i in range(R3)]
                v1 = [B_("v1_%d" % i, [128, 512], BF16) for i in range(R3)]
                v2 = [B_("v2_%d" % i, [128, 512], BF16) for i in range(R3)]
                ysm = [B_("ysm%d" % i, [16, 512]) for i in range(R3)]
                P1 = [Pp("P1_%d" % i, [128, 512]) for i in range(2)]
                P2 = [Pp("P2_%d" % i, [128, 512]) for i in range(2)]
                uf = [B_("uf%d" % i, [128, D]) for i in range(2)]
                vf = [B_("vf%d" % i, [128, D]) for i in range(2)]
                ub = [B_("ub%d" % i, [128, D], BF16) for i in range(2)]
                vbt = [B_("vbt%d" % i, [128, D], BF16) for i in range(2)]
                uts = [B_("uts%d" % i, [128, D], BF16) for i in range(2)]
                pTu = [Pp("pTu%d" % i, [128, 1024], BF16) for i in range(2)]

                def m0_tile(et):
                    pr = et % 2
                    rows = slice(et * 128, (et + 1) * 128)
                    S.dma("sp", uf[pr][:], I["expert_u"][rows, :], w=["uf%d" % pr], stream="uf%d" % pr)
                    S.dma("sp", vf[pr][:], I["expert_v"][rows, :], w=["vf%d" % pr], stream="vf%d" % pr)
                    yield
                    cp("act", ub[pr][:], uf[pr][:], ["uf%d" % pr], ["ub%d" % pr])
                    cp("act", vbt[pr][:], vf[pr][:], ["vf%d" % pr], ["vbt%d" % pr])
                    yield
                    for kc in range(8):
                        tr(pTu[pr][:, kc * 128:(kc + 1) * 128], ub[pr][:, kc * 128:(kc + 1) * 128], ident_b,
                           ["ub%d" % pr, "cstb"], ["pTu%d" % pr])
                    S.dma("sp", Vb[rows, :], vbt[pr][:], r=["vbt%d" % pr], w=["Vb"], stream="vbo%d" % pr)
                    yield
                    cp("act", uts[pr][:], pTu[pr][:, :], ["pTu%d" % pr], ["uts%d" % pr])
                    yield
                    S.dma("sp", UTb[et], uts[pr][:], r=["uts%d" % pr], w=["UTb"], stream="uto%d" % pr)
                    yield
                PY = [Pp("PY%d" % i, [128, 512]) for i in range(2)]
                for k in range(16):
                    ts("dve", iot[:, k * 128:(k + 1) * 128], cst[:, C_IOTA:C_IOTA + 128], float(128 * k), None, ALU.add,
                       None, ["cst"], ["iot"])

                def tables(g):
                    gp = g % 3
                    fg = fcol[:, g:g + 1]
                    for (tab, key, off) in ((SIN[gp], "SIN%d" % gp, 0.0), (COS[gp], "COS%d" % gp, 0.25)):
                        act(u1[:], iot[:], AF.Identity, ["iot", "fcol"], ["u1"], scale=fg, bias=off)
                        ts("dve", k1[:], u1[:], MAGIC, MAGIC, ALU.add, ALU.subtract, ["u1"], ["k1"])
                        tt("dve", fr[:], u1[:], k1[:], ALU.subtract, ["u1", "k1"], ["fr"])
                        act(tab[:], fr[:], AF.Sin, ["fr"], [key], scale=TWO_PI)

                pieces = [(g, q, bb) for g in range(64) for q in range(4) for bb in range(NB)]

                def info(n):
                    g, q, bb = pieces[n]
                    return g, q, bb, g // 8, g % 3, n % R3, q * 512, bb * SEQ + q * 512

                def st_a(n):
                    g, q, bb, j, gp, pb, t0, tok = info(n)
                    if q == 0 and bb == 0 and g + 1 < 64:
                        tables(g + 1)
                    if g % 8 == 0 and q == 0 and bb == 0:
                        S.dma("sp", uT[j % 2][:], UTs[j * 128:(j + 1) * 128, :], r=["UTs"], w=["uT%d" % (j % 2)],
                              stream="uT%d" % (j % 2))
                    uk = "uT%d" % (j % 2)
                    mm(P1[n % 2][:, :], Blk1[:, g, :], uT[j % 2][:, tok:tok + 512], True, True, ["Blk1", uk], ["P1_%d" % (n % 2)])
                    mm(P2[n % 2][:, :], Blk2[:, g, :], uT[j % 2][:, tok:tok + 512], True, True, ["Blk2", uk], ["P2_%d" % (n % 2)])

                def st_b(n):
                    g, q, bb, j, gp, pb, t0, tok = info(n)
                    cp("act", p1b[pb][:], P1[n % 2][:, :], ["P1_%d" % (n % 2)], ["p1b%d" % pb])
                    cp("act", p2b[pb][:], P2[n % 2][:, :], ["P2_%d" % (n % 2)], ["p2b%d" % pb])

                def st_c(n):
                    g, q, bb, j, gp, pb, t0, tok = info(n)
                    tt("dve", w1[pb][:], p1b[pb][:], COS[gp][:, t0:t0 + 512], ALU.mult, ["p1b%d" % pb, "COS%d" % gp],
                       ["w1_%d" % pb])
                    tt("dve", w2[pb][:], p2b[pb][:], SIN[gp][:, t0:t0 + 512], ALU.mult, ["p2b%d" % pb, "SIN%d" % gp],
                       ["w2_%d" % pb])

                def st_d(n):
                    g, q, bb, j, gp, pb, t0, tok = info(n)
                    tt("pool", ww[pb][:], w1[pb][:], w2[pb][:], ALU.add, ["w1_%d" % pb, "w2_%d" % pb], ["ww%d" % pb])

                def st_e(n):
                    g, q, bb, j, gp, pb, t0, tok = info(n)
                    zc, zp = zz[bb][q % R3], zz[bb][(q - 1) % R3]
                    zck, zpk = "zz%d_%d" % (bb, q % R3), "zz%d_%d" % (bb, (q - 1) % R3)
                    init = 0.0 if q == 0 else zp[:, 511:512]
                    S.op("dve", lambda e: e.tensor_tensor_scan(
                        out=zc[:], data0=rcol[:, g:g + 1].to_broadcast([128, 512]), data1=ww[pb][:],
                        initial=init, op0=ALU.mult, op1=ALU.add), r=["rcol", "ww%d" % pb, zpk], w=[zck])

                def st_f(n):
                    g, q, bb, j, gp, pb, t0, tok = info(n)
                    cp("act", zb[pb][:], zz[bb][q % R3][:], ["zz%d_%d" % (bb, q % R3)], ["zb%d" % pb])

                def st_g(n):
                    g, q, bb, j, gp, pb, t0, tok = info(n)
                    tt("dve", v1[pb][:], zb[pb][:], COS[gp][:, t0:t0 + 512], ALU.mult, ["zb%d" % pb, "COS%d" % gp],
                       ["v1_%d" % pb])
                    tt("pool", v2[pb][:], zb[pb][:], SIN[gp][:, t0:t0 + 512], ALU.mult, ["zb%d" % pb, "SIN%d" % gp],
                       ["v2_%d" % pb])

                def st_h(n):
                    g, q, bb, j, gp, pb, t0, tok = info(n)
                    pp = n % 2
                    mm(PY[pp][0:16, :], CL1[:, g, :], v1[pb][:], True, False, ["CL1", "v1_%d" % pb], ["PY%d" % pp])
                    mm(PY[pp][0:16, :], CL2[:, g, :], v2[pb][:], False, True, ["CL2", "v2_%d" % pb], ["PY%d" % pp])

                def st_i(n):
                    g, q, bb, j, gp, pb, t0, tok = info(n)
                    pp = n % 2
                    yk = "ysm%d" % pb
                    cp("act", ysm[pb][0:16, :], PY[pp][0:16, :], ["PY%d" % pp], [yk])
                    S.dma("sp", Y5s[g * 16:(g + 1) * 16, tok:tok + 512], ysm[pb][0:16, :], r=[yk], w=["Y5s"], stream=yk)

                tables(0)
                stages_c = [st_a, st_b, st_c, st_d, st_e, st_f, st_g, st_h, st_i]
                side = []
                nxt_et = 0
                for k in range(len(pieces) + len(stages_c) - 1):
                    for s_ in reversed(range(len(stages_c))):
                        n = k - s_
                        if 0 <= n < len(pieces):
                            stages_c[s_](n)
                    if k % 4 == 0 and nxt_et < 128:
                        side.append(m0_tile(nxt_et))
                        nxt_et += 1
                    for g_ in list(side):
                        try:
                            next(g_)
                        except StopIteration:
                            side.remove(g_)
                while side or nxt_et < 128:
                    if nxt_et < 128:
                        side.append(m0_tile(nxt_et))
                        nxt_et += 1
                    for g_ in list(side):
                        try:
                            next(g_)
                        except StopIteration:
                            side.remove(g_)
                S.flush()

            with ExitStack() as p2:
                B_ = lambda n, sh, dt=F32: p2.enter_context(nc.sbuf_tensor("C2_" + n, sh, dt))
                Pp = lambda n, sh, dt=F32: p2.enter_context(nc.psum_tensor("C2_" + n, sh, dt))
                y5 = [B_("y5_%d" % i, [128, 512]) for i in range(3)]
                uu = [B_("uu%d" % i, [128, 512], BF16) for i in range(3)]
                yv = [B_("yv%d" % i, [128, 512]) for i in range(3)]
                vb = [B_("vb%d" % i, [128, 512], BF16) for i in range(3)]
                sg = [B_("sg%d" % i, [128, 512]) for i in range(3)]
                oo = [B_("oo%d" % i, [128, 8, 512]) for i in range(2)]
                sq = [B_("sq%d" % i, [128, 512]) for i in range(3)]
                rs5 = [B_("rs5_%d" % i, [128, 512]) for i in range(2)]
                ycb = [B_("ycb%d" % i, [128, 512], BF16) for i in range(2)]
                PG = [Pp("PG%d" % i, [128, 512]) for i in range(2)]
                PSS = [Pp("PSS%d" % i, [128, 512]) for i in range(2)]
                NBK = T // 512

                def cinfo(n):
                    return n // 8, n % 8, n % 3, (n // 8) * 512

                def c_a(n):
                    blk, j, r3, tok = cinfo(n)
                    S.dma("sp", y5[r3][:], Y5s[j * 128:(j + 1) * 128, tok:tok + 512], r=["Y5s"], w=["y5_%d" % r3],
                          stream="y5_%d" % r3)
                    S.dma("sp", uu[r3][:], UTs[j * 128:(j + 1) * 128, tok:tok + 512], r=["UTs"], w=["uu%d" % r3],
                          stream="uu%d" % r3)

                def c_b(n):
                    blk, j, r3, tok = cinfo(n)
                    stt(yv[r3][:], uu[r3][:], D5c[:, j:j + 1], y5[r3][:], ALU.mult, ALU.add,
                        ["uu%d" % r3, "D5c", "y5_%d" % r3], ["yv%d" % r3])

                def c_c(n):
                    blk, j, r3, tok = cinfo(n)
                    act(vb[r3][:], yv[r3][:], AF.Gelu, ["yv%d" % r3], ["vb%d" % r3])

                def c_d(n):
                    blk, j, r3, tok = cinfo(n)
                    mm(PG[n % 2][:, :], Wg[:, j, :], vb[r3][:], True, True, ["Wg", "vb%d" % r3], ["PG%d" % (n % 2)])

                def c_e(n):
                    blk, j, r3, tok = cinfo(n)
                    act(sg[r3][:], PG[n % 2][:, :], AF.Sigmoid, ["PG%d" % (n % 2), "glub"], ["sg%d" % r3],
                        bias=glub[:, j:j + 1])

                def c_f(n):
                    blk, j, r3, tok = cinfo(n)
                    tt("dve", oo[blk % 2][:, j, :], vb[r3][:], sg[r3][:], ALU.mult, ["vb%d" % r3, "sg%d" % r3],
                       ["oo%d_%d" % (blk % 2, j)])

                def c_g(n):
                    blk, j, r3, tok = cinfo(n)
                    tt("pool", sq[r3][:], oo[blk % 2][:, j, :], oo[blk % 2][:, j, :], ALU.mult,
                       ["oo%d_%d" % (blk % 2, j)], ["sq%d" % r3])

                def c_h(n):
                    blk, j, r3, tok = cinfo(n)
                    mm(PSS[blk % 2][:, :], ones_f, sq[r3][:], j == 0, j == 7, ["cst", "sq%d" % r3], ["PSS%d" % (blk % 2)])

                def c_tail(blk):
                    bp = blk % 2
                    tok = blk * 512
                    rk = "rs5_%d" % bp
                    ts("dve", rs5[bp][:], PSS[bp][:, :], 1.0 / 1024, EPS, ALU.mult, ALU.add, ["PSS%d" % bp], [rk])
                    yield
                    act(rs5[bp][:], rs5[bp][:], AF.Sqrt, [rk], [rk])
                    yield
                    S.op("dve", lambda e: e.reciprocal(out=rs5[bp][:], in_=rs5[bp][:]), r=[rk], w=[rk])
                    yield
                    for j in range(8):
                        jp = j % 2
                        stt(ycb[jp][:], oo[bp][:, j, :], g5c[:, j:j + 1], rs5[bp][:], ALU.mult, ALU.mult,
                            ["oo%d_%d" % (bp, j), "g5c", rk], ["ycb%d" % jp])
                        S.dma("sp", YCs[1024 + j * 128:1024 + (j + 1) * 128, tok:tok + 512], ycb[jp][:],
                              r=["ycb%d" % jp], w=["YCs"], stream="ycb%d" % jp)
                        yield

                st2 = [c_a, c_b, c_c, c_d, c_e, c_f, c_g, c_h]
                NI2 = NBK * 8
                side2 = []
                for k in range(NI2 + len(st2) - 1):
                    for s_ in reversed(range(len(st2))):
                        n = k - s_
                        if 0 <= n < NI2:
                            st2[s_](n)
                    nh = k - (len(st2) - 1)
                    if nh >= 0 and nh % 8 == 7:
                        side2.append(c_tail(nh // 8))
                    for g_ in list(side2):
                        try:
                            next(g_)
                        except StopIteration:
                            side2.remove(g_)
                while side2:
                    for g_ in list(side2):
                        try:
                            next(g_)
                        except StopIteration:
                            side2.remove(g_)
                S.flush()
        if stop_after == 3:
            return nc

        with ExitStack() as ph:
            A = lambda n, sh, dt=F32: ph.enter_context(nc.sbuf_tensor("D_" + n, sh, dt))
            P = lambda n, sh, dt=F32: ph.enter_context(nc.psum_tensor("D_" + n, sh, dt))
            wout = A("wout", [128, 16, D], BF16)
            wq = A("wq", [128, 8, 2048], BF16)
            skf = A("skf", [128, 16, 128])
            skT = A("skT", [128, 16, 128], BF16)
            GT1 = A("GT1", [128, D]); G2 = A("G2", [128, D]); SH2 = A("SH2", [128, D])
            yct = [A("yct%d" % i, [128, 16, 128], BF16) for i in range(2)]
            xin = [A("xin%d" % i, [128, D]) for i in range(2)]
            t1 = A("t1", [128, D]); x1 = [A("x1_%d" % i, [128, D]) for i in range(2)]
            junk = A("junk", [128, D], BF16)
            ss2 = A("ss2", [128, 32])
            hb2 = A("hb2", [128, D], BF16)
            h2T = [A("h2T%d" % i, [128, 8, 128], BF16) for i in range(2)]
            qT = A("qT", [128, 16, 128], BF16)
            scb = [A("sc_%d" % i, [128, 16, 128]) for i in range(2)]; sc2 = A("sc2", [128, 16, 128])
            v8 = A("v8", [128, 16, 16]); i8 = A("i8", [128, 16, 16], U32); i8f = A("i8f", [128, 16, 16])
            cand = A("cand", [128, 8, 256]); cand2 = A("cand2", [128, 8, 256])
            c8 = A("c8", [128, 8, 16]); p8 = A("p8", [128, 8, 16], U32)
            ge = A("ge", [128, 8, 16]); gs = A("gs", [128, 8]); gg = A("gg", [128, 8, 16])
            ra_i = A("ra_i", [128, 128], I32); rb_i = A("rb_i", [128, 128], I32)
            raf = A("raf", [128, 128]); rbf = A("rbf", [128, 128])
            oh = A("oh", [128, 128, 16]); oh2 = A("oh2", [128, 128, 16])
            isel = A("isel", [128, 128]); jsel = A("jsel", [128, 128])
            rstg = [A("rstg%d" % i, [128, 3, 128], BF16) for i in range(2)]
            pT = P("pT", [128, 1024], BF16)
            pM = P("pM", [128, 1024])
            pq = [P("pq%d" % i, [128, 512]) for i in range(2)]
            psc = [P("psc%d" % i, [128, 512]) for i in range(2)]
            pTi = P("pTi", [128, 512])
            iota16 = cst[:, C_IOTA16:C_IOTA16 + 16]

            woutv = I["w_out"].rearrange("(ct p) d -> p ct d", p=128)
            for q4 in range(4):
                S.dma("pool", wout[:, q4 * 4:(q4 + 1) * 4, :], woutv[:, q4 * 4:(q4 + 1) * 4, :], w=["wout"], stream="wout")
            wqv = I["w_query"].rearrange("(kc p) n -> p kc n", p=128)
            for q4 in range(4):
                S.dma("pool", wq[:, q4 * 2:(q4 + 1) * 2, :], wqv[:, q4 * 2:(q4 + 1) * 2, :], w=["wq"], stream="wq")
            S.dma("sp", skf[:], I["sub_keys"].rearrange("m k d -> k m d"), w=["skf"], stream="skf")
            for m in range(16):
                tr(pTi[:, (m % 4) * 128:(m % 4 + 1) * 128], skf[:, m, :], ident_f, ["skf", "cst"], ["pTi"])
                cp("dve", skT[:, m, :], pTi[:, (m % 4) * 128:(m % 4 + 1) * 128], ["pTi"], ["skT"])
            YCv = YCs.rearrange("(ct p) t -> p ct t", p=128)
            H2v = H2Ts.rearrange("(kc p) t -> p kc t", p=128)
            def tile_vars(i):
                return i // 16, i % 2, i * 128

            def front(i):
                b, par, tok0 = tile_vars(i)
                sck = "sc%d" % par
                if i % 16 == 0:
                    S.dma("sp", GT1[:], MODs[b:b + 1, 2048:3072].partition_broadcast(128), r=["MODs"], w=["GT1"], stream="d0")
                    S.dma("sp", G2[:], MODs[b:b + 1, 4096:5120].partition_broadcast(128), r=["MODs"], w=["G2"], stream="d1")
                    S.dma("sp", SH2[:], MODs[b:b + 1, 3072:4096].partition_broadcast(128), r=["MODs"], w=["SH2"], stream="d2")
                yk, xk, x1k, hk = "yct%d" % par, "xin%d" % par, "x1_%d" % par, "h2T%d" % par
                S.dma("sp", yct[par][:], YCv[:, :, tok0:tok0 + 128], r=["YCs"], w=[yk], stream=yk)
                S.dma("sp", xin[par][:], I["x"][tok0:tok0 + 128, :], w=[xk], stream=xk)
                yield
                for half in range(2):
                    for ct in range(16):
                        mm(pM[:, half * 512:(half + 1) * 512], yct[par][:, ct, :], wout[:, ct, half * 512:(half + 1) * 512],
                           ct == 0, ct == 15, [yk, "wout"], ["pM"])
                yield
                tt("dve", t1[:], pM[:, :], GT1[:], ALU.mult, ["pM", "GT1"], ["t1"])
                yield
                tt("pool", x1[par][:], t1[:], xin[par][:], ALU.add, ["t1", xk], [x1k])
                S.dma("sp", X1s[tok0:tok0 + 128, :], x1[par][:], r=[x1k], w=["X1s"], stream=x1k)
                yield
                act(junk[:], x1[par][:], AF.Square, [x1k], ["junk", "ss2"], accum_out=ss2[:, i:i + 1])
                yield
                col = ss2[:, i:i + 1]
                ts("dve", col, col, 1.0 / D, EPS, ALU.mult, ALU.add, ["ss2"], ["ss2"])
                yield
                act(col, col, AF.Sqrt, ["ss2"], ["ss2"])
                yield
                S.op("dve", lambda e: e.reciprocal(out=col, in_=col), r=["ss2"], w=["ss2"])
                stt(t1[:], x1[par][:], ss2[:, i:i + 1], G2[:], ALU.mult, ALU.mult, [x1k, "ss2", "G2"], ["t1"])
                yield
                tt("pool", hb2[:], t1[:], SH2[:], ALU.add, ["t1", "SH2"], ["hb2"])
                yield
                for kc in range(8):
                    tr(pT[:, kc * 128:(kc + 1) * 128], hb2[:, kc * 128:(kc + 1) * 128], ident_b, ["hb2", "cstb"], ["pT"])
                yield
                cp("act", h2T[par][:, :, :], pT[:, :].rearrange("p (k t) -> p k t", k=8), ["pT"], [hk])
                S.dma("sp", H2v[:, :, tok0:tok0 + 128], h2T[par][:, :, :], r=[hk], w=["H2Ts"], stream=hk)
                yield
                for m4 in range(4):
                    pp = m4 % 2
                    for mi in range(4):
                        m = m4 * 4 + mi
                        for kc in range(8):
                            mm(pq[pp][:, mi * 128:(mi + 1) * 128], wq[:, kc, m * 128:(m + 1) * 128], h2T[par][:, kc, :],
                               kc == 0, kc == 7, ["wq", hk], ["pq%d" % pp])
                    cp("act", qT[:, m4 * 4:(m4 + 1) * 4, :], pq[pp][:, :].rearrange("p (m t) -> p m t", m=4),
                       ["pq%d" % pp], ["qT%d" % m4])
                    yield
                for m4 in range(4):
                    pp = m4 % 2
                    for mi in range(4):
                        m = m4 * 4 + mi
                        mm(psc[pp][:, mi * 128:(mi + 1) * 128], qT[:, m, :], skT[:, m, :], True, True,
                           ["qT%d" % m4, "skT"], ["psc%d" % pp])
                    cp("act", scb[par][:, m4 * 4:(m4 + 1) * 4, :], psc[pp][:, :].rearrange("p (m k) -> p m k", m=4),
                       ["psc%d" % pp], [sck])
                    yield

            def back(i):
                b, par, tok0 = tile_vars(i)
                sck = "sc%d" % par
                for m in range(16):
                    S.op("dve", lambda e, m=m: e.max(out=v8[:, m, 0:8], in_=scb[par][:, m, :]), r=[sck], w=["v8a%d" % m])
                yield
                for m in range(16):
                    S.op("dve", lambda e, m=m: e.max_index(out=i8[:, m, 0:8], in_max=v8[:, m, 0:8], in_values=scb[par][:, m, :]),
                         r=[sck, "v8a%d" % m], w=["i8a%d" % m])
                    S.op("dve", lambda e, m=m: e.match_replace(out=sc2[:, m, :], in_to_replace=v8[:, m, 0:8],
                                                               in_values=scb[par][:, m, :], imm_value=-1e30),
                         r=[sck, "v8a%d" % m], w=["sc2_%d" % m])
                    if m % 4 == 3:
                        yield
                for m in range(16):
                    S.op("dve", lambda e, m=m: e.max(out=v8[:, m, 8:16], in_=sc2[:, m, :]), r=["sc2_%d" % m],
                         w=["v8b%d" % m])
                yield
                for m in range(16):
                    S.op("dve", lambda e, m=m: e.max_index(out=i8[:, m, 8:16], in_max=v8[:, m, 8:16],
                                                           in_values=sc2[:, m, :]), r=["sc2_%d" % m, "v8b%d" % m],
                         w=["i8b%d" % m])
                yield
                v8keys = ["v8a%d" % m for m in range(16)] + ["v8b%d" % m for m in range(16)]
                i8keys = ["i8a%d" % m for m in range(16)] + ["i8b%d" % m for m in range(16)]
                cp("dve", i8f[:], i8[:], i8keys, ["i8f"])
                v8v = v8[:, :, :].rearrange("p (h c) r -> p h c r", c=2)
                i8v = i8f[:, :, :].rearrange("p (h c) r -> p h c r", c=2)
                tt("dve", cand[:, :, :].rearrange("p h (r c) -> p h r c", c=16),
                   v8v[:, :, 0, :].unsqueeze(3).to_broadcast([128, 8, 16, 16]),
                   v8v[:, :, 1, :].unsqueeze(2).to_broadcast([128, 8, 16, 16]), ALU.add, v8keys, ["cand"])
                yield
                for h in range(8):
                    S.op("dve", lambda e, h=h: e.max(out=c8[:, h, 0:8], in_=cand[:, h, :]), r=["cand"], w=["c8a%d" % h])
                yield
                for h in range(8):
                    S.op("dve", lambda e, h=h: e.max_index(out=p8[:, h, 0:8], in_max=c8[:, h, 0:8], in_values=cand[:, h, :]),
                         r=["cand", "c8a%d" % h], w=["p8a%d" % h])
                    S.op("dve", lambda e, h=h: e.match_replace(out=cand2[:, h, :], in_to_replace=c8[:, h, 0:8],
                                                               in_values=cand[:, h, :], imm_value=-1e30),
                         r=["cand", "c8a%d" % h], w=["cand2_%d" % h])
                    if h % 4 == 3:
                        yield
                for h in range(8):
                    S.op("dve", lambda e, h=h: e.max(out=c8[:, h, 8:16], in_=cand2[:, h, :]), r=["cand2_%d" % h],
                         w=["c8b%d" % h])
                yield
                for h in range(8):
                    S.op("dve", lambda e, h=h: e.max_index(out=p8[:, h, 8:16], in_max=c8[:, h, 8:16],
                                                           in_values=cand2[:, h, :]), r=["cand2_%d" % h, "c8b%d" % h],
                         w=["p8b%d" % h])
                yield
                c8keys = ["c8a%d" % h for h in range(8)] + ["c8b%d" % h for h in range(8)]
                p8keys = ["p8a%d" % h for h in range(8)] + ["p8b%d" % h for h in range(8)]
                tt("dve", ge[:], c8[:], c8[:, :, 0:1].to_broadcast([128, 8, 16]), ALU.subtract, c8keys, ["ge"])
                yield
                act(ge[:], ge[:], AF.Exp, ["ge"], ["ge"])
                yield
                S.op("dve", lambda e: e.tensor_reduce(out=gs[:], in_=ge[:], axis=AX.X, op=ALU.add), r=["ge"], w=["gs"])
                S.op("dve", lambda e: e.reciprocal(out=gs[:], in_=gs[:]), r=["gs"], w=["gs"])
                tt("dve", gg[:], ge[:], gs[:, :].unsqueeze(2).to_broadcast([128, 8, 16]), ALU.mult, ["ge", "gs"], ["gg"])
                p8i = p8[:, :, :].rearrange("p h k -> p (h k)").bitcast(I32)
                S.op("dve", lambda e: e.tensor_single_scalar(out=ra_i[:], in_=p8i, scalar=4, op=ALU.logical_shift_right),
                     r=p8keys, w=["ra_i"])
                S.op("dve", lambda e: e.tensor_single_scalar(out=rb_i[:], in_=p8i, scalar=15, op=ALU.bitwise_and),
                     r=p8keys, w=["rb_i"])
                cp("dve", raf[:], ra_i[:], ["ra_i"], ["raf"])
                cp("dve", rbf[:], rb_i[:], ["rb_i"], ["rbf"])
                yield
                io3 = iota16.unsqueeze(1).to_broadcast([128, 128, 16])
                for (rf, rk, ci, ohh, ok, sel, sk_) in ((raf, "raf", 0, oh, "oh", isel, "isel"),
                                                        (rbf, "rbf", 1, oh2, "oh2", jsel, "jsel")):
                    eng = "dve" if ci == 0 else "pool"
                    tt("dve", ohh[:], rf[:, :].unsqueeze(2).to_broadcast([128, 128, 16]), io3, ALU.is_equal,
                       [rk, "cst"], [ok])
                    tt(eng, ohh[:, :, :].rearrange("p (h k) r -> p h k r", h=8),
                       ohh[:, :, :].rearrange("p (h k) r -> p h k r", h=8),
                       i8v[:, :, ci, :].unsqueeze(2).to_broadcast([128, 8, 16, 16]), ALU.mult, [ok, "i8f"], [ok])
                    S.op("dve", lambda e, sel=sel, ohh=ohh: e.tensor_reduce(out=sel[:], in_=ohh[:], axis=AX.X, op=ALU.add),
                         r=[ok], w=[sk_])
                    yield
                tr(pTi[:, 0:128], isel[:], ident_f, ["isel", "cst"], ["pTi"])
                tr(pTi[:, 128:256], jsel[:], ident_f, ["jsel", "cst"], ["pTi"])
                tr(pTi[:, 256:384], gg[:, :, :].rearrange("p h k -> p (h k)"), ident_f, ["gg", "cst"], ["pTi"])
                rk_ = "rstg%d" % par
                cp("act", rstg[par][:, :, :], pTi[:, 0:384].rearrange("p (a t) -> p a t", a=3), ["pTi"], [rk_])
                S.dma("sp", RTs[:, :, tok0:tok0 + 128], rstg[par][:, :, :], r=[rk_], w=["RTs"], stream=rk_)

            def interleave(gens):
                gens = [g_ for g_ in gens if g_ is not None]
                while gens:
                    for g_ in list(gens):
                        try:
                            next(g_)
                        except StopIteration:
                            gens.remove(g_)

            interleave([front(0)])
            for i in range(NT):
                interleave([front(i + 1) if i + 1 < NT else None, back(i)])
            S.flush()
        if stop_after == 4:
            return nc

        with ExitStack() as ph:
            A = lambda n, sh, dt=F32: ph.enter_context(nc.sbuf_tensor("M_" + n, sh, dt))
            P = lambda n, sh, dt=F32: ph.enter_context(nc.psum_tensor("M_" + n, sh, dt))
            TB = 256
            G0 = A("G0", [128, 64, TB], BF16)
            G1 = A("G1", [128, 64, TB], BF16)
            utb = [A("utb%d" % i, [128, 2, D], BF16) for i in range(4)]
            vtb = [A("vtb%d" % i, [128, 2, D], BF16) for i in range(4)]
            Pm = [A("Pm%d" % i, [128, 8, 64], BF16) for i in range(4)]
            Q0 = [A("Q0%d" % i, [128, 8, 128], BF16) for i in range(4)]
            Qm = [A("Qm%d" % i, [128, 8, 128], BF16) for i in range(4)]
            h2b = [A("h2b%d" % i, [128, 8, TB], BF16) for i in range(2)]
            rt = [A("rt%d" % i, [128, 3, TB], BF16) for i in range(2)]
            Ag = [A("Ag%d" % i, [128, TB], BF16) for i in range(2)]
            GA = [A("GA%d" % i, [128, TB], BF16) for i in range(2)]
            x1t = [A("x1t%d" % i, [128, D]) for i in range(2)]
            GT2 = A("GT2", [128, D]); nfg = A("nfg", [128, D])
            tm = [A("tm%d" % i, [128, D]) for i in range(2)]; x2 = A("x2", [128, D]); junkm = A("junkm", [128, D], BF16)
            ot = [A("ot%d" % i, [128, D]) for i in range(2)]
            ssf = A("ssf", [128, 32])
            pO = [P("pO%d" % i, [128, 1024]) for i in range(2)]
            pA = [P("pA%d" % i, [128, 512]) for i in range(2)]
            pG = [P("pG%d" % i, [128, 512]) for i in range(2)]
            iota_b = cstb[:, C_IOTA:C_IOTA + 128]
            io32 = iota_b.unsqueeze(1).to_broadcast([128, 32, 128])
            io8 = iota_b.unsqueeze(1).to_broadcast([128, 8, 128])
            H2v = H2Ts.rearrange("(kc p) t -> p kc t", p=128)
            UTv = UTb.rearrange("e p x -> p e x")
            Vv = Vb.rearrange("(e p) d -> p e d", p=128)
            S.dma("sp", nfg[:], I["norm_f_g"][0:1, :].partition_broadcast(128), w=["nfg"], stream="m0")
            Gh = [G0, G1]
            io64 = [iota_b[:, 64 * hf:64 * hf + 64].unsqueeze(1).to_broadcast([128, 8, 64]) for hf in range(2)]
            NBLK = T // TB
            cnts = {"g": 0, "s": 0}
            late = []

            def run_late():
                for f_ in late:
                    f_()
                del late[:]

            def build(blk, hf):
                bp = blk % 2
                tok = blk * TB
                hk, rk = "h2b%d" % bp, "rt%d" % bp
                if hf == 0:
                    S.dma("sp", h2b[bp][:], H2v[:, :, tok:tok + TB], r=["H2Ts"], w=[hk], stream=hk)
                    S.dma("sp", rt[bp][:], RTs[:, :, tok:tok + TB], r=["RTs"], w=[rk], stream=rk)
                    yield
                NG = TB // 8
                base = cnts["s"]
                cnts["s"] += NG

                def dve_part(k):
                    sp_ = (base + k) % 4
                    tsl = slice(k * 8, (k + 1) * 8)
                    bcn = lambda a_, n_: rt[bp][:, a_, tsl].unsqueeze(2).to_broadcast([128, 8, n_])
                    tt("dve", Pm[sp_][:], bcn(0, 64), io64[hf], ALU.is_equal, [rk, "cstb"], ["Pm%d" % sp_])
                    tt("dve", Q0[sp_][:], bcn(1, 128), io8, ALU.is_equal, [rk, "cstb"], ["Q0%d" % sp_])

                def pool_part(k):
                    sp_ = (base + k) % 4
                    tsl = slice(k * 8, (k + 1) * 8)
                    tt("pool", Qm[sp_][:], Q0[sp_][:], rt[bp][:, 2, tsl].unsqueeze(2).to_broadcast([128, 8, 128]),
                       ALU.mult, ["Q0%d" % sp_, rk], ["Qm%d" % sp_])

                def mm_part(k):
                    sp_ = (base + k) % 4
                    for t4 in range(2):
                        gp = cnts["g"] % 2
                        cnts["g"] += 1
                        for ti in range(4):
                            t = t4 * 4 + ti
                            mm(pG[gp][:, ti * 64:(ti + 1) * 64], Qm[sp_][:, t, :], Pm[sp_][:, t, :], True, True,
                               ["Qm%d" % sp_, "Pm%d" % sp_], ["pG%d" % gp])
                        t0 = k * 8 + t4 * 4
                        late.append(lambda gp=gp, t0=t0: cp(
                            "act", Gh[hf][:, :, t0:t0 + 4], pG[gp][:, 0:256].rearrange("p (t i) -> p i t", t=4),
                            ["pG%d" % gp], ["G%d" % hf]))

                dve_part(0)
                dve_part(1)
                dve_part(2)
                yield
                pool_part(0)
                pool_part(1)
                yield
                for k in range(NG):
                    mm_part(k)
                    if k + 3 < NG:
                        late.append(lambda k=k: dve_part(k + 3))
                    if k + 2 < NG:
                        late.append(lambda k=k: pool_part(k + 2))
                    yield

            def final(blk):
                tok = blk * TB
                b_ = tok // SEQ
                if tok % SEQ == 0:
                    S.dma("sp", GT2[:], MODs[b_:b_ + 1, 5120:6144].partition_broadcast(128), r=["MODs"], w=["GT2"],
                          stream="m1")
                for t2 in range(2):
                    tt("dve", tm[t2][:], pO[t2][:, :], GT2[:], ALU.mult, ["pO%d" % t2, "GT2"], ["tm%d" % t2])
                yield
                for t2 in range(2):
                    ti = blk * 2 + t2
                    tk0 = tok + t2 * 128
                    xk, ok_ = "x1t%d" % t2, "ot%d" % t2
                    col = ssf[:, ti % 32:ti % 32 + 1]
                    S.dma("sp", x1t[t2][:], X1s[tk0:tk0 + 128, :], r=["X1s"], w=[xk], stream=xk)
                    yield
                    tt("pool", x2[:], tm[t2][:], x1t[t2][:], ALU.add, ["tm%d" % t2, xk], ["x2"])
                    yield
                    act(junkm[:], x2[:], AF.Square, ["x2"], ["junkm", "ssf"], accum_out=col)
                    yield
                    ts("dve", col, col, 1.0 / D, EPS, ALU.mult, ALU.add, ["ssf"], ["ssf"])
                    yield
                    act(col, col, AF.Sqrt, ["ssf"], ["ssf"])
                    yield
                    S.op("dve", lambda e, col=col: e.reciprocal(out=col, in_=col), r=["ssf"], w=["ssf"])
                    stt(ot[t2][:], x2[:], col, nfg[:], ALU.mult, ALU.mult, ["x2", "ssf", "nfg"], [ok_])
                    yield
                    S.dma("sp", out[tk0:tk0 + 128, :], ot[t2][:], r=[ok_], w=["out"], stream=ok_)
                    yield

            def prefetch(gg):
                if gg >= NBLK * 64:
                    return
                up = gg % 4
                i0 = (gg % 64) * 2
                S.dma("sp", utb[up][:], UTv[:, i0:i0 + 2, :], r=["UTb"], w=["utb%d" % up], stream="utb%d" % up)
                S.dma("sp", vtb[up][:], Vv[:, i0:i0 + 2, :], r=["Vb"], w=["vtb%d" % up], stream="vtb%d" % up)

            def emitA(blk, i):
                bp = blk % 2
                gg = blk * 64 + i // 2
                up = gg % 4
                if gg == 0 and i == 0:
                    prefetch(0)
                    prefetch(1)
                    prefetch(2)
                ap_ = i % 2
                for kc in range(8):
                    mm(pA[ap_][:, 0:256], utb[up][:, i % 2, kc * 128:(kc + 1) * 128], h2b[bp][:, kc, :],
                       kc == 0, kc == 7, ["utb%d" % up, "h2b%d" % bp], ["pA%d" % ap_])

            def emitG(blk, i):
                ap_ = i % 2
                hf = i // 64
                act(Ag[ap_][:], pA[ap_][:, 0:256], AF.Gelu, ["pA%d" % ap_], ["Ag%d" % ap_])
                tt("dve", GA[ap_][:], Ag[ap_][:], Gh[hf][:, i % 64, :], ALU.mult, ["Ag%d" % ap_, "G%d" % hf], ["GA%d" % ap_])

            def emitVm(blk, i):
                gg = blk * 64 + i // 2
                up = gg % 4
                vk = "vtb%d" % up
                ap_ = i % 2
                for t2 in range(2):
                    for half in range(2):
                        mm(pO[t2][:, half * 512:(half + 1) * 512], GA[ap_][:, t2 * 128:(t2 + 1) * 128],
                           vtb[up][:, i % 2, half * 512:(half + 1) * 512], i == 0, i == 127,
                           ["GA%d" % ap_, vk], ["pO%d" % t2])
                if i % 2 == 0:
                    prefetch(gg + 3)

            def step(gens):
                for g_ in list(gens):
                    try:
                        next(g_)
                    except StopIteration:
                        gens.remove(g_)

            for _ in build(0, 0):
                run_late()
            run_late()
            side = []
            for blk in range(NBLK):
                side.append(build(blk, 1))
                emitA(blk, 0)
                emitA(blk, 1)
                emitG(blk, 0)
                bld = side[-1]
                for i in range(128):
                    if i == 63:
                        for _ in bld:
                            run_late()
                        run_late()
                    if i == 64 and blk + 1 < NBLK:
                        bld = build(blk + 1, 0)
                        side.append(bld)
                    step(side)
                    if i + 2 < 128:
                        emitA(blk, i + 2)
                    if i + 1 < 128:
                        emitG(blk, i + 1)
                    run_late()
                    emitVm(blk, i)
                for _ in bld:
                    run_late()
                run_late()
                fg = final(blk)
                next(fg)
                side.append(fg)
            while side:
                step(side)
                run_late()
            S.flush()
        return nc


def prep_inputs(inputs):
    sq = lambda a: np.ascontiguousarray(a[0]) if a.shape[0] == 1 and a.ndim >= 2 else np.ascontiguousarray(a)
    shared = {}
    for n, sh in IN_SPECS:
        if n in ("x", "c", "consts"):
            continue
        a = np.asarray(inputs[n], dtype=np.float32)
        shared[n] = np.ascontiguousarray(a.reshape(sh))
    shared["consts"] = make_consts()
    x = np.asarray(inputs["x"], dtype=np.float32)
    c = np.asarray(inputs["c"], dtype=np.float32)
    maps = []
    for i in range(NCORES):
        m = dict(shared)
        m["x"] = np.ascontiguousarray(x[i * NB:(i + 1) * NB].reshape(T, D))
        m["c"] = np.ascontiguousarray(c[i * NB:(i + 1) * NB])
        maps.append(m)
    return maps


def kernel(**inputs):
    nc = build()
    maps = prep_inputs(inputs)
    res = run_bass_kernel_spmd(nc, maps, core_ids=list(range(NCORES)))
    outs = [np.asarray(r["out"]).reshape(NB, SEQ, D) for r in res.results]
    return np.concatenate(outs, axis=0).astype(np.float32)
```

```python
import os
from contextlib import ExitStack

import numpy as np
import concourse.bass as bass
import concourse.mybir as mybir
from concourse.bass_utils import run_bass_kernel_spmd

F32 = mybir.dt.float32
BF16 = mybir.dt.bfloat16
I32 = mybir.dt.int32
U32 = mybir.dt.uint32
AF = mybir.ActivationFunctionType
ALU = mybir.AluOpType
AX = mybir.AxisListType

NCORES = 8
D = 1024
NB = 2
SEQ = 2048
T = NB * SEQ
NT = T // 128
INW = 4112
EPS = 1e-6
MAGIC = 12582912.0
TWO_PI = 6.283185307179586


class _Op:
    __slots__ = ("eng", "fn", "dom", "order", "waits", "target", "val", "is_dma")

    def __init__(self, eng, fn, dom, order, is_dma):
        self.eng, self.fn, self.dom, self.order, self.is_dma = eng, fn, dom, order, is_dma
        self.waits = []
        self.target = is_dma
        self.val = None


class Sched:
    CE = ("pe", "act", "dve", "pool")

    def __init__(self, nc, stack):
        self.nc = nc
        self.stack = stack
        self.q = {k: [] for k in ("pe", "act", "dve", "pool", "sp")}
        self.sem = {k: stack.enter_context(nc.semaphore("c_" + k)) for k in self.CE}
        self.cnt = {k: 0 for k in self.CE}
        self.order = {k: 0 for k in self.CE}
        self.dsem = {}
        self.dcnt = {}
        self.dslot = {}
        self.dfree = []
        self.dorder = {}
        self.waited = {k: {} for k in self.q}
        self.lastw = {}
        self.readers = {}
        self.lastop = {}
        self.ninst = 0

    def _need(self, eng, p, out):
        if self.waited[eng].get(p.dom, 0) >= p.order:
            return
        cur = out.get(p.dom)
        if cur is None or cur.order < p.order:
            out[p.dom] = p

    def _deps(self, eng, r, w, is_dma=False):
        need = {}
        for b in r:
            p = self.lastw.get(b)
            if p is not None and (is_dma or not (p.eng == eng and eng == "pe" and not p.is_dma)):
                self._need(eng, p, need)
        for b in w:
            p = self.lastw.get(b)
            if p is not None and (is_dma or p.is_dma or p.eng != eng or eng != "pe"):
                self._need(eng, p, need)
            for p in self.readers.get(b, ()):
                if is_dma or p.is_dma or p.eng != eng or eng != "pe":
                    self._need(eng, p, need)
        return need

    def _add(self, op, need, r, w):
        for dom, p in need.items():
            self.waited[op.eng][dom] = p.order
            p.target = True
            op.waits.append(p)
        self.q[op.eng].append(op)
        self.lastop[op.dom] = op
        for b in r:
            self.readers.setdefault(b, []).append(op)
        for b in w:
            self.lastw[b] = op
            self.readers[b] = []
        self.ninst += 1

    def op(self, eng, fn, r=(), w=()):
        need = self._deps(eng, r, w)
        self.order[eng] += 1
        o = _Op(eng, fn, eng, self.order[eng], False)
        self._add(o, need, r, w)

    def dma(self, eng, out, in_, r=(), w=(), stream="d", **kw):
        if eng == "pool":
            key = "swd_%d" % len(self.dsem)
            self.dsem[key] = self.stack.enter_context(self.nc.semaphore(key))
            self.dcnt[key] = 0
            self.dorder[key] = 0
            self.dslot["__swd__" + key] = key
            stream = "__swd__" + key
        if stream not in self.dslot:
            if self.dfree:
                self.dslot[stream] = self.dfree.pop()
            else:
                k = "dma_%d" % len(self.dsem)
                self.dsem[k] = self.stack.enter_context(self.nc.semaphore(k))
                self.dcnt[k] = 0
                self.dorder[k] = 0
                self.dslot[stream] = k
        key = self.dslot[stream]
        need = self._deps(eng, r, w, is_dma=True)
        self.dorder[key] += 1
        fn = lambda e, out=out, in_=in_, kw=kw: e.dma_start(out=out, in_=in_, **kw)
        o = _Op(eng, fn, key, self.dorder[key], True)
        self._add(o, need, r, w)

    def barrier(self):
        lasts = list(self.lastop.values())
        for eng in self.q:
            need = {}
            for p in lasts:
                self._need(eng, p, need)
            if need:
                o = _Op(eng, None, None, 0, False)
                for dom, p in need.items():
                    self.waited[eng][dom] = p.order
                    p.target = True
                    o.waits.append(p)
                self.q[eng].append(o)
        self.lastw = {}
        self.readers = {}

    def flush(self):
        nc = self.nc
        self.barrier()
        q = self.q
        for eng in q:
            for o in q[eng]:
                if o.fn is None:
                    continue
                if o.is_dma:
                    self.dcnt[o.dom] += 16
                    o.val = self.dcnt[o.dom]
                elif o.target:
                    self.cnt[eng] += 1
                    o.val = self.cnt[eng]
        sem, dsem = self.sem, self.dsem

        def run(e, ops):
            for o in ops:
                for p in o.waits:
                    e.wait_ge(dsem[p.dom] if p.is_dma else sem[p.dom], p.val)
                if o.fn is None:
                    continue
                ins = o.fn(e)
                if o.is_dma:
                    ins.then_inc(dsem[o.dom], 16)
                elif o.target:
                    ins.then_inc(sem[o.eng], 1)

        with nc.Block() as block:
            @block.tensor
            def _(e):
                run(e, q["pe"])

            @block.scalar
            def _(e):
                run(e, q["act"])

            @block.vector
            def _(e):
                run(e, q["dve"])

            @block.gpsimd
            def _(e):
                run(e, q["pool"])

            @block.sync
            def _(e):
                run(e, q["sp"])
        for k in q:
            q[k] = []
        self.dfree.extend(v for k_, v in self.dslot.items() if not k_.startswith("__swd__"))
        self.dslot = {}
        self.lastop = {}


C_IDENT, C_TRIU, C_TRIS, C_ONES, C_IOTA, C_BD16, C_RM8, C_IOTA16, C_END = (
    0, 128, 256, 384, 512, 640, 768, 776, 792)


def make_consts():
    c = np.zeros((128, C_END), np.float32)
    k = np.arange(128)
    c[:, C_IDENT:C_IDENT + 128] = np.eye(128)
    c[:, C_TRIU:C_TRIU + 128] = (k[:, None] <= k[None, :])
    c[:, C_TRIS:C_TRIS + 128] = (k[:, None] > k[None, :])
    c[:, C_ONES:C_ONES + 128] = 1.0
    c[:, C_IOTA:C_IOTA + 128] = k[None, :]
    c[:, C_BD16:C_BD16 + 128] = (k[:, None] // 16 == k[None, :] // 16)
    c[:, C_RM8:C_RM8 + 8] = (k[:, None] // 16 == np.arange(8)[None, :])
    c[:, C_IOTA16:C_IOTA16 + 16] = np.arange(16)[None, :]
    return c


def skew_pipeline(stages, n_items):
    ns = len(stages)
    for k in range(n_items + ns - 1):
        for s_ in reversed(range(ns)):
            n = k - s_
            if 0 <= n < n_items:
                stages[s_](n)


IN_SPECS = [
    ("x", [T, D]), ("c", [NB, D]), ("w_ada", [D, 6 * D]), ("b_ada", [1, 6 * D]),
    ("norm1_g", [1, D]), ("w_in", [D, INW]), ("conv_w", [4, 2048]), ("conv_b", [1, 2048]),
    ("dt_bias", [1, 16]), ("a_log", [1, 16]), ("d_ssd", [1, 16]), ("norm_ssd_g", [1, D]),
    ("s5_a_re", [64, 64]), ("s5_a_im", [64, 64]), ("s5_log_dt", [1, 64]),
    ("s5_b_re", [64, 64, 16]), ("s5_b_im", [64, 64, 16]), ("s5_c_re", [64, 16, 64]),
    ("s5_c_im", [64, 16, 64]), ("s5_d", [64, 16]), ("glu_w", [64, 16, 16]), ("glu_b", [64, 16]),
    ("norm_s5_g", [1, D]), ("w_out", [2 * D, D]), ("norm2_g", [1, D]), ("w_query", [D, 2048]),
    ("sub_keys", [16, 128, 128]), ("expert_u", [16384, D]), ("expert_v", [16384, D]),
    ("norm_f_g", [1, D]), ("consts", [128, C_END]),
]


def build(debug=(), stop_after=None):
    nc = bass.Bass("TRN2", target_bir_lowering=False)
    I = {n: nc.dram_tensor(n, sh, F32, kind="ExternalInput").ap() for n, sh in IN_SPECS}
    out = nc.dram_tensor("out", [T, D], F32, kind="ExternalOutput").ap()

    def SCR(name, shape, dt):
        kind = "ExternalOutput" if name in debug else "Internal"
        return nc.dram_tensor(name, shape, dt, kind=kind).ap()

    MODs = SCR("MODs", [NB, 6 * D], F32)
    XCs = SCR("XCs", [2048, T], BF16)
    UTs = SCR("UTs", [1024, T], BF16)
    Zs = SCR("Zs", [T, D], BF16)
    DTs = SCR("DTs", [T, 16], F32)
    YCs = SCR("YCs", [2048, T], BF16)
    Y5s = SCR("Y5s", [1024, T], F32)
    X1s = SCR("X1s", [T, D], F32)
    H2Ts = SCR("H2Ts", [D, T], BF16)
    RTs = SCR("RTs", [128, 3, T], BF16)
    UTb = SCR("UTb", [128, 128, 1024], BF16)
    Vb = SCR("Vb", [16384, D], BF16)

    with ExitStack() as top:
        S = Sched(nc, top)

        def mm(out_, lhsT, rhs, start, stop, r, w):
            S.op("pe", lambda e: e.matmul(out_, lhsT=lhsT, rhs=rhs, start=start, stop=stop), r=r, w=w)

        def tr(out_, in_, ident, r, w):
            S.op("pe", lambda e: e.transpose(out=out_, in_=in_, identity=ident), r=r, w=w)

        def act(out_, in_, func, r, w, eng="act", **kw):
            S.op(eng, lambda e: e.activation(out=out_, in_=in_, func=func, **kw), r=r, w=w)

        def tt(eng, out_, in0, in1, op, r, w):
            S.op(eng, lambda e: e.tensor_tensor(out=out_, in0=in0, in1=in1, op=op), r=r, w=w)

        def ts(eng, out_, in0, s1, s2, op0, op1, r, w):
            if s2 is None:
                S.op(eng, lambda e: e.tensor_scalar(out=out_, in0=in0, scalar1=s1, scalar2=None, op0=op0), r=r, w=w)
            else:
                S.op(eng, lambda e: e.tensor_scalar(out=out_, in0=in0, scalar1=s1, scalar2=s2, op0=op0, op1=op1),
                     r=r, w=w)

        def stt(out_, in0, scalar, in1, op0, op1, r, w):
            S.op("dve", lambda e: e.scalar_tensor_tensor(out=out_, in0=in0, scalar=scalar, in1=in1, op0=op0, op1=op1),
                 r=r, w=w)

        def cp(eng, out_, in_, r, w):
            if eng == "act":
                act(out_, in_, AF.Copy, r, w)
            else:
                S.op(eng, lambda e: e.tensor_copy(out=out_, in_=in_), r=r, w=w)

        def rsqrt(col, n, r, w):
            ts("dve", col, col, 1.0 / n, EPS, ALU.mult, ALU.add, r, w)
            act(col, col, AF.Sqrt, w, w)
            S.op("dve", lambda e: e.reciprocal(out=col, in_=col), r=w, w=w)

        cst = top.enter_context(nc.sbuf_tensor("cst", [128, C_END], F32))
        cstb = top.enter_context(nc.sbuf_tensor("cstb", [128, C_END], BF16))
        S.dma("sp", cst[:], I["consts"][:, :], w=["cst"], stream="cst")
        cp("dve", cstb[:], cst[:], ["cst"], ["cstb"])
        ident_f = cst[:, C_IDENT:C_IDENT + 128]
        ident_b = cstb[:, C_IDENT:C_IDENT + 128]
        triu_f = cst[:, C_TRIU:C_TRIU + 128]
        tris_f = cst[:, C_TRIS:C_TRIS + 128]
        ones_f = cst[:, C_ONES:C_ONES + 128]
        ones_b = cstb[:, C_ONES:C_ONES + 128]

        with ExitStack() as ph:
            A = lambda n, sh, dt=F32: ph.enter_context(nc.sbuf_tensor(n, sh, dt))
            cT = A("cT", [128, 8, NB])
            rep = A("rep", [128, 8, NB, 128])
            bada = A("bada", [128, 6 * D])
            g1bc = A("g1bc", [128, D])
            g2bc = A("g2bc", [128, D])
            wa = [A("wa%d" % i, [128, 8, 512]) for i in range(2)]
            modbc = [A("modbc%d" % b, [128, 6 * D]) for b in range(NB)]
            pm = [ph.enter_context(nc.psum_tensor("pm%d" % i, [128, 512], F32)) for i in range(2)]
            for b in range(NB):
                S.dma("sp", cT[:, :, b], I["c"][b:b + 1, :].rearrange("o (kc p) -> p (o kc)", p=128), w=["cT"],
                      stream="p0", allow_slow_non_contiguous=True)
            S.dma("sp", bada[:], I["b_ada"][0:1, :].partition_broadcast(128), w=["bada"], stream="p0b")
            S.dma("sp", g1bc[:], I["norm1_g"][0:1, :].partition_broadcast(128), w=["g1bc"], stream="p0c")
            S.dma("sp", g2bc[:], I["norm2_g"][0:1, :].partition_broadcast(128), w=["g2bc"], stream="p0d")
            act(cT[:], cT[:], AF.Silu, ["cT"], ["cT"])
            cp("dve", rep[:], cT[:].unsqueeze(3).to_broadcast([128, 8, NB, 128]), ["cT"], ["rep"])
            wav = I["w_ada"].rearrange("(kc p) n -> p kc n", p=128)
            for n in range(12):
                wb = wa[n % 2]
                wk = "wa%d" % (n % 2)
                S.dma("sp", wb[:], wav[:, :, n * 512:(n + 1) * 512], w=[wk], stream=wk)
                for b in range(NB):
                    pk = "pm%d" % b
                    for kc in range(8):
                        mm(pm[b][:, :], rep[:, kc, b, :], wb[:, kc, :], kc == 0, kc == 7, ["rep", wk], [pk])
                    tt("dve", modbc[b][:, n * 512:(n + 1) * 512], pm[b][:, :], bada[:, n * 512:(n + 1) * 512],
                       ALU.add, [pk, "bada"], ["modbc%d" % b])
            for b in range(NB):
                mk = "modbc%d" % b
                stt(modbc[b][:, 1024:2048], modbc[b][:, 1024:2048], 1.0, g1bc[:], ALU.add, ALU.mult, [mk, "g1bc"], [mk])
                stt(modbc[b][:, 4096:5120], modbc[b][:, 4096:5120], 1.0, g2bc[:], ALU.add, ALU.mult, [mk, "g2bc"], [mk])
                S.dma("sp", MODs[b:b + 1, :], modbc[b][0:1, :], r=[mk], w=["MODs"], stream="p0s")
            S.flush()
        if stop_after == 0:
            return nc

        with ExitStack() as ph:
            A = lambda n, sh, dt=F32: ph.enter_context(nc.sbuf_tensor(n, sh, dt))
            P = lambda n, sh, dt=F32: ph.enter_context(nc.psum_tensor(n, sh, dt))
            win = A("win", [128, 8, INW], BF16)
            hT = A("hT", [128, 8, SEQ], BF16)
            G1 = A("G1", [128, D])
            SH1 = A("SH1", [128, D])
            xin = [A("xin%d" % i, [128, D]) for i in range(5)]
            t1 = [A("t1_%d" % i, [128, D]) for i in range(3)]
            junk = A("junk", [128, D], BF16)
            hb = [A("hb%d" % i, [128, D], BF16) for i in range(3)]
            ss = A("ss", [128, 16])
            zst = [A("zst%d" % i, [128, D], BF16) for i in range(2)]
            dts = [A("dts%d" % i, [128, 16]) for i in range(2)]
            xpad = [A("xpad%d" % i, [128, 3 + SEQ]) for i in range(2)]
            acc = [A("acc%d" % i, [128, SEQ]) for i in range(2)]
            xo = [A("xo%d" % i, [128, SEQ], BF16) for i in range(2)]
            cw = A("cw", [128, 16, 4])
            cb = A("cb", [128, 16])
            pT = [P("pT%d" % i, [128, 1024], BF16) for i in range(2)]
            pz = P("pz", [128, 1024])
            pdt = P("pdt", [128, 16])
            pc = [P("pc%d" % i, [128, 512]) for i in range(2)]

            winv = I["w_in"].rearrange("(kc p) n -> p kc n", p=128)
            for kc in range(8):
                S.dma("pool", win[:, kc, :], winv[:, kc, :], w=["win%d" % kc], stream="win")
            for k in range(4):
                S.dma("sp", cw[:, :, k], I["conv_w"][k:k + 1, :].rearrange("o (ct p) -> p (o ct)", p=128), w=["cw"],
                      stream="cw", allow_slow_non_contiguous=True)
            S.dma("sp", cb[:], I["conv_b"].rearrange("o (ct p) -> p (o ct)", p=128), w=["cb"], stream="cb",
                  allow_slow_non_contiguous=True)
            for i in range(2):
                S.op("dve", lambda e, i=i: e.memset(xpad[i][:, 0:3], 0.0), w=["xpad%d" % i])

            def tokgen(b, i):
                tok0 = b * SEQ + i * 128
                r3, r2 = i % 3, i % 2
                xk, tk_, hk, pk = "xin%d" % (i % 5), "t1_%d" % r3, "hb%d" % r3, "pT%d" % r2
                xt = xin[i % 5]
                col = ss[:, i:i + 1]
                S.dma("sp", xt[:], I["x"][tok0:tok0 + 128, :], w=[xk], stream=xk)
                yield
                act(junk[:], xt[:], AF.Square, [xk], ["junk", "ss%d" % i], accum_out=col)
                yield
                ts("dve", col, col, 1.0 / D, EPS, ALU.mult, ALU.add, ["ss%d" % i], ["ss%d" % i])
                yield
                act(col, col, AF.Sqrt, ["ss%d" % i], ["ss%d" % i])
                yield
                S.op("dve", lambda e: e.reciprocal(out=col, in_=col), r=["ss%d" % i], w=["ss%d" % i])
                stt(t1[r3][:], xt[:], col, G1[:], ALU.mult, ALU.mult, [xk, "ss%d" % i, "G1"], [tk_])
                yield
                tt("pool", hb[r3][:], t1[r3][:], SH1[:], ALU.add, [tk_, "SH1"], [hk])
                yield
                for kc in range(8):
                    tr(pT[r2][:, kc * 128:(kc + 1) * 128], hb[r3][:, kc * 128:(kc + 1) * 128], ident_b, [hk, "cstb"], [pk])
                yield
                cp("act", hT[:, :, i * 128:(i + 1) * 128], pT[r2][:, :].rearrange("p (k t) -> p k t", k=8), [pk],
                   ["hT%d" % i])
                yield
                for half in range(2):
                    for kc in range(8):
                        mm(pz[:, half * 512:(half + 1) * 512], hT[:, kc, i * 128:(i + 1) * 128],
                           win[:, kc, half * 512:(half + 1) * 512], kc == 0, kc == 7, ["hT%d" % i, "win%d" % kc], ["pz"])
                for kc in range(8):
                    mm(pdt[:, :], hT[:, kc, i * 128:(i + 1) * 128], win[:, kc, 3072:3088], kc == 0, kc == 7,
                       ["hT%d" % i, "win%d" % kc], ["pdt"])
                yield
                zk, dk = "zst%d" % r2, "dts%d" % r2
                cp("act", zst[r2][:], pz[:, :], ["pz"], [zk])
                cp("dve", dts[r2][:], pdt[:, :], ["pdt"], [dk])
                S.dma("sp", Zs[tok0:tok0 + 128, :], zst[r2][:], r=[zk], w=["Zs"], stream=zk)
                S.dma("sp", DTs[tok0:tok0 + 128, :], dts[r2][:], r=[dk], w=["DTs"], stream=dk)
                yield

            def chgen(b, ct):
                col0 = 1024 + ct * 128 if ct < 16 else 3088 + (ct - 16) * 128
                par = ct % 2
                xpk, xok, ak = "xpad%d" % par, "xo%d" % par, "acc%d" % par
                for blk in range(4):
                    pck = "pc%d" % (blk % 2)
                    for kc in range(8):
                        mm(pc[blk % 2][:, :], win[:, kc, col0:col0 + 128], hT[:, kc, blk * 512:(blk + 1) * 512],
                           kc == 0, kc == 7, ["win%d" % kc] + ["hT%d" % j for j in range(blk * 4, blk * 4 + 4)], [pck])
                    if ct < 16:
                        cp("act", xpad[par][:, 3 + blk * 512:3 + (blk + 1) * 512], pc[blk % 2][:, :], [pck], [xpk])
                    else:
                        cp("act", xo[par][:, blk * 512:(blk + 1) * 512], pc[blk % 2][:, :], [pck], [xok])
                    if blk % 2 == 1:
                        yield
                if ct < 16:
                    xp = xpad[par]
                    ts("dve", acc[par][:], xp[:, 3:3 + SEQ], cw[:, ct, 3:4], cb[:, ct:ct + 1], ALU.mult, ALU.add,
                       [xpk, "cw", "cb"], [ak])
                    yield
                    for k in (2, 1, 0):
                        stt(acc[par][:], xp[:, k:k + SEQ], cw[:, ct, k:k + 1], acc[par][:], ALU.mult, ALU.add,
                            [xpk, "cw", ak], [ak])
                        yield
                    act(xo[par][:], acc[par][:], AF.Silu, [ak], [xok])
                    yield
                    S.dma("sp", XCs[ct * 128:(ct + 1) * 128, b * SEQ:(b + 1) * SEQ], xo[par][:], r=[xok], w=["XCs"],
                          stream=xok)
                else:
                    S.dma("sp", UTs[(ct - 16) * 128:(ct - 15) * 128, b * SEQ:(b + 1) * SEQ], xo[par][:], r=[xok],
                          w=["UTs"], stream=xok)
                yield

            def run_skewed(gens, every):
                active = []
                k = 0
                while gens or active:
                    if gens and k % every == 0:
                        active.append(gens.pop(0))
                    for g_ in list(active):
                        try:
                            next(g_)
                        except StopIteration:
                            active.remove(g_)
                    k += 1

            for b in range(NB):
                S.dma("sp", G1[:], MODs[b:b + 1, 1024:2048].partition_broadcast(128), r=["MODs"], w=["G1"], stream="g1")
                S.dma("sp", SH1[:], MODs[b:b + 1, 0:1024].partition_broadcast(128), r=["MODs"], w=["SH1"], stream="sh1")
                run_skewed([tokgen(b, i) for i in range(16)], 1)
                run_skewed([chgen(b, ct) for ct in range(24)], 4)
            S.flush()
        if stop_after == 1:
            return nc

        with ExitStack() as ph:
            A = lambda n, sh, dt=F32: ph.enter_context(nc.sbuf_tensor("B_" + n, sh, dt))
            P = lambda n, sh, dt=F32: ph.enter_context(nc.psum_tensor("B_" + n, sh, dt))
            dtb = A("dtb", [128, 16]); abc = A("abc", [128, 16]); d16 = A("d16", [128, 16])
            dssd = A("dssd", [128, D]); normg = A("normg", [128, D])
            S32 = A("S32", [128, 16, 64]); Sbf = A("Sbf", [128, 16, 64], BF16)
            RC = 4
            xct = [A("xct%d" % i, [128, 16, 128], BF16) for i in range(RC)]
            zt = [A("zt%d" % i, [128, D], BF16) for i in range(RC)]
            dtr = [A("dtr%d" % i, [128, 16]) for i in range(RC)]
            dtt = [A("dtt%d" % i, [128, 16]) for i in range(RC)]
            da = [A("da%d" % i, [128, 16]) for i in range(RC)]
            c3 = [A("c3_%d" % i, [128, 48]) for i in range(RC)]
            e3 = [A("e3_%d" % i, [128, 48]) for i in range(RC)]
            xs = [A("xs%d" % i, [128, D], BF16) for i in range(RC)]
            Btok = [A("Btok%d" % i, [128, 512], BF16) for i in range(RC)]
            xdt = [A("xdt%d" % i, [128, D], BF16) for i in range(RC)]
            xdd = [A("xdd%d" % i, [128, D], BF16) for i in range(RC)]
            CBm = [A("CBm%d" % i, [128, 4, 128]) for i in range(RC)]
            RH = 3
            lD = [A("lD%d" % i, [128, 128]) for i in range(RH)]
            Lm = [A("Lm%d" % i, [128, 128]) for i in range(RH)]
            Mt = [A("Mt%d" % i, [128, 128], BF16) for i in range(RH)]
            yo = A("yo", [128, D]); y1 = A("y1", [128, D]); xD = A("xD", [128, D]); sz = A("sz", [128, D])
            yg = A("yg", [128, D]); junkb = A("junkb", [128, 256], BF16); ssq = A("ssq", [128, 4])
            yn = A("yn", [128, D]); ynb = A("ynb", [128, D], BF16)
            ynT = [A("ynT%d" % i, [128, 8, 128], BF16) for i in range(2)]
            pTr = P("pTr", [128, 1024], BF16)
            pDs = [P("pD%d" % i, [128, 512]) for i in range(2)]
            pSl = [P("pS%d" % i, [128, 512]) for i in range(2)]
            pPro = P("pPro", [128, 512])
            pY = P("pY", [128, 512])
            pYo = P("pYo", [128, 512])

            S.dma("sp", dtb[:], I["dt_bias"][0:1, :].partition_broadcast(128), w=["dtb"], stream="b0")
            S.dma("sp", abc[:], I["a_log"][0:1, :].partition_broadcast(128), w=["abc"], stream="b1")
            S.dma("sp", d16[:], I["d_ssd"][0:1, :].partition_broadcast(128), w=["d16"], stream="b2")
            S.dma("sp", normg[:], I["norm_ssd_g"][0:1, :].partition_broadcast(128), w=["normg"], stream="b3")
            act(abc[:], abc[:], AF.Exp, ["abc"], ["abc"])
            ts("dve", abc[:], abc[:], -1.0, None, ALU.mult, None, ["abc"], ["abc"])
            cp("dve", dssd[:, :].rearrange("p (h q) -> p h q", q=64), d16[:, :].unsqueeze(2).to_broadcast([128, 16, 64]),
               ["d16"], ["dssd"])
            XCv = XCs.rearrange("(ct p) t -> p ct t", p=128)
            YCv = YCs.rearrange("(ct p) t -> p ct t", p=128)
            v3 = lambda ap: ap.rearrange("p (h q) -> p h q", q=64)
            bc3 = lambda col: col.unsqueeze(2).to_broadcast([128, 16, 64])
            NCH = NB * 16

            def prologue(ci):
                r_ = ci % RC
                tok0 = ci * 128
                K_ = lambda nm: "%s%d" % (nm, r_)
                X = xct[r_]
                S.dma("sp", X[:], XCv[:, :, tok0:tok0 + 128], r=["XCs"], w=[K_("xct")], stream=K_("xct"))
                S.dma("sp", zt[r_][:], Zs[tok0:tok0 + 128, :], r=["Zs"], w=[K_("zt")], stream=K_("zt"))
                S.dma("sp", dtr[r_][:], DTs[tok0:tok0 + 128, :], r=["DTs"], w=[K_("dtr")], stream=K_("dtr"))
                yield
                tt("dve", dtt[r_][:], dtr[r_][:], dtb[:], ALU.add, [K_("dtr"), "dtb"], [K_("dtt")])
                yield
                act(dtt[r_][:], dtt[r_][:], AF.Exp, [K_("dtt")], [K_("dtt")])
                act(dtt[r_][:], dtt[r_][:], AF.Ln, [K_("dtt")], [K_("dtt")], bias=1.0)
                yield
                tt("dve", da[r_][:], dtt[r_][:], abc[:], ALU.mult, [K_("dtt"), "abc"], [K_("da")])
                yield
                mm(pPro[:, 0:16], triu_f, da[r_][:], True, True, ["cst", K_("da")], ["pm_cs"])
                mm(pPro[:, 16:32], ones_f, da[r_][:], True, True, ["cst", K_("da")], ["pm_cs"])
                cp("dve", c3[r_][:, 0:32], pPro[:, 0:32], ["pm_cs"], [K_("c3")])
                tt("dve", c3[r_][:, 32:48], c3[r_][:, 16:32], c3[r_][:, 0:16], ALU.subtract, [K_("c3")], [K_("c3")])
                yield
                act(e3[r_][:], c3[r_][:], AF.Exp, [K_("c3")], [K_("e3")])
                yield
                for j in range(8):
                    tr(pTr[:, j * 128:(j + 1) * 128], X[:, j, :], ident_b, [K_("xct"), "cstb"], ["pTr"])
                cp("act", xs[r_][:], pTr[:, :], ["pTr"], [K_("xs")])
                yield
                for g in range(4):
                    tr(pTr[:, g * 128:(g + 1) * 128], X[:, 8 + g, :], ident_b, [K_("xct"), "cstb"], ["pTr"])
                cp("act", Btok[r_][:], pTr[:, 0:512], ["pTr"], [K_("Btok")])
                yield
                tt("dve", v3(xdt[r_][:, :]), v3(xs[r_][:, :]), bc3(dtt[r_][:, :]), ALU.mult, [K_("xs"), K_("dtt")],
                   [K_("xdt")])
                tt("dve", v3(xdd[r_][:, :]), v3(xdt[r_][:, :]), bc3(e3[r_][:, 32:48]), ALU.mult, [K_("xdt"), K_("e3")],
                   [K_("xdd")])
                yield
                for g in range(4):
                    mm(pPro[:, 128:256], X[:, 8 + g, :], X[:, 12 + g, :], True, True, [K_("xct")], ["pm_cb"])
                    tt("dve", CBm[r_][:, g, :], pPro[:, 128:256], triu_f, ALU.mult, ["pm_cb", "cst"], [K_("CBm") + "_%d" % g])
                    yield

            def epilogue(ci):
                r_ = ci % RC
                tok0 = ci * 128
                c = ci % 16
                K_ = lambda nm: "%s%d" % (nm, r_)
                yield
                tt("pool", xD[:], xs[r_][:], dssd[:], ALU.mult, [K_("xs"), "dssd"], ["xD"])
                yield
                tt("dve", y1[:], y1[:], xD[:], ALU.add, ["y1", "xD"], ["y1"])
                act(sz[:], zt[r_][:], AF.Silu, [K_("zt")], ["sz"])
                yield
                tt("dve", yg[:], y1[:], sz[:], ALU.mult, ["y1", "sz"], ["yg"])
                yield
                for G_ in range(4):
                    act(junkb[:], yg[:, G_ * 256:(G_ + 1) * 256], AF.Square, ["yg"], ["junkb", "ssq"],
                        accum_out=ssq[:, G_:G_ + 1])
                yield
                ts("dve", ssq[:], ssq[:], 1.0 / 256, EPS, ALU.mult, ALU.add, ["ssq"], ["ssq"])
                yield
                act(ssq[:], ssq[:], AF.Sqrt, ["ssq"], ["ssq"])
                yield
                S.op("dve", lambda e: e.reciprocal(out=ssq[:], in_=ssq[:]), r=["ssq"], w=["ssq"])
                tt("dve", yn[:, :].rearrange("p (g q) -> p g q", q=256), yg[:, :].rearrange("p (g q) -> p g q", q=256),
                   ssq[:, :].unsqueeze(2).to_broadcast([128, 4, 256]), ALU.mult, ["yg", "ssq"], ["yn"])
                yield
                tt("pool", ynb[:], yn[:], normg[:], ALU.mult, ["yn", "normg"], ["ynb"])
                yield
                for j in range(8):
                    tr(pTr[:, j * 128:(j + 1) * 128], ynb[:, j * 128:(j + 1) * 128], ident_b, ["ynb", "cstb"], ["pTr"])
                yk = "ynT%d" % (ci % 2)
                cp("act", ynT[ci % 2][:, :, :], pTr[:, :].rearrange("p (k t) -> p k t", k=8), ["pTr"], [yk])
                S.dma("sp", YCv[:, 0:8, tok0:tok0 + 128], ynT[ci % 2][:, :, :], r=[yk], w=["YCs"], stream=yk)
                yield

            def e1_half(ci, hf):
                r_ = ci % RC
                cs_ = slice(hf * 512, (hf + 1) * 512)
                v3h = lambda ap: ap.rearrange("p (h q) -> p h q", q=64)
                if ci % 16 == 0:
                    cp("dve", y1[:, cs_], pY[:, :], ["pY"], ["y1"])
                else:
                    tt("dve", v3h(yo[:, cs_]), v3h(pYo[:, :]),
                       e3[r_][:, hf * 8:hf * 8 + 8].unsqueeze(2).to_broadcast([128, 8, 64]), ALU.mult,
                       ["pYo", "e3_%d" % r_], ["yo"])
                    tt("dve", y1[:, cs_], yo[:, cs_], pY[:, :], ALU.add, ["yo", "pY"], ["y1"])

            def hinfo(n):
                ci, h = n // 16, n % 16
                return ci, h, h // 4, ci % RC, n % RH, ci % 16

            def h_a(n):
                ci, h, g, r_, hr, c = hinfo(n)
                tt("pool", lD[hr][:], tris_f, da[r_][:, h:h + 1].to_broadcast([128, 128]), ALU.mult,
                   ["cst", "da%d" % r_], ["lD%d" % hr])

            def h_b(n):
                ci, h, g, r_, hr, c = hinfo(n)
                mm(pDs[n % 2][:, 0:128], lD[hr][:], triu_f, True, True, ["lD%d" % hr, "cst"], ["pD%d" % (n % 2)])

            def h_c(n):
                ci, h, g, r_, hr, c = hinfo(n)
                act(Lm[hr][:], pDs[n % 2][:, 0:128], AF.Exp, ["pD%d" % (n % 2)], ["Lm%d" % hr])

            def h_d(n):
                ci, h, g, r_, hr, c = hinfo(n)
                tt("dve", Mt[hr][:], Lm[hr][:], CBm[r_][:, g, :], ALU.mult, ["Lm%d" % hr, "CBm%d_%d" % (r_, g)],
                   ["Mt%d" % hr])

            def h_e(n):
                ci, h, g, r_, hr, c = hinfo(n)
                X = xct[r_]
                mm(pY[:, (h % 8) * 64:(h % 8 + 1) * 64], Mt[hr][:], xdt[r_][:, h * 64:(h + 1) * 64], True, True,
                   ["Mt%d" % hr, "xdt%d" % r_], ["pY"])
                if c != 0:
                    mm(pYo[:, (h % 8) * 64:(h % 8 + 1) * 64], X[:, 12 + g, :], Sbf[:, h, :], True, True,
                       ["xct%d" % r_, "Sbf%d" % h], ["pYo"])
                mm(pSl[n % 2][:, 0:64], Btok[r_][:, g * 128:(g + 1) * 128], xdd[r_][:, h * 64:(h + 1) * 64],
                   True, True, ["Btok%d" % r_, "xdd%d" % r_], ["pS%d" % (n % 2)])

            def h_f(n):
                ci, h, g, r_, hr, c = hinfo(n)
                if c == 0:
                    cp("dve", S32[:, h, :], pSl[n % 2][:, 0:64], ["pS%d" % (n % 2)], ["S32_%d" % h])
                else:
                    stt(S32[:, h, :], S32[:, h, :], e3[r_][:, 16 + h:17 + h], pSl[n % 2][:, 0:64],
                        ALU.mult, ALU.add, ["S32_%d" % h, "e3_%d" % r_, "pS%d" % (n % 2)], ["S32_%d" % h])

            def h_g(n):
                ci, h, g, r_, hr, c = hinfo(n)
                cp("pool", Sbf[:, h, :], S32[:, h, :], ["S32_%d" % h], ["Sbf%d" % h])

            stages = [h_a, h_b, h_c, h_d, h_e, h_f, h_g]
            NS = len(stages)
            for _ in prologue(0):
                pass
            for _ in prologue(1):
                pass
            side = []
            NI_ = NCH * 16
            for k in range(NI_ + NS - 1):
                for s_ in reversed(range(NS)):
                    n = k - s_
                    if 0 <= n < NI_:
                        stages[s_](n)
                if k >= 11 and (k - 11) % 16 == 0:
                    e1_half((k - 11) // 16, 0)
                if k >= 19 and (k - 19) % 16 == 0:
                    e1_half((k - 19) // 16, 1)
                    side.append(epilogue((k - 19) // 16))
                if k % 16 == 0 and k // 16 + 2 < NCH:
                    side.append(prologue(k // 16 + 2))
                for g_ in list(side):
                    try:
                        next(g_)
                    except StopIteration:
                        side.remove(g_)
            while side:
                for g_ in list(side):
                    try:
                        next(g_)
                    except StopIteration:
                        side.remove(g_)
            S.flush()
        if stop_after == 2:
            return nc

        with ExitStack() as ph:
            A = lambda n, sh, dt=F32: ph.enter_context(nc.sbuf_tensor(n, sh, dt))
            Blk1 = A("Blk1", [128, 64, 128], BF16)
            Blk2 = A("Blk2", [128, 64, 128], BF16)
            CL1 = A("CL1", [128, 64, 16], BF16)
            CL2 = A("CL2", [128, 64, 16], BF16)
            rcol = A("rcol", [128, 64])
            fcol = A("fcol", [128, 64])
            D5c = A("D5c", [128, 8])
            glub = A("glub", [128, 8])
            g5c = A("g5c", [128, 8])
            Wg = A("Wg", [128, 8, 128], BF16)
            rm8 = cst[:, C_RM8:C_RM8 + 8]
            INV2PI = 1.0 / TWO_PI

            def sin_turns(out_, f_ap, tk, tf, keys_in, kout, e1="dve", e2="dve"):
                ts(e1, tk, f_ap, MAGIC, MAGIC, ALU.add, ALU.subtract, keys_in, ["_tk"])
                tt(e2, tf, f_ap, tk, ALU.subtract, keys_in + ["_tk"], ["_tf"])
                act(out_, tf, AF.Sin, ["_tf"], [kout], scale=TWO_PI)

            with ExitStack() as p0:
                B_ = lambda n, sh, dt=F32: p0.enter_context(nc.sbuf_tensor(n, sh, dt))
                lrT = B_("lrT", [128, 64]); liT = B_("liT", [128, 64]); dtg = B_("dtg", [128, 64])
                ldt = B_("ldt", [128, 64]); f2 = B_("f2", [128, 64]); tk = B_("tk", [128, 64]); tf = B_("tf", [128, 64])
                sn = B_("sn", [128, 64]); cs_ = B_("cs_", [128, 64]); lbr = B_("lbr", [128, 64]); lbi = B_("lbi", [128, 64])
                den = B_("den", [128, 64]); t_a = B_("t_a", [128, 64]); t_b = B_("t_b", [128, 64])
                cre = B_("cre", [128, 64]); cim = B_("cim", [128, 64])
                br = B_("br", [64, 64, 16]); bi = B_("bi", [64, 64, 16])
                bbr = B_("bbr", [64, 64, 16]); bbi = B_("bbi", [64, 64, 16]); t_c = B_("t_c", [64, 64, 16])
                Dre = B_("Dre", [128, 8, 64]); Dim = B_("Dim", [128, 8, 64]); nDre = B_("nDre", [128, 8, 64])
                cc1 = B_("cc1", [128, 128]); cc2 = B_("cc2", [128, 128]); wrow = B_("wrow", [128, 8, 16])
                ptc = p0.enter_context(nc.psum_tensor("ptc", [128, 128], F32))
                for half in range(2):
                    S.dma("sp", lrT[half * 64:(half + 1) * 64, :], I["s5_a_re"].rearrange("g p -> p g"), w=["lrT"],
                          stream="c0a", allow_slow_non_contiguous=True)
                    S.dma("sp", liT[half * 64:(half + 1) * 64, :], I["s5_a_im"].rearrange("g p -> p g"), w=["liT"],
                          stream="c0b", allow_slow_non_contiguous=True)
                S.dma("sp", dtg[:], I["s5_log_dt"][0:1, :].partition_broadcast(128), w=["dtg"], stream="c0c")
                act(dtg[:], dtg[:], AF.Exp, ["dtg"], ["dtg"])
                tt("dve", ldt[:], lrT[:], dtg[:], ALU.mult, ["lrT", "dtg"], ["ldt"])
                act(rcol[:], ldt[:], AF.Exp, ["ldt"], ["rcol"])
                tt("dve", fcol[:], liT[:], dtg[:], ALU.mult, ["liT", "dtg"], ["fcol"])
                ts("dve", fcol[:], fcol[:], INV2PI, None, ALU.mult, None, ["fcol"], ["fcol"])
                sin_turns(sn[:], fcol[:], tk[:], tf[:], ["fcol"], "sn")
                ts("dve", f2[:], fcol[:], 0.25, None, ALU.add, None, ["fcol"], ["f2"])
                sin_turns(cs_[:], f2[:], tk[:], tf[:], ["f2"], "cs_")
                tt("dve", lbr[:], rcol[:], cs_[:], ALU.mult, ["rcol", "cs_"], ["lbr"])
                tt("dve", lbi[:], rcol[:], sn[:], ALU.mult, ["rcol", "sn"], ["lbi"])
                ts("dve", lbr[:], lbr[:], -1.0, None, ALU.add, None, ["lbr"], ["lbr"])
                tt("dve", den[:], lrT[:], lrT[:], ALU.mult, ["lrT"], ["den"])
                tt("dve", t_a[:], liT[:], liT[:], ALU.mult, ["liT"], ["t_a"])
                tt("dve", den[:], den[:], t_a[:], ALU.add, ["den", "t_a"], ["den"])
                S.op("dve", lambda e: e.reciprocal(out=den[:], in_=den[:]), r=["den"], w=["den"])
                tt("dve", t_a[:], lbr[:], lrT[:], ALU.mult, ["lbr", "lrT"], ["t_a"])
                tt("dve", t_b[:], lbi[:], liT[:], ALU.mult, ["lbi", "liT"], ["t_b"])
                tt("dve", t_a[:], t_a[:], t_b[:], ALU.add, ["t_a", "t_b"], ["t_a"])
                tt("dve", cre[:], t_a[:], den[:], ALU.mult, ["t_a", "den"], ["cre"])
                tt("dve", t_a[:], lbi[:], lrT[:], ALU.mult, ["lbi", "lrT"], ["t_a"])
                tt("dve", t_b[:], lbr[:], liT[:], ALU.mult, ["lbr", "liT"], ["t_b"])
                tt("dve", t_a[:], t_a[:], t_b[:], ALU.subtract, ["t_a", "t_b"], ["t_a"])
                tt("dve", cim[:], t_a[:], den[:], ALU.mult, ["t_a", "den"], ["cim"])
                for q4 in range(4):
                    gs = slice(q4 * 16, (q4 + 1) * 16)
                    S.dma("sp", br[:, gs, :], I["s5_b_re"][gs].rearrange("g p h -> p g h"), w=["br"], stream="c0d")
                    S.dma("sp", bi[:, gs, :], I["s5_b_im"][gs].rearrange("g p h -> p g h"), w=["bi"], stream="c0e")
                bcr = cre[0:64, :].unsqueeze(2).to_broadcast([64, 64, 16])
                bci = cim[0:64, :].unsqueeze(2).to_broadcast([64, 64, 16])
                tt("dve", bbr[:], br[:], bcr, ALU.mult, ["br", "cre"], ["bbr"])
                tt("dve", t_c[:], bi[:], bci, ALU.mult, ["bi", "cim"], ["t_c"])
                tt("dve", bbr[:], bbr[:], t_c[:], ALU.subtract, ["bbr", "t_c"], ["bbr"])
                tt("dve", bbi[:], bi[:], bcr, ALU.mult, ["bi", "cre"], ["bbi"])
                tt("dve", t_c[:], br[:], bci, ALU.mult, ["br", "cim"], ["t_c"])
                tt("dve", bbi[:], bbi[:], t_c[:], ALU.add, ["bbi", "t_c"], ["bbi"])
                for j in range(8):
                    tr(ptc[:, 0:64], bbr[:, j * 8:(j + 1) * 8, :].rearrange("p g h -> p (g h)"), ident_f[0:64, 0:64], ["bbr", "cst"], ["ptc"])
                    cp("dve", Dre[:, j, :], ptc[:, 0:64], ["ptc"], ["Dre"])
                    tr(ptc[:, 64:128], bbi[:, j * 8:(j + 1) * 8, :].rearrange("p g h -> p (g h)"), ident_f[0:64, 0:64], ["bbi", "cst"], ["ptc2"])
                    cp("dve", Dim[:, j, :], ptc[:, 64:128], ["ptc2"], ["Dim"])
                ts("dve", nDre[:], Dre[:], -1.0, None, ALU.mult, None, ["Dre"], ["nDre"])
                rmb = rm8.unsqueeze(2).to_broadcast([128, 8, 64])
                for j in range(8):
                    gs = slice(j * 8, (j + 1) * 8)
                    bcD = lambda t: t[:, j, :].unsqueeze(1).to_broadcast([128, 8, 64])
                    tt("dve", Blk1[:, gs, 0:64], bcD(Dre), rmb, ALU.mult, ["Dre", "cst"], ["Blk1"])
                    tt("dve", Blk1[:, gs, 64:128], bcD(Dim), rmb, ALU.mult, ["Dim", "cst"], ["Blk1"])
                    tt("dve", Blk2[:, gs, 0:64], bcD(Dim), rmb, ALU.mult, ["Dim", "cst"], ["Blk2"])
                    tt("dve", Blk2[:, gs, 64:128], bcD(nDre), rmb, ALU.mult, ["nDre", "cst"], ["Blk2"])
                crv = I["s5_c_re"].rearrange("g h p -> (g h) p")
                civ = I["s5_c_im"].rearrange("g h p -> (g h) p")
                for j in range(8):
                    rs_ = slice(j * 128, (j + 1) * 128)
                    S.dma("sp", cc1[:, 0:64], crv[rs_, :], w=["cc1"], stream="c0f")
                    S.dma("sp", cc1[:, 64:128], civ[rs_, :], w=["cc1"], stream="c0f")
                    S.dma("sp", cc2[:, 0:64], civ[rs_, :], w=["cc2"], stream="c0g")
                    S.dma("sp", cc2[:, 64:128], crv[rs_, :], w=["cc2"], stream="c0g")
                    tr(ptc[:, :], cc1[:], ident_f, ["cc1", "cst"], ["ptc", "ptc2"])
                    gs = slice(j * 8, (j + 1) * 8)
                    cp("dve", CL1[0:64, gs, :], ptc[0:64, :].rearrange("p (g h) -> p g h", h=16), ["ptc"], ["CL1"])
                    ts("dve", CL1[64:128, gs, :], ptc[64:128, :].rearrange("p (g h) -> p g h", h=16), -1.0, None,
                       ALU.mult, None, ["ptc"], ["CL1"])
                    tr(ptc[:, :], cc2[:], ident_f, ["cc2", "cst"], ["ptc", "ptc2"])
                    ts("dve", CL2[:, gs, :], ptc[:, :].rearrange("p (g h) -> p g h", h=16), -1.0, None, ALU.mult, None,
                       ["ptc"], ["CL2"])
                S.dma("sp", D5c[:], I["s5_d"].rearrange("(j gl) h -> (gl h) j", gl=8), w=["D5c"], stream="c0h",
                      allow_slow_non_contiguous=True)
                S.dma("sp", glub[:], I["glu_b"].rearrange("(j gl) h -> (gl h) j", gl=8), w=["glub"], stream="c0i",
                      allow_slow_non_contiguous=True)
                S.dma("sp", g5c[:], I["norm_s5_g"].rearrange("o (j p) -> p (o j)", p=128), w=["g5c"], stream="c0j",
                      allow_slow_non_contiguous=True)
                S.dma("sp", wrow[:], I["glu_w"].rearrange("(j gl) h k -> (gl h) j k", gl=8), w=["wrow"], stream="c0k")
                for j in range(8):
                    tt("dve", Wg[:, j, :].rearrange("p (g k) -> p g k", k=16),
                       wrow[:, j, :].unsqueeze(1).to_broadcast([128, 8, 16]),
                       rm8.unsqueeze(2).to_broadcast([128, 8, 16]), ALU.mult, ["wrow", "cst"], ["Wg"])
                S.flush()

            with ExitStack() as p1:
                B_ = lambda n, sh, dt=F32: p1.enter_context(nc.sbuf_tensor(n, sh, dt))
                Pp = lambda n, sh, dt=F32: p1.enter_context(nc.psum_tensor(n, sh, dt))
                iot = B_("iot", [128, SEQ])
                SIN = [B_("SIN%d" % i, [128, SEQ], BF16) for i in range(3)]
                COS = [B_("COS%d" % i, [128, SEQ], BF16) for i in range(3)]
                u1 = B_("u1", [128, SEQ]); k1 = B_("k1", [128, SEQ]); fr = B_("fr", [128, SEQ])
                uT = [B_("uT%d" % i, [128, T], BF16) for i in range(2)]
                R3 = 3
                p1b = [B_("p1b%d" % i, [128, 512], BF16) for i in range(R3)]
                p2b = [B_("p2b%d" % i, [128, 512], BF16) for i in range(R3)]
                w1 = [B_("w1_%d" % i, [128, 512], BF16) for i in range(R3)]
                w2 = [B_("w2_%d" % i, [128, 512], BF16) for i in range(R3)]
                ww = [B_("ww%d" % i, [128, 512]) for i in range(R3)]
                zz = [[B_("zz%d_%d" % (bb, i), [128, 512]) for i in range(R3)] for bb in range(NB)]
                zb = [B_("zb%d" % i, [128, 512], BF16) for i in range(R3)]
                v1 = [B_("v1_%d" % i, [128, 512], BF16) for i in range(R3)]
                v2 = [B_("v2_%d" % i, [128, 512], BF16) for i in range(R3)]
                ysm = [B_("ysm%d" % i, [16, 512]) for i in range(R3)]
                P1 = [Pp("P1_%d" % i, [128, 512]) for i in range(2)]
                P2 = [Pp("P2_%d" % i, [128, 512]) for i in range(2)]
                uf = [B_("uf%d" % i, [128, D]) for i in range(2)]
                vf = [B_("vf%d" % i, [128, D]) for i in range(2)]
                ub = [B_("ub%d" % i, [128, D], BF16) for i in range(2)]
                vbt = [B_("vbt%d" % i, [128, D], BF16) for i in range(2)]
                uts = [B_("uts%d" % i, [128, D], BF16) for i in range(2)]
                pTu = [Pp("pTu%d" % i, [128, 1024], BF16) for i in range(2)]

                def m0_tile(et):
                    pr = et % 2
                    rows = slice(et * 128, (et + 1) * 128)
                    S.dma("sp", uf[pr][:], I["expert_u"][rows, :], w=["uf%d" % pr], stream="uf%d" % pr)
                    S.dma("sp", vf[pr][:], I["expert_v"][rows, :], w=["vf%d" % pr], stream="vf%d" % pr)
                    yield
                    cp("act", ub[pr][:], uf[pr][:], ["uf%d" % pr], ["ub%d" % pr])
                    cp("act", vbt[pr][:], vf[pr][:], ["vf%d" % pr], ["vbt%d" % pr])
                    yield
                    for kc in range(8):
                        tr(pTu[pr][:, kc * 128:(kc + 1) * 128], ub[pr][:, kc * 128:(kc + 1) * 128], ident_b,
                           ["ub%d" % pr, "cstb"], ["pTu%d" % pr])
                    S.dma("sp", Vb[rows, :], vbt[pr][:], r=["vbt%d" % pr], w=["Vb"], stream="vbo%d" % pr)
                    yield
                    cp("act", uts[pr][:], pTu[pr][:, :], ["pTu%d" % pr], ["uts%d" % pr])
                    yield
                    S.dma("sp", UTb[et], uts[pr][:], r=["uts%d" % pr], w=["UTb"], stream="uto%d" % pr)
                    yield
                PY = [Pp("PY%d" % i, [128, 512]) for i in range(2)]
                for k in range(16):
                    ts("dve", iot[:, k * 128:(k + 1) * 128], cst[:, C_IOTA:C_IOTA + 128], float(128 * k), None, ALU.add,
                       None, ["cst"], ["iot"])

                def tables(g):
                    gp = g % 3
                    fg = fcol[:, g:g + 1]
                    for (tab, key, off) in ((SIN[gp], "SIN%d" % gp, 0.0), (COS[gp], "COS%d" % gp, 0.25)):
                        act(u1[:], iot[:], AF.Identity, ["iot", "fcol"], ["u1"], scale=fg, bias=off)
                        ts("dve", k1[:], u1[:], MAGIC, MAGIC, ALU.add, ALU.subtract, ["u1"], ["k1"])
                        tt("dve", fr[:], u1[:], k1[:], ALU.subtract, ["u1", "k1"], ["fr"])
                        act(tab[:], fr[:], AF.Sin, ["fr"], [key], scale=TWO_PI)

                pieces = [(g, q, bb) for g in range(64) for q in range(4) for bb in range(NB)]

                def info(n):
                    g, q, bb = pieces[n]
                    return g, q, bb, g // 8, g % 3, n % R3, q * 512, bb * SEQ + q * 512

                def st_a(n):
                    g, q, bb, j, gp, pb, t0, tok = info(n)
                    if q == 0 and bb == 0 and g + 1 < 64:
                        tables(g + 1)
                    if g % 8 == 0 and q == 0 and bb == 0:
                        S.dma("sp", uT[j % 2][:], UTs[j * 128:(j + 1) * 128, :], r=["UTs"], w=["uT%d" % (j % 2)],
                              stream="uT%d" % (j % 2))
                    uk = "uT%d" % (j % 2)
                    mm(P1[n % 2][:, :], Blk1[:, g, :], uT[j % 2][:, tok:tok + 512], True, True, ["Blk1", uk], ["P1_%d" % (n % 2)])
                    mm(P2[n % 2][:, :], Blk2[:, g, :], uT[j % 2][:, tok:tok + 512], True, True, ["Blk2", uk], ["P2_%d" % (n % 2)])

                def st_b(n):
                    g, q, bb, j, gp, pb, t0, tok = info(n)
                    cp("act", p1b[pb][:], P1[n % 2][:, :], ["P1_%d" % (n % 2)], ["p1b%d" % pb])
                    cp("act", p2b[pb][:], P2[n % 2][:, :], ["P2_%d" % (n % 2)], ["p2b%d" % pb])

                def st_c(n):
                    g, q, bb, j, gp, pb, t0, tok = info(n)
                    tt("dve", w1[pb][:], p1b[pb][:], COS[gp][:, t0:t0 + 512], ALU.mult, ["p1b%d" % pb, "COS%d" % gp],
                       ["w1_%d" % pb])
                    tt("dve", w2[pb][:], p2b[pb][:], SIN[gp][:, t0:t0 + 512], ALU.mult, ["p2b%d" % pb, "SIN%d" % gp],
                       ["w2_%d" % pb])

                def st_d(n):
                    g, q, bb, j, gp, pb, t0, tok = info(n)
                    tt("pool", ww[pb][:], w1[pb][:], w2[pb][:], ALU.add, ["w1_%d" % pb, "w2_%d" % pb], ["ww%d" % pb])

                def st_e(n):
                    g, q, bb, j, gp, pb, t0, tok = info(n)
                    zc, zp = zz[bb][q % R3], zz[bb][(q - 1) % R3]
                    zck, zpk = "zz%d_%d" % (bb, q % R3), "zz%d_%d" % (bb, (q - 1) % R3)
                    init = 0.0 if q == 0 else zp[:, 511:512]
                    S.op("dve", lambda e: e.tensor_tensor_scan(
                        out=zc[:], data0=rcol[:, g:g + 1].to_broadcast([128, 512]), data1=ww[pb][:],
                        initial=init, op0=ALU.mult, op1=ALU.add), r=["rcol", "ww%d" % pb, zpk], w=[zck])

                def st_f(n):
                    g, q, bb, j, gp, pb, t0, tok = info(n)
                    cp("act", zb[pb][:], zz[bb][q % R3][:], ["zz%d_%d" % (bb, q % R3)], ["zb%d" % pb])

                def st_g(n):
                    g, q, bb, j, gp, pb, t0, tok = info(n)
                    tt("dve", v1[pb][:], zb[pb][:], COS[gp][:, t0:t0 + 512], ALU.mult, ["zb%d" % pb, "COS%d" % gp],
                       ["v1_%d" % pb])
                    tt("pool", v2[pb][:], zb[pb][:], SIN[gp][:, t0:t0 + 512], ALU.mult, ["zb%d" % pb, "SIN%d" % gp],
                       ["v2_%d" % pb])

                def st_h(n):
                    g, q, bb, j, gp, pb, t0, tok = info(n)
                    pp = n % 2
                    mm(PY[pp][0:16, :], CL1[:, g, :], v1[pb][:], True, False, ["CL1", "v1_%d" % pb], ["PY%d" % pp])
                    mm(PY[pp][0:16, :], CL2[:, g, :], v2[pb][:], False, True, ["CL2", "v2_%d" % pb], ["PY%d" % pp])

                def st_i(n):
                    g, q, bb, j, gp, pb, t0, tok = info(n)
                    pp = n % 2
                    yk = "ysm%d" % pb
                    cp("act", ysm[pb][0:16, :], PY[pp][0:16, :], ["PY%d" % pp], [yk])
                    S.dma("sp", Y5s[g * 16:(g + 1) * 16, tok:tok + 512], ysm[pb][0:16, :], r=[yk], w=["Y5s"], stream=yk)

                tables(0)
                stages_c = [st_a, st_b, st_c, st_d, st_e, st_f, st_g, st_h, st_i]
                side = []
                nxt_et = 0
                for k in range(len(pieces) + len(stages_c) - 1):
                    for s_ in reversed(range(len(stages_c))):
                        n = k - s_
                        if 0 <= n < len(pieces):
                            stages_c[s_](n)
                    if k % 4 == 0 and nxt_et < 128:
                        side.append(m0_tile(nxt_et))
                        nxt_et += 1
                    for g_ in list(side):
                        try:
                            next(g_)
                        except StopIteration:
                            side.remove(g_)
                while side or nxt_et < 128:
                    if nxt_et < 128:
                        side.append(m0_tile(nxt_et))
                        nxt_et += 1
                    for g_ in list(side):
                        try:
                            next(g_)
                        except StopIteration:
                            side.remove(g_)
                S.flush()

            with ExitStack() as p2:
                B_ = lambda n, sh, dt=F32: p2.enter_context(nc.sbuf_tensor("C2_" + n, sh, dt))
                Pp = lambda n, sh, dt=F32: p2.enter_context(nc.psum_tensor("C2_" + n, sh, dt))
                y5 = [B_("y5_%d" % i, [128, 512]) for i in range(3)]
                uu = [B_("uu%d" % i, [128, 512], BF16) for i in range(3)]
                yv = [B_("yv%d" % i, [128, 512]) for i in range(3)]
                vb = [B_("vb%d" % i, [128, 512], BF16) for i in range(3)]
                sg = [B_("sg%d" % i, [128, 512]) for i in range(3)]
                oo = [B_("oo%d" % i, [128, 8, 512]) for i in range(2)]
                sq = [B_("sq%d" % i, [128, 512]) for i in range(3)]
                rs5 = [B_("rs5_%d" % i, [128, 512]) for i in range(2)]
                ycb = [B_("ycb%d" % i, [128, 512], BF16) for i in range(2)]
                PG = [Pp("PG%d" % i, [128, 512]) for i in range(2)]
                PSS = [Pp("PSS%d" % i, [128, 512]) for i in range(2)]
                NBK = T // 512

                def cinfo(n):
                    return n // 8, n % 8, n % 3, (n // 8) * 512

                def c_a(n):
                    blk, j, r3, tok = cinfo(n)
                    S.dma("sp", y5[r3][:], Y5s[j * 128:(j + 1) * 128, tok:tok + 512], r=["Y5s"], w=["y5_%d" % r3],
                          stream="y5_%d" % r3)
                    S.dma("sp", uu[r3][:], UTs[j * 128:(j + 1) * 128, tok:tok + 512], r=["UTs"], w=["uu%d" % r3],
                          stream="uu%d" % r3)

                def c_b(n):
                    blk, j, r3, tok = cinfo(n)
                    stt(yv[r3][:], uu[r3][:], D5c[:, j:j + 1], y5[r3][:], ALU.mult, ALU.add,
                        ["uu%d" % r3, "D5c", "y5_%d" % r3], ["yv%d" % r3])

                def c_c(n):
                    blk, j, r3, tok = cinfo(n)
                    act(vb[r3][:], yv[r3][:], AF.Gelu, ["yv%d" % r3], ["vb%d" % r3])

                def c_d(n):
                    blk, j, r3, tok = cinfo(n)
                    mm(PG[n % 2][:, :], Wg[:, j, :], vb[r3][:], True, True, ["Wg", "vb%d" % r3], ["PG%d" % (n % 2)])

                def c_e(n):
                    blk, j, r3, tok = cinfo(n)
                    act(sg[r3][:], PG[n % 2][:, :], AF.Sigmoid, ["PG%d" % (n % 2), "glub"], ["sg%d" % r3],
                        bias=glub[:, j:j + 1])

                def c_f(n):
                    blk, j, r3, tok = cinfo(n)
                    tt("dve", oo[blk % 2][:, j, :], vb[r3][:], sg[r3][:], ALU.mult, ["vb%d" % r3, "sg%d" % r3],
                       ["oo%d_%d" % (blk % 2, j)])

                def c_g(n):
                    blk, j, r3, tok = cinfo(n)
                    tt("pool", sq[r3][:], oo[blk % 2][:, j, :], oo[blk % 2][:, j, :], ALU.mult,
                       ["oo%d_%d" % (blk % 2, j)], ["sq%d" % r3])

                def c_h(n):
                    blk, j, r3, tok = cinfo(n)
                    mm(PSS[blk % 2][:, :], ones_f, sq[r3][:], j == 0, j == 7, ["cst", "sq%d" % r3], ["PSS%d" % (blk % 2)])

                def c_tail(blk):
                    bp = blk % 2
                    tok = blk * 512
                    rk = "rs5_%d" % bp
                    ts("dve", rs5[bp][:], PSS[bp][:, :], 1.0 / 1024, EPS, ALU.mult, ALU.add, ["PSS%d" % bp], [rk])
                    yield
                    act(rs5[bp][:], rs5[bp][:], AF.Sqrt, [rk], [rk])
                    yield
                    S.op("dve", lambda e: e.reciprocal(out=rs5[bp][:], in_=rs5[bp][:]), r=[rk], w=[rk])
                    yield
                    for j in range(8):
                        jp = j % 2
                        stt(ycb[jp][:], oo[bp][:, j, :], g5c[:, j:j + 1], rs5[bp][:], ALU.mult, ALU.mult,
                            ["oo%d_%d" % (bp, j), "g5c", rk], ["ycb%d" % jp])
                        S.dma("sp", YCs[1024 + j * 128:1024 + (j + 1) * 128, tok:tok + 512], ycb[jp][:],
                              r=["ycb%d" % jp], w=["YCs"], stream="ycb%d" % jp)
                        yield

                st2 = [c_a, c_b, c_c, c_d, c_e, c_f, c_g, c_h]
                NI2 = NBK * 8
                side2 = []
                for k in range(NI2 + len(st2) - 1):
                    for s_ in reversed(range(len(st2))):
                        n = k - s_
                        if 0 <= n < NI2:
                            st2[s_](n)
                    nh = k - (len(st2) - 1)
                    if nh >= 0 and nh % 8 == 7:
                        side2.append(c_tail(nh // 8))
                    for g_ in list(side2):
                        try:
                            next(g_)
                        except StopIteration:
                            side2.remove(g_)
                while side2:
                    for g_ in list(side2):
                        try:
                            next(g_)
                        except StopIteration:
                            side2.remove(g_)
                S.flush()
        if stop_after == 3:
            return nc

        with ExitStack() as ph:
            A = lambda n, sh, dt=F32: ph.enter_context(nc.sbuf_tensor("D_" + n, sh, dt))
            P = lambda n, sh, dt=F32: ph.enter_context(nc.psum_tensor("D_" + n, sh, dt))
            wout = A("wout", [128, 16, D], BF16)
            wq = A("wq", [128, 8, 2048], BF16)
            skf = A("skf", [128, 16, 128])
            skT = A("skT", [128, 16, 128], BF16)
            GT1 = A("GT1", [128, D]); G2 = A("G2", [128, D]); SH2 = A("SH2", [128, D])
            yct = [A("yct%d" % i, [128, 16, 128], BF16) for i in range(2)]
            xin = [A("xin%d" % i, [128, D]) for i in range(2)]
            t1 = A("t1", [128, D]); x1 = [A("x1_%d" % i, [128, D]) for i in range(2)]
            junk = A("junk", [128, D], BF16)
            ss2 = A("ss2", [128, 32])
            hb2 = A("hb2", [128, D], BF16)
            h2T = [A("h2T%d" % i, [128, 8, 128], BF16) for i in range(2)]
            qT = A("qT", [128, 16, 128], BF16)
            scb = [A("sc_%d" % i, [128, 16, 128]) for i in range(2)]; sc2 = A("sc2", [128, 16, 128])
            v8 = A("v8", [128, 16, 16]); i8 = A("i8", [128, 16, 16], U32); i8f = A("i8f", [128, 16, 16])
            cand = A("cand", [128, 8, 256]); cand2 = A("cand2", [128, 8, 256])
            c8 = A("c8", [128, 8, 16]); p8 = A("p8", [128, 8, 16], U32)
            ge = A("ge", [128, 8, 16]); gs = A("gs", [128, 8]); gg = A("gg", [128, 8, 16])
            ra_i = A("ra_i", [128, 128], I32); rb_i = A("rb_i", [128, 128], I32)
            raf = A("raf", [128, 128]); rbf = A("rbf", [128, 128])
            oh = A("oh", [128, 128, 16]); oh2 = A("oh2", [128, 128, 16])
            isel = A("isel", [128, 128]); jsel = A("jsel", [128, 128])
            rstg = [A("rstg%d" % i, [128, 3, 128], BF16) for i in range(2)]
            pT = P("pT", [128, 1024], BF16)
            pM = P("pM", [128, 1024])
            pq = [P("pq%d" % i, [128, 512]) for i in range(2)]
            psc = [P("psc%d" % i, [128, 512]) for i in range(2)]
            pTi = P("pTi", [128, 512])
            iota16 = cst[:, C_IOTA16:C_IOTA16 + 16]

            woutv = I["w_out"].rearrange("(ct p) d -> p ct d", p=128)
            for q4 in range(4):
                S.dma("pool", wout[:, q4 * 4:(q4 + 1) * 4, :], woutv[:, q4 * 4:(q4 + 1) * 4, :], w=["wout"], stream="wout")
            wqv = I["w_query"].rearrange("(kc p) n -> p kc n", p=128)
            for q4 in range(4):
                S.dma("pool", wq[:, q4 * 2:(q4 + 1) * 2, :], wqv[:, q4 * 2:(q4 + 1) * 2, :], w=["wq"], stream="wq")
            S.dma("sp", skf[:], I["sub_keys"].rearrange("m k d -> k m d"), w=["skf"], stream="skf")
            for m in range(16):
                tr(pTi[:, (m % 4) * 128:(m % 4 + 1) * 128], skf[:, m, :], ident_f, ["skf", "cst"], ["pTi"])
                cp("dve", skT[:, m, :], pTi[:, (m % 4) * 128:(m % 4 + 1) * 128], ["pTi"], ["skT"])
            YCv = YCs.rearrange("(ct p) t -> p ct t", p=128)
            H2v = H2Ts.rearrange("(kc p) t -> p kc t", p=128)
            def tile_vars(i):
                return i // 16, i % 2, i * 128

            def front(i):
                b, par, tok0 = tile_vars(i)
                sck = "sc%d" % par
                if i % 16 == 0:
                    S.dma("sp", GT1[:], MODs[b:b + 1, 2048:3072].partition_broadcast(128), r=["MODs"], w=["GT1"], stream="d0")
                    S.dma("sp", G2[:], MODs[b:b + 1, 4096:5120].partition_broadcast(128), r=["MODs"], w=["G2"], stream="d1")
                    S.dma("sp", SH2[:], MODs[b:b + 1, 3072:4096].partition_broadcast(128), r=["MODs"], w=["SH2"], stream="d2")
                yk, xk, x1k, hk = "yct%d" % par, "xin%d" % par, "x1_%d" % par, "h2T%d" % par
                S.dma("sp", yct[par][:], YCv[:, :, tok0:tok0 + 128], r=["YCs"], w=[yk], stream=yk)
                S.dma("sp", xin[par][:], I["x"][tok0:tok0 + 128, :], w=[xk], stream=xk)
                yield
                for half in range(2):
                    for ct in range(16):
                        mm(pM[:, half * 512:(half + 1) * 512], yct[par][:, ct, :], wout[:, ct, half * 512:(half + 1) * 512],
                           ct == 0, ct == 15, [yk, "wout"], ["pM"])
                yield
                tt("dve", t1[:], pM[:, :], GT1[:], ALU.mult, ["pM", "GT1"], ["t1"])
                yield
                tt("pool", x1[par][:], t1[:], xin[par][:], ALU.add, ["t1", xk], [x1k])
                S.dma("sp", X1s[tok0:tok0 + 128, :], x1[par][:], r=[x1k], w=["X1s"], stream=x1k)
                yield
                act(junk[:], x1[par][:], AF.Square, [x1k], ["junk", "ss2"], accum_out=ss2[:, i:i + 1])
                yield
                col = ss2[:, i:i + 1]
                ts("dve", col, col, 1.0 / D, EPS, ALU.mult, ALU.add, ["ss2"], ["ss2"])
                yield
                act(col, col, AF.Sqrt, ["ss2"], ["ss2"])
                yield
                S.op("dve", lambda e: e.reciprocal(out=col, in_=col), r=["ss2"], w=["ss2"])
                stt(t1[:], x1[par][:], ss2[:, i:i + 1], G2[:], ALU.mult, ALU.mult, [x1k, "ss2", "G2"], ["t1"])
                yield
                tt("pool", hb2[:], t1[:], SH2[:], ALU.add, ["t1", "SH2"], ["hb2"])
                yield
                for kc in range(8):
                    tr(pT[:, kc * 128:(kc + 1) * 128], hb2[:, kc * 128:(kc + 1) * 128], ident_b, ["hb2", "cstb"], ["pT"])
                yield
                cp("act", h2T[par][:, :, :], pT[:, :].rearrange("p (k t) -> p k t", k=8), ["pT"], [hk])
                S.dma("sp", H2v[:, :, tok0:tok0 + 128], h2T[par][:, :, :], r=[hk], w=["H2Ts"], stream=hk)
                yield
                for m4 in range(4):
                    pp = m4 % 2
                    for mi in range(4):
                        m = m4 * 4 + mi
                        for kc in range(8):
                            mm(pq[pp][:, mi * 128:(mi + 1) * 128], wq[:, kc, m * 128:(m + 1) * 128], h2T[par][:, kc, :],
                               kc == 0, kc == 7, ["wq", hk], ["pq%d" % pp])
                    cp("act", qT[:, m4 * 4:(m4 + 1) * 4, :], pq[pp][:, :].rearrange("p (m t) -> p m t", m=4),
                       ["pq%d" % pp], ["qT%d" % m4])
                    yield
                for m4 in range(4):
                    pp = m4 % 2
                    for mi in range(4):
                        m = m4 * 4 + mi
                        mm(psc[pp][:, mi * 128:(mi + 1) * 128], qT[:, m, :], skT[:, m, :], True, True,
                           ["qT%d" % m4, "skT"], ["psc%d" % pp])
                    cp("act", scb[par][:, m4 * 4:(m4 + 1) * 4, :], psc[pp][:, :].rearrange("p (m k) -> p m k", m=4),
                       ["psc%d" % pp], [sck])
                    yield

            def back(i):
                b, par, tok0 = tile_vars(i)
                sck = "sc%d" % par
                for m in range(16):
                    S.op("dve", lambda e, m=m: e.max(out=v8[:, m, 0:8], in_=scb[par][:, m, :]), r=[sck], w=["v8a%d" % m])
                yield
                for m in range(16):
                    S.op("dve", lambda e, m=m: e.max_index(out=i8[:, m, 0:8], in_max=v8[:, m, 0:8], in_values=scb[par][:, m, :]),
                         r=[sck, "v8a%d" % m], w=["i8a%d" % m])
                    S.op("dve", lambda e, m=m: e.match_replace(out=sc2[:, m, :], in_to_replace=v8[:, m, 0:8],
                                                               in_values=scb[par][:, m, :], imm_value=-1e30),
                         r=[sck, "v8a%d" % m], w=["sc2_%d" % m])
                    if m % 4 == 3:
                        yield
                for m in range(16):
                    S.op("dve", lambda e, m=m: e.max(out=v8[:, m, 8:16], in_=sc2[:, m, :]), r=["sc2_%d" % m],
                         w=["v8b%d" % m])
                yield
                for m in range(16):
                    S.op("dve", lambda e, m=m: e.max_index(out=i8[:, m, 8:16], in_max=v8[:, m, 8:16],
                                                           in_values=sc2[:, m, :]), r=["sc2_%d" % m, "v8b%d" % m],
                         w=["i8b%d" % m])
                yield
                v8keys = ["v8a%d" % m for m in range(16)] + ["v8b%d" % m for m in range(16)]
                i8keys = ["i8a%d" % m for m in range(16)] + ["i8b%d" % m for m in range(16)]
                cp("dve", i8f[:], i8[:], i8keys, ["i8f"])
                v8v = v8[:, :, :].rearrange("p (h c) r -> p h c r", c=2)
                i8v = i8f[:, :, :].rearrange("p (h c) r -> p h c r", c=2)
                tt("dve", cand[:, :, :].rearrange("p h (r c) -> p h r c", c=16),
                   v8v[:, :, 0, :].unsqueeze(3).to_broadcast([128, 8, 16, 16]),
                   v8v[:, :, 1, :].unsqueeze(2).to_broadcast([128, 8, 16, 16]), ALU.add, v8keys, ["cand"])
                yield
                for h in range(8):
                    S.op("dve", lambda e, h=h: e.max(out=c8[:, h, 0:8], in_=cand[:, h, :]), r=["cand"], w=["c8a%d" % h])
                yield
                for h in range(8):
                    S.op("dve", lambda e, h=h: e.max_index(out=p8[:, h, 0:8], in_max=c8[:, h, 0:8], in_values=cand[:, h, :]),
                         r=["cand", "c8a%d" % h], w=["p8a%d" % h])
                    S.op("dve", lambda e, h=h: e.match_replace(out=cand2[:, h, :], in_to_replace=c8[:, h, 0:8],
                                                               in_values=cand[:, h, :], imm_value=-1e30),
                         r=["cand", "c8a%d" % h], w=["cand2_%d" % h])
                    if h % 4 == 3:
                        yield
                for h in range(8):
                    S.op("dve", lambda e, h=h: e.max(out=c8[:, h, 8:16], in_=cand2[:, h, :]), r=["cand2_%d" % h],
                         w=["c8b%d" % h])
                yield
                for h in range(8):
                    S.op("dve", lambda e, h=h: e.max_index(out=p8[:, h, 8:16], in_max=c8[:, h, 8:16],
                                                           in_values=cand2[:, h, :]), r=["cand2_%d" % h, "c8b%d" % h],
                         w=["p8b%d" % h])
                yield
                c8keys = ["c8a%d" % h for h in range(8)] + ["c8b%d" % h for h in range(8)]
                p8keys = ["p8a%d" % h for h in range(8)] + ["p8b%d" % h for h in range(8)]
                tt("dve", ge[:], c8[:], c8[:, :, 0:1].to_broadcast([128, 8, 16]), ALU.subtract, c8keys, ["ge"])
                yield
                act(ge[:], ge[:], AF.Exp, ["ge"], ["ge"])
                yield
                S.op("dve", lambda e: e.tensor_reduce(out=gs[:], in_=ge[:], axis=AX.X, op=ALU.add), r=["ge"], w=["gs"])
                S.op("dve", lambda e: e.reciprocal(out=gs[:], in_=gs[:]), r=["gs"], w=["gs"])
                tt("dve", gg[:], ge[:], gs[:, :].unsqueeze(2).to_broadcast([128, 8, 16]), ALU.mult, ["ge", "gs"], ["gg"])
                p8i = p8[:, :, :].rearrange("p h k -> p (h k)").bitcast(I32)
                S.op("dve", lambda e: e.tensor_single_scalar(out=ra_i[:], in_=p8i, scalar=4, op=ALU.logical_shift_right),
                     r=p8keys, w=["ra_i"])
                S.op("dve", lambda e: e.tensor_single_scalar(out=rb_i[:], in_=p8i, scalar=15, op=ALU.bitwise_and),
                     r=p8keys, w=["rb_i"])
                cp("dve", raf[:], ra_i[:], ["ra_i"], ["raf"])
                cp("dve", rbf[:], rb_i[:], ["rb_i"], ["rbf"])
                yield
                io3 = iota16.unsqueeze(1).to_broadcast([128, 128, 16])
                for (rf, rk, ci, ohh, ok, sel, sk_) in ((raf, "raf", 0, oh, "oh", isel, "isel"),
                                                        (rbf, "rbf", 1, oh2, "oh2", jsel, "jsel")):
                    eng = "dve" if ci == 0 else "pool"
                    tt("dve", ohh[:], rf[:, :].unsqueeze(2).to_broadcast([128, 128, 16]), io3, ALU.is_equal,
                       [rk, "cst"], [ok])
                    tt(eng, ohh[:, :, :].rearrange("p (h k) r -> p h k r", h=8),
                       ohh[:, :, :].rearrange("p (h k) r -> p h k r", h=8),
                       i8v[:, :, ci, :].unsqueeze(2).to_broadcast([128, 8, 16, 16]), ALU.mult, [ok, "i8f"], [ok])
                    S.op("dve", lambda e, sel=sel, ohh=ohh: e.tensor_reduce(out=sel[:], in_=ohh[:], axis=AX.X, op=ALU.add),
                         r=[ok], w=[sk_])
                    yield
                tr(pTi[:, 0:128], isel[:], ident_f, ["isel", "cst"], ["pTi"])
                tr(pTi[:, 128:256], jsel[:], ident_f, ["jsel", "cst"], ["pTi"])
                tr(pTi[:, 256:384], gg[:, :, :].rearrange("p h k -> p (h k)"), ident_f, ["gg", "cst"], ["pTi"])
                rk_ = "rstg%d" % par
                cp("act", rstg[par][:, :, :], pTi[:, 0:384].rearrange("p (a t) -> p a t", a=3), ["pTi"], [rk_])
                S.dma("sp", RTs[:, :, tok0:tok0 + 128], rstg[par][:, :, :], r=[rk_], w=["RTs"], stream=rk_)

            def interleave(gens):
                gens = [g_ for g_ in gens if g_ is not None]
                while gens:
                    for g_ in list(gens):
                        try:
                            next(g_)
                        except StopIteration:
                            gens.remove(g_)

            interleave([front(0)])
            for i in range(NT):
                interleave([front(i + 1) if i + 1 < NT else None, back(i)])
            S.flush()
        if stop_after == 4:
            return nc

        with ExitStack() as ph:
            A = lambda n, sh, dt=F32: ph.enter_context(nc.sbuf_tensor("M_" + n, sh, dt))
            P = lambda n, sh, dt=F32: ph.enter_context(nc.psum_tensor("M_" + n, sh, dt))
            TB = 256
            G0 = A("G0", [128, 64, TB], BF16)
            G1 = A("G1", [128, 64, TB], BF16)
            utb = [A("utb%d" % i, [128, 2, D], BF16) for i in range(4)]
            vtb = [A("vtb%d" % i, [128, 2, D], BF16) for i in range(4)]
            Pm = [A("Pm%d" % i, [128, 8, 64], BF16) for i in range(4)]
            Q0 = [A("Q0%d" % i, [128, 8, 128], BF16) for i in range(4)]
            Qm = [A("Qm%d" % i, [128, 8, 128], BF16) for i in range(4)]
            h2b = [A("h2b%d" % i, [128, 8, TB], BF16) for i in range(2)]
            rt = [A("rt%d" % i, [128, 3, TB], BF16) for i in range(2)]
            Ag = [A("Ag%d" % i, [128, TB], BF16) for i in range(2)]
            GA = [A("GA%d" % i, [128, TB], BF16) for i in range(2)]
            x1t = [A("x1t%d" % i, [128, D]) for i in range(2)]
            GT2 = A("GT2", [128, D]); nfg = A("nfg", [128, D])
            tm = [A("tm%d" % i, [128, D]) for i in range(2)]; x2 = A("x2", [128, D]); junkm = A("junkm", [128, D], BF16)
            ot = [A("ot%d" % i, [128, D]) for i in range(2)]
            ssf = A("ssf", [128, 32])
            pO = [P("pO%d" % i, [128, 1024]) for i in range(2)]
            pA = [P("pA%d" % i, [128, 512]) for i in range(2)]
            pG = [P("pG%d" % i, [128, 512]) for i in range(2)]
            iota_b = cstb[:, C_IOTA:C_IOTA + 128]
            io32 = iota_b.unsqueeze(1).to_broadcast([128, 32, 128])
            io8 = iota_b.unsqueeze(1).to_broadcast([128, 8, 128])
            H2v = H2Ts.rearrange("(kc p) t -> p kc t", p=128)
            UTv = UTb.rearrange("e p x -> p e x")
            Vv = Vb.rearrange("(e p) d -> p e d", p=128)
            S.dma("sp", nfg[:], I["norm_f_g"][0:1, :].partition_broadcast(128), w=["nfg"], stream="m0")
            Gh = [G0, G1]
            io64 = [iota_b[:, 64 * hf:64 * hf + 64].unsqueeze(1).to_broadcast([128, 8, 64]) for hf in range(2)]
            NBLK = T // TB
            cnts = {"g": 0, "s": 0}
            late = []

            def run_late():
                for f_ in late:
                    f_()
                del late[:]

            def build(blk, hf):
                bp = blk % 2
                tok = blk * TB
                hk, rk = "h2b%d" % bp, "rt%d" % bp
                if hf == 0:
                    S.dma("sp", h2b[bp][:], H2v[:, :, tok:tok + TB], r=["H2Ts"], w=[hk], stream=hk)
                    S.dma("sp", rt[bp][:], RTs[:, :, tok:tok + TB], r=["RTs"], w=[rk], stream=rk)
                    yield
                NG = TB // 8
                base = cnts["s"]
                cnts["s"] += NG

                def dve_part(k):
                    sp_ = (base + k) % 4
                    tsl = slice(k * 8, (k + 1) * 8)
                    bcn = lambda a_, n_: rt[bp][:, a_, tsl].unsqueeze(2).to_broadcast([128, 8, n_])
                    tt("dve", Pm[sp_][:], bcn(0, 64), io64[hf], ALU.is_equal, [rk, "cstb"], ["Pm%d" % sp_])
                    tt("dve", Q0[sp_][:], bcn(1, 128), io8, ALU.is_equal, [rk, "cstb"], ["Q0%d" % sp_])

                def pool_part(k):
                    sp_ = (base + k) % 4
                    tsl = slice(k * 8, (k + 1) * 8)
                    tt("pool", Qm[sp_][:], Q0[sp_][:], rt[bp][:, 2, tsl].unsqueeze(2).to_broadcast([128, 8, 128]),
                       ALU.mult, ["Q0%d" % sp_, rk], ["Qm%d" % sp_])

                def mm_part(k):
                    sp_ = (base + k) % 4
                    for t4 in range(2):
                        gp = cnts["g"] % 2
                        cnts["g"] += 1
                        for ti in range(4):
                            t = t4 * 4 + ti
                            mm(pG[gp][:, ti * 64:(ti + 1) * 64], Qm[sp_][:, t, :], Pm[sp_][:, t, :], True, True,
                               ["Qm%d" % sp_, "Pm%d" % sp_], ["pG%d" % gp])
                        t0 = k * 8 + t4 * 4
                        late.append(lambda gp=gp, t0=t0: cp(
                            "act", Gh[hf][:, :, t0:t0 + 4], pG[gp][:, 0:256].rearrange("p (t i) -> p i t", t=4),
                            ["pG%d" % gp], ["G%d" % hf]))

                dve_part(0)
                dve_part(1)
                dve_part(2)
                yield
                pool_part(0)
                pool_part(1)
                yield
                for k in range(NG):
                    mm_part(k)
                    if k + 3 < NG:
                        late.append(lambda k=k: dve_part(k + 3))
                    if k + 2 < NG:
                        late.append(lambda k=k: pool_part(k + 2))
                    yield

            def final(blk):
                tok = blk * TB
                b_ = tok // SEQ
                if tok % SEQ == 0:
                    S.dma("sp", GT2[:], MODs[b_:b_ + 1, 5120:6144].partition_broadcast(128), r=["MODs"], w=["GT2"],
                          stream="m1")
                for t2 in range(2):
                    tt("dve", tm[t2][:], pO[t2][:, :], GT2[:], ALU.mult, ["pO%d" % t2, "GT2"], ["tm%d" % t2])
                yield
                for t2 in range(2):
                    ti = blk * 2 + t2
                    tk0 = tok + t2 * 128
                    xk, ok_ = "x1t%d" % t2, "ot%d" % t2
                    col = ssf[:, ti % 32:ti % 32 + 1]
                    S.dma("sp", x1t[t2][:], X1s[tk0:tk0 + 128, :], r=["X1s"], w=[xk], stream=xk)
                    yield
                    tt("pool", x2[:], tm[t2][:], x1t[t2][:], ALU.add, ["tm%d" % t2, xk], ["x2"])
                    yield
                    act(junkm[:], x2[:], AF.Square, ["x2"], ["junkm", "ssf"], accum_out=col)
                    yield
                    ts("dve", col, col, 1.0 / D, EPS, ALU.mult, ALU.add, ["ssf"], ["ssf"])
                    yield
                    act(col, col, AF.Sqrt, ["ssf"], ["ssf"])
                    yield
                    S.op("dve", lambda e, col=col: e.reciprocal(out=col, in_=col), r=["ssf"], w=["ssf"])
                    stt(ot[t2][:], x2[:], col, nfg[:], ALU.mult, ALU.mult, ["x2", "ssf", "nfg"], [ok_])
                    yield
                    S.dma("sp", out[tk0:tk0 + 128, :], ot[t2][:], r=[ok_], w=["out"], stream=ok_)
                    yield

            def prefetch(gg):
                if gg >= NBLK * 64:
                    return
                up = gg % 4
                i0 = (gg % 64) * 2
                S.dma("sp", utb[up][:], UTv[:, i0:i0 + 2, :], r=["UTb"], w=["utb%d" % up], stream="utb%d" % up)
                S.dma("sp", vtb[up][:], Vv[:, i0:i0 + 2, :], r=["Vb"], w=["vtb%d" % up], stream="vtb%d" % up)

            def emitA(blk, i):
                bp = blk % 2
                gg = blk * 64 + i // 2
                up = gg % 4
                if gg == 0 and i == 0:
                    prefetch(0)
                    prefetch(1)
                    prefetch(2)
                ap_ = i % 2
                for kc in range(8):
                    mm(pA[ap_][:, 0:256], utb[up][:, i % 2, kc * 128:(kc + 1) * 128], h2b[bp][:, kc, :],
                       kc == 0, kc == 7, ["utb%d" % up, "h2b%d" % bp], ["pA%d" % ap_])

            def emitG(blk, i):
                ap_ = i % 2
                hf = i // 64
                act(Ag[ap_][:], pA[ap_][:, 0:256], AF.Gelu, ["pA%d" % ap_], ["Ag%d" % ap_])
                tt("dve", GA[ap_][:], Ag[ap_][:], Gh[hf][:, i % 64, :], ALU.mult, ["Ag%d" % ap_, "G%d" % hf], ["GA%d" % ap_])

            def emitVm(blk, i):
                gg = blk * 64 + i // 2
                up = gg % 4
                vk = "vtb%d" % up
                ap_ = i % 2
                for t2 in range(2):
                    for half in range(2):
                        mm(pO[t2][:, half * 512:(half + 1) * 512], GA[ap_][:, t2 * 128:(t2 + 1) * 128],
                           vtb[up][:, i % 2, half * 512:(half + 1) * 512], i == 0, i == 127,
                           ["GA%d" % ap_, vk], ["pO%d" % t2])
                if i % 2 == 0:
                    prefetch(gg + 3)

            def step(gens, skip=None):
                for g_ in list(gens):
                    if g_ is skip:
                        continue
                    try:
                        next(g_)
                    except StopIteration:
                        gens.remove(g_)

            for _ in build(0, 0):
                run_late()
            run_late()
            side = []
            for blk in range(NBLK):
                side.append(build(blk, 1))
                emitA(blk, 0)
                emitA(blk, 1)
                emitG(blk, 0)
                bld = side[-1]
                for i in range(128):
                    if i == 63:
                        for _ in bld:
                            run_late()
                        run_late()
                    if i == 64 and blk + 1 < NBLK:
                        bld = build(blk + 1, 0)
                        side.append(bld)
                    ip = i % 64
                    if ip % 2 == 0 or ip >= 56:
                        step(side)
                    else:
                        step(side, skip=bld)
                    if i + 2 < 128:
                        emitA(blk, i + 2)
                    if i + 1 < 128:
                        emitG(blk, i + 1)
                    run_late()
                    emitVm(blk, i)
                for _ in bld:
                    run_late()
                run_late()
                fg = final(blk)
                next(fg)
                side.append(fg)
            while side:
                step(side)
                run_late()
            S.flush()
        return nc


def prep_inputs(inputs):
    sq = lambda a: np.ascontiguousarray(a[0]) if a.shape[0] == 1 and a.ndim >= 2 else np.ascontiguousarray(a)
    shared = {}
    for n, sh in IN_SPECS:
        if n in ("x", "c", "consts"):
            continue
        a = np.asarray(inputs[n], dtype=np.float32)
        shared[n] = np.ascontiguousarray(a.reshape(sh))
    shared["consts"] = make_consts()
    x = np.asarray(inputs["x"], dtype=np.float32)
    c = np.asarray(inputs["c"], dtype=np.float32)
    maps = []
    for i in range(NCORES):
        m = dict(shared)
        m["x"] = np.ascontiguousarray(x[i * NB:(i + 1) * NB].reshape(T, D))
        m["c"] = np.ascontiguousarray(c[i * NB:(i + 1) * NB])
        maps.append(m)
    return maps


def kernel(**inputs):
    nc = build()
    maps = prep_inputs(inputs)
    res = run_bass_kernel_spmd(nc, maps, core_ids=list(range(NCORES)))
    outs = [np.asarray(r["out"]).reshape(NB, SEQ, D) for r in res.results]
    return np.concatenate(outs, axis=0).astype(np.float32)
```

```python
import os
from contextlib import ExitStack

import numpy as np
import concourse.bass as bass
import concourse.mybir as mybir
from concourse.bass_utils import run_bass_kernel_spmd

F32 = mybir.dt.float32
BF16 = mybir.dt.bfloat16
I32 = mybir.dt.int32
U32 = mybir.dt.uint32
AF = mybir.ActivationFunctionType
ALU = mybir.AluOpType
AX = mybir.AxisListType

NCORES = 8
D = 1024
NB = 2
SEQ = 2048
T = NB * SEQ
NT = T // 128
INW = 4112
EPS = 1e-6
MAGIC = 12582912.0
TWO_PI = 6.283185307179586


class _Op:
    __slots__ = ("eng", "fn", "dom", "order", "waits", "target", "val", "is_dma")

    def __init__(self, eng, fn, dom, order, is_dma):
        self.eng, self.fn, self.dom, self.order, self.is_dma = eng, fn, dom, order, is_dma
        self.waits = []
        self.target = is_dma
        self.val = None


class Sched:
    CE = ("pe", "act", "dve", "pool")

    def __init__(self, nc, stack):
        self.nc = nc
        self.stack = stack
        self.q = {k: [] for k in ("pe", "act", "dve", "pool", "sp")}
        self.sem = {k: stack.enter_context(nc.semaphore("c_" + k)) for k in self.CE}
        self.cnt = {k: 0 for k in self.CE}
        self.order = {k: 0 for k in self.CE}
        self.dsem = {}
        self.dcnt = {}
        self.dslot = {}
        self.dfree = []
        self.dorder = {}
        self.waited = {k: {} for k in self.q}
        self.lastw = {}
        self.readers = {}
        self.lastop = {}
        self.ninst = 0

    def _need(self, eng, p, out):
        if self.waited[eng].get(p.dom, 0) >= p.order:
            return
        cur = out.get(p.dom)
        if cur is None or cur.order < p.order:
            out[p.dom] = p

    def _deps(self, eng, r, w, is_dma=False):
        need = {}
        for b in r:
            p = self.lastw.get(b)
            if p is not None and (is_dma or not (p.eng == eng and eng == "pe" and not p.is_dma)):
                self._need(eng, p, need)
        for b in w:
            p = self.lastw.get(b)
            if p is not None and (is_dma or p.is_dma or p.eng != eng or eng != "pe"):
                self._need(eng, p, need)
            for p in self.readers.get(b, ()):
                if is_dma or p.is_dma or p.eng != eng or eng != "pe":
                    self._need(eng, p, need)
        return need

    def _add(self, op, need, r, w):
        for dom, p in need.items():
            self.waited[op.eng][dom] = p.order
            p.target = True
            op.waits.append(p)
        self.q[op.eng].append(op)
        self.lastop[op.dom] = op
        for b in r:
            self.readers.setdefault(b, []).append(op)
        for b in w:
            self.lastw[b] = op
            self.readers[b] = []
        self.ninst += 1

    def op(self, eng, fn, r=(), w=()):
        need = self._deps(eng, r, w)
        self.order[eng] += 1
        o = _Op(eng, fn, eng, self.order[eng], False)
        self._add(o, need, r, w)

    def dma(self, eng, out, in_, r=(), w=(), stream="d", **kw):
        if eng == "pool":
            key = "swd_%d" % len(self.dsem)
            self.dsem[key] = self.stack.enter_context(self.nc.semaphore(key))
            self.dcnt[key] = 0
            self.dorder[key] = 0
            self.dslot["__swd__" + key] = key
            stream = "__swd__" + key
        if stream not in self.dslot:
            if self.dfree:
                self.dslot[stream] = self.dfree.pop()
            else:
                k = "dma_%d" % len(self.dsem)
                self.dsem[k] = self.stack.enter_context(self.nc.semaphore(k))
                self.dcnt[k] = 0
                self.dorder[k] = 0
                self.dslot[stream] = k
        key = self.dslot[stream]
        need = self._deps(eng, r, w, is_dma=True)
        self.dorder[key] += 1
        fn = lambda e, out=out, in_=in_, kw=kw: e.dma_start(out=out, in_=in_, **kw)
        o = _Op(eng, fn, key, self.dorder[key], True)
        self._add(o, need, r, w)

    def barrier(self):
        lasts = list(self.lastop.values())
        for eng in self.q:
            need = {}
            for p in lasts:
                self._need(eng, p, need)
            if need:
                o = _Op(eng, None, None, 0, False)
                for dom, p in need.items():
                    self.waited[eng][dom] = p.order
                    p.target = True
                    o.waits.append(p)
                self.q[eng].append(o)
        self.lastw = {}
        self.readers = {}

    def flush(self):
        nc = self.nc
        self.barrier()
        q = self.q
        for eng in q:
            for o in q[eng]:
                if o.fn is None:
                    continue
                if o.is_dma:
                    self.dcnt[o.dom] += 16
                    o.val = self.dcnt[o.dom]
                elif o.target:
                    self.cnt[eng] += 1
                    o.val = self.cnt[eng]
        sem, dsem = self.sem, self.dsem

        def run(e, ops):
            for o in ops:
                for p in o.waits:
                    e.wait_ge(dsem[p.dom] if p.is_dma else sem[p.dom], p.val)
                if o.fn is None:
                    continue
                ins = o.fn(e)
                if o.is_dma:
                    ins.then_inc(dsem[o.dom], 16)
                elif o.target:
                    ins.then_inc(sem[o.eng], 1)

        with nc.Block() as block:
            @block.tensor
            def _(e):
                run(e, q["pe"])

            @block.scalar
            def _(e):
                run(e, q["act"])

            @block.vector
            def _(e):
                run(e, q["dve"])

            @block.gpsimd
            def _(e):
                run(e, q["pool"])

            @block.sync
            def _(e):
                run(e, q["sp"])
        for k in q:
            q[k] = []
        self.dfree.extend(v for k_, v in self.dslot.items() if not k_.startswith("__swd__"))
        self.dslot = {}
        self.lastop = {}


C_IDENT, C_TRIU, C_TRIS, C_ONES, C_IOTA, C_BD16, C_RM8, C_IOTA16, C_END = (
    0, 128, 256, 384, 512, 640, 768, 776, 792)


def make_consts():
    c = np.zeros((128, C_END), np.float32)
    k = np.arange(128)
    c[:, C_IDENT:C_IDENT + 128] = np.eye(128)
    c[:, C_TRIU:C_TRIU + 128] = (k[:, None] <= k[None, :])
    c[:, C_TRIS:C_TRIS + 128] = (k[:, None] > k[None, :])
    c[:, C_ONES:C_ONES + 128] = 1.0
    c[:, C_IOTA:C_IOTA + 128] = k[None, :]
    c[:, C_BD16:C_BD16 + 128] = (k[:, None] // 16 == k[None, :] // 16)
    c[:, C_RM8:C_RM8 + 8] = (k[:, None] // 16 == np.arange(8)[None, :])
    c[:, C_IOTA16:C_IOTA16 + 16] = np.arange(16)[None, :]
    return c


def skew_pipeline(stages, n_items):
    ns = len(stages)
    for k in range(n_items + ns - 1):
        for s_ in reversed(range(ns)):
            n = k - s_
            if 0 <= n < n_items:
                stages[s_](n)


IN_SPECS = [
    ("x", [T, D]), ("c", [NB, D]), ("w_ada", [D, 6 * D]), ("b_ada", [1, 6 * D]),
    ("norm1_g", [1, D]), ("w_in", [D, INW]), ("conv_w", [4, 2048]), ("conv_b", [1, 2048]),
    ("dt_bias", [1, 16]), ("a_log", [1, 16]), ("d_ssd", [1, 16]), ("norm_ssd_g", [1, D]),
    ("s5_a_re", [64, 64]), ("s5_a_im", [64, 64]), ("s5_log_dt", [1, 64]),
    ("s5_b_re", [64, 64, 16]), ("s5_b_im", [64, 64, 16]), ("s5_c_re", [64, 16, 64]),
    ("s5_c_im", [64, 16, 64]), ("s5_d", [64, 16]), ("glu_w", [64, 16, 16]), ("glu_b", [64, 16]),
    ("norm_s5_g", [1, D]), ("w_out", [2 * D, D]), ("norm2_g", [1, D]), ("w_query", [D, 2048]),
    ("sub_keys", [16, 128, 128]), ("expert_u", [16384, D]), ("expert_v", [16384, D]),
    ("norm_f_g", [1, D]), ("consts", [128, C_END]),
]


def build(debug=(), stop_after=None):
    nc = bass.Bass("TRN2", target_bir_lowering=False)
    I = {n: nc.dram_tensor(n, sh, F32, kind="ExternalInput").ap() for n, sh in IN_SPECS}
    out = nc.dram_tensor("out", [T, D], F32, kind="ExternalOutput").ap()

    def SCR(name, shape, dt):
        kind = "ExternalOutput" if name in debug else "Internal"
        return nc.dram_tensor(name, shape, dt, kind=kind).ap()

    MODs = SCR("MODs", [NB, 6 * D], F32)
    XCs = SCR("XCs", [2048, T], BF16)
    UTs = SCR("UTs", [1024, T], BF16)
    Zs = SCR("Zs", [T, D], BF16)
    DTs = SCR("DTs", [T, 16], F32)
    YCs = SCR("YCs", [2048, T], BF16)
    Y5s = SCR("Y5s", [1024, T], F32)
    X1s = SCR("X1s", [T, D], F32)
    H2Ts = SCR("H2Ts", [D, T], BF16)
    RTs = SCR("RTs", [128, 3, T], BF16)
    UTb = SCR("UTb", [128, 128, 1024], BF16)
    Vb = SCR("Vb", [16384, D], BF16)

    with ExitStack() as top:
        S = Sched(nc, top)

        def mm(out_, lhsT, rhs, start, stop, r, w):
            S.op("pe", lambda e: e.matmul(out_, lhsT=lhsT, rhs=rhs, start=start, stop=stop), r=r, w=w)

        def tr(out_, in_, ident, r, w):
            S.op("pe", lambda e: e.transpose(out=out_, in_=in_, identity=ident), r=r, w=w)

        def act(out_, in_, func, r, w, eng="act", **kw):
            S.op(eng, lambda e: e.activation(out=out_, in_=in_, func=func, **kw), r=r, w=w)

        def tt(eng, out_, in0, in1, op, r, w):
            S.op(eng, lambda e: e.tensor_tensor(out=out_, in0=in0, in1=in1, op=op), r=r, w=w)

        def ts(eng, out_, in0, s1, s2, op0, op1, r, w):
            if s2 is None:
                S.op(eng, lambda e: e.tensor_scalar(out=out_, in0=in0, scalar1=s1, scalar2=None, op0=op0), r=r, w=w)
            else:
                S.op(eng, lambda e: e.tensor_scalar(out=out_, in0=in0, scalar1=s1, scalar2=s2, op0=op0, op1=op1),
                     r=r, w=w)

        def stt(out_, in0, scalar, in1, op0, op1, r, w):
            S.op("dve", lambda e: e.scalar_tensor_tensor(out=out_, in0=in0, scalar=scalar, in1=in1, op0=op0, op1=op1),
                 r=r, w=w)

        def cp(eng, out_, in_, r, w):
            if eng == "act":
                act(out_, in_, AF.Copy, r, w)
            else:
                S.op(eng, lambda e: e.tensor_copy(out=out_, in_=in_), r=r, w=w)

        def rsqrt(col, n, r, w):
            ts("dve", col, col, 1.0 / n, EPS, ALU.mult, ALU.add, r, w)
            act(col, col, AF.Sqrt, w, w)
            S.op("dve", lambda e: e.reciprocal(out=col, in_=col), r=w, w=w)

        cst = top.enter_context(nc.sbuf_tensor("cst", [128, C_END], F32))
        cstb = top.enter_context(nc.sbuf_tensor("cstb", [128, C_END], BF16))
        S.dma("sp", cst[:], I["consts"][:, :], w=["cst"], stream="cst")
        cp("dve", cstb[:], cst[:], ["cst"], ["cstb"])
        ident_f = cst[:, C_IDENT:C_IDENT + 128]
        ident_b = cstb[:, C_IDENT:C_IDENT + 128]
        triu_f = cst[:, C_TRIU:C_TRIU + 128]
        tris_f = cst[:, C_TRIS:C_TRIS + 128]
        ones_f = cst[:, C_ONES:C_ONES + 128]
        ones_b = cstb[:, C_ONES:C_ONES + 128]

        with ExitStack() as ph:
            A = lambda n, sh, dt=F32: ph.enter_context(nc.sbuf_tensor(n, sh, dt))
            cT = A("cT", [128, 8, NB])
            rep = A("rep", [128, 8, NB, 128])
            bada = A("bada", [128, 6 * D])
            g1bc = A("g1bc", [128, D])
            g2bc = A("g2bc", [128, D])
            wa = [A("wa%d" % i, [128, 8, 512]) for i in range(2)]
            modbc = [A("modbc%d" % b, [128, 6 * D]) for b in range(NB)]
            pm = [ph.enter_context(nc.psum_tensor("pm%d" % i, [128, 512], F32)) for i in range(2)]
            for b in range(NB):
                S.dma("sp", cT[:, :, b], I["c"][b:b + 1, :].rearrange("o (kc p) -> p (o kc)", p=128), w=["cT"],
                      stream="p0", allow_slow_non_contiguous=True)
            S.dma("sp", bada[:], I["b_ada"][0:1, :].partition_broadcast(128), w=["bada"], stream="p0b")
            S.dma("sp", g1bc[:], I["norm1_g"][0:1, :].partition_broadcast(128), w=["g1bc"], stream="p0c")
            S.dma("sp", g2bc[:], I["norm2_g"][0:1, :].partition_broadcast(128), w=["g2bc"], stream="p0d")
            act(cT[:], cT[:], AF.Silu, ["cT"], ["cT"])
            cp("dve", rep[:], cT[:].unsqueeze(3).to_broadcast([128, 8, NB, 128]), ["cT"], ["rep"])
            wav = I["w_ada"].rearrange("(kc p) n -> p kc n", p=128)
            for n in range(12):
                wb = wa[n % 2]
                wk = "wa%d" % (n % 2)
                S.dma("sp", wb[:], wav[:, :, n * 512:(n + 1) * 512], w=[wk], stream=wk)
                for b in range(NB):
                    pk = "pm%d" % b
                    for kc in range(8):
                        mm(pm[b][:, :], rep[:, kc, b, :], wb[:, kc, :], kc == 0, kc == 7, ["rep", wk], [pk])
                    tt("dve", modbc[b][:, n * 512:(n + 1) * 512], pm[b][:, :], bada[:, n * 512:(n + 1) * 512],
                       ALU.add, [pk, "bada"], ["modbc%d" % b])
            for b in range(NB):
                mk = "modbc%d" % b
                stt(modbc[b][:, 1024:2048], modbc[b][:, 1024:2048], 1.0, g1bc[:], ALU.add, ALU.mult, [mk, "g1bc"], [mk])
                stt(modbc[b][:, 4096:5120], modbc[b][:, 4096:5120], 1.0, g2bc[:], ALU.add, ALU.mult, [mk, "g2bc"], [mk])
                S.dma("sp", MODs[b:b + 1, :], modbc[b][0:1, :], r=[mk], w=["MODs"], stream="p0s")
            S.flush()
        if stop_after == 0:
            return nc

        with ExitStack() as ph:
            A = lambda n, sh, dt=F32: ph.enter_context(nc.sbuf_tensor(n, sh, dt))
            P = lambda n, sh, dt=F32: ph.enter_context(nc.psum_tensor(n, sh, dt))
            win = A("win", [128, 8, INW], BF16)
            hT = A("hT", [128, 8, SEQ], BF16)
            G1 = A("G1", [128, D])
            SH1 = A("SH1", [128, D])
            xin = [A("xin%d" % i, [128, D]) for i in range(5)]
            t1 = [A("t1_%d" % i, [128, D]) for i in range(3)]
            junk = A("junk", [128, D], BF16)
            hb = [A("hb%d" % i, [128, D], BF16) for i in range(3)]
            ss = A("ss", [128, 16])
            zst = [A("zst%d" % i, [128, D], BF16) for i in range(2)]
            dts = [A("dts%d" % i, [128, 16]) for i in range(2)]
            xpad = [A("xpad%d" % i, [128, 3 + SEQ]) for i in range(2)]
            acc = [A("acc%d" % i, [128, SEQ]) for i in range(2)]
            xo = [A("xo%d" % i, [128, SEQ], BF16) for i in range(2)]
            cw = A("cw", [128, 16, 4])
            cb = A("cb", [128, 16])
            pT = [P("pT%d" % i, [128, 1024], BF16) for i in range(2)]
            pz = P("pz", [128, 1024])
            pdt = P("pdt", [128, 16])
            pc = [P("pc%d" % i, [128, 512]) for i in range(2)]

            winv = I["w_in"].rearrange("(kc p) n -> p kc n", p=128)
            for kc in range(8):
                S.dma("pool", win[:, kc, :], winv[:, kc, :], w=["win%d" % kc], stream="win")
            for k in range(4):
                S.dma("sp", cw[:, :, k], I["conv_w"][k:k + 1, :].rearrange("o (ct p) -> p (o ct)", p=128), w=["cw"],
                      stream="cw", allow_slow_non_contiguous=True)
            S.dma("sp", cb[:], I["conv_b"].rearrange("o (ct p) -> p (o ct)", p=128), w=["cb"], stream="cb",
                  allow_slow_non_contiguous=True)
            for i in range(2):
                S.op("dve", lambda e, i=i: e.memset(xpad[i][:, 0:3], 0.0), w=["xpad%d" % i])

            def tokgen(b, i):
                tok0 = b * SEQ + i * 128
                r3, r2 = i % 3, i % 2
                xk, tk_, hk, pk = "xin%d" % (i % 5), "t1_%d" % r3, "hb%d" % r3, "pT%d" % r2
                xt = xin[i % 5]
                col = ss[:, i:i + 1]
                S.dma("sp", xt[:], I["x"][tok0:tok0 + 128, :], w=[xk], stream=xk)
                yield
                act(junk[:], xt[:], AF.Square, [xk], ["junk", "ss%d" % i], accum_out=col)
                yield
                ts("dve", col, col, 1.0 / D, EPS, ALU.mult, ALU.add, ["ss%d" % i], ["ss%d" % i])
                yield
                act(col, col, AF.Sqrt, ["ss%d" % i], ["ss%d" % i])
                yield
                S.op("dve", lambda e: e.reciprocal(out=col, in_=col), r=["ss%d" % i], w=["ss%d" % i])
                stt(t1[r3][:], xt[:], col, G1[:], ALU.mult, ALU.mult, [xk, "ss%d" % i, "G1"], [tk_])
                yield
                tt("pool", hb[r3][:], t1[r3][:], SH1[:], ALU.add, [tk_, "SH1"], [hk])
                yield
                for kc in range(8):
                    tr(pT[r2][:, kc * 128:(kc + 1) * 128], hb[r3][:, kc * 128:(kc + 1) * 128], ident_b, [hk, "cstb"], [pk])
                yield
                cp("act", hT[:, :, i * 128:(i + 1) * 128], pT[r2][:, :].rearrange("p (k t) -> p k t", k=8), [pk],
                   ["hT%d" % i])
                yield
                for half in range(2):
                    for kc in range(8):
                        mm(pz[:, half * 512:(half + 1) * 512], hT[:, kc, i * 128:(i + 1) * 128],
                           win[:, kc, half * 512:(half + 1) * 512], kc == 0, kc == 7, ["hT%d" % i, "win%d" % kc], ["pz"])
                for kc in range(8):
                    mm(pdt[:, :], hT[:, kc, i * 128:(i + 1) * 128], win[:, kc, 3072:3088], kc == 0, kc == 7,
                       ["hT%d" % i, "win%d" % kc], ["pdt"])
                yield
                zk, dk = "zst%d" % r2, "dts%d" % r2
                cp("act", zst[r2][:], pz[:, :], ["pz"], [zk])
                cp("dve", dts[r2][:], pdt[:, :], ["pdt"], [dk])
                S.dma("sp", Zs[tok0:tok0 + 128, :], zst[r2][:], r=[zk], w=["Zs"], stream=zk)
                S.dma("sp", DTs[tok0:tok0 + 128, :], dts[r2][:], r=[dk], w=["DTs"], stream=dk)
                yield

            def chgen(b, ct):
                col0 = 1024 + ct * 128 if ct < 16 else 3088 + (ct - 16) * 128
                par = ct % 2
                xpk, xok, ak = "xpad%d" % par, "xo%d" % par, "acc%d" % par
                for blk in range(4):
                    pck = "pc%d" % (blk % 2)
                    for kc in range(8):
                        mm(pc[blk % 2][:, :], win[:, kc, col0:col0 + 128], hT[:, kc, blk * 512:(blk + 1) * 512],
                           kc == 0, kc == 7, ["win%d" % kc] + ["hT%d" % j for j in range(blk * 4, blk * 4 + 4)], [pck])
                    if ct < 16:
                        cp("act", xpad[par][:, 3 + blk * 512:3 + (blk + 1) * 512], pc[blk % 2][:, :], [pck], [xpk])
                    else:
                        cp("act", xo[par][:, blk * 512:(blk + 1) * 512], pc[blk % 2][:, :], [pck], [xok])
                    if blk % 2 == 1:
                        yield
                if ct < 16:
                    xp = xpad[par]
                    ts("dve", acc[par][:], xp[:, 3:3 + SEQ], cw[:, ct, 3:4], cb[:, ct:ct + 1], ALU.mult, ALU.add,
                       [xpk, "cw", "cb"], [ak])
                    yield
                    for k in (2, 1, 0):
                        stt(acc[par][:], xp[:, k:k + SEQ], cw[:, ct, k:k + 1], acc[par][:], ALU.mult, ALU.add,
                            [xpk, "cw", ak], [ak])
                        yield
                    act(xo[par][:], acc[par][:], AF.Silu, [ak], [xok])
                    yield
                    S.dma("sp", XCs[ct * 128:(ct + 1) * 128, b * SEQ:(b + 1) * SEQ], xo[par][:], r=[xok], w=["XCs"],
                          stream=xok)
                else:
                    S.dma("sp", UTs[(ct - 16) * 128:(ct - 15) * 128, b * SEQ:(b + 1) * SEQ], xo[par][:], r=[xok],
                          w=["UTs"], stream=xok)
                yield

            def run_skewed(gens, every):
                active = []
                k = 0
                while gens or active:
                    if gens and k % every == 0:
                        active.append(gens.pop(0))
                    for g_ in list(active):
                        try:
                            next(g_)
                        except StopIteration:
                            active.remove(g_)
                    k += 1

            for b in range(NB):
                S.dma("sp", G1[:], MODs[b:b + 1, 1024:2048].partition_broadcast(128), r=["MODs"], w=["G1"], stream="g1")
                S.dma("sp", SH1[:], MODs[b:b + 1, 0:1024].partition_broadcast(128), r=["MODs"], w=["SH1"], stream="sh1")
                run_skewed([tokgen(b, i) for i in range(16)], 1)
                run_skewed([chgen(b, ct) for ct in range(24)], 4)
            S.flush()
        if stop_after == 1:
            return nc

        with ExitStack() as ph:
            A = lambda n, sh, dt=F32: ph.enter_context(nc.sbuf_tensor("B_" + n, sh, dt))
            P = lambda n, sh, dt=F32: ph.enter_context(nc.psum_tensor("B_" + n, sh, dt))
            dtb = A("dtb", [128, 16]); abc = A("abc", [128, 16]); d16 = A("d16", [128, 16])
            dssd = A("dssd", [128, D]); normg = A("normg", [128, D])
            S32 = A("S32", [128, 16, 64]); Sbf = A("Sbf", [128, 16, 64], BF16)
            RC = 4
            xct = [A("xct%d" % i, [128, 16, 128], BF16) for i in range(RC)]
            zt = [A("zt%d" % i, [128, D], BF16) for i in range(RC)]
            dtr = [A("dtr%d" % i, [128, 16]) for i in range(RC)]
            dtt = [A("dtt%d" % i, [128, 16]) for i in range(RC)]
            da = [A("da%d" % i, [128, 16]) for i in range(RC)]
            c3 = [A("c3_%d" % i, [128, 48]) for i in range(RC)]
            e3 = [A("e3_%d" % i, [128, 48]) for i in range(RC)]
            xs = [A("xs%d" % i, [128, D], BF16) for i in range(RC)]
            Btok = [A("Btok%d" % i, [128, 512], BF16) for i in range(RC)]
            xdt = [A("xdt%d" % i, [128, D], BF16) for i in range(RC)]
            xdd = [A("xdd%d" % i, [128, D], BF16) for i in range(RC)]
            CBm = [A("CBm%d" % i, [128, 4, 128]) for i in range(RC)]
            RH = 3
            lD = [A("lD%d" % i, [128, 128]) for i in range(RH)]
            Lm = [A("Lm%d" % i, [128, 128]) for i in range(RH)]
            Mt = [A("Mt%d" % i, [128, 128], BF16) for i in range(RH)]
            yo = A("yo", [128, D]); y1 = A("y1", [128, D]); xD = A("xD", [128, D]); sz = A("sz", [128, D])
            yg = A("yg", [128, D]); junkb = A("junkb", [128, 256], BF16); ssq = A("ssq", [128, 4])
            yn = A("yn", [128, D]); ynb = A("ynb", [128, D], BF16)
            ynT = [A("ynT%d" % i, [128, 8, 128], BF16) for i in range(2)]
            pTr = P("pTr", [128, 1024], BF16)
            pDs = [P("pD%d" % i, [128, 512]) for i in range(2)]
            pSl = [P("pS%d" % i, [128, 512]) for i in range(2)]
            pPro = P("pPro", [128, 512])
            pY = P("pY", [128, 512])
            pYo = P("pYo", [128, 512])

            S.dma("sp", dtb[:], I["dt_bias"][0:1, :].partition_broadcast(128), w=["dtb"], stream="b0")
            S.dma("sp", abc[:], I["a_log"][0:1, :].partition_broadcast(128), w=["abc"], stream="b1")
            S.dma("sp", d16[:], I["d_ssd"][0:1, :].partition_broadcast(128), w=["d16"], stream="b2")
            S.dma("sp", normg[:], I["norm_ssd_g"][0:1, :].partition_broadcast(128), w=["normg"], stream="b3")
            act(abc[:], abc[:], AF.Exp, ["abc"], ["abc"])
            ts("dve", abc[:], abc[:], -1.0, None, ALU.mult, None, ["abc"], ["abc"])
            cp("dve", dssd[:, :].rearrange("p (h q) -> p h q", q=64), d16[:, :].unsqueeze(2).to_broadcast([128, 16, 64]),
               ["d16"], ["dssd"])
            XCv = XCs.rearrange("(ct p) t -> p ct t", p=128)
            YCv = YCs.rearrange("(ct p) t -> p ct t", p=128)
            v3 = lambda ap: ap.rearrange("p (h q) -> p h q", q=64)
            bc3 = lambda col: col.unsqueeze(2).to_broadcast([128, 16, 64])
            NCH = NB * 16

            def prologue(ci):
                r_ = ci % RC
                tok0 = ci * 128
                K_ = lambda nm: "%s%d" % (nm, r_)
                X = xct[r_]
                S.dma("sp", X[:], XCv[:, :, tok0:tok0 + 128], r=["XCs"], w=[K_("xct")], stream=K_("xct"))
                S.dma("sp", zt[r_][:], Zs[tok0:tok0 + 128, :], r=["Zs"], w=[K_("zt")], stream=K_("zt"))
                S.dma("sp", dtr[r_][:], DTs[tok0:tok0 + 128, :], r=["DTs"], w=[K_("dtr")], stream=K_("dtr"))
                yield
                tt("dve", dtt[r_][:], dtr[r_][:], dtb[:], ALU.add, [K_("dtr"), "dtb"], [K_("dtt")])
                yield
                act(dtt[r_][:], dtt[r_][:], AF.Exp, [K_("dtt")], [K_("dtt")])
                act(dtt[r_][:], dtt[r_][:], AF.Ln, [K_("dtt")], [K_("dtt")], bias=1.0)
                yield
                tt("dve", da[r_][:], dtt[r_][:], abc[:], ALU.mult, [K_("dtt"), "abc"], [K_("da")])
                yield
                mm(pPro[:, 0:16], triu_f, da[r_][:], True, True, ["cst", K_("da")], ["pm_cs"])
                mm(pPro[:, 16:32], ones_f, da[r_][:], True, True, ["cst", K_("da")], ["pm_cs"])
                cp("dve", c3[r_][:, 0:32], pPro[:, 0:32], ["pm_cs"], [K_("c3")])
                tt("dve", c3[r_][:, 32:48], c3[r_][:, 16:32], c3[r_][:, 0:16], ALU.subtract, [K_("c3")], [K_("c3")])
                yield
                act(e3[r_][:], c3[r_][:], AF.Exp, [K_("c3")], [K_("e3")])
                yield
                for j in range(8):
                    tr(pTr[:, j * 128:(j + 1) * 128], X[:, j, :], ident_b, [K_("xct"), "cstb"], ["pTr"])
                cp("act", xs[r_][:], pTr[:, :], ["pTr"], [K_("xs")])
                yield
                for g in range(4):
                    tr(pTr[:, g * 128:(g + 1) * 128], X[:, 8 + g, :], ident_b, [K_("xct"), "cstb"], ["pTr"])
                cp("act", Btok[r_][:], pTr[:, 0:512], ["pTr"], [K_("Btok")])
                yield
                tt("dve", v3(xdt[r_][:, :]), v3(xs[r_][:, :]), bc3(dtt[r_][:, :]), ALU.mult, [K_("xs"), K_("dtt")],
                   [K_("xdt")])
                tt("dve", v3(xdd[r_][:, :]), v3(xdt[r_][:, :]), bc3(e3[r_][:, 32:48]), ALU.mult, [K_("xdt"), K_("e3")],
                   [K_("xdd")])
                yield
                for g in range(4):
                    mm(pPro[:, 128:256], X[:, 8 + g, :], X[:, 12 + g, :], True, True, [K_("xct")], ["pm_cb"])
                    tt("dve", CBm[r_][:, g, :], pPro[:, 128:256], triu_f, ALU.mult, ["pm_cb", "cst"], [K_("CBm") + "_%d" % g])
                    yield

            def epilogue(ci):
                r_ = ci % RC
                tok0 = ci * 128
                c = ci % 16
                K_ = lambda nm: "%s%d" % (nm, r_)
                yield
                tt("pool", xD[:], xs[r_][:], dssd[:], ALU.mult, [K_("xs"), "dssd"], ["xD"])
                yield
                tt("dve", y1[:], y1[:], xD[:], ALU.add, ["y1", "xD"], ["y1"])
                act(sz[:], zt[r_][:], AF.Silu, [K_("zt")], ["sz"])
                yield
                tt("dve", yg[:], y1[:], sz[:], ALU.mult, ["y1", "sz"], ["yg"])
                yield
                for G_ in range(4):
                    act(junkb[:], yg[:, G_ * 256:(G_ + 1) * 256], AF.Square, ["yg"], ["junkb", "ssq"],
                        accum_out=ssq[:, G_:G_ + 1])
                yield
                ts("dve", ssq[:], ssq[:], 1.0 / 256, EPS, ALU.mult, ALU.add, ["ssq"], ["ssq"])
                yield
                act(ssq[:], ssq[:], AF.Sqrt, ["ssq"], ["ssq"])
                yield
                S.op("dve", lambda e: e.reciprocal(out=ssq[:], in_=ssq[:]), r=["ssq"], w=["ssq"])
                tt("dve", yn[:, :].rearrange("p (g q) -> p g q", q=256), yg[:, :].rearrange("p (g q) -> p g q", q=256),
                   ssq[:, :].unsqueeze(2).to_broadcast([128, 4, 256]), ALU.mult, ["yg", "ssq"], ["yn"])
                yield
                tt("pool", ynb[:], yn[:], normg[:], ALU.mult, ["yn", "normg"], ["ynb"])
                yield
                for j in range(8):
                    tr(pTr[:, j * 128:(j + 1) * 128], ynb[:, j * 128:(j + 1) * 128], ident_b, ["ynb", "cstb"], ["pTr"])
                yk = "ynT%d" % (ci % 2)
                cp("act", ynT[ci % 2][:, :, :], pTr[:, :].rearrange("p (k t) -> p k t", k=8), ["pTr"], [yk])
                S.dma("sp", YCv[:, 0:8, tok0:tok0 + 128], ynT[ci % 2][:, :, :], r=[yk], w=["YCs"], stream=yk)
                yield

            def e1_half(ci, hf):
                r_ = ci % RC
                cs_ = slice(hf * 512, (hf + 1) * 512)
                v3h = lambda ap: ap.rearrange("p (h q) -> p h q", q=64)
                if ci % 16 == 0:
                    cp("dve", y1[:, cs_], pY[:, :], ["pY"], ["y1"])
                else:
                    tt("dve", v3h(yo[:, cs_]), v3h(pYo[:, :]),
                       e3[r_][:, hf * 8:hf * 8 + 8].unsqueeze(2).to_broadcast([128, 8, 64]), ALU.mult,
                       ["pYo", "e3_%d" % r_], ["yo"])
                    tt("dve", y1[:, cs_], yo[:, cs_], pY[:, :], ALU.add, ["yo", "pY"], ["y1"])

            def hinfo(n):
                ci, h = n // 16, n % 16
                return ci, h, h // 4, ci % RC, n % RH, ci % 16

            def h_a(n):
                ci, h, g, r_, hr, c = hinfo(n)
                tt("pool", lD[hr][:], tris_f, da[r_][:, h:h + 1].to_broadcast([128, 128]), ALU.mult,
                   ["cst", "da%d" % r_], ["lD%d" % hr])

            def h_b(n):
                ci, h, g, r_, hr, c = hinfo(n)
                mm(pDs[n % 2][:, 0:128], lD[hr][:], triu_f, True, True, ["lD%d" % hr, "cst"], ["pD%d" % (n % 2)])

            def h_c(n):
                ci, h, g, r_, hr, c = hinfo(n)
                act(Lm[hr][:], pDs[n % 2][:, 0:128], AF.Exp, ["pD%d" % (n % 2)], ["Lm%d" % hr])

            def h_d(n):
                ci, h, g, r_, hr, c = hinfo(n)
                tt("dve", Mt[hr][:], Lm[hr][:], CBm[r_][:, g, :], ALU.mult, ["Lm%d" % hr, "CBm%d_%d" % (r_, g)],
                   ["Mt%d" % hr])

            def h_e(n):
                ci, h, g, r_, hr, c = hinfo(n)
                X = xct[r_]
                mm(pY[:, (h % 8) * 64:(h % 8 + 1) * 64], Mt[hr][:], xdt[r_][:, h * 64:(h + 1) * 64], True, True,
                   ["Mt%d" % hr, "xdt%d" % r_], ["pY"])
                if c != 0:
                    mm(pYo[:, (h % 8) * 64:(h % 8 + 1) * 64], X[:, 12 + g, :], Sbf[:, h, :], True, True,
                       ["xct%d" % r_, "Sbf%d" % h], ["pYo"])
                mm(pSl[n % 2][:, 0:64], Btok[r_][:, g * 128:(g + 1) * 128], xdd[r_][:, h * 64:(h + 1) * 64],
                   True, True, ["Btok%d" % r_, "xdd%d" % r_], ["pS%d" % (n % 2)])

            def h_f(n):
                ci, h, g, r_, hr, c = hinfo(n)
                if c == 0:
                    cp("dve", S32[:, h, :], pSl[n % 2][:, 0:64], ["pS%d" % (n % 2)], ["S32_%d" % h])
                else:
                    stt(S32[:, h, :], S32[:, h, :], e3[r_][:, 16 + h:17 + h], pSl[n % 2][:, 0:64],
                        ALU.mult, ALU.add, ["S32_%d" % h, "e3_%d" % r_, "pS%d" % (n % 2)], ["S32_%d" % h])

            def h_g(n):
                ci, h, g, r_, hr, c = hinfo(n)
                cp("pool", Sbf[:, h, :], S32[:, h, :], ["S32_%d" % h], ["Sbf%d" % h])

            stages = [h_a, h_b, h_c, h_d, h_e, h_f, h_g]
            NS = len(stages)
            for _ in prologue(0):
                pass
            for _ in prologue(1):
                pass
            side = []
            NI_ = NCH * 16
            for k in range(NI_ + NS - 1):
                for s_ in reversed(range(NS)):
                    n = k - s_
                    if 0 <= n < NI_:
                        stages[s_](n)
                if k >= 11 and (k - 11) % 16 == 0:
                    e1_half((k - 11) // 16, 0)
                if k >= 19 and (k - 19) % 16 == 0:
                    e1_half((k - 19) // 16, 1)
                    side.append(epilogue((k - 19) // 16))
                if k % 16 == 0 and k // 16 + 2 < NCH:
                    side.append(prologue(k // 16 + 2))
                for g_ in list(side):
                    try:
                        next(g_)
                    except StopIteration:
                        side.remove(g_)
            while side:
                for g_ in list(side):
                    try:
                        next(g_)
                    except StopIteration:
                        side.remove(g_)
            S.flush()
        if stop_after == 2:
            return nc

        with ExitStack() as ph:
            A = lambda n, sh, dt=F32: ph.enter_context(nc.sbuf_tensor(n, sh, dt))
            Blk1 = A("Blk1", [128, 64, 128], BF16)
            Blk2 = A("Blk2", [128, 64, 128], BF16)
            CL1 = A("CL1", [128, 64, 16], BF16)
            CL2 = A("CL2", [128, 64, 16], BF16)
            rcol = A("rcol", [128, 64])
            fcol = A("fcol", [128, 64])
            D5c = A("D5c", [128, 8])
            glub = A("glub", [128, 8])
            g5c = A("g5c", [128, 8])
            Wg = A("Wg", [128, 8, 128], BF16)
            rm8 = cst[:, C_RM8:C_RM8 + 8]
            INV2PI = 1.0 / TWO_PI

            def sin_turns(out_, f_ap, tk, tf, keys_in, kout, e1="dve", e2="dve"):
                ts(e1, tk, f_ap, MAGIC, MAGIC, ALU.add, ALU.subtract, keys_in, ["_tk"])
                tt(e2, tf, f_ap, tk, ALU.subtract, keys_in + ["_tk"], ["_tf"])
                act(out_, tf, AF.Sin, ["_tf"], [kout], scale=TWO_PI)

            with ExitStack() as p0:
                B_ = lambda n, sh, dt=F32: p0.enter_context(nc.sbuf_tensor(n, sh, dt))
                lrT = B_("lrT", [128, 64]); liT = B_("liT", [128, 64]); dtg = B_("dtg", [128, 64])
                ldt = B_("ldt", [128, 64]); f2 = B_("f2", [128, 64]); tk = B_("tk", [128, 64]); tf = B_("tf", [128, 64])
                sn = B_("sn", [128, 64]); cs_ = B_("cs_", [128, 64]); lbr = B_("lbr", [128, 64]); lbi = B_("lbi", [128, 64])
                den = B_("den", [128, 64]); t_a = B_("t_a", [128, 64]); t_b = B_("t_b", [128, 64])
                cre = B_("cre", [128, 64]); cim = B_("cim", [128, 64])
                br = B_("br", [64, 64, 16]); bi = B_("bi", [64, 64, 16])
                bbr = B_("bbr", [64, 64, 16]); bbi = B_("bbi", [64, 64, 16]); t_c = B_("t_c", [64, 64, 16])
                Dre = B_("Dre", [128, 8, 64]); Dim = B_("Dim", [128, 8, 64]); nDre = B_("nDre", [128, 8, 64])
                cc1 = B_("cc1", [128, 128]); cc2 = B_("cc2", [128, 128]); wrow = B_("wrow", [128, 8, 16])
                ptc = p0.enter_context(nc.psum_tensor("ptc", [128, 128], F32))
                for half in range(2):
                    S.dma("sp", lrT[half * 64:(half + 1) * 64, :], I["s5_a_re"].rearrange("g p -> p g"), w=["lrT"],
                          stream="c0a", allow_slow_non_contiguous=True)
                    S.dma("sp", liT[half * 64:(half + 1) * 64, :], I["s5_a_im"].rearrange("g p -> p g"), w=["liT"],
                          stream="c0b", allow_slow_non_contiguous=True)
                S.dma("sp", dtg[:], I["s5_log_dt"][0:1, :].partition_broadcast(128), w=["dtg"], stream="c0c")
                act(dtg[:], dtg[:], AF.Exp, ["dtg"], ["dtg"])
                tt("dve", ldt[:], lrT[:], dtg[:], ALU.mult, ["lrT", "dtg"], ["ldt"])
                act(rcol[:], ldt[:], AF.Exp, ["ldt"], ["rcol"])
                tt("dve", fcol[:], liT[:], dtg[:], ALU.mult, ["liT", "dtg"], ["fcol"])
                ts("dve", fcol[:], fcol[:], INV2PI, None, ALU.mult, None, ["fcol"], ["fcol"])
                sin_turns(sn[:], fcol[:], tk[:], tf[:], ["fcol"], "sn")
                ts("dve", f2[:], fcol[:], 0.25, None, ALU.add, None, ["fcol"], ["f2"])
                sin_turns(cs_[:], f2[:], tk[:], tf[:], ["f2"], "cs_")
                tt("dve", lbr[:], rcol[:], cs_[:], ALU.mult, ["rcol", "cs_"], ["lbr"])
                tt("dve", lbi[:], rcol[:], sn[:], ALU.mult, ["rcol", "sn"], ["lbi"])
                ts("dve", lbr[:], lbr[:], -1.0, None, ALU.add, None, ["lbr"], ["lbr"])
                tt("dve", den[:], lrT[:], lrT[:], ALU.mult, ["lrT"], ["den"])
                tt("dve", t_a[:], liT[:], liT[:], ALU.mult, ["liT"], ["t_a"])
                tt("dve", den[:], den[:], t_a[:], ALU.add, ["den", "t_a"], ["den"])
                S.op("dve", lambda e: e.reciprocal(out=den[:], in_=den[:]), r=["den"], w=["den"])
                tt("dve", t_a[:], lbr[:], lrT[:], ALU.mult, ["lbr", "lrT"], ["t_a"])
                tt("dve", t_b[:], lbi[:], liT[:], ALU.mult, ["lbi", "liT"], ["t_b"])
                tt("dve", t_a[:], t_a[:], t_b[:], ALU.add, ["t_a", "t_b"], ["t_a"])
                tt("dve", cre[:], t_a[:], den[:], ALU.mult, ["t_a", "den"], ["cre"])
                tt("dve", t_a[:], lbi[:], lrT[:], ALU.mult, ["lbi", "lrT"], ["t_a"])
                tt("dve", t_b[:], lbr[:], liT[:], ALU.mult, ["lbr", "liT"], ["t_b"])
                tt("dve", t_a[:], t_a[:], t_b[:], ALU.subtract, ["t_a", "t_b"], ["t_a"])
                tt("dve", cim[:], t_a[:], den[:], ALU.mult, ["t_a", "den"], ["cim"])
                for q4 in range(4):
                    gs = slice(q4 * 16, (q4 + 1) * 16)
                    S.dma("sp", br[:, gs, :], I["s5_b_re"][gs].rearrange("g p h -> p g h"), w=["br"], stream="c0d")
                    S.dma("sp", bi[:, gs, :], I["s5_b_im"][gs].rearrange("g p h -> p g h"), w=["bi"], stream="c0e")
                bcr = cre[0:64, :].unsqueeze(2).to_broadcast([64, 64, 16])
                bci = cim[0:64, :].unsqueeze(2).to_broadcast([64, 64, 16])
                tt("dve", bbr[:], br[:], bcr, ALU.mult, ["br", "cre"], ["bbr"])
                tt("dve", t_c[:], bi[:], bci, ALU.mult, ["bi", "cim"], ["t_c"])
                tt("dve", bbr[:], bbr[:], t_c[:], ALU.subtract, ["bbr", "t_c"], ["bbr"])
                tt("dve", bbi[:], bi[:], bcr, ALU.mult, ["bi", "cre"], ["bbi"])
                tt("dve", t_c[:], br[:], bci, ALU.mult, ["br", "cim"], ["t_c"])
                tt("dve", bbi[:], bbi[:], t_c[:], ALU.add, ["bbi", "t_c"], ["bbi"])
                for j in range(8):
                    tr(ptc[:, 0:64], bbr[:, j * 8:(j + 1) * 8, :].rearrange("p g h -> p (g h)"), ident_f[0:64, 0:64], ["bbr", "cst"], ["ptc"])
                    cp("dve", Dre[:, j, :], ptc[:, 0:64], ["ptc"], ["Dre"])
                    tr(ptc[:, 64:128], bbi[:, j * 8:(j + 1) * 8, :].rearrange("p g h -> p (g h)"), ident_f[0:64, 0:64], ["bbi", "cst"], ["ptc2"])
                    cp("dve", Dim[:, j, :], ptc[:, 64:128], ["ptc2"], ["Dim"])
                ts("dve", nDre[:], Dre[:], -1.0, None, ALU.mult, None, ["Dre"], ["nDre"])
                rmb = rm8.unsqueeze(2).to_broadcast([128, 8, 64])
                for j in range(8):
                    gs = slice(j * 8, (j + 1) * 8)
                    bcD = lambda t: t[:, j, :].unsqueeze(1).to_broadcast([128, 8, 64])
                    tt("dve", Blk1[:, gs, 0:64], bcD(Dre), rmb, ALU.mult, ["Dre", "cst"], ["Blk1"])
                    tt("dve", Blk1[:, gs, 64:128], bcD(Dim), rmb, ALU.mult, ["Dim", "cst"], ["Blk1"])
                    tt("dve", Blk2[:, gs, 0:64], bcD(Dim), rmb, ALU.mult, ["Dim", "cst"], ["Blk2"])
                    tt("dve", Blk2[:, gs, 64:128], bcD(nDre), rmb, ALU.mult, ["nDre", "cst"], ["Blk2"])
                crv = I["s5_c_re"].rearrange("g h p -> (g h) p")
                civ = I["s5_c_im"].rearrange("g h p -> (g h) p")
                for j in range(8):
                    rs_ = slice(j * 128, (j + 1) * 128)
                    S.dma("sp", cc1[:, 0:64], crv[rs_, :], w=["cc1"], stream="c0f")
                    S.dma("sp", cc1[:, 64:128], civ[rs_, :], w=["cc1"], stream="c0f")
                    S.dma("sp", cc2[:, 0:64], civ[rs_, :], w=["cc2"], stream="c0g")
                    S.dma("sp", cc2[:, 64:128], crv[rs_, :], w=["cc2"], stream="c0g")
                    tr(ptc[:, :], cc1[:], ident_f, ["cc1", "cst"], ["ptc", "ptc2"])
                    gs = slice(j * 8, (j + 1) * 8)
                    cp("dve", CL1[0:64, gs, :], ptc[0:64, :].rearrange("p (g h) -> p g h", h=16), ["ptc"], ["CL1"])
                    ts("dve", CL1[64:128, gs, :], ptc[64:128, :].rearrange("p (g h) -> p g h", h=16), -1.0, None,
                       ALU.mult, None, ["ptc"], ["CL1"])
                    tr(ptc[:, :], cc2[:], ident_f, ["cc2", "cst"], ["ptc", "ptc2"])
                    ts("dve", CL2[:, gs, :], ptc[:, :].rearrange("p (g h) -> p g h", h=16), -1.0, None, ALU.mult, None,
                       ["ptc"], ["CL2"])
                S.dma("sp", D5c[:], I["s5_d"].rearrange("(j gl) h -> (gl h) j", gl=8), w=["D5c"], stream="c0h",
                      allow_slow_non_contiguous=True)
                S.dma("sp", glub[:], I["glu_b"].rearrange("(j gl) h -> (gl h) j", gl=8), w=["glub"], stream="c0i",
                      allow_slow_non_contiguous=True)
                S.dma("sp", g5c[:], I["norm_s5_g"].rearrange("o (j p) -> p (o j)", p=128), w=["g5c"], stream="c0j",
                      allow_slow_non_contiguous=True)
                S.dma("sp", wrow[:], I["glu_w"].rearrange("(j gl) h k -> (gl h) j k", gl=8), w=["wrow"], stream="c0k")
                for j in range(8):
                    tt("dve", Wg[:, j, :].rearrange("p (g k) -> p g k", k=16),
                       wrow[:, j, :].unsqueeze(1).to_broadcast([128, 8, 16]),
                       rm8.unsqueeze(2).to_broadcast([128, 8, 16]), ALU.mult, ["wrow", "cst"], ["Wg"])
                S.flush()

            with ExitStack() as p1:
                B_ = lambda n, sh, dt=F32: p1.enter_context(nc.sbuf_tensor(n, sh, dt))
                Pp = lambda n, sh, dt=F32: p1.enter_context(nc.psum_tensor(n, sh, dt))
                iot = B_("iot", [128, SEQ])
                SIN = [B_("SIN%d" % i, [128, SEQ], BF16) for i in range(3)]
                COS = [B_("COS%d" % i, [128, SEQ], BF16) for i in range(3)]
                u1 = B_("u1", [128, SEQ]); k1 = B_("k1", [128, SEQ]); fr = B_("fr", [128, SEQ])
                uT = [B_("uT%d" % i, [128, T], BF16) for i in range(2)]
                R3 = 3
                p1b = [B_("p1b%d" % i, [128, 512], BF16) for i in range(R3)]
                p2b = [B_("p2b%d" % i, [128, 512], BF16) for i in range(R3)]
                w1 = [B_("w1_%d" % i, [128, 512], BF16) for i in range(R3)]
                w2 = [B_("w2_%d" % i, [128, 512], BF16) for i in range(R3)]
                ww = [B_("ww%d" % i, [128, 512]) for i in range(R3)]
                zz = [[B_("zz%d_%d" % (bb, i), [128, 512]) for i in range(R3)] for bb in range(NB)]
                zb = [B_("zb%d" % i, [128, 512], BF16) for i in range(R3)]
                v1 = [B_("v1_%d" % i, [128, 512], BF16) for i in range(R3)]
                v2 = [B_("v2_%d" % i, [128, 512], BF16) for i in range(R3)]
                ysm = [B_("ysm%d" % i, [16, 512]) for i in range(R3)]
                P1 = [Pp("P1_%d" % i, [128, 512]) for i in range(2)]
                P2 = [Pp("P2_%d" % i, [128, 512]) for i in range(2)]
                uf = [B_("uf%d" % i, [128, D]) for i in range(2)]
                vf = [B_("vf%d" % i, [128, D]) for i in range(2)]
                ub = [B_("ub%d" % i, [128, D], BF16) for i in range(2)]
                vbt = [B_("vbt%d" % i, [128, D], BF16) for i in range(2)]
                uts = [B_("uts%d" % i, [128, D], BF16) for i in range(2)]
                pTu = [Pp("pTu%d" % i, [128, 1024], BF16) for i in range(2)]

                def m0_tile(et):
                    pr = et % 2
                    rows = slice(et * 128, (et + 1) * 128)
                    S.dma("sp", uf[pr][:], I["expert_u"][rows, :], w=["uf%d" % pr], stream="uf%d" % pr)
                    S.dma("sp", vf[pr][:], I["expert_v"][rows, :], w=["vf%d" % pr], stream="vf%d" % pr)
                    yield
                    cp("act", ub[pr][:], uf[pr][:], ["uf%d" % pr], ["ub%d" % pr])
                    cp("act", vbt[pr][:], vf[pr][:], ["vf%d" % pr], ["vbt%d" % pr])
                    yield
                    for kc in range(8):
                        tr(pTu[pr][:, kc * 128:(kc + 1) * 128], ub[pr][:, kc * 128:(kc + 1) * 128], ident_b,
                           ["ub%d" % pr, "cstb"], ["pTu%d" % pr])
                    S.dma("sp", Vb[rows, :], vbt[pr][:], r=["vbt%d" % pr], w=["Vb"], stream="vbo%d" % pr)
                    yield
                    cp("act", uts[pr][:], pTu[pr][:, :], ["pTu%d" % pr], ["uts%d" % pr])
                    yield
                    S.dma("sp", UTb[et], uts[pr][:], r=["uts%d" % pr], w=["UTb"], stream="uto%d" % pr)
                    yield
                PY = [Pp("PY%d" % i, [128, 512]) for i in range(2)]
                for k in range(16):
                    ts("dve", iot[:, k * 128:(k + 1) * 128], cst[:, C_IOTA:C_IOTA + 128], float(128 * k), None, ALU.add,
                       None, ["cst"], ["iot"])

                def tables(g):
                    gp = g % 3
                    fg = fcol[:, g:g + 1]
                    for (tab, key, off) in ((SIN[gp], "SIN%d" % gp, 0.0), (COS[gp], "COS%d" % gp, 0.25)):
                        act(u1[:], iot[:], AF.Identity, ["iot", "fcol"], ["u1"], scale=fg, bias=off)
                        yield
                        ts("dve", k1[:], u1[:], MAGIC, MAGIC, ALU.add, ALU.subtract, ["u1"], ["k1"])
                        yield
                        tt("dve", fr[:], u1[:], k1[:], ALU.subtract, ["u1", "k1"], ["fr"])
                        yield
                        act(tab[:], fr[:], AF.Sin, ["fr"], [key], scale=TWO_PI)
                        yield

                pieces = [(g, q, bb) for g in range(64) for q in range(4) for bb in range(NB)]

                def info(n):
                    g, q, bb = pieces[n]
                    return g, q, bb, g // 8, g % 3, n % R3, q * 512, bb * SEQ + q * 512

                def st_a(n):
                    g, q, bb, j, gp, pb, t0, tok = info(n)
                    if q == 0 and bb == 0 and g + 1 < 64:
                        side.append(tables(g + 1))
                    if g % 8 == 0 and q == 0 and bb == 0:
                        S.dma("sp", uT[j % 2][:], UTs[j * 128:(j + 1) * 128, :], r=["UTs"], w=["uT%d" % (j % 2)],
                              stream="uT%d" % (j % 2))
                    uk = "uT%d" % (j % 2)
                    mm(P1[n % 2][:, :], Blk1[:, g, :], uT[j % 2][:, tok:tok + 512], True, True, ["Blk1", uk], ["P1_%d" % (n % 2)])
                    mm(P2[n % 2][:, :], Blk2[:, g, :], uT[j % 2][:, tok:tok + 512], True, True, ["Blk2", uk], ["P2_%d" % (n % 2)])

                def st_b(n):
                    g, q, bb, j, gp, pb, t0, tok = info(n)
                    cp("act", p1b[pb][:], P1[n % 2][:, :], ["P1_%d" % (n % 2)], ["p1b%d" % pb])
                    cp("act", p2b[pb][:], P2[n % 2][:, :], ["P2_%d" % (n % 2)], ["p2b%d" % pb])

                def st_c(n):
                    g, q, bb, j, gp, pb, t0, tok = info(n)
                    tt("dve", w1[pb][:], p1b[pb][:], COS[gp][:, t0:t0 + 512], ALU.mult, ["p1b%d" % pb, "COS%d" % gp],
                       ["w1_%d" % pb])
                    tt("dve", w2[pb][:], p2b[pb][:], SIN[gp][:, t0:t0 + 512], ALU.mult, ["p2b%d" % pb, "SIN%d" % gp],
                       ["w2_%d" % pb])

                def st_d(n):
                    g, q, bb, j, gp, pb, t0, tok = info(n)
                    tt("pool", ww[pb][:], w1[pb][:], w2[pb][:], ALU.add, ["w1_%d" % pb, "w2_%d" % pb], ["ww%d" % pb])

                def st_e(n):
                    g, q, bb, j, gp, pb, t0, tok = info(n)
                    zc, zp = zz[bb][q % R3], zz[bb][(q - 1) % R3]
                    zck, zpk = "zz%d_%d" % (bb, q % R3), "zz%d_%d" % (bb, (q - 1) % R3)
                    init = 0.0 if q == 0 else zp[:, 511:512]
                    S.op("dve", lambda e: e.tensor_tensor_scan(
                        out=zc[:], data0=rcol[:, g:g + 1].to_broadcast([128, 512]), data1=ww[pb][:],
                        initial=init, op0=ALU.mult, op1=ALU.add), r=["rcol", "ww%d" % pb, zpk], w=[zck])

                def st_f(n):
                    g, q, bb, j, gp, pb, t0, tok = info(n)
                    cp("act", zb[pb][:], zz[bb][q % R3][:], ["zz%d_%d" % (bb, q % R3)], ["zb%d" % pb])

                def st_g(n):
                    g, q, bb, j, gp, pb, t0, tok = info(n)
                    tt("dve", v1[pb][:], zb[pb][:], COS[gp][:, t0:t0 + 512], ALU.mult, ["zb%d" % pb, "COS%d" % gp],
                       ["v1_%d" % pb])
                    tt("pool", v2[pb][:], zb[pb][:], SIN[gp][:, t0:t0 + 512], ALU.mult, ["zb%d" % pb, "SIN%d" % gp],
                       ["v2_%d" % pb])

                def st_h(n):
                    g, q, bb, j, gp, pb, t0, tok = info(n)
                    pp = n % 2
                    mm(PY[pp][0:16, :], CL1[:, g, :], v1[pb][:], True, False, ["CL1", "v1_%d" % pb], ["PY%d" % pp])
                    mm(PY[pp][0:16, :], CL2[:, g, :], v2[pb][:], False, True, ["CL2", "v2_%d" % pb], ["PY%d" % pp])

                def st_i(n):
                    g, q, bb, j, gp, pb, t0, tok = info(n)
                    pp = n % 2
                    yk = "ysm%d" % pb
                    cp("act", ysm[pb][0:16, :], PY[pp][0:16, :], ["PY%d" % pp], [yk])
                    S.dma("sp", Y5s[g * 16:(g + 1) * 16, tok:tok + 512], ysm[pb][0:16, :], r=[yk], w=["Y5s"], stream=yk)

                for _ in tables(0):
                    pass
                stages_c = [st_a, st_b, st_c, st_d, st_e, st_f, st_g, st_h, st_i]
                side = []
                nxt_et = 0
                for k in range(len(pieces) + len(stages_c) - 1):
                    for s_ in reversed(range(len(stages_c))):
                        n = k - s_
                        if 0 <= n < len(pieces):
                            stages_c[s_](n)
                    if k % 4 == 0 and nxt_et < 128:
                        side.append(m0_tile(nxt_et))
                        nxt_et += 1
                    for g_ in list(side):
                        try:
                            next(g_)
                        except StopIteration:
                            side.remove(g_)
                while side or nxt_et < 128:
                    if nxt_et < 128:
                        side.append(m0_tile(nxt_et))
                        nxt_et += 1
                    for g_ in list(side):
                        try:
                            next(g_)
                        except StopIteration:
                            side.remove(g_)
                S.flush()

            with ExitStack() as p2:
                B_ = lambda n, sh, dt=F32: p2.enter_context(nc.sbuf_tensor("C2_" + n, sh, dt))
                Pp = lambda n, sh, dt=F32: p2.enter_context(nc.psum_tensor("C2_" + n, sh, dt))
                y5 = [B_("y5_%d" % i, [128, 512]) for i in range(3)]
                uu = [B_("uu%d" % i, [128, 512], BF16) for i in range(3)]
                yv = [B_("yv%d" % i, [128, 512]) for i in range(3)]
                vb = [B_("vb%d" % i, [128, 512], BF16) for i in range(3)]
                sg = [B_("sg%d" % i, [128, 512]) for i in range(3)]
                oo = [B_("oo%d" % i, [128, 8, 512]) for i in range(2)]
                sq = [B_("sq%d" % i, [128, 512]) for i in range(3)]
                rs5 = [B_("rs5_%d" % i, [128, 512]) for i in range(2)]
                ycb = [B_("ycb%d" % i, [128, 512], BF16) for i in range(2)]
                PG = [Pp("PG%d" % i, [128, 512]) for i in range(2)]
                PSS = [Pp("PSS%d" % i, [128, 512]) for i in range(2)]
                NBK = T // 512

                def cinfo(n):
                    return n // 8, n % 8, n % 3, (n // 8) * 512

                def c_a(n):
                    blk, j, r3, tok = cinfo(n)
                    S.dma("sp", y5[r3][:], Y5s[j * 128:(j + 1) * 128, tok:tok + 512], r=["Y5s"], w=["y5_%d" % r3],
                          stream="y5_%d" % r3)
                    S.dma("sp", uu[r3][:], UTs[j * 128:(j + 1) * 128, tok:tok + 512], r=["UTs"], w=["uu%d" % r3],
                          stream="uu%d" % r3)

                def c_b(n):
                    blk, j, r3, tok = cinfo(n)
                    stt(yv[r3][:], uu[r3][:], D5c[:, j:j + 1], y5[r3][:], ALU.mult, ALU.add,
                        ["uu%d" % r3, "D5c", "y5_%d" % r3], ["yv%d" % r3])

                def c_c(n):
                    blk, j, r3, tok = cinfo(n)
                    act(vb[r3][:], yv[r3][:], AF.Gelu, ["yv%d" % r3], ["vb%d" % r3])

                def c_d(n):
                    blk, j, r3, tok = cinfo(n)
                    mm(PG[n % 2][:, :], Wg[:, j, :], vb[r3][:], True, True, ["Wg", "vb%d" % r3], ["PG%d" % (n % 2)])

                def c_e(n):
                    blk, j, r3, tok = cinfo(n)
                    act(sg[r3][:], PG[n % 2][:, :], AF.Sigmoid, ["PG%d" % (n % 2), "glub"], ["sg%d" % r3],
                        bias=glub[:, j:j + 1])

                def c_f(n):
                    blk, j, r3, tok = cinfo(n)
                    tt("dve", oo[blk % 2][:, j, :], vb[r3][:], sg[r3][:], ALU.mult, ["vb%d" % r3, "sg%d" % r3],
                       ["oo%d_%d" % (blk % 2, j)])

                def c_g(n):
                    blk, j, r3, tok = cinfo(n)
                    tt("pool", sq[r3][:], oo[blk % 2][:, j, :], oo[blk % 2][:, j, :], ALU.mult,
                       ["oo%d_%d" % (blk % 2, j)], ["sq%d" % r3])

                def c_h(n):
                    blk, j, r3, tok = cinfo(n)
                    mm(PSS[blk % 2][:, :], ones_f, sq[r3][:], j == 0, j == 7, ["cst", "sq%d" % r3], ["PSS%d" % (blk % 2)])

                def c_tail(blk):
                    bp = blk % 2
                    tok = blk * 512
                    rk = "rs5_%d" % bp
                    ts("dve", rs5[bp][:], PSS[bp][:, :], 1.0 / 1024, EPS, ALU.mult, ALU.add, ["PSS%d" % bp], [rk])
                    yield
                    act(rs5[bp][:], rs5[bp][:], AF.Sqrt, [rk], [rk])
                    yield
                    S.op("dve", lambda e: e.reciprocal(out=rs5[bp][:], in_=rs5[bp][:]), r=[rk], w=[rk])
                    yield
                    for j in range(8):
                        jp = j % 2
                        stt(ycb[jp][:], oo[bp][:, j, :], g5c[:, j:j + 1], rs5[bp][:], ALU.mult, ALU.mult,
                            ["oo%d_%d" % (bp, j), "g5c", rk], ["ycb%d" % jp])
                        S.dma("sp", YCs[1024 + j * 128:1024 + (j + 1) * 128, tok:tok + 512], ycb[jp][:],
                              r=["ycb%d" % jp], w=["YCs"], stream="ycb%d" % jp)
                        yield

                st2 = [c_a, c_b, c_c, c_d, c_e, c_f, c_g, c_h]
                NI2 = NBK * 8
                side2 = []
                for k in range(NI2 + len(st2) - 1):
                    for s_ in reversed(range(len(st2))):
                        n = k - s_
                        if 0 <= n < NI2:
                            st2[s_](n)
                    nh = k - (len(st2) - 1)
                    if nh >= 0 and nh % 8 == 7:
                        side2.append(c_tail(nh // 8))
                    for g_ in list(side2):
                        try:
                            next(g_)
                        except StopIteration:
                            side2.remove(g_)
                while side2:
                    for g_ in list(side2):
                        try:
                            next(g_)
                        except StopIteration:
                            side2.remove(g_)
                S.flush()
        if stop_after == 3:
            return nc

        with ExitStack() as ph:
            A = lambda n, sh, dt=F32: ph.enter_context(nc.sbuf_tensor("D_" + n, sh, dt))
            P = lambda n, sh, dt=F32: ph.enter_context(nc.psum_tensor("D_" + n, sh, dt))
            wout = A("wout", [128, 16, D], BF16)
            wq = A("wq", [128, 8, 2048], BF16)
            skf = A("skf", [128, 16, 128])
            skT = A("skT", [128, 16, 128], BF16)
            GT1 = A("GT1", [128, D]); G2 = A("G2", [128, D]); SH2 = A("SH2", [128, D])
            yct = [A("yct%d" % i, [128, 16, 128], BF16) for i in range(2)]
            xin = [A("xin%d" % i, [128, D]) for i in range(2)]
            t1 = A("t1", [128, D]); x1 = [A("x1_%d" % i, [128, D]) for i in range(2)]
            junk = A("junk", [128, D], BF16)
            ss2 = A("ss2", [128, 32])
            hb2 = A("hb2", [128, D], BF16)
            h2T = [A("h2T%d" % i, [128, 8, 128], BF16) for i in range(2)]
            qT = A("qT", [128, 16, 128], BF16)
            scb = [A("sc_%d" % i, [128, 16, 128]) for i in range(2)]; sc2 = A("sc2", [128, 16, 128])
            v8 = A("v8", [128, 16, 16]); i8 = A("i8", [128, 16, 16], U32); i8f = A("i8f", [128, 16, 16])
            cand = A("cand", [128, 8, 256]); cand2 = A("cand2", [128, 8, 256])
            c8 = A("c8", [128, 8, 16]); p8 = A("p8", [128, 8, 16], U32)
            ge = A("ge", [128, 8, 16]); gs = A("gs", [128, 8]); gg = A("gg", [128, 8, 16])
            ra_i = A("ra_i", [128, 128], I32); rb_i = A("rb_i", [128, 128], I32)
            raf = A("raf", [128, 128]); rbf = A("rbf", [128, 128])
            oh = A("oh", [128, 128, 16]); oh2 = A("oh2", [128, 128, 16])
            isel = A("isel", [128, 128]); jsel = A("jsel", [128, 128])
            rstg = [A("rstg%d" % i, [128, 3, 128], BF16) for i in range(2)]
            pT = P("pT", [128, 1024], BF16)
            pM = P("pM", [128, 1024])
            pq = [P("pq%d" % i, [128, 512]) for i in range(2)]
            psc = [P("psc%d" % i, [128, 512]) for i in range(2)]
            pTi = P("pTi", [128, 512])
            iota16 = cst[:, C_IOTA16:C_IOTA16 + 16]

            woutv = I["w_out"].rearrange("(ct p) d -> p ct d", p=128)
            for q4 in range(4):
                S.dma("pool", wout[:, q4 * 4:(q4 + 1) * 4, :], woutv[:, q4 * 4:(q4 + 1) * 4, :], w=["wout"], stream="wout")
            wqv = I["w_query"].rearrange("(kc p) n -> p kc n", p=128)
            for q4 in range(4):
                S.dma("pool", wq[:, q4 * 2:(q4 + 1) * 2, :], wqv[:, q4 * 2:(q4 + 1) * 2, :], w=["wq"], stream="wq")
            S.dma("sp", skf[:], I["sub_keys"].rearrange("m k d -> k m d"), w=["skf"], stream="skf")
            for m in range(16):
                tr(pTi[:, (m % 4) * 128:(m % 4 + 1) * 128], skf[:, m, :], ident_f, ["skf", "cst"], ["pTi"])
                cp("dve", skT[:, m, :], pTi[:, (m % 4) * 128:(m % 4 + 1) * 128], ["pTi"], ["skT"])
            YCv = YCs.rearrange("(ct p) t -> p ct t", p=128)
            H2v = H2Ts.rearrange("(kc p) t -> p kc t", p=128)
            def tile_vars(i):
                return i // 16, i % 2, i * 128

            def front(i):
                b, par, tok0 = tile_vars(i)
                sck = "sc%d" % par
                if i % 16 == 0:
                    S.dma("sp", GT1[:], MODs[b:b + 1, 2048:3072].partition_broadcast(128), r=["MODs"], w=["GT1"], stream="d0")
                    S.dma("sp", G2[:], MODs[b:b + 1, 4096:5120].partition_broadcast(128), r=["MODs"], w=["G2"], stream="d1")
                    S.dma("sp", SH2[:], MODs[b:b + 1, 3072:4096].partition_broadcast(128), r=["MODs"], w=["SH2"], stream="d2")
                yk, xk, x1k, hk = "yct%d" % par, "xin%d" % par, "x1_%d" % par, "h2T%d" % par
                S.dma("sp", yct[par][:], YCv[:, :, tok0:tok0 + 128], r=["YCs"], w=[yk], stream=yk)
                S.dma("sp", xin[par][:], I["x"][tok0:tok0 + 128, :], w=[xk], stream=xk)
                yield
                for half in range(2):
                    for ct in range(16):
                        mm(pM[:, half * 512:(half + 1) * 512], yct[par][:, ct, :], wout[:, ct, half * 512:(half + 1) * 512],
                           ct == 0, ct == 15, [yk, "wout"], ["pM"])
                yield
                tt("dve", t1[:], pM[:, :], GT1[:], ALU.mult, ["pM", "GT1"], ["t1"])
                yield
                tt("pool", x1[par][:], t1[:], xin[par][:], ALU.add, ["t1", xk], [x1k])
                S.dma("sp", X1s[tok0:tok0 + 128, :], x1[par][:], r=[x1k], w=["X1s"], stream=x1k)
                yield
                act(junk[:], x1[par][:], AF.Square, [x1k], ["junk", "ss2"], accum_out=ss2[:, i:i + 1])
                yield
                col = ss2[:, i:i + 1]
                ts("dve", col, col, 1.0 / D, EPS, ALU.mult, ALU.add, ["ss2"], ["ss2"])
                yield
                act(col, col, AF.Sqrt, ["ss2"], ["ss2"])
                yield
                S.op("dve", lambda e: e.reciprocal(out=col, in_=col), r=["ss2"], w=["ss2"])
                stt(t1[:], x1[par][:], ss2[:, i:i + 1], G2[:], ALU.mult, ALU.mult, [x1k, "ss2", "G2"], ["t1"])
                yield
                tt("pool", hb2[:], t1[:], SH2[:], ALU.add, ["t1", "SH2"], ["hb2"])
                yield
                for kc in range(8):
                    tr(pT[:, kc * 128:(kc + 1) * 128], hb2[:, kc * 128:(kc + 1) * 128], ident_b, ["hb2", "cstb"], ["pT"])
                yield
                cp("act", h2T[par][:, :, :], pT[:, :].rearrange("p (k t) -> p k t", k=8), ["pT"], [hk])
                S.dma("sp", H2v[:, :, tok0:tok0 + 128], h2T[par][:, :, :], r=[hk], w=["H2Ts"], stream=hk)
                yield
                for m4 in range(4):
                    pp = m4 % 2
                    for mi in range(4):
                        m = m4 * 4 + mi
                        for kc in range(8):
                            mm(pq[pp][:, mi * 128:(mi + 1) * 128], wq[:, kc, m * 128:(m + 1) * 128], h2T[par][:, kc, :],
                               kc == 0, kc == 7, ["wq", hk], ["pq%d" % pp])
                    cp("act", qT[:, m4 * 4:(m4 + 1) * 4, :], pq[pp][:, :].rearrange("p (m t) -> p m t", m=4),
                       ["pq%d" % pp], ["qT%d" % m4])
                    yield
                for m4 in range(4):
                    pp = m4 % 2
                    for mi in range(4):
                        m = m4 * 4 + mi
                        mm(psc[pp][:, mi * 128:(mi + 1) * 128], qT[:, m, :], skT[:, m, :], True, True,
                           ["qT%d" % m4, "skT"], ["psc%d" % pp])
                    cp("act", scb[par][:, m4 * 4:(m4 + 1) * 4, :], psc[pp][:, :].rearrange("p (m k) -> p m k", m=4),
                       ["psc%d" % pp], [sck])
                    yield

            def back(i):
                b, par, tok0 = tile_vars(i)
                sck = "sc%d" % par
                for m in range(16):
                    S.op("dve", lambda e, m=m: e.max(out=v8[:, m, 0:8], in_=scb[par][:, m, :]), r=[sck], w=["v8a%d" % m])
                yield
                for m in range(16):
                    S.op("dve", lambda e, m=m: e.max_index(out=i8[:, m, 0:8], in_max=v8[:, m, 0:8], in_values=scb[par][:, m, :]),
                         r=[sck, "v8a%d" % m], w=["i8a%d" % m])
                    S.op("dve", lambda e, m=m: e.match_replace(out=sc2[:, m, :], in_to_replace=v8[:, m, 0:8],
                                                               in_values=scb[par][:, m, :], imm_value=-1e30),
                         r=[sck, "v8a%d" % m], w=["sc2_%d" % m])
                    if m % 4 == 3:
                        yield
                for m in range(16):
                    S.op("dve", lambda e, m=m: e.max(out=v8[:, m, 8:16], in_=sc2[:, m, :]), r=["sc2_%d" % m],
                         w=["v8b%d" % m])
                yield
                for m in range(16):
                    S.op("dve", lambda e, m=m: e.max_index(out=i8[:, m, 8:16], in_max=v8[:, m, 8:16],
                                                           in_values=sc2[:, m, :]), r=["sc2_%d" % m, "v8b%d" % m],
                         w=["i8b%d" % m])
                yield
                v8keys = ["v8a%d" % m for m in range(16)] + ["v8b%d" % m for m in range(16)]
                i8keys = ["i8a%d" % m for m in range(16)] + ["i8b%d" % m for m in range(16)]
                cp("dve", i8f[:], i8[:], i8keys, ["i8f"])
                v8v = v8[:, :, :].rearrange("p (h c) r -> p h c r", c=2)
                i8v = i8f[:, :, :].rearrange("p (h c) r -> p h c r", c=2)
                tt("dve", cand[:, :, :].rearrange("p h (r c) -> p h r c", c=16),
                   v8v[:, :, 0, :].unsqueeze(3).to_broadcast([128, 8, 16, 16]),
                   v8v[:, :, 1, :].unsqueeze(2).to_broadcast([128, 8, 16, 16]), ALU.add, v8keys, ["cand"])
                yield
                for h in range(8):
                    S.op("dve", lambda e, h=h: e.max(out=c8[:, h, 0:8], in_=cand[:, h, :]), r=["cand"], w=["c8a%d" % h])
                yield
                for h in range(8):
                    S.op("dve", lambda e, h=h: e.max_index(out=p8[:, h, 0:8], in_max=c8[:, h, 0:8], in_values=cand[:, h, :]),
                         r=["cand", "c8a%d" % h], w=["p8a%d" % h])
                    S.op("dve", lambda e, h=h: e.match_replace(out=cand2[:, h, :], in_to_replace=c8[:, h, 0:8],
                                                               in_values=cand[:, h, :], imm_value=-1e30),
                         r=["cand", "c8a%d" % h], w=["cand2_%d" % h])
                    if h % 4 == 3:
                        yield
                for h in range(8):
                    S.op("dve", lambda e, h=h: e.max(out=c8[:, h, 8:16], in_=cand2[:, h, :]), r=["cand2_%d" % h],
                         w=["c8b%d" % h])
                yield
                for h in range(8):
                    S.op("dve", lambda e, h=h: e.max_index(out=p8[:, h, 8:16], in_max=c8[:, h, 8:16],
                                                           in_values=cand2[:, h, :]), r=["cand2_%d" % h, "c8b%d" % h],
                         w=["p8b%d" % h])
                yield
                c8keys = ["c8a%d" % h for h in range(8)] + ["c8b%d" % h for h in range(8)]
                p8keys = ["p8a%d" % h for h in range(8)] + ["p8b%d" % h for h in range(8)]
                tt("dve", ge[:], c8[:], c8[:, :, 0:1].to_broadcast([128, 8, 16]), ALU.subtract, c8keys, ["ge"])
                yield
                act(ge[:], ge[:], AF.Exp, ["ge"], ["ge"])
                yield
                S.op("dve", lambda e: e.tensor_reduce(out=gs[:], in_=ge[:], axis=AX.X, op=ALU.add), r=["ge"], w=["gs"])
                S.op("dve", lambda e: e.reciprocal(out=gs[:], in_=gs[:]), r=["gs"], w=["gs"])
                tt("dve", gg[:], ge[:], gs[:, :].unsqueeze(2).to_broadcast([128, 8, 16]), ALU.mult, ["ge", "gs"], ["gg"])
                p8i = p8[:, :, :].rearrange("p h k -> p (h k)").bitcast(I32)
                S.op("dve", lambda e: e.tensor_single_scalar(out=ra_i[:], in_=p8i, scalar=4, op=ALU.logical_shift_right),
                     r=p8keys, w=["ra_i"])
                S.op("dve", lambda e: e.tensor_single_scalar(out=rb_i[:], in_=p8i, scalar=15, op=ALU.bitwise_and),
                     r=p8keys, w=["rb_i"])
                cp("dve", raf[:], ra_i[:], ["ra_i"], ["raf"])
                cp("dve", rbf[:], rb_i[:], ["rb_i"], ["rbf"])
                yield
                io3 = iota16.unsqueeze(1).to_broadcast([128, 128, 16])
                for (rf, rk, ci, ohh, ok, sel, sk_) in ((raf, "raf", 0, oh, "oh", isel, "isel"),
                                                        (rbf, "rbf", 1, oh2, "oh2", jsel, "jsel")):
                    eng = "dve" if ci == 0 else "pool"
                    tt("dve", ohh[:], rf[:, :].unsqueeze(2).to_broadcast([128, 128, 16]), io3, ALU.is_equal,
                       [rk, "cst"], [ok])
                    tt(eng, ohh[:, :, :].rearrange("p (h k) r -> p h k r", h=8),
                       ohh[:, :, :].rearrange("p (h k) r -> p h k r", h=8),
                       i8v[:, :, ci, :].unsqueeze(2).to_broadcast([128, 8, 16, 16]), ALU.mult, [ok, "i8f"], [ok])
                    S.op("dve", lambda e, sel=sel, ohh=ohh: e.tensor_reduce(out=sel[:], in_=ohh[:], axis=AX.X, op=ALU.add),
                         r=[ok], w=[sk_])
                    yield
                tr(pTi[:, 0:128], isel[:], ident_f, ["isel", "cst"], ["pTi"])
                tr(pTi[:, 128:256], jsel[:], ident_f, ["jsel", "cst"], ["pTi"])
                tr(pTi[:, 256:384], gg[:, :, :].rearrange("p h k -> p (h k)"), ident_f, ["gg", "cst"], ["pTi"])
                rk_ = "rstg%d" % par
                cp("act", rstg[par][:, :, :], pTi[:, 0:384].rearrange("p (a t) -> p a t", a=3), ["pTi"], [rk_])
                S.dma("sp", RTs[:, :, tok0:tok0 + 128], rstg[par][:, :, :], r=[rk_], w=["RTs"], stream=rk_)

            def interleave(gens):
                gens = [g_ for g_ in gens if g_ is not None]
                while gens:
                    for g_ in list(gens):
                        try:
                            next(g_)
                        except StopIteration:
                            gens.remove(g_)

            interleave([front(0)])
            for i in range(NT):
                interleave([front(i + 1) if i + 1 < NT else None, back(i)])
            S.flush()
        if stop_after == 4:
            return nc

        with ExitStack() as ph:
            A = lambda n, sh, dt=F32: ph.enter_context(nc.sbuf_tensor("M_" + n, sh, dt))
            P = lambda n, sh, dt=F32: ph.enter_context(nc.psum_tensor("M_" + n, sh, dt))
            TB = 256
            G0 = A("G0", [128, 64, TB], BF16)
            G1 = A("G1", [128, 64, TB], BF16)
            utb = [A("utb%d" % i, [128, 2, D], BF16) for i in range(4)]
            vtb = [A("vtb%d" % i, [128, 2, D], BF16) for i in range(4)]
            Pm = [A("Pm%d" % i, [128, 8, 64], BF16) for i in range(4)]
            Q0 = [A("Q0%d" % i, [128, 8, 128], BF16) for i in range(4)]
            Qm = [A("Qm%d" % i, [128, 8, 128], BF16) for i in range(4)]
            h2b = [A("h2b%d" % i, [128, 8, TB], BF16) for i in range(2)]
            rt = [A("rt%d" % i, [128, 3, TB], BF16) for i in range(2)]
            Ag = [A("Ag%d" % i, [128, TB], BF16) for i in range(2)]
            GA = [A("GA%d" % i, [128, TB], BF16) for i in range(2)]
            x1t = [A("x1t%d" % i, [128, D]) for i in range(2)]
            GT2 = A("GT2", [128, D]); nfg = A("nfg", [128, D])
            tm = [A("tm%d" % i, [128, D]) for i in range(2)]; x2 = A("x2", [128, D]); junkm = A("junkm", [128, D], BF16)
            ot = [A("ot%d" % i, [128, D]) for i in range(2)]
            ssf = A("ssf", [128, 32])
            pO = [P("pO%d" % i, [128, 1024]) for i in range(2)]
            pA = [P("pA%d" % i, [128, 512]) for i in range(2)]
            pG = [P("pG%d" % i, [128, 512]) for i in range(2)]
            iota_b = cstb[:, C_IOTA:C_IOTA + 128]
            io32 = iota_b.unsqueeze(1).to_broadcast([128, 32, 128])
            io8 = iota_b.unsqueeze(1).to_broadcast([128, 8, 128])
            H2v = H2Ts.rearrange("(kc p) t -> p kc t", p=128)
            UTv = UTb.rearrange("e p x -> p e x")
            Vv = Vb.rearrange("(e p) d -> p e d", p=128)
            S.dma("sp", nfg[:], I["norm_f_g"][0:1, :].partition_broadcast(128), w=["nfg"], stream="m0")
            Gh = [G0, G1]
            io64 = [iota_b[:, 64 * hf:64 * hf + 64].unsqueeze(1).to_broadcast([128, 8, 64]) for hf in range(2)]
            NBLK = T // TB
            cnts = {"g": 0, "s": 0}
            late = []

            def run_late():
                for f_ in late:
                    f_()
                del late[:]

            def build(blk, hf):
                bp = blk % 2
                tok = blk * TB
                hk, rk = "h2b%d" % bp, "rt%d" % bp
                if hf == 0:
                    S.dma("sp", h2b[bp][:], H2v[:, :, tok:tok + TB], r=["H2Ts"], w=[hk], stream=hk)
                    S.dma("sp", rt[bp][:], RTs[:, :, tok:tok + TB], r=["RTs"], w=[rk], stream=rk)
                    yield
                NG = TB // 8
                base = cnts["s"]
                cnts["s"] += NG

                def dve_part(k):
                    sp_ = (base + k) % 4
                    tsl = slice(k * 8, (k + 1) * 8)
                    bcn = lambda a_, n_: rt[bp][:, a_, tsl].unsqueeze(2).to_broadcast([128, 8, n_])
                    tt("dve", Pm[sp_][:], bcn(0, 64), io64[hf], ALU.is_equal, [rk, "cstb"], ["Pm%d" % sp_])
                    tt("dve", Q0[sp_][:], bcn(1, 128), io8, ALU.is_equal, [rk, "cstb"], ["Q0%d" % sp_])

                def pool_part(k):
                    sp_ = (base + k) % 4
                    tsl = slice(k * 8, (k + 1) * 8)
                    tt("pool", Qm[sp_][:], Q0[sp_][:], rt[bp][:, 2, tsl].unsqueeze(2).to_broadcast([128, 8, 128]),
                       ALU.mult, ["Q0%d" % sp_, rk], ["Qm%d" % sp_])

                def mm_part(k):
                    sp_ = (base + k) % 4
                    for t4 in range(2):
                        gp = cnts["g"] % 2
                        cnts["g"] += 1
                        for ti in range(4):
                            t = t4 * 4 + ti
                            mm(pG[gp][:, ti * 64:(ti + 1) * 64], Qm[sp_][:, t, :], Pm[sp_][:, t, :], True, True,
                               ["Qm%d" % sp_, "Pm%d" % sp_], ["pG%d" % gp])
                        t0 = k * 8 + t4 * 4
                        late.append(lambda gp=gp, t0=t0: cp(
                            "act", Gh[hf][:, :, t0:t0 + 4], pG[gp][:, 0:256].rearrange("p (t i) -> p i t", t=4),
                            ["pG%d" % gp], ["G%d" % hf]))

                dve_part(0)
                dve_part(1)
                dve_part(2)
                yield
                pool_part(0)
                pool_part(1)
                yield
                for k in range(NG):
                    mm_part(k)
                    if k + 3 < NG:
                        late.append(lambda k=k: dve_part(k + 3))
                    if k + 2 < NG:
                        late.append(lambda k=k: pool_part(k + 2))
                    yield

            def final(blk):
                tok = blk * TB
                b_ = tok // SEQ
                if tok % SEQ == 0:
                    S.dma("sp", GT2[:], MODs[b_:b_ + 1, 5120:6144].partition_broadcast(128), r=["MODs"], w=["GT2"],
                          stream="m1")
                for t2 in range(2):
                    tt("dve", tm[t2][:], pO[t2][:, :], GT2[:], ALU.mult, ["pO%d" % t2, "GT2"], ["tm%d" % t2])
                yield
                for t2 in range(2):
                    ti = blk * 2 + t2
                    tk0 = tok + t2 * 128
                    xk, ok_ = "x1t%d" % t2, "ot%d" % t2
                    col = ssf[:, ti % 32:ti % 32 + 1]
                    S.dma("sp", x1t[t2][:], X1s[tk0:tk0 + 128, :], r=["X1s"], w=[xk], stream=xk)
                    yield
                    tt("pool", x2[:], tm[t2][:], x1t[t2][:], ALU.add, ["tm%d" % t2, xk], ["x2"])
                    yield
                    act(junkm[:], x2[:], AF.Square, ["x2"], ["junkm", "ssf"], accum_out=col)
                    yield
                    ts("dve", col, col, 1.0 / D, EPS, ALU.mult, ALU.add, ["ssf"], ["ssf"])
                    yield
                    act(col, col, AF.Sqrt, ["ssf"], ["ssf"])
                    yield
                    S.op("dve", lambda e, col=col: e.reciprocal(out=col, in_=col), r=["ssf"], w=["ssf"])
                    stt(ot[t2][:], x2[:], col, nfg[:], ALU.mult, ALU.mult, ["x2", "ssf", "nfg"], [ok_])
                    yield
                    S.dma("sp", out[tk0:tk0 + 128, :], ot[t2][:], r=[ok_], w=["out"], stream=ok_)
                    yield

            def prefetch(gg):
                if gg >= NBLK * 64:
                    return
                up = gg % 4
                i0 = (gg % 64) * 2
                S.dma("sp", utb[up][:], UTv[:, i0:i0 + 2, :], r=["UTb"], w=["utb%d" % up], stream="utb%d" % up)
                S.dma("sp", vtb[up][:], Vv[:, i0:i0 + 2, :], r=["Vb"], w=["vtb%d" % up], stream="vtb%d" % up)

            def emitA(blk, i):
                bp = blk % 2
                gg = blk * 64 + i // 2
                up = gg % 4
                if gg == 0 and i == 0:
                    prefetch(0)
                    prefetch(1)
                    prefetch(2)
                ap_ = i % 2
                for kc in range(8):
                    mm(pA[ap_][:, 0:256], utb[up][:, i % 2, kc * 128:(kc + 1) * 128], h2b[bp][:, kc, :],
                       kc == 0, kc == 7, ["utb%d" % up, "h2b%d" % bp], ["pA%d" % ap_])

            def emitG(blk, i):
                ap_ = i % 2
                hf = i // 64
                act(Ag[ap_][:], pA[ap_][:, 0:256], AF.Gelu, ["pA%d" % ap_], ["Ag%d" % ap_])
                tt("dve", GA[ap_][:], Ag[ap_][:], Gh[hf][:, i % 64, :], ALU.mult, ["Ag%d" % ap_, "G%d" % hf], ["GA%d" % ap_])

            def emitVm(blk, i):
                gg = blk * 64 + i // 2
                up = gg % 4
                vk = "vtb%d" % up
                ap_ = i % 2
                for t2 in range(2):
                    for half in range(2):
                        mm(pO[t2][:, half * 512:(half + 1) * 512], GA[ap_][:, t2 * 128:(t2 + 1) * 128],
                           vtb[up][:, i % 2, half * 512:(half + 1) * 512], i == 0, i == 127,
                           ["GA%d" % ap_, vk], ["pO%d" % t2])
                if i % 2 == 0:
                    prefetch(gg + 3)

            def step(gens, skip=None):
                for g_ in list(gens):
                    if g_ is skip:
                        continue
                    try:
                        next(g_)
                    except StopIteration:
                        gens.remove(g_)

            for _ in build(0, 0):
                run_late()
            run_late()
            side = []
            for blk in range(NBLK):
                side.append(build(blk, 1))
                emitA(blk, 0)
                emitA(blk, 1)
                emitG(blk, 0)
                bld = side[-1]
                for i in range(128):
                    if i == 63:
                        for _ in bld:
                            run_late()
                        run_late()
                    if i == 64 and blk + 1 < NBLK:
                        bld = build(blk + 1, 0)
                        side.append(bld)
                    ip = i % 64
                    if ((ip + 1) * 37) // 64 > (ip * 37) // 64:
                        step(side)
                    else:
                        step(side, skip=bld)
                    if i + 2 < 128:
                        emitA(blk, i + 2)
                    if i + 1 < 128:
                        emitG(blk, i + 1)
                    run_late()
                    emitVm(blk, i)
                for _ in bld:
                    run_late()
                run_late()
                fg = final(blk)
                next(fg)
                side.append(fg)
            while side:
                step(side)
                run_late()
            S.flush()
        return nc


def prep_inputs(inputs):
    sq = lambda a: np.ascontiguousarray(a[0]) if a.shape[0] == 1 and a.ndim >= 2 else np.ascontiguousarray(a)
    shared = {}
    for n, sh in IN_SPECS:
        if n in ("x", "c", "consts"):
            continue
        a = np.asarray(inputs[n], dtype=np.float32)
        shared[n] = np.ascontiguousarray(a.reshape(sh))
    shared["consts"] = make_consts()
    x = np.asarray(inputs["x"], dtype=np.float32)
    c = np.asarray(inputs["c"], dtype=np.float32)
    maps = []
    for i in range(NCORES):
        m = dict(shared)
        m["x"] = np.ascontiguousarray(x[i * NB:(i + 1) * NB].reshape(T, D))
        m["c"] = np.ascontiguousarray(c[i * NB:(i + 1) * NB])
        maps.append(m)
    return maps


def kernel(**inputs):
    nc = build()
    maps = prep_inputs(inputs)
    res = run_bass_kernel_spmd(nc, maps, core_ids=list(range(NCORES)))
    outs = [np.asarray(r["out"]).reshape(NB, SEQ, D) for r in res.results]
    return np.concatenate(outs, axis=0).astype(np.float32)
```

```python
import os
from contextlib import ExitStack

import numpy as np
import concourse.bass as bass
import concourse.mybir as mybir
from concourse.bass_utils import run_bass_kernel_spmd

F32 = mybir.dt.float32
BF16 = mybir.dt.bfloat16
I32 = mybir.dt.int32
U32 = mybir.dt.uint32
AF = mybir.ActivationFunctionType
ALU = mybir.AluOpType
AX = mybir.AxisListType

NCORES = 8
D = 1024
NB = 2
SEQ = 2048
T = NB * SEQ
NT = T // 128
INW = 4112
EPS = 1e-6
MAGIC = 12582912.0
TWO_PI = 6.283185307179586


class _Op:
    __slots__ = ("eng", "fn", "dom", "order", "waits", "target", "val", "is_dma")

    def __init__(self, eng, fn, dom, order, is_dma):
        self.eng, self.fn, self.dom, self.order, self.is_dma = eng, fn, dom, order, is_dma
        self.waits = []
        self.target = is_dma
        self.val = None


class Sched:
    CE = ("pe", "act", "dve", "pool")

    def __init__(self, nc, stack):
        self.nc = nc
        self.stack = stack
        self.q = {k: [] for k in ("pe", "act", "dve", "pool", "sp")}
        self.sem = {k: stack.enter_context(nc.semaphore("c_" + k)) for k in self.CE}
        self.cnt = {k: 0 for k in self.CE}
        self.order = {k: 0 for k in self.CE}
        self.dsem = {}
        self.dcnt = {}
        self.dslot = {}
        self.dfree = []
        self.dorder = {}
        self.waited = {k: {} for k in self.q}
        self.lastw = {}
        self.readers = {}
        self.lastop = {}
        self.ninst = 0

    def _need(self, eng, p, out):
        if self.waited[eng].get(p.dom, 0) >= p.order:
            return
        cur = out.get(p.dom)
        if cur is None or cur.order < p.order:
            out[p.dom] = p

    def _deps(self, eng, r, w, is_dma=False):
        need = {}
        for b in r:
            p = self.lastw.get(b)
            if p is not None and (is_dma or not (p.eng == eng and eng == "pe" and not p.is_dma)):
                self._need(eng, p, need)
        for b in w:
            p = self.lastw.get(b)
            if p is not None and (is_dma or p.is_dma or p.eng != eng or eng != "pe"):
                self._need(eng, p, need)
            for p in self.readers.get(b, ()):
                if is_dma or p.is_dma or p.eng != eng or eng != "pe":
                    self._need(eng, p, need)
        return need

    def _add(self, op, need, r, w):
        for dom, p in need.items():
            self.waited[op.eng][dom] = p.order
            p.target = True
            op.waits.append(p)
        self.q[op.eng].append(op)
        self.lastop[op.dom] = op
        for b in r:
            self.readers.setdefault(b, []).append(op)
        for b in w:
            self.lastw[b] = op
            self.readers[b] = []
        self.ninst += 1

    def op(self, eng, fn, r=(), w=()):
        need = self._deps(eng, r, w)
        self.order[eng] += 1
        o = _Op(eng, fn, eng, self.order[eng], False)
        self._add(o, need, r, w)

    def dma(self, eng, out, in_, r=(), w=(), stream="d", **kw):
        if eng == "pool":
            key = "swd_%d" % len(self.dsem)
            self.dsem[key] = self.stack.enter_context(self.nc.semaphore(key))
            self.dcnt[key] = 0
            self.dorder[key] = 0
            self.dslot["__swd__" + key] = key
            stream = "__swd__" + key
        if stream not in self.dslot:
            if self.dfree:
                self.dslot[stream] = self.dfree.pop()
            else:
                k = "dma_%d" % len(self.dsem)
                self.dsem[k] = self.stack.enter_context(self.nc.semaphore(k))
                self.dcnt[k] = 0
                self.dorder[k] = 0
                self.dslot[stream] = k
        key = self.dslot[stream]
        need = self._deps(eng, r, w, is_dma=True)
        self.dorder[key] += 1
        fn = lambda e, out=out, in_=in_, kw=kw: e.dma_start(out=out, in_=in_, **kw)
        o = _Op(eng, fn, key, self.dorder[key], True)
        self._add(o, need, r, w)

    def barrier(self):
        lasts = list(self.lastop.values())
        for eng in self.q:
            need = {}
            for p in lasts:
                self._need(eng, p, need)
            if need:
                o = _Op(eng, None, None, 0, False)
                for dom, p in need.items():
                    self.waited[eng][dom] = p.order
                    p.target = True
                    o.waits.append(p)
                self.q[eng].append(o)
        self.lastw = {}
        self.readers = {}

    def flush(self):
        nc = self.nc
        self.barrier()
        q = self.q
        for eng in q:
            for o in q[eng]:
                if o.fn is None:
                    continue
                if o.is_dma:
                    self.dcnt[o.dom] += 16
                    o.val = self.dcnt[o.dom]
                elif o.target:
                    self.cnt[eng] += 1
                    o.val = self.cnt[eng]
        sem, dsem = self.sem, self.dsem

        def run(e, ops):
            for o in ops:
                for p in o.waits:
                    e.wait_ge(dsem[p.dom] if p.is_dma else sem[p.dom], p.val)
                if o.fn is None:
                    continue
                ins = o.fn(e)
                if o.is_dma:
                    ins.then_inc(dsem[o.dom], 16)
                elif o.target:
                    ins.then_inc(sem[o.eng], 1)

        with nc.Block() as block:
            @block.tensor
            def _(e):
                run(e, q["pe"])

            @block.scalar
            def _(e):
                run(e, q["act"])

            @block.vector
            def _(e):
                run(e, q["dve"])

            @block.gpsimd
            def _(e):
                run(e, q["pool"])

            @block.sync
            def _(e):
                run(e, q["sp"])
        for k in q:
            q[k] = []
        self.dfree.extend(v for k_, v in self.dslot.items() if not k_.startswith("__swd__"))
        self.dslot = {}
        self.lastop = {}


C_IDENT, C_TRIU, C_TRIS, C_ONES, C_IOTA, C_BD16, C_RM8, C_IOTA16, C_END = (
    0, 128, 256, 384, 512, 640, 768, 776, 792)


def make_consts():
    c = np.zeros((128, C_END), np.float32)
    k = np.arange(128)
    c[:, C_IDENT:C_IDENT + 128] = np.eye(128)
    c[:, C_TRIU:C_TRIU + 128] = (k[:, None] <= k[None, :])
    c[:, C_TRIS:C_TRIS + 128] = (k[:, None] > k[None, :])
    c[:, C_ONES:C_ONES + 128] = 1.0
    c[:, C_IOTA:C_IOTA + 128] = k[None, :]
    c[:, C_BD16:C_BD16 + 128] = (k[:, None] // 16 == k[None, :] // 16)
    c[:, C_RM8:C_RM8 + 8] = (k[:, None] // 16 == np.arange(8)[None, :])
    c[:, C_IOTA16:C_IOTA16 + 16] = np.arange(16)[None, :]
    return c


def skew_pipeline(stages, n_items):
    ns = len(stages)
    for k in range(n_items + ns - 1):
        for s_ in reversed(range(ns)):
            n = k - s_
            if 0 <= n < n_items:
                stages[s_](n)


IN_SPECS = [
    ("x", [T, D]), ("c", [NB, D]), ("w_ada", [D, 6 * D]), ("b_ada", [1, 6 * D]),
    ("norm1_g", [1, D]), ("w_in", [D, INW]), ("conv_w", [4, 2048]), ("conv_b", [1, 2048]),
    ("dt_bias", [1, 16]), ("a_log", [1, 16]), ("d_ssd", [1, 16]), ("norm_ssd_g", [1, D]),
    ("s5_a_re", [64, 64]), ("s5_a_im", [64, 64]), ("s5_log_dt", [1, 64]),
    ("s5_b_re", [64, 64, 16]), ("s5_b_im", [64, 64, 16]), ("s5_c_re", [64, 16, 64]),
    ("s5_c_im", [64, 16, 64]), ("s5_d", [64, 16]), ("glu_w", [64, 16, 16]), ("glu_b", [64, 16]),
    ("norm_s5_g", [1, D]), ("w_out", [2 * D, D]), ("norm2_g", [1, D]), ("w_query", [D, 2048]),
    ("sub_keys", [16, 128, 128]), ("expert_u", [16384, D]), ("expert_v", [16384, D]),
    ("norm_f_g", [1, D]), ("consts", [128, C_END]),
]


def build(debug=(), stop_after=None):
    nc = bass.Bass("TRN2", target_bir_lowering=False)
    I = {n: nc.dram_tensor(n, sh, F32, kind="ExternalInput").ap() for n, sh in IN_SPECS}
    out = nc.dram_tensor("out", [T, D], F32, kind="ExternalOutput").ap()

    def SCR(name, shape, dt):
        kind = "ExternalOutput" if name in debug else "Internal"
        return nc.dram_tensor(name, shape, dt, kind=kind).ap()

    MODs = SCR("MODs", [NB, 6 * D], F32)
    XCs = SCR("XCs", [2048, T], BF16)
    UTs = SCR("UTs", [1024, T], BF16)
    Zs = SCR("Zs", [T, D], BF16)
    DTs = SCR("DTs", [T, 16], F32)
    YCs = SCR("YCs", [2048, T], BF16)
    Y5s = SCR("Y5s", [1024, T], F32)
    X1s = SCR("X1s", [T, D], F32)
    H2Ts = SCR("H2Ts", [D, T], BF16)
    RTs = SCR("RTs", [128, 3, T], BF16)
    UTb = SCR("UTb", [128, 128, 1024], BF16)
    Vb = SCR("Vb", [16384, D], BF16)

    with ExitStack() as top:
        S = Sched(nc, top)

        def mm(out_, lhsT, rhs, start, stop, r, w):
            S.op("pe", lambda e: e.matmul(out_, lhsT=lhsT, rhs=rhs, start=start, stop=stop), r=r, w=w)

        def tr(out_, in_, ident, r, w):
            S.op("pe", lambda e: e.transpose(out=out_, in_=in_, identity=ident), r=r, w=w)

        def act(out_, in_, func, r, w, eng="act", **kw):
            S.op(eng, lambda e: e.activation(out=out_, in_=in_, func=func, **kw), r=r, w=w)

        def tt(eng, out_, in0, in1, op, r, w):
            S.op(eng, lambda e: e.tensor_tensor(out=out_, in0=in0, in1=in1, op=op), r=r, w=w)

        def ts(eng, out_, in0, s1, s2, op0, op1, r, w):
            if s2 is None:
                S.op(eng, lambda e: e.tensor_scalar(out=out_, in0=in0, scalar1=s1, scalar2=None, op0=op0), r=r, w=w)
            else:
                S.op(eng, lambda e: e.tensor_scalar(out=out_, in0=in0, scalar1=s1, scalar2=s2, op0=op0, op1=op1),
                     r=r, w=w)

        def stt(out_, in0, scalar, in1, op0, op1, r, w):
            S.op("dve", lambda e: e.scalar_tensor_tensor(out=out_, in0=in0, scalar=scalar, in1=in1, op0=op0, op1=op1),
                 r=r, w=w)

        def cp(eng, out_, in_, r, w):
            if eng == "act":
                act(out_, in_, AF.Copy, r, w)
            else:
                S.op(eng, lambda e: e.tensor_copy(out=out_, in_=in_), r=r, w=w)

        def rsqrt(col, n, r, w):
            ts("dve", col, col, 1.0 / n, EPS, ALU.mult, ALU.add, r, w)
            act(col, col, AF.Sqrt, w, w)
            S.op("dve", lambda e: e.reciprocal(out=col, in_=col), r=w, w=w)

        cst = top.enter_context(nc.sbuf_tensor("cst", [128, C_END], F32))
        cstb = top.enter_context(nc.sbuf_tensor("cstb", [128, C_END], BF16))
        S.dma("sp", cst[:], I["consts"][:, :], w=["cst"], stream="cst")
        cp("dve", cstb[:], cst[:], ["cst"], ["cstb"])
        ident_f = cst[:, C_IDENT:C_IDENT + 128]
        ident_b = cstb[:, C_IDENT:C_IDENT + 128]
        triu_f = cst[:, C_TRIU:C_TRIU + 128]
        tris_f = cst[:, C_TRIS:C_TRIS + 128]
        ones_f = cst[:, C_ONES:C_ONES + 128]
        ones_b = cstb[:, C_ONES:C_ONES + 128]

        with ExitStack() as ph:
            A = lambda n, sh, dt=F32: ph.enter_context(nc.sbuf_tensor(n, sh, dt))
            cT = A("cT", [128, 8, NB])
            rep = A("rep", [128, 8, NB, 128])
            bada = A("bada", [128, 6 * D])
            g1bc = A("g1bc", [128, D])
            g2bc = A("g2bc", [128, D])
            wa = [A("wa%d" % i, [128, 8, 512]) for i in range(2)]
            modbc = [A("modbc%d" % b, [128, 6 * D]) for b in range(NB)]
            pm = [ph.enter_context(nc.psum_tensor("pm%d" % i, [128, 512], F32)) for i in range(2)]
            for b in range(NB):
                S.dma("sp", cT[:, :, b], I["c"][b:b + 1, :].rearrange("o (kc p) -> p (o kc)", p=128), w=["cT"],
                      stream="p0", allow_slow_non_contiguous=True)
            S.dma("sp", bada[:], I["b_ada"][0:1, :].partition_broadcast(128), w=["bada"], stream="p0b")
            S.dma("sp", g1bc[:], I["norm1_g"][0:1, :].partition_broadcast(128), w=["g1bc"], stream="p0c")
            S.dma("sp", g2bc[:], I["norm2_g"][0:1, :].partition_broadcast(128), w=["g2bc"], stream="p0d")
            act(cT[:], cT[:], AF.Silu, ["cT"], ["cT"])
            cp("dve", rep[:], cT[:].unsqueeze(3).to_broadcast([128, 8, NB, 128]), ["cT"], ["rep"])
            wav = I["w_ada"].rearrange("(kc p) n -> p kc n", p=128)
            for n in range(12):
                wb = wa[n % 2]
                wk = "wa%d" % (n % 2)
                S.dma("sp", wb[:], wav[:, :, n * 512:(n + 1) * 512], w=[wk], stream=wk)
                for b in range(NB):
                    pk = "pm%d" % b
                    for kc in range(8):
                        mm(pm[b][:, :], rep[:, kc, b, :], wb[:, kc, :], kc == 0, kc == 7, ["rep", wk], [pk])
                    tt("dve", modbc[b][:, n * 512:(n + 1) * 512], pm[b][:, :], bada[:, n * 512:(n + 1) * 512],
                       ALU.add, [pk, "bada"], ["modbc%d" % b])
            for b in range(NB):
                mk = "modbc%d" % b
                stt(modbc[b][:, 1024:2048], modbc[b][:, 1024:2048], 1.0, g1bc[:], ALU.add, ALU.mult, [mk, "g1bc"], [mk])
                stt(modbc[b][:, 4096:5120], modbc[b][:, 4096:5120], 1.0, g2bc[:], ALU.add, ALU.mult, [mk, "g2bc"], [mk])
                S.dma("sp", MODs[b:b + 1, :], modbc[b][0:1, :], r=[mk], w=["MODs"], stream="p0s")
            S.flush()
        if stop_after == 0:
            return nc

        with ExitStack() as ph:
            A = lambda n, sh, dt=F32: ph.enter_context(nc.sbuf_tensor(n, sh, dt))
            P = lambda n, sh, dt=F32: ph.enter_context(nc.psum_tensor(n, sh, dt))
            win = A("win", [128, 8, INW], BF16)
            hT = A("hT", [128, 8, SEQ], BF16)
            G1 = A("G1", [128, D])
            SH1 = A("SH1", [128, D])
            xin = [A("xin%d" % i, [128, D]) for i in range(5)]
            t1 = [A("t1_%d" % i, [128, D]) for i in range(3)]
            junk = A("junk", [128, D], BF16)
            hb = [A("hb%d" % i, [128, D], BF16) for i in range(3)]
            ss = A("ss", [128, 16])
            zst = [A("zst%d" % i, [128, D], BF16) for i in range(2)]
            dts = [A("dts%d" % i, [128, 16]) for i in range(2)]
            xpad = [A("xpad%d" % i, [128, 3 + SEQ]) for i in range(2)]
            acc = [A("acc%d" % i, [128, SEQ]) for i in range(2)]
            xo = [A("xo%d" % i, [128, SEQ], BF16) for i in range(2)]
            cw = A("cw", [128, 16, 4])
            cb = A("cb", [128, 16])
            pT = [P("pT%d" % i, [128, 1024], BF16) for i in range(2)]
            pz = P("pz", [128, 1024])
            pdt = P("pdt", [128, 16])
            pc = [P("pc%d" % i, [128, 512]) for i in range(2)]

            winv = I["w_in"].rearrange("(kc p) n -> p kc n", p=128)
            for kc in range(8):
                S.dma("pool", win[:, kc, :], winv[:, kc, :], w=["win%d" % kc], stream="win")
            for k in range(4):
                S.dma("sp", cw[:, :, k], I["conv_w"][k:k + 1, :].rearrange("o (ct p) -> p (o ct)", p=128), w=["cw"],
                      stream="cw", allow_slow_non_contiguous=True)
            S.dma("sp", cb[:], I["conv_b"].rearrange("o (ct p) -> p (o ct)", p=128), w=["cb"], stream="cb",
                  allow_slow_non_contiguous=True)
            for i in range(2):
                S.op("dve", lambda e, i=i: e.memset(xpad[i][:, 0:3], 0.0), w=["xpad%d" % i])

            def tokgen(b, i):
                tok0 = b * SEQ + i * 128
                r3, r2 = i % 3, i % 2
                xk, tk_, hk, pk = "xin%d" % (i % 5), "t1_%d" % r3, "hb%d" % r3, "pT%d" % r2
                xt = xin[i % 5]
                col = ss[:, i:i + 1]
                S.dma("sp", xt[:], I["x"][tok0:tok0 + 128, :], w=[xk], stream=xk)
                yield
                act(junk[:], xt[:], AF.Square, [xk], ["junk", "ss%d" % i], accum_out=col)
                yield
                ts("dve", col, col, 1.0 / D, EPS, ALU.mult, ALU.add, ["ss%d" % i], ["ss%d" % i])
                yield
                act(col, col, AF.Sqrt, ["ss%d" % i], ["ss%d" % i])
                yield
                S.op("dve", lambda e: e.reciprocal(out=col, in_=col), r=["ss%d" % i], w=["ss%d" % i])
                stt(t1[r3][:], xt[:], col, G1[:], ALU.mult, ALU.mult, [xk, "ss%d" % i, "G1"], [tk_])
                yield
                tt("pool", hb[r3][:], t1[r3][:], SH1[:], ALU.add, [tk_, "SH1"], [hk])
                yield
                for kc in range(8):
                    tr(pT[r2][:, kc * 128:(kc + 1) * 128], hb[r3][:, kc * 128:(kc + 1) * 128], ident_b, [hk, "cstb"], [pk])
                yield
                cp("act", hT[:, :, i * 128:(i + 1) * 128], pT[r2][:, :].rearrange("p (k t) -> p k t", k=8), [pk],
                   ["hT%d" % i])
                yield
                for half in range(2):
                    for kc in range(8):
                        mm(pz[:, half * 512:(half + 1) * 512], hT[:, kc, i * 128:(i + 1) * 128],
                           win[:, kc, half * 512:(half + 1) * 512], kc == 0, kc == 7, ["hT%d" % i, "win%d" % kc], ["pz"])
                for kc in range(8):
                    mm(pdt[:, :], hT[:, kc, i * 128:(i + 1) * 128], win[:, kc, 3072:3088], kc == 0, kc == 7,
                       ["hT%d" % i, "win%d" % kc], ["pdt"])
                yield
                zk, dk = "zst%d" % r2, "dts%d" % r2
                cp("act", zst[r2][:], pz[:, :], ["pz"], [zk])
                cp("dve", dts[r2][:], pdt[:, :], ["pdt"], [dk])
                S.dma("sp", Zs[tok0:tok0 + 128, :], zst[r2][:], r=[zk], w=["Zs"], stream=zk)
                S.dma("sp", DTs[tok0:tok0 + 128, :], dts[r2][:], r=[dk], w=["DTs"], stream=dk)
                yield

            def chgen(b, ct):
                col0 = 1024 + ct * 128 if ct < 16 else 3088 + (ct - 16) * 128
                par = ct % 2
                xpk, xok, ak = "xpad%d" % par, "xo%d" % par, "acc%d" % par
                for blk in range(4):
                    pck = "pc%d" % (blk % 2)
                    for kc in range(8):
                        mm(pc[blk % 2][:, :], win[:, kc, col0:col0 + 128], hT[:, kc, blk * 512:(blk + 1) * 512],
                           kc == 0, kc == 7, ["win%d" % kc] + ["hT%d" % j for j in range(blk * 4, blk * 4 + 4)], [pck])
                    if ct < 16:
                        cp("act", xpad[par][:, 3 + blk * 512:3 + (blk + 1) * 512], pc[blk % 2][:, :], [pck], [xpk])
                    else:
                        cp("act", xo[par][:, blk * 512:(blk + 1) * 512], pc[blk % 2][:, :], [pck], [xok])
                    if blk % 2 == 1:
                        yield
                if ct < 16:
                    xp = xpad[par]
                    ts("dve", acc[par][:], xp[:, 3:3 + SEQ], cw[:, ct, 3:4], cb[:, ct:ct + 1], ALU.mult, ALU.add,
                       [xpk, "cw", "cb"], [ak])
                    yield
                    for k in (2, 1, 0):
                        stt(acc[par][:], xp[:, k:k + SEQ], cw[:, ct, k:k + 1], acc[par][:], ALU.mult, ALU.add,
                            [xpk, "cw", ak], [ak])
                        yield
                    act(xo[par][:], acc[par][:], AF.Silu, [ak], [xok])
                    yield
                    S.dma("sp", XCs[ct * 128:(ct + 1) * 128, b * SEQ:(b + 1) * SEQ], xo[par][:], r=[xok], w=["XCs"],
                          stream=xok)
                else:
                    S.dma("sp", UTs[(ct - 16) * 128:(ct - 15) * 128, b * SEQ:(b + 1) * SEQ], xo[par][:], r=[xok],
                          w=["UTs"], stream=xok)
                yield

            def run_skewed(gens, every):
                active = []
                k = 0
                while gens or active:
                    if gens and k % every == 0:
                        active.append(gens.pop(0))
                    for g_ in list(active):
                        try:
                            next(g_)
                        except StopIteration:
                            active.remove(g_)
                    k += 1

            for b in range(NB):
                S.dma("sp", G1[:], MODs[b:b + 1, 1024:2048].partition_broadcast(128), r=["MODs"], w=["G1"], stream="g1")
                S.dma("sp", SH1[:], MODs[b:b + 1, 0:1024].partition_broadcast(128), r=["MODs"], w=["SH1"], stream="sh1")
                run_skewed([tokgen(b, i) for i in range(16)], 1)
                run_skewed([chgen(b, ct) for ct in range(24)], 4)
            S.flush()
        if stop_after == 1:
            return nc

        with ExitStack() as ph:
            A = lambda n, sh, dt=F32: ph.enter_context(nc.sbuf_tensor("B_" + n, sh, dt))
            P = lambda n, sh, dt=F32: ph.enter_context(nc.psum_tensor("B_" + n, sh, dt))
            dtb = A("dtb", [128, 16]); abc = A("abc", [128, 16]); d16 = A("d16", [128, 16])
            dssd = A("dssd", [128, D]); normg = A("normg", [128, D])
            S32 = A("S32", [128, 16, 64]); Sbf = A("Sbf", [128, 16, 64], BF16)
            RC = 4
            xct = [A("xct%d" % i, [128, 16, 128], BF16) for i in range(RC)]
            zt = [A("zt%d" % i, [128, D], BF16) for i in range(RC)]
            dtr = [A("dtr%d" % i, [128, 16]) for i in range(RC)]
            dtt = [A("dtt%d" % i, [128, 16]) for i in range(RC)]
            da = [A("da%d" % i, [128, 16]) for i in range(RC)]
            c3 = [A("c3_%d" % i, [128, 48]) for i in range(RC)]
            e3 = [A("e3_%d" % i, [128, 48]) for i in range(RC)]
            xs = [A("xs%d" % i, [128, D], BF16) for i in range(RC)]
            Btok = [A("Btok%d" % i, [128, 512], BF16) for i in range(RC)]
            xdt = [A("xdt%d" % i, [128, D], BF16) for i in range(RC)]
            xdd = [A("xdd%d" % i, [128, D], BF16) for i in range(RC)]
            CBm = [A("CBm%d" % i, [128, 4, 128]) for i in range(RC)]
            RH = 3
            lD = [A("lD%d" % i, [128, 128]) for i in range(RH)]
            Lm = [A("Lm%d" % i, [128, 128]) for i in range(RH)]
            Mt = [A("Mt%d" % i, [128, 128], BF16) for i in range(RH)]
            yo = A("yo", [128, D]); y1 = A("y1", [128, D]); xD = A("xD", [128, D]); sz = A("sz", [128, D])
            yg = A("yg", [128, D]); junkb = A("junkb", [128, 256], BF16); ssq = A("ssq", [128, 4])
            yn = A("yn", [128, D]); ynb = A("ynb", [128, D], BF16)
            ynT = [A("ynT%d" % i, [128, 8, 128], BF16) for i in range(2)]
            pTr = P("pTr", [128, 1024], BF16)
            pDs = [P("pD%d" % i, [128, 512]) for i in range(2)]
            pSl = [P("pS%d" % i, [128, 512]) for i in range(2)]
            pPro = P("pPro", [128, 512])
            pY = P("pY", [128, 512])
            pYo = P("pYo", [128, 512])

            S.dma("sp", dtb[:], I["dt_bias"][0:1, :].partition_broadcast(128), w=["dtb"], stream="b0")
            S.dma("sp", abc[:], I["a_log"][0:1, :].partition_broadcast(128), w=["abc"], stream="b1")
            S.dma("sp", d16[:], I["d_ssd"][0:1, :].partition_broadcast(128), w=["d16"], stream="b2")
            S.dma("sp", normg[:], I["norm_ssd_g"][0:1, :].partition_broadcast(128), w=["normg"], stream="b3")
            act(abc[:], abc[:], AF.Exp, ["abc"], ["abc"])
            ts("dve", abc[:], abc[:], -1.0, None, ALU.mult, None, ["abc"], ["abc"])
            cp("dve", dssd[:, :].rearrange("p (h q) -> p h q", q=64), d16[:, :].unsqueeze(2).to_broadcast([128, 16, 64]),
               ["d16"], ["dssd"])
            XCv = XCs.rearrange("(ct p) t -> p ct t", p=128)
            YCv = YCs.rearrange("(ct p) t -> p ct t", p=128)
            v3 = lambda ap: ap.rearrange("p (h q) -> p h q", q=64)
            bc3 = lambda col: col.unsqueeze(2).to_broadcast([128, 16, 64])
            NCH = NB * 16

            def prologue(ci):
                r_ = ci % RC
                tok0 = ci * 128
                K_ = lambda nm: "%s%d" % (nm, r_)
                X = xct[r_]
                S.dma("sp", X[:], XCv[:, :, tok0:tok0 + 128], r=["XCs"], w=[K_("xct")], stream=K_("xct"))
                S.dma("sp", zt[r_][:], Zs[tok0:tok0 + 128, :], r=["Zs"], w=[K_("zt")], stream=K_("zt"))
                S.dma("sp", dtr[r_][:], DTs[tok0:tok0 + 128, :], r=["DTs"], w=[K_("dtr")], stream=K_("dtr"))
                yield
                tt("dve", dtt[r_][:], dtr[r_][:], dtb[:], ALU.add, [K_("dtr"), "dtb"], [K_("dtt")])
                yield
                act(dtt[r_][:], dtt[r_][:], AF.Exp, [K_("dtt")], [K_("dtt")])
                act(dtt[r_][:], dtt[r_][:], AF.Ln, [K_("dtt")], [K_("dtt")], bias=1.0)
                yield
                tt("dve", da[r_][:], dtt[r_][:], abc[:], ALU.mult, [K_("dtt"), "abc"], [K_("da")])
                yield
                mm(pPro[:, 0:16], triu_f, da[r_][:], True, True, ["cst", K_("da")], ["pm_cs"])
                mm(pPro[:, 16:32], ones_f, da[r_][:], True, True, ["cst", K_("da")], ["pm_cs"])
                cp("dve", c3[r_][:, 0:32], pPro[:, 0:32], ["pm_cs"], [K_("c3")])
                tt("dve", c3[r_][:, 32:48], c3[r_][:, 16:32], c3[r_][:, 0:16], ALU.subtract, [K_("c3")], [K_("c3")])
                yield
                act(e3[r_][:], c3[r_][:], AF.Exp, [K_("c3")], [K_("e3")])
                yield
                for j in range(8):
                    tr(pTr[:, j * 128:(j + 1) * 128], X[:, j, :], ident_b, [K_("xct"), "cstb"], ["pTr"])
                cp("act", xs[r_][:], pTr[:, :], ["pTr"], [K_("xs")])
                yield
                for g in range(4):
                    tr(pTr[:, g * 128:(g + 1) * 128], X[:, 8 + g, :], ident_b, [K_("xct"), "cstb"], ["pTr"])
                cp("act", Btok[r_][:], pTr[:, 0:512], ["pTr"], [K_("Btok")])
                yield
                tt("dve", v3(xdt[r_][:, :]), v3(xs[r_][:, :]), bc3(dtt[r_][:, :]), ALU.mult, [K_("xs"), K_("dtt")],
                   [K_("xdt")])
                tt("dve", v3(xdd[r_][:, :]), v3(xdt[r_][:, :]), bc3(e3[r_][:, 32:48]), ALU.mult, [K_("xdt"), K_("e3")],
                   [K_("xdd")])
                yield
                for g in range(4):
                    mm(pPro[:, 128:256], X[:, 8 + g, :], X[:, 12 + g, :], True, True, [K_("xct")], ["pm_cb"])
                    tt("dve", CBm[r_][:, g, :], pPro[:, 128:256], triu_f, ALU.mult, ["pm_cb", "cst"], [K_("CBm") + "_%d" % g])
                    yield

            def epilogue(ci):
                r_ = ci % RC
                tok0 = ci * 128
                c = ci % 16
                K_ = lambda nm: "%s%d" % (nm, r_)
                yield
                tt("pool", xD[:], xs[r_][:], dssd[:], ALU.mult, [K_("xs"), "dssd"], ["xD"])
                yield
                tt("dve", y1[:], y1[:], xD[:], ALU.add, ["y1", "xD"], ["y1"])
                act(sz[:], zt[r_][:], AF.Silu, [K_("zt")], ["sz"])
                yield
                tt("dve", yg[:], y1[:], sz[:], ALU.mult, ["y1", "sz"], ["yg"])
                yield
                for G_ in range(4):
                    act(junkb[:], yg[:, G_ * 256:(G_ + 1) * 256], AF.Square, ["yg"], ["junkb", "ssq"],
                        accum_out=ssq[:, G_:G_ + 1])
                yield
                ts("dve", ssq[:], ssq[:], 1.0 / 256, EPS, ALU.mult, ALU.add, ["ssq"], ["ssq"])
                yield
                act(ssq[:], ssq[:], AF.Sqrt, ["ssq"], ["ssq"])
                yield
                S.op("dve", lambda e: e.reciprocal(out=ssq[:], in_=ssq[:]), r=["ssq"], w=["ssq"])
                tt("dve", yn[:, :].rearrange("p (g q) -> p g q", q=256), yg[:, :].rearrange("p (g q) -> p g q", q=256),
                   ssq[:, :].unsqueeze(2).to_broadcast([128, 4, 256]), ALU.mult, ["yg", "ssq"], ["yn"])
                yield
                tt("pool", ynb[:], yn[:], normg[:], ALU.mult, ["yn", "normg"], ["ynb"])
                yield
                for j in range(8):
                    tr(pTr[:, j * 128:(j + 1) * 128], ynb[:, j * 128:(j + 1) * 128], ident_b, ["ynb", "cstb"], ["pTr"])
                yk = "ynT%d" % (ci % 2)
                cp("act", ynT[ci % 2][:, :, :], pTr[:, :].rearrange("p (k t) -> p k t", k=8), ["pTr"], [yk])
                S.dma("sp", YCv[:, 0:8, tok0:tok0 + 128], ynT[ci % 2][:, :, :], r=[yk], w=["YCs"], stream=yk)
                yield

            def e1_half(ci, hf):
                r_ = ci % RC
                cs_ = slice(hf * 512, (hf + 1) * 512)
                v3h = lambda ap: ap.rearrange("p (h q) -> p h q", q=64)
                if ci % 16 == 0:
                    cp("dve", y1[:, cs_], pY[:, :], ["pY"], ["y1"])
                else:
                    tt("dve", v3h(yo[:, cs_]), v3h(pYo[:, :]),
                       e3[r_][:, hf * 8:hf * 8 + 8].unsqueeze(2).to_broadcast([128, 8, 64]), ALU.mult,
                       ["pYo", "e3_%d" % r_], ["yo"])
                    tt("dve", y1[:, cs_], yo[:, cs_], pY[:, :], ALU.add, ["yo", "pY"], ["y1"])

            def hinfo(n):
                ci, h = n // 16, n % 16
                return ci, h, h // 4, ci % RC, n % RH, ci % 16

            def h_a(n):
                ci, h, g, r_, hr, c = hinfo(n)
                tt("pool", lD[hr][:], tris_f, da[r_][:, h:h + 1].to_broadcast([128, 128]), ALU.mult,
                   ["cst", "da%d" % r_], ["lD%d" % hr])

            def h_b(n):
                ci, h, g, r_, hr, c = hinfo(n)
                mm(pDs[n % 2][:, 0:128], lD[hr][:], triu_f, True, True, ["lD%d" % hr, "cst"], ["pD%d" % (n % 2)])

            def h_c(n):
                ci, h, g, r_, hr, c = hinfo(n)
                act(Lm[hr][:], pDs[n % 2][:, 0:128], AF.Exp, ["pD%d" % (n % 2)], ["Lm%d" % hr])

            def h_d(n):
                ci, h, g, r_, hr, c = hinfo(n)
                tt("dve", Mt[hr][:], Lm[hr][:], CBm[r_][:, g, :], ALU.mult, ["Lm%d" % hr, "CBm%d_%d" % (r_, g)],
                   ["Mt%d" % hr])

            def h_e(n):
                ci, h, g, r_, hr, c = hinfo(n)
                X = xct[r_]
                mm(pY[:, (h % 8) * 64:(h % 8 + 1) * 64], Mt[hr][:], xdt[r_][:, h * 64:(h + 1) * 64], True, True,
                   ["Mt%d" % hr, "xdt%d" % r_], ["pY"])
                if c != 0:
                    mm(pYo[:, (h % 8) * 64:(h % 8 + 1) * 64], X[:, 12 + g, :], Sbf[:, h, :], True, True,
                       ["xct%d" % r_, "Sbf%d" % h], ["pYo"])
                mm(pSl[n % 2][:, 0:64], Btok[r_][:, g * 128:(g + 1) * 128], xdd[r_][:, h * 64:(h + 1) * 64],
                   True, True, ["Btok%d" % r_, "xdd%d" % r_], ["pS%d" % (n % 2)])

            def h_f(n):
                ci, h, g, r_, hr, c = hinfo(n)
                if c == 0:
                    cp("dve", S32[:, h, :], pSl[n % 2][:, 0:64], ["pS%d" % (n % 2)], ["S32_%d" % h])
                else:
                    stt(S32[:, h, :], S32[:, h, :], e3[r_][:, 16 + h:17 + h], pSl[n % 2][:, 0:64],
                        ALU.mult, ALU.add, ["S32_%d" % h, "e3_%d" % r_, "pS%d" % (n % 2)], ["S32_%d" % h])

            def h_g(n):
                ci, h, g, r_, hr, c = hinfo(n)
                cp("pool", Sbf[:, h, :], S32[:, h, :], ["S32_%d" % h], ["Sbf%d" % h])

            stages = [h_a, h_b, h_c, h_d, h_e, h_f, h_g]
            NS = len(stages)
            for _ in prologue(0):
                pass
            for _ in prologue(1):
                pass
            side = []
            NI_ = NCH * 16
            for k in range(NI_ + NS - 1):
                for s_ in reversed(range(NS)):
                    n = k - s_
                    if 0 <= n < NI_:
                        stages[s_](n)
                if k >= 11 and (k - 11) % 16 == 0:
                    e1_half((k - 11) // 16, 0)
                if k >= 19 and (k - 19) % 16 == 0:
                    e1_half((k - 19) // 16, 1)
                    side.append(epilogue((k - 19) // 16))
                if k % 16 == 0 and k // 16 + 2 < NCH:
                    side.append(prologue(k // 16 + 2))
                for g_ in list(side):
                    try:
                        next(g_)
                    except StopIteration:
                        side.remove(g_)
            while side:
                for g_ in list(side):
                    try:
                        next(g_)
                    except StopIteration:
                        side.remove(g_)
            S.flush()
        if stop_after == 2:
            return nc

        with ExitStack() as ph:
            A = lambda n, sh, dt=F32: ph.enter_context(nc.sbuf_tensor(n, sh, dt))
            Blk1 = A("Blk1", [128, 64, 128], BF16)
            Blk2 = A("Blk2", [128, 64, 128], BF16)
            CL1 = A("CL1", [128, 64, 16], BF16)
            CL2 = A("CL2", [128, 64, 16], BF16)
            rcol = A("rcol", [128, 64])
            fcol = A("fcol", [128, 64])
            D5c = A("D5c", [128, 8])
            glub = A("glub", [128, 8])
            g5c = A("g5c", [128, 8])
            Wg = A("Wg", [128, 8, 128], BF16)
            rm8 = cst[:, C_RM8:C_RM8 + 8]
            INV2PI = 1.0 / TWO_PI

            def sin_turns(out_, f_ap, tk, tf, keys_in, kout, e1="dve", e2="dve"):
                ts(e1, tk, f_ap, MAGIC, MAGIC, ALU.add, ALU.subtract, keys_in, ["_tk"])
                tt(e2, tf, f_ap, tk, ALU.subtract, keys_in + ["_tk"], ["_tf"])
                act(out_, tf, AF.Sin, ["_tf"], [kout], scale=TWO_PI)

            with ExitStack() as p0:
                B_ = lambda n, sh, dt=F32: p0.enter_context(nc.sbuf_tensor(n, sh, dt))
                lrT = B_("lrT", [128, 64]); liT = B_("liT", [128, 64]); dtg = B_("dtg", [128, 64])
                ldt = B_("ldt", [128, 64]); f2 = B_("f2", [128, 64]); tk = B_("tk", [128, 64]); tf = B_("tf", [128, 64])
                sn = B_("sn", [128, 64]); cs_ = B_("cs_", [128, 64]); lbr = B_("lbr", [128, 64]); lbi = B_("lbi", [128, 64])
                den = B_("den", [128, 64]); t_a = B_("t_a", [128, 64]); t_b = B_("t_b", [128, 64])
                cre = B_("cre", [128, 64]); cim = B_("cim", [128, 64])
                br = B_("br", [64, 64, 16]); bi = B_("bi", [64, 64, 16])
                bbr = B_("bbr", [64, 64, 16]); bbi = B_("bbi", [64, 64, 16]); t_c = B_("t_c", [64, 64, 16])
                Dre = B_("Dre", [128, 8, 64]); Dim = B_("Dim", [128, 8, 64]); nDre = B_("nDre", [128, 8, 64])
                cc1 = B_("cc1", [128, 128]); cc2 = B_("cc2", [128, 128]); wrow = B_("wrow", [128, 8, 16])
                ptc = p0.enter_context(nc.psum_tensor("ptc", [128, 128], F32))
                for half in range(2):
                    S.dma("sp", lrT[half * 64:(half + 1) * 64, :], I["s5_a_re"].rearrange("g p -> p g"), w=["lrT"],
                          stream="c0a", allow_slow_non_contiguous=True)
                    S.dma("sp", liT[half * 64:(half + 1) * 64, :], I["s5_a_im"].rearrange("g p -> p g"), w=["liT"],
                          stream="c0b", allow_slow_non_contiguous=True)
                S.dma("sp", dtg[:], I["s5_log_dt"][0:1, :].partition_broadcast(128), w=["dtg"], stream="c0c")
                act(dtg[:], dtg[:], AF.Exp, ["dtg"], ["dtg"])
                tt("dve", ldt[:], lrT[:], dtg[:], ALU.mult, ["lrT", "dtg"], ["ldt"])
                act(rcol[:], ldt[:], AF.Exp, ["ldt"], ["rcol"])
                tt("dve", fcol[:], liT[:], dtg[:], ALU.mult, ["liT", "dtg"], ["fcol"])
                ts("dve", fcol[:], fcol[:], INV2PI, None, ALU.mult, None, ["fcol"], ["fcol"])
                sin_turns(sn[:], fcol[:], tk[:], tf[:], ["fcol"], "sn")
                ts("dve", f2[:], fcol[:], 0.25, None, ALU.add, None, ["fcol"], ["f2"])
                sin_turns(cs_[:], f2[:], tk[:], tf[:], ["f2"], "cs_")
                tt("dve", lbr[:], rcol[:], cs_[:], ALU.mult, ["rcol", "cs_"], ["lbr"])
                tt("dve", lbi[:], rcol[:], sn[:], ALU.mult, ["rcol", "sn"], ["lbi"])
                ts("dve", lbr[:], lbr[:], -1.0, None, ALU.add, None, ["lbr"], ["lbr"])
                tt("dve", den[:], lrT[:], lrT[:], ALU.mult, ["lrT"], ["den"])
                tt("dve", t_a[:], liT[:], liT[:], ALU.mult, ["liT"], ["t_a"])
                tt("dve", den[:], den[:], t_a[:], ALU.add, ["den", "t_a"], ["den"])
                S.op("dve", lambda e: e.reciprocal(out=den[:], in_=den[:]), r=["den"], w=["den"])
                tt("dve", t_a[:], lbr[:], lrT[:], ALU.mult, ["lbr", "lrT"], ["t_a"])
                tt("dve", t_b[:], lbi[:], liT[:], ALU.mult, ["lbi", "liT"], ["t_b"])
                tt("dve", t_a[:], t_a[:], t_b[:], ALU.add, ["t_a", "t_b"], ["t_a"])
                tt("dve", cre[:], t_a[:], den[:], ALU.mult, ["t_a", "den"], ["cre"])
                tt("dve", t_a[:], lbi[:], lrT[:], ALU.mult, ["lbi", "lrT"], ["t_a"])
                tt("dve", t_b[:], lbr[:], liT[:], ALU.mult, ["lbr", "liT"], ["t_b"])
                tt("dve", t_a[:], t_a[:], t_b[:], ALU.subtract, ["t_a", "t_b"], ["t_a"])
                tt("dve", cim[:], t_a[:], den[:], ALU.mult, ["t_a", "den"], ["cim"])
                for q4 in range(4):
                    gs = slice(q4 * 16, (q4 + 1) * 16)
                    S.dma("sp", br[:, gs, :], I["s5_b_re"][gs].rearrange("g p h -> p g h"), w=["br"], stream="c0d")
                    S.dma("sp", bi[:, gs, :], I["s5_b_im"][gs].rearrange("g p h -> p g h"), w=["bi"], stream="c0e")
                bcr = cre[0:64, :].unsqueeze(2).to_broadcast([64, 64, 16])
                bci = cim[0:64, :].unsqueeze(2).to_broadcast([64, 64, 16])
                tt("dve", bbr[:], br[:], bcr, ALU.mult, ["br", "cre"], ["bbr"])
                tt("dve", t_c[:], bi[:], bci, ALU.mult, ["bi", "cim"], ["t_c"])
                tt("dve", bbr[:], bbr[:], t_c[:], ALU.subtract, ["bbr", "t_c"], ["bbr"])
                tt("dve", bbi[:], bi[:], bcr, ALU.mult, ["bi", "cre"], ["bbi"])
                tt("dve", t_c[:], br[:], bci, ALU.mult, ["br", "cim"], ["t_c"])
                tt("dve", bbi[:], bbi[:], t_c[:], ALU.add, ["bbi", "t_c"], ["bbi"])
                for j in range(8):
                    tr(ptc[:, 0:64], bbr[:, j * 8:(j + 1) * 8, :].rearrange("p g h -> p (g h)"), ident_f[0:64, 0:64], ["bbr", "cst"], ["ptc"])
                    cp("dve", Dre[:, j, :], ptc[:, 0:64], ["ptc"], ["Dre"])
                    tr(ptc[:, 64:128], bbi[:, j * 8:(j + 1) * 8, :].rearrange("p g h -> p (g h)"), ident_f[0:64, 0:64], ["bbi", "cst"], ["ptc2"])
                    cp("dve", Dim[:, j, :], ptc[:, 64:128], ["ptc2"], ["Dim"])
                ts("dve", nDre[:], Dre[:], -1.0, None, ALU.mult, None, ["Dre"], ["nDre"])
                rmb = rm8.unsqueeze(2).to_broadcast([128, 8, 64])
                for j in range(8):
                    gs = slice(j * 8, (j + 1) * 8)
                    bcD = lambda t: t[:, j, :].unsqueeze(1).to_broadcast([128, 8, 64])
                    tt("dve", Blk1[:, gs, 0:64], bcD(Dre), rmb, ALU.mult, ["Dre", "cst"], ["Blk1"])
                    tt("dve", Blk1[:, gs, 64:128], bcD(Dim), rmb, ALU.mult, ["Dim", "cst"], ["Blk1"])
                    tt("dve", Blk2[:, gs, 0:64], bcD(Dim), rmb, ALU.mult, ["Dim", "cst"], ["Blk2"])
                    tt("dve", Blk2[:, gs, 64:128], bcD(nDre), rmb, ALU.mult, ["nDre", "cst"], ["Blk2"])
                crv = I["s5_c_re"].rearrange("g h p -> (g h) p")
                civ = I["s5_c_im"].rearrange("g h p -> (g h) p")
                for j in range(8):
                    rs_ = slice(j * 128, (j + 1) * 128)
                    S.dma("sp", cc1[:, 0:64], crv[rs_, :], w=["cc1"], stream="c0f")
                    S.dma("sp", cc1[:, 64:128], civ[rs_, :], w=["cc1"], stream="c0f")
                    S.dma("sp", cc2[:, 0:64], civ[rs_, :], w=["cc2"], stream="c0g")
                    S.dma("sp", cc2[:, 64:128], crv[rs_, :], w=["cc2"], stream="c0g")
                    tr(ptc[:, :], cc1[:], ident_f, ["cc1", "cst"], ["ptc", "ptc2"])
                    gs = slice(j * 8, (j + 1) * 8)
                    cp("dve", CL1[0:64, gs, :], ptc[0:64, :].rearrange("p (g h) -> p g h", h=16), ["ptc"], ["CL1"])
                    ts("dve", CL1[64:128, gs, :], ptc[64:128, :].rearrange("p (g h) -> p g h", h=16), -1.0, None,
                       ALU.mult, None, ["ptc"], ["CL1"])
                    tr(ptc[:, :], cc2[:], ident_f, ["cc2", "cst"], ["ptc", "ptc2"])
                    ts("dve", CL2[:, gs, :], ptc[:, :].rearrange("p (g h) -> p g h", h=16), -1.0, None, ALU.mult, None,
                       ["ptc"], ["CL2"])
                S.dma("sp", D5c[:], I["s5_d"].rearrange("(j gl) h -> (gl h) j", gl=8), w=["D5c"], stream="c0h",
                      allow_slow_non_contiguous=True)
                S.dma("sp", glub[:], I["glu_b"].rearrange("(j gl) h -> (gl h) j", gl=8), w=["glub"], stream="c0i",
                      allow_slow_non_contiguous=True)
                S.dma("sp", g5c[:], I["norm_s5_g"].rearrange("o (j p) -> p (o j)", p=128), w=["g5c"], stream="c0j",
                      allow_slow_non_contiguous=True)
                S.dma("sp", wrow[:], I["glu_w"].rearrange("(j gl) h k -> (gl h) j k", gl=8), w=["wrow"], stream="c0k")
                for j in range(8):
                    tt("dve", Wg[:, j, :].rearrange("p (g k) -> p g k", k=16),
                       wrow[:, j, :].unsqueeze(1).to_broadcast([128, 8, 16]),
                       rm8.unsqueeze(2).to_broadcast([128, 8, 16]), ALU.mult, ["wrow", "cst"], ["Wg"])
                S.flush()

            with ExitStack() as p1:
                B_ = lambda n, sh, dt=F32: p1.enter_context(nc.sbuf_tensor(n, sh, dt))
                Pp = lambda n, sh, dt=F32: p1.enter_context(nc.psum_tensor(n, sh, dt))
                iot = B_("iot", [128, SEQ])
                SIN = [B_("SIN%d" % i, [128, SEQ], BF16) for i in range(3)]
                COS = [B_("COS%d" % i, [128, SEQ], BF16) for i in range(3)]
                u1 = B_("u1", [128, SEQ]); k1 = B_("k1", [128, SEQ]); fr = B_("fr", [128, SEQ])
                uT = [B_("uT%d" % i, [128, T], BF16) for i in range(2)]
                R3 = 3
                p1b = [B_("p1b%d" % i, [128, 512], BF16) for i in range(R3)]
                p2b = [B_("p2b%d" % i, [128, 512], BF16) for i in range(R3)]
                w1 = [B_("w1_%d" % i, [128, 512], BF16) for i in range(R3)]
                w2 = [B_("w2_%d" % i, [128, 512], BF16) for i in range(R3)]
                ww = [B_("ww%d" % i, [128, 512]) for i in range(R3)]
                zz = [[B_("zz%d_%d" % (bb, i), [128, 512]) for i in range(R3)] for bb in range(NB)]
                zb = [B_("zb%d" % i, [128, 512], BF16) for i in range(R3)]
                v1 = [B_("v1_%d" % i, [128, 512], BF16) for i in range(R3)]
                v2 = [B_("v2_%d" % i, [128, 512], BF16) for i in range(R3)]
                ysm = [B_("ysm%d" % i, [16, 512]) for i in range(R3)]
                P1 = [Pp("P1_%d" % i, [128, 512]) for i in range(2)]
                P2 = [Pp("P2_%d" % i, [128, 512]) for i in range(2)]
                uf = [B_("uf%d" % i, [128, D]) for i in range(2)]
                vf = [B_("vf%d" % i, [128, D]) for i in range(2)]
                ub = [B_("ub%d" % i, [128, D], BF16) for i in range(2)]
                vbt = [B_("vbt%d" % i, [128, D], BF16) for i in range(2)]
                uts = [B_("uts%d" % i, [128, D], BF16) for i in range(2)]
                pTu = [Pp("pTu%d" % i, [128, 1024], BF16) for i in range(2)]

                def m0_tile(et):
                    pr = et % 2
                    rows = slice(et * 128, (et + 1) * 128)
                    S.dma("sp", uf[pr][:], I["expert_u"][rows, :], w=["uf%d" % pr], stream="uf%d" % pr)
                    S.dma("sp", vf[pr][:], I["expert_v"][rows, :], w=["vf%d" % pr], stream="vf%d" % pr)
                    yield
                    cp("act", ub[pr][:], uf[pr][:], ["uf%d" % pr], ["ub%d" % pr])
                    cp("act", vbt[pr][:], vf[pr][:], ["vf%d" % pr], ["vbt%d" % pr])
                    yield
                    for kc in range(8):
                        tr(pTu[pr][:, kc * 128:(kc + 1) * 128], ub[pr][:, kc * 128:(kc + 1) * 128], ident_b,
                           ["ub%d" % pr, "cstb"], ["pTu%d" % pr])
                    S.dma("sp", Vb[rows, :], vbt[pr][:], r=["vbt%d" % pr], w=["Vb"], stream="vbo%d" % pr)
                    yield
                    cp("act", uts[pr][:], pTu[pr][:, :], ["pTu%d" % pr], ["uts%d" % pr])
                    yield
                    S.dma("sp", UTb[et], uts[pr][:], r=["uts%d" % pr], w=["UTb"], stream="uto%d" % pr)
                    yield
                PY = [Pp("PY%d" % i, [128, 512]) for i in range(2)]
                for k in range(16):
                    ts("dve", iot[:, k * 128:(k + 1) * 128], cst[:, C_IOTA:C_IOTA + 128], float(128 * k), None, ALU.add,
                       None, ["cst"], ["iot"])

                def tables(g):
                    gp = g % 3
                    fg = fcol[:, g:g + 1]
                    for (tab, key, off) in ((SIN[gp], "SIN%d" % gp, 0.0), (COS[gp], "COS%d" % gp, 0.25)):
                        act(u1[:], iot[:], AF.Identity, ["iot", "fcol"], ["u1"], scale=fg, bias=off)
                        yield
                        act(k1[:], u1[:], AF.Identity, ["u1"], ["k1"], scale=1.0, bias=MAGIC)
                        yield
                        stt(fr[:], k1[:], MAGIC, u1[:], ALU.subtract, ALU.subtract, ["u1", "k1"], ["fr"])
                        yield
                        act(tab[:], fr[:], AF.Sin, ["fr"], [key], scale=-TWO_PI)
                        yield

                pieces = [(g, q, bb) for g in range(64) for q in range(4) for bb in range(NB)]

                def info(n):
                    g, q, bb = pieces[n]
                    return g, q, bb, g // 8, g % 3, n % R3, q * 512, bb * SEQ + q * 512

                def st_a(n):
                    g, q, bb, j, gp, pb, t0, tok = info(n)
                    if q == 0 and bb == 0 and g + 1 < 64:
                        side.append(tables(g + 1))
                    if g % 8 == 0 and q == 0 and bb == 0:
                        S.dma("sp", uT[j % 2][:], UTs[j * 128:(j + 1) * 128, :], r=["UTs"], w=["uT%d" % (j % 2)],
                              stream="uT%d" % (j % 2))
                    uk = "uT%d" % (j % 2)
                    mm(P1[n % 2][:, :], Blk1[:, g, :], uT[j % 2][:, tok:tok + 512], True, True, ["Blk1", uk], ["P1_%d" % (n % 2)])
                    mm(P2[n % 2][:, :], Blk2[:, g, :], uT[j % 2][:, tok:tok + 512], True, True, ["Blk2", uk], ["P2_%d" % (n % 2)])

                def st_b(n):
                    g, q, bb, j, gp, pb, t0, tok = info(n)
                    cp("act", p1b[pb][:], P1[n % 2][:, :], ["P1_%d" % (n % 2)], ["p1b%d" % pb])
                    cp("act", p2b[pb][:], P2[n % 2][:, :], ["P2_%d" % (n % 2)], ["p2b%d" % pb])

                def st_c(n):
                    g, q, bb, j, gp, pb, t0, tok = info(n)
                    tt("dve", w1[pb][:], p1b[pb][:], COS[gp][:, t0:t0 + 512], ALU.mult, ["p1b%d" % pb, "COS%d" % gp],
                       ["w1_%d" % pb])
                    tt("dve", w2[pb][:], p2b[pb][:], SIN[gp][:, t0:t0 + 512], ALU.mult, ["p2b%d" % pb, "SIN%d" % gp],
                       ["w2_%d" % pb])

                def st_d(n):
                    g, q, bb, j, gp, pb, t0, tok = info(n)
                    tt("pool", ww[pb][:], w1[pb][:], w2[pb][:], ALU.add, ["w1_%d" % pb, "w2_%d" % pb], ["ww%d" % pb])

                def st_e(n):
                    g, q, bb, j, gp, pb, t0, tok = info(n)
                    zc, zp = zz[bb][q % R3], zz[bb][(q - 1) % R3]
                    zck, zpk = "zz%d_%d" % (bb, q % R3), "zz%d_%d" % (bb, (q - 1) % R3)
                    init = 0.0 if q == 0 else zp[:, 511:512]
                    S.op("dve", lambda e: e.tensor_tensor_scan(
                        out=zc[:], data0=rcol[:, g:g + 1].to_broadcast([128, 512]), data1=ww[pb][:],
                        initial=init, op0=ALU.mult, op1=ALU.add), r=["rcol", "ww%d" % pb, zpk], w=[zck])

                def st_f(n):
                    g, q, bb, j, gp, pb, t0, tok = info(n)
                    cp("act", zb[pb][:], zz[bb][q % R3][:], ["zz%d_%d" % (bb, q % R3)], ["zb%d" % pb])

                def st_g(n):
                    g, q, bb, j, gp, pb, t0, tok = info(n)
                    tt("dve", v1[pb][:], zb[pb][:], COS[gp][:, t0:t0 + 512], ALU.mult, ["zb%d" % pb, "COS%d" % gp],
                       ["v1_%d" % pb])
                    tt("pool", v2[pb][:], zb[pb][:], SIN[gp][:, t0:t0 + 512], ALU.mult, ["zb%d" % pb, "SIN%d" % gp],
                       ["v2_%d" % pb])

                def st_h(n):
                    g, q, bb, j, gp, pb, t0, tok = info(n)
                    pp = n % 2
                    mm(PY[pp][0:16, :], CL1[:, g, :], v1[pb][:], True, False, ["CL1", "v1_%d" % pb], ["PY%d" % pp])
                    mm(PY[pp][0:16, :], CL2[:, g, :], v2[pb][:], False, True, ["CL2", "v2_%d" % pb], ["PY%d" % pp])

                def st_i(n):
                    g, q, bb, j, gp, pb, t0, tok = info(n)
                    pp = n % 2
                    yk = "ysm%d" % pb
                    cp("act", ysm[pb][0:16, :], PY[pp][0:16, :], ["PY%d" % pp], [yk])
                    S.dma("sp", Y5s[g * 16:(g + 1) * 16, tok:tok + 512], ysm[pb][0:16, :], r=[yk], w=["Y5s"], stream=yk)

                for _ in tables(0):
                    pass
                stages_c = [st_a, st_b, st_c, st_d, st_e, st_f, st_g, st_h, st_i]
                side = []
                nxt_et = 0
                for k in range(len(pieces) + len(stages_c) - 1):
                    for s_ in reversed(range(len(stages_c))):
                        n = k - s_
                        if 0 <= n < len(pieces):
                            stages_c[s_](n)
                    if k % 4 == 0 and nxt_et < 128:
                        side.append(m0_tile(nxt_et))
                        nxt_et += 1
                    for g_ in list(side):
                        try:
                            next(g_)
                        except StopIteration:
                            side.remove(g_)
                while side or nxt_et < 128:
                    if nxt_et < 128:
                        side.append(m0_tile(nxt_et))
                        nxt_et += 1
                    for g_ in list(side):
                        try:
                            next(g_)
                        except StopIteration:
                            side.remove(g_)
                S.flush()

            with ExitStack() as p2:
                B_ = lambda n, sh, dt=F32: p2.enter_context(nc.sbuf_tensor("C2_" + n, sh, dt))
                Pp = lambda n, sh, dt=F32: p2.enter_context(nc.psum_tensor("C2_" + n, sh, dt))
                y5 = [B_("y5_%d" % i, [128, 512]) for i in range(3)]
                uu = [B_("uu%d" % i, [128, 512], BF16) for i in range(3)]
                yv = [B_("yv%d" % i, [128, 512]) for i in range(3)]
                vb = [B_("vb%d" % i, [128, 512], BF16) for i in range(3)]
                sg = [B_("sg%d" % i, [128, 512]) for i in range(3)]
                oo = [B_("oo%d" % i, [128, 8, 512]) for i in range(2)]
                sq = [B_("sq%d" % i, [128, 512]) for i in range(3)]
                rs5 = [B_("rs5_%d" % i, [128, 512]) for i in range(2)]
                ycb = [B_("ycb%d" % i, [128, 512], BF16) for i in range(2)]
                PG = [Pp("PG%d" % i, [128, 512]) for i in range(2)]
                PSS = [Pp("PSS%d" % i, [128, 512]) for i in range(2)]
                NBK = T // 512

                def cinfo(n):
                    return n // 8, n % 8, n % 3, (n // 8) * 512

                def c_a(n):
                    blk, j, r3, tok = cinfo(n)
                    S.dma("sp", y5[r3][:], Y5s[j * 128:(j + 1) * 128, tok:tok + 512], r=["Y5s"], w=["y5_%d" % r3],
                          stream="y5_%d" % r3)
                    S.dma("sp", uu[r3][:], UTs[j * 128:(j + 1) * 128, tok:tok + 512], r=["UTs"], w=["uu%d" % r3],
                          stream="uu%d" % r3)

                def c_b(n):
                    blk, j, r3, tok = cinfo(n)
                    stt(yv[r3][:], uu[r3][:], D5c[:, j:j + 1], y5[r3][:], ALU.mult, ALU.add,
                        ["uu%d" % r3, "D5c", "y5_%d" % r3], ["yv%d" % r3])

                def c_c(n):
                    blk, j, r3, tok = cinfo(n)
                    act(vb[r3][:], yv[r3][:], AF.Gelu, ["yv%d" % r3], ["vb%d" % r3])

                def c_d(n):
                    blk, j, r3, tok = cinfo(n)
                    mm(PG[n % 2][:, :], Wg[:, j, :], vb[r3][:], True, True, ["Wg", "vb%d" % r3], ["PG%d" % (n % 2)])

                def c_e(n):
                    blk, j, r3, tok = cinfo(n)
                    act(sg[r3][:], PG[n % 2][:, :], AF.Sigmoid, ["PG%d" % (n % 2), "glub"], ["sg%d" % r3],
                        bias=glub[:, j:j + 1])

                def c_f(n):
                    blk, j, r3, tok = cinfo(n)
                    tt("dve", oo[blk % 2][:, j, :], vb[r3][:], sg[r3][:], ALU.mult, ["vb%d" % r3, "sg%d" % r3],
                       ["oo%d_%d" % (blk % 2, j)])

                def c_g(n):
                    blk, j, r3, tok = cinfo(n)
                    tt("pool", sq[r3][:], oo[blk % 2][:, j, :], oo[blk % 2][:, j, :], ALU.mult,
                       ["oo%d_%d" % (blk % 2, j)], ["sq%d" % r3])

                def c_h(n):
                    blk, j, r3, tok = cinfo(n)
                    mm(PSS[blk % 2][:, :], ones_f, sq[r3][:], j == 0, j == 7, ["cst", "sq%d" % r3], ["PSS%d" % (blk % 2)])

                def c_tail(blk):
                    bp = blk % 2
                    tok = blk * 512
                    rk = "rs5_%d" % bp
                    ts("dve", rs5[bp][:], PSS[bp][:, :], 1.0 / 1024, EPS, ALU.mult, ALU.add, ["PSS%d" % bp], [rk])
                    yield
                    act(rs5[bp][:], rs5[bp][:], AF.Sqrt, [rk], [rk])
                    yield
                    S.op("dve", lambda e: e.reciprocal(out=rs5[bp][:], in_=rs5[bp][:]), r=[rk], w=[rk])
                    yield
                    for j in range(8):
                        jp = j % 2
                        stt(ycb[jp][:], oo[bp][:, j, :], g5c[:, j:j + 1], rs5[bp][:], ALU.mult, ALU.mult,
                            ["oo%d_%d" % (bp, j), "g5c", rk], ["ycb%d" % jp])
                        S.dma("sp", YCs[1024 + j * 128:1024 + (j + 1) * 128, tok:tok + 512], ycb[jp][:],
                              r=["ycb%d" % jp], w=["YCs"], stream="ycb%d" % jp)
                        yield

                st2 = [c_a, c_b, c_c, c_d, c_e, c_f, c_g, c_h]
                NI2 = NBK * 8
                side2 = []
                for k in range(NI2 + len(st2) - 1):
                    for s_ in reversed(range(len(st2))):
                        n = k - s_
                        if 0 <= n < NI2:
                            st2[s_](n)
                    nh = k - (len(st2) - 1)
                    if nh >= 0 and nh % 8 == 7:
                        side2.append(c_tail(nh // 8))
                    for g_ in list(side2):
                        try:
                            next(g_)
                        except StopIteration:
                            side2.remove(g_)
                while side2:
                    for g_ in list(side2):
                        try:
                            next(g_)
                        except StopIteration:
                            side2.remove(g_)
                S.flush()
        if stop_after == 3:
            return nc

        with ExitStack() as ph:
            A = lambda n, sh, dt=F32: ph.enter_context(nc.sbuf_tensor("D_" + n, sh, dt))
            P = lambda n, sh, dt=F32: ph.enter_context(nc.psum_tensor("D_" + n, sh, dt))
            wout = A("wout", [128, 16, D], BF16)
            wq = A("wq", [128, 8, 2048], BF16)
            skf = A("skf", [128, 16, 128])
            skT = A("skT", [128, 16, 128], BF16)
            GT1 = A("GT1", [128, D]); G2 = A("G2", [128, D]); SH2 = A("SH2", [128, D])
            yct = [A("yct%d" % i, [128, 16, 128], BF16) for i in range(2)]
            xin = [A("xin%d" % i, [128, D]) for i in range(2)]
            t1 = A("t1", [128, D]); x1 = [A("x1_%d" % i, [128, D]) for i in range(2)]
            junk = A("junk", [128, D], BF16)
            ss2 = A("ss2", [128, 32])
            hb2 = A("hb2", [128, D], BF16)
            h2T = [A("h2T%d" % i, [128, 8, 128], BF16) for i in range(2)]
            qT = A("qT", [128, 16, 128], BF16)
            scb = [A("sc_%d" % i, [128, 16, 128]) for i in range(2)]; sc2 = A("sc2", [128, 16, 128])
            v8 = A("v8", [128, 16, 16]); i8 = A("i8", [128, 16, 16], U32); i8f = A("i8f", [128, 16, 16])
            cand = A("cand", [128, 8, 256]); cand2 = A("cand2", [128, 8, 256])
            c8 = A("c8", [128, 8, 16]); p8 = A("p8", [128, 8, 16], U32)
            ge = A("ge", [128, 8, 16]); gs = A("gs", [128, 8]); gg = A("gg", [128, 8, 16])
            ra_i = A("ra_i", [128, 128], I32); rb_i = A("rb_i", [128, 128], I32)
            raf = A("raf", [128, 128]); rbf = A("rbf", [128, 128])
            oh = A("oh", [128, 128, 16]); oh2 = A("oh2", [128, 128, 16])
            isel = A("isel", [128, 128]); jsel = A("jsel", [128, 128])
            rstg = [A("rstg%d" % i, [128, 3, 128], BF16) for i in range(2)]
            pT = P("pT", [128, 1024], BF16)
            pM = P("pM", [128, 1024])
            pq = [P("pq%d" % i, [128, 512]) for i in range(2)]
            psc = [P("psc%d" % i, [128, 512]) for i in range(2)]
            pTi = P("pTi", [128, 512])
            iota16 = cst[:, C_IOTA16:C_IOTA16 + 16]

            woutv = I["w_out"].rearrange("(ct p) d -> p ct d", p=128)
            for q4 in range(4):
                S.dma("pool", wout[:, q4 * 4:(q4 + 1) * 4, :], woutv[:, q4 * 4:(q4 + 1) * 4, :], w=["wout"], stream="wout")
            wqv = I["w_query"].rearrange("(kc p) n -> p kc n", p=128)
            for q4 in range(4):
                S.dma("pool", wq[:, q4 * 2:(q4 + 1) * 2, :], wqv[:, q4 * 2:(q4 + 1) * 2, :], w=["wq"], stream="wq")
            S.dma("sp", skf[:], I["sub_keys"].rearrange("m k d -> k m d"), w=["skf"], stream="skf")
            for m in range(16):
                tr(pTi[:, (m % 4) * 128:(m % 4 + 1) * 128], skf[:, m, :], ident_f, ["skf", "cst"], ["pTi"])
                cp("dve", skT[:, m, :], pTi[:, (m % 4) * 128:(m % 4 + 1) * 128], ["pTi"], ["skT"])
            YCv = YCs.rearrange("(ct p) t -> p ct t", p=128)
            H2v = H2Ts.rearrange("(kc p) t -> p kc t", p=128)
            def tile_vars(i):
                return i // 16, i % 2, i * 128

            def front(i):
                b, par, tok0 = tile_vars(i)
                sck = "sc%d" % par
                if i % 16 == 0:
                    S.dma("sp", GT1[:], MODs[b:b + 1, 2048:3072].partition_broadcast(128), r=["MODs"], w=["GT1"], stream="d0")
                    S.dma("sp", G2[:], MODs[b:b + 1, 4096:5120].partition_broadcast(128), r=["MODs"], w=["G2"], stream="d1")
                    S.dma("sp", SH2[:], MODs[b:b + 1, 3072:4096].partition_broadcast(128), r=["MODs"], w=["SH2"], stream="d2")
                yk, xk, x1k, hk = "yct%d" % par, "xin%d" % par, "x1_%d" % par, "h2T%d" % par
                S.dma("sp", yct[par][:], YCv[:, :, tok0:tok0 + 128], r=["YCs"], w=[yk], stream=yk)
                S.dma("sp", xin[par][:], I["x"][tok0:tok0 + 128, :], w=[xk], stream=xk)
                yield
                for half in range(2):
                    for ct in range(16):
                        mm(pM[:, half * 512:(half + 1) * 512], yct[par][:, ct, :], wout[:, ct, half * 512:(half + 1) * 512],
                           ct == 0, ct == 15, [yk, "wout"], ["pM"])
                yield
                tt("dve", t1[:], pM[:, :], GT1[:], ALU.mult, ["pM", "GT1"], ["t1"])
                yield
                tt("pool", x1[par][:], t1[:], xin[par][:], ALU.add, ["t1", xk], [x1k])
                S.dma("sp", X1s[tok0:tok0 + 128, :], x1[par][:], r=[x1k], w=["X1s"], stream=x1k)
                yield
                act(junk[:], x1[par][:], AF.Square, [x1k], ["junk", "ss2"], accum_out=ss2[:, i:i + 1])
                yield
                col = ss2[:, i:i + 1]
                ts("dve", col, col, 1.0 / D, EPS, ALU.mult, ALU.add, ["ss2"], ["ss2"])
                yield
                act(col, col, AF.Sqrt, ["ss2"], ["ss2"])
                yield
                S.op("dve", lambda e: e.reciprocal(out=col, in_=col), r=["ss2"], w=["ss2"])
                stt(t1[:], x1[par][:], ss2[:, i:i + 1], G2[:], ALU.mult, ALU.mult, [x1k, "ss2", "G2"], ["t1"])
                yield
                tt("pool", hb2[:], t1[:], SH2[:], ALU.add, ["t1", "SH2"], ["hb2"])
                yield
                for kc in range(8):
                    tr(pT[:, kc * 128:(kc + 1) * 128], hb2[:, kc * 128:(kc + 1) * 128], ident_b, ["hb2", "cstb"], ["pT"])
                yield
                cp("act", h2T[par][:, :, :], pT[:, :].rearrange("p (k t) -> p k t", k=8), ["pT"], [hk])
                S.dma("sp", H2v[:, :, tok0:tok0 + 128], h2T[par][:, :, :], r=[hk], w=["H2Ts"], stream=hk)
                yield
                for m4 in range(4):
                    pp = m4 % 2
                    for mi in range(4):
                        m = m4 * 4 + mi
                        for kc in range(8):
                            mm(pq[pp][:, mi * 128:(mi + 1) * 128], wq[:, kc, m * 128:(m + 1) * 128], h2T[par][:, kc, :],
                               kc == 0, kc == 7, ["wq", hk], ["pq%d" % pp])
                    cp("act", qT[:, m4 * 4:(m4 + 1) * 4, :], pq[pp][:, :].rearrange("p (m t) -> p m t", m=4),
                       ["pq%d" % pp], ["qT%d" % m4])
                    yield
                for m4 in range(4):
                    pp = m4 % 2
                    for mi in range(4):
                        m = m4 * 4 + mi
                        mm(psc[pp][:, mi * 128:(mi + 1) * 128], qT[:, m, :], skT[:, m, :], True, True,
                           ["qT%d" % m4, "skT"], ["psc%d" % pp])
                    cp("act", scb[par][:, m4 * 4:(m4 + 1) * 4, :], psc[pp][:, :].rearrange("p (m k) -> p m k", m=4),
                       ["psc%d" % pp], [sck])
                    yield

            def back(i):
                b, par, tok0 = tile_vars(i)
                sck = "sc%d" % par
                for m in range(16):
                    S.op("dve", lambda e, m=m: e.max(out=v8[:, m, 0:8], in_=scb[par][:, m, :]), r=[sck], w=["v8a%d" % m])
                yield
                for m in range(16):
                    S.op("dve", lambda e, m=m: e.max_index(out=i8[:, m, 0:8], in_max=v8[:, m, 0:8], in_values=scb[par][:, m, :]),
                         r=[sck, "v8a%d" % m], w=["i8a%d" % m])
                    S.op("dve", lambda e, m=m: e.match_replace(out=sc2[:, m, :], in_to_replace=v8[:, m, 0:8],
                                                               in_values=scb[par][:, m, :], imm_value=-1e30),
                         r=[sck, "v8a%d" % m], w=["sc2_%d" % m])
                    if m % 4 == 3:
                        yield
                for m in range(16):
                    S.op("dve", lambda e, m=m: e.max(out=v8[:, m, 8:16], in_=sc2[:, m, :]), r=["sc2_%d" % m],
                         w=["v8b%d" % m])
                yield
                for m in range(16):
                    S.op("dve", lambda e, m=m: e.max_index(out=i8[:, m, 8:16], in_max=v8[:, m, 8:16],
                                                           in_values=sc2[:, m, :]), r=["sc2_%d" % m, "v8b%d" % m],
                         w=["i8b%d" % m])
                yield
                v8keys = ["v8a%d" % m for m in range(16)] + ["v8b%d" % m for m in range(16)]
                i8keys = ["i8a%d" % m for m in range(16)] + ["i8b%d" % m for m in range(16)]
                cp("dve", i8f[:], i8[:], i8keys, ["i8f"])
                v8v = v8[:, :, :].rearrange("p (h c) r -> p h c r", c=2)
                i8v = i8f[:, :, :].rearrange("p (h c) r -> p h c r", c=2)
                tt("dve", cand[:, :, :].rearrange("p h (r c) -> p h r c", c=16),
                   v8v[:, :, 0, :].unsqueeze(3).to_broadcast([128, 8, 16, 16]),
                   v8v[:, :, 1, :].unsqueeze(2).to_broadcast([128, 8, 16, 16]), ALU.add, v8keys, ["cand"])
                yield
                for h in range(8):
                    S.op("dve", lambda e, h=h: e.max(out=c8[:, h, 0:8], in_=cand[:, h, :]), r=["cand"], w=["c8a%d" % h])
                yield
                for h in range(8):
                    S.op("dve", lambda e, h=h: e.max_index(out=p8[:, h, 0:8], in_max=c8[:, h, 0:8], in_values=cand[:, h, :]),
                         r=["cand", "c8a%d" % h], w=["p8a%d" % h])
                    S.op("dve", lambda e, h=h: e.match_replace(out=cand2[:, h, :], in_to_replace=c8[:, h, 0:8],
                                                               in_values=cand[:, h, :], imm_value=-1e30),
                         r=["cand", "c8a%d" % h], w=["cand2_%d" % h])
                    if h % 4 == 3:
                        yield
                for h in range(8):
                    S.op("dve", lambda e, h=h: e.max(out=c8[:, h, 8:16], in_=cand2[:, h, :]), r=["cand2_%d" % h],
                         w=["c8b%d" % h])
                yield
                for h in range(8):
                    S.op("dve", lambda e, h=h: e.max_index(out=p8[:, h, 8:16], in_max=c8[:, h, 8:16],
                                                           in_values=cand2[:, h, :]), r=["cand2_%d" % h, "c8b%d" % h],
                         w=["p8b%d" % h])
                yield
                c8keys = ["c8a%d" % h for h in range(8)] + ["c8b%d" % h for h in range(8)]
                p8keys = ["p8a%d" % h for h in range(8)] + ["p8b%d" % h for h in range(8)]
                tt("dve", ge[:], c8[:], c8[:, :, 0:1].to_broadcast([128, 8, 16]), ALU.subtract, c8keys, ["ge"])
                yield
                act(ge[:], ge[:], AF.Exp, ["ge"], ["ge"])
                yield
                S.op("dve", lambda e: e.tensor_reduce(out=gs[:], in_=ge[:], axis=AX.X, op=ALU.add), r=["ge"], w=["gs"])
                S.op("dve", lambda e: e.reciprocal(out=gs[:], in_=gs[:]), r=["gs"], w=["gs"])
                tt("dve", gg[:], ge[:], gs[:, :].unsqueeze(2).to_broadcast([128, 8, 16]), ALU.mult, ["ge", "gs"], ["gg"])
                p8i = p8[:, :, :].rearrange("p h k -> p (h k)").bitcast(I32)
                S.op("dve", lambda e: e.tensor_single_scalar(out=ra_i[:], in_=p8i, scalar=4, op=ALU.logical_shift_right),
                     r=p8keys, w=["ra_i"])
                S.op("dve", lambda e: e.tensor_single_scalar(out=rb_i[:], in_=p8i, scalar=15, op=ALU.bitwise_and),
                     r=p8keys, w=["rb_i"])
                cp("dve", raf[:], ra_i[:], ["ra_i"], ["raf"])
                cp("dve", rbf[:], rb_i[:], ["rb_i"], ["rbf"])
                yield
                io3 = iota16.unsqueeze(1).to_broadcast([128, 128, 16])
                for (rf, rk, ci, ohh, ok, sel, sk_) in ((raf, "raf", 0, oh, "oh", isel, "isel"),
                                                        (rbf, "rbf", 1, oh2, "oh2", jsel, "jsel")):
                    eng = "dve" if ci == 0 else "pool"
                    tt("dve", ohh[:], rf[:, :].unsqueeze(2).to_broadcast([128, 128, 16]), io3, ALU.is_equal,
                       [rk, "cst"], [ok])
                    tt(eng, ohh[:, :, :].rearrange("p (h k) r -> p h k r", h=8),
                       ohh[:, :, :].rearrange("p (h k) r -> p h k r", h=8),
                       i8v[:, :, ci, :].unsqueeze(2).to_broadcast([128, 8, 16, 16]), ALU.mult, [ok, "i8f"], [ok])
                    S.op("dve", lambda e, sel=sel, ohh=ohh: e.tensor_reduce(out=sel[:], in_=ohh[:], axis=AX.X, op=ALU.add),
                         r=[ok], w=[sk_])
                    yield
                tr(pTi[:, 0:128], isel[:], ident_f, ["isel", "cst"], ["pTi"])
                tr(pTi[:, 128:256], jsel[:], ident_f, ["jsel", "cst"], ["pTi"])
                tr(pTi[:, 256:384], gg[:, :, :].rearrange("p h k -> p (h k)"), ident_f, ["gg", "cst"], ["pTi"])
                rk_ = "rstg%d" % par
                cp("act", rstg[par][:, :, :], pTi[:, 0:384].rearrange("p (a t) -> p a t", a=3), ["pTi"], [rk_])
                S.dma("sp", RTs[:, :, tok0:tok0 + 128], rstg[par][:, :, :], r=[rk_], w=["RTs"], stream=rk_)

            def interleave(gens):
                gens = [g_ for g_ in gens if g_ is not None]
                while gens:
                    for g_ in list(gens):
                        try:
                            next(g_)
                        except StopIteration:
                            gens.remove(g_)

            interleave([front(0)])
            for i in range(NT):
                interleave([front(i + 1) if i + 1 < NT else None, back(i)])
            S.flush()
        if stop_after == 4:
            return nc

        with ExitStack() as ph:
            A = lambda n, sh, dt=F32: ph.enter_context(nc.sbuf_tensor("M_" + n, sh, dt))
            P = lambda n, sh, dt=F32: ph.enter_context(nc.psum_tensor("M_" + n, sh, dt))
            TB = 256
            G0 = A("G0", [128, 64, TB], BF16)
            G1 = A("G1", [128, 64, TB], BF16)
            utb = [A("utb%d" % i, [128, 2, D], BF16) for i in range(4)]
            vtb = [A("vtb%d" % i, [128, 2, D], BF16) for i in range(4)]
            Pm = [A("Pm%d" % i, [128, 8, 64], BF16) for i in range(4)]
            Q0 = [A("Q0%d" % i, [128, 8, 128], BF16) for i in range(4)]
            Qm = [A("Qm%d" % i, [128, 8, 128], BF16) for i in range(4)]
            h2b = [A("h2b%d" % i, [128, 8, TB], BF16) for i in range(2)]
            rt = [A("rt%d" % i, [128, 3, TB], BF16) for i in range(2)]
            Ag = [A("Ag%d" % i, [128, TB], BF16) for i in range(2)]
            GA = [A("GA%d" % i, [128, TB], BF16) for i in range(2)]
            x1t = [A("x1t%d" % i, [128, D]) for i in range(2)]
            GT2 = A("GT2", [128, D]); nfg = A("nfg", [128, D])
            tm = [A("tm%d" % i, [128, D]) for i in range(2)]; x2 = A("x2", [128, D]); junkm = A("junkm", [128, D], BF16)
            ot = [A("ot%d" % i, [128, D]) for i in range(2)]
            ssf = A("ssf", [128, 32])
            pO = [P("pO%d" % i, [128, 1024]) for i in range(2)]
            pA = [P("pA%d" % i, [128, 512]) for i in range(2)]
            pG = [P("pG%d" % i, [128, 512]) for i in range(2)]
            iota_b = cstb[:, C_IOTA:C_IOTA + 128]
            io32 = iota_b.unsqueeze(1).to_broadcast([128, 32, 128])
            io8 = iota_b.unsqueeze(1).to_broadcast([128, 8, 128])
            H2v = H2Ts.rearrange("(kc p) t -> p kc t", p=128)
            UTv = UTb.rearrange("e p x -> p e x")
            Vv = Vb.rearrange("(e p) d -> p e d", p=128)
            S.dma("sp", nfg[:], I["norm_f_g"][0:1, :].partition_broadcast(128), w=["nfg"], stream="m0")
            Gh = [G0, G1]
            io64 = [iota_b[:, 64 * hf:64 * hf + 64].unsqueeze(1).to_broadcast([128, 8, 64]) for hf in range(2)]
            NBLK = T // TB
            cnts = {"g": 0, "s": 0}
            late = []

            def run_late():
                for f_ in late:
                    f_()
                del late[:]

            def build(blk, hf):
                bp = blk % 2
                tok = blk * TB
                hk, rk = "h2b%d" % bp, "rt%d" % bp
                if hf == 0:
                    S.dma("sp", h2b[bp][:], H2v[:, :, tok:tok + TB], r=["H2Ts"], w=[hk], stream=hk)
                    S.dma("sp", rt[bp][:], RTs[:, :, tok:tok + TB], r=["RTs"], w=[rk], stream=rk)
                    yield
                NG = TB // 8
                base = cnts["s"]
                cnts["s"] += NG

                def dve_part(k):
                    sp_ = (base + k) % 4
                    tsl = slice(k * 8, (k + 1) * 8)
                    bcn = lambda a_, n_: rt[bp][:, a_, tsl].unsqueeze(2).to_broadcast([128, 8, n_])
                    tt("dve", Pm[sp_][:], bcn(0, 64), io64[hf], ALU.is_equal, [rk, "cstb"], ["Pm%d" % sp_])
                    tt("dve", Q0[sp_][:], bcn(1, 128), io8, ALU.is_equal, [rk, "cstb"], ["Q0%d" % sp_])

                def pool_part(k):
                    sp_ = (base + k) % 4
                    tsl = slice(k * 8, (k + 1) * 8)
                    tt("pool", Qm[sp_][:], Q0[sp_][:], rt[bp][:, 2, tsl].unsqueeze(2).to_broadcast([128, 8, 128]),
                       ALU.mult, ["Q0%d" % sp_, rk], ["Qm%d" % sp_])

                def mm_part(k):
                    sp_ = (base + k) % 4
                    for t4 in range(2):
                        gp = cnts["g"] % 2
                        cnts["g"] += 1
                        for ti in range(4):
                            t = t4 * 4 + ti
                            mm(pG[gp][:, ti * 64:(ti + 1) * 64], Qm[sp_][:, t, :], Pm[sp_][:, t, :], True, True,
                               ["Qm%d" % sp_, "Pm%d" % sp_], ["pG%d" % gp])
                        t0 = k * 8 + t4 * 4
                        late.append(lambda gp=gp, t0=t0: cp(
                            "act", Gh[hf][:, :, t0:t0 + 4], pG[gp][:, 0:256].rearrange("p (t i) -> p i t", t=4),
                            ["pG%d" % gp], ["G%d" % hf]))

                dve_part(0)
                dve_part(1)
                dve_part(2)
                yield
                pool_part(0)
                pool_part(1)
                yield
                for k in range(NG):
                    mm_part(k)
                    if k + 3 < NG:
                        late.append(lambda k=k: dve_part(k + 3))
                    if k + 2 < NG:
                        late.append(lambda k=k: pool_part(k + 2))
                    yield

            def final(blk):
                tok = blk * TB
                b_ = tok // SEQ
                if tok % SEQ == 0:
                    S.dma("sp", GT2[:], MODs[b_:b_ + 1, 5120:6144].partition_broadcast(128), r=["MODs"], w=["GT2"],
                          stream="m1")
                for t2 in range(2):
                    tt("dve", tm[t2][:], pO[t2][:, :], GT2[:], ALU.mult, ["pO%d" % t2, "GT2"], ["tm%d" % t2])
                yield
                for t2 in range(2):
                    ti = blk * 2 + t2
                    tk0 = tok + t2 * 128
                    xk, ok_ = "x1t%d" % t2, "ot%d" % t2
                    col = ssf[:, ti % 32:ti % 32 + 1]
                    S.dma("sp", x1t[t2][:], X1s[tk0:tk0 + 128, :], r=["X1s"], w=[xk], stream=xk)
                    yield
                    tt("pool", x2[:], tm[t2][:], x1t[t2][:], ALU.add, ["tm%d" % t2, xk], ["x2"])
                    yield
                    act(junkm[:], x2[:], AF.Square, ["x2"], ["junkm", "ssf"], accum_out=col)
                    yield
                    ts("dve", col, col, 1.0 / D, EPS, ALU.mult, ALU.add, ["ssf"], ["ssf"])
                    yield
                    act(col, col, AF.Sqrt, ["ssf"], ["ssf"])
                    yield
                    S.op("dve", lambda e, col=col: e.reciprocal(out=col, in_=col), r=["ssf"], w=["ssf"])
                    stt(ot[t2][:], x2[:], col, nfg[:], ALU.mult, ALU.mult, ["x2", "ssf", "nfg"], [ok_])
                    yield
                    S.dma("sp", out[tk0:tk0 + 128, :], ot[t2][:], r=[ok_], w=["out"], stream=ok_)
                    yield

            def prefetch(gg):
                if gg >= NBLK * 64:
                    return
                up = gg % 4
                i0 = (gg % 64) * 2
                S.dma("sp", utb[up][:], UTv[:, i0:i0 + 2, :], r=["UTb"], w=["utb%d" % up], stream="utb%d" % up)
                S.dma("sp", vtb[up][:], Vv[:, i0:i0 + 2, :], r=["Vb"], w=["vtb%d" % up], stream="vtb%d" % up)

            def emitA(blk, i):
                bp = blk % 2
                gg = blk * 64 + i // 2
                up = gg % 4
                if gg == 0 and i == 0:
                    prefetch(0)
                    prefetch(1)
                    prefetch(2)
                ap_ = i % 2
                for kc in range(8):
                    mm(pA[ap_][:, 0:256], utb[up][:, i % 2, kc * 128:(kc + 1) * 128], h2b[bp][:, kc, :],
                       kc == 0, kc == 7, ["utb%d" % up, "h2b%d" % bp], ["pA%d" % ap_])

            def emitG(blk, i):
                ap_ = i % 2
                hf = i // 64
                act(Ag[ap_][:], pA[ap_][:, 0:256], AF.Gelu, ["pA%d" % ap_], ["Ag%d" % ap_])
                tt("dve", GA[ap_][:], Ag[ap_][:], Gh[hf][:, i % 64, :], ALU.mult, ["Ag%d" % ap_, "G%d" % hf], ["GA%d" % ap_])

            def emitVm(blk, i):
                gg = blk * 64 + i // 2
                up = gg % 4
                vk = "vtb%d" % up
                ap_ = i % 2
                for t2 in range(2):
                    for half in range(2):
                        mm(pO[t2][:, half * 512:(half + 1) * 512], GA[ap_][:, t2 * 128:(t2 + 1) * 128],
                           vtb[up][:, i % 2, half * 512:(half + 1) * 512], i == 0, i == 127,
                           ["GA%d" % ap_, vk], ["pO%d" % t2])
                if i % 2 == 0:
                    prefetch(gg + 3)

            def step(gens, skip=None):
                for g_ in list(gens):
                    if g_ is skip:
                        continue
                    try:
                        next(g_)
                    except StopIteration:
                        gens.remove(g_)

            for _ in build(0, 0):
                run_late()
            run_late()
            side = []
            for blk in range(NBLK):
                side.append(build(blk, 1))
                emitA(blk, 0)
                emitA(blk, 1)
                emitG(blk, 0)
                bld = side[-1]
                for i in range(128):
                    if i == 63:
                        for _ in bld:
                            run_late()
                        run_late()
                    if i == 64 and blk + 1 < NBLK:
                        bld = build(blk + 1, 0)
                        side.append(bld)
                    ip = i % 64
                    if ((ip + 1) * 37) // 64 > (ip * 37) // 64:
                        step(side)
                    else:
                        step(side, skip=bld)
                    if i + 2 < 128:
                        emitA(blk, i + 2)
                    if i + 1 < 128:
                        emitG(blk, i + 1)
                    run_late()
                    emitVm(blk, i)
                for _ in bld:
                    run_late()
                run_late()
                fg = final(blk)
                next(fg)
                side.append(fg)
            while side:
                step(side)
                run_late()
            S.flush()
        return nc


def prep_inputs(inputs):
    sq = lambda a: np.ascontiguousarray(a[0]) if a.shape[0] == 1 and a.ndim >= 2 else np.ascontiguousarray(a)
    shared = {}
    for n, sh in IN_SPECS:
        if n in ("x", "c", "consts"):
            continue
        a = np.asarray(inputs[n], dtype=np.float32)
        shared[n] = np.ascontiguousarray(a.reshape(sh))
    shared["consts"] = make_consts()
    x = np.asarray(inputs["x"], dtype=np.float32)
    c = np.asarray(inputs["c"], dtype=np.float32)
    maps = []
    for i in range(NCORES):
        m = dict(shared)
        m["x"] = np.ascontiguousarray(x[i * NB:(i + 1) * NB].reshape(T, D))
        m["c"] = np.ascontiguousarray(c[i * NB:(i + 1) * NB])
        maps.append(m)
    return maps


def kernel(**inputs):
    nc = build()
    maps = prep_inputs(inputs)
    res = run_bass_kernel_spmd(nc, maps, core_ids=list(range(NCORES)))
    outs = [np.asarray(r["out"]).reshape(NB, SEQ, D) for r in res.results]
    return np.concatenate(outs, axis=0).astype(np.float32)
```

```python
import os
from contextlib import ExitStack

import numpy as np
import concourse.bass as bass
import concourse.mybir as mybir
from concourse.bass_utils import run_bass_kernel_spmd

F32 = mybir.dt.float32
BF16 = mybir.dt.bfloat16
I32 = mybir.dt.int32
U32 = mybir.dt.uint32
AF = mybir.ActivationFunctionType
ALU = mybir.AluOpType
AX = mybir.AxisListType

NCORES = 8
D = 1024
NB = 2
SEQ = 2048
T = NB * SEQ
NT = T // 128
INW = 4112
EPS = 1e-6
MAGIC = 12582912.0
TWO_PI = 6.283185307179586


class _Op:
    __slots__ = ("eng", "fn", "dom", "order", "waits", "target", "val", "is_dma")

    def __init__(self, eng, fn, dom, order, is_dma):
        self.eng, self.fn, self.dom, self.order, self.is_dma = eng, fn, dom, order, is_dma
        self.waits = []
        self.target = is_dma
        self.val = None


class Sched:
    CE = ("pe", "act", "dve", "pool")

    def __init__(self, nc, stack):
        self.nc = nc
        self.stack = stack
        self.q = {k: [] for k in ("pe", "act", "dve", "pool", "sp")}
        self.sem = {k: stack.enter_context(nc.semaphore("c_" + k)) for k in self.CE}
        self.cnt = {k: 0 for k in self.CE}
        self.order = {k: 0 for k in self.CE}
        self.dsem = {}
        self.dcnt = {}
        self.dslot = {}
        self.dfree = []
        self.dorder = {}
        self.waited = {k: {} for k in self.q}
        self.lastw = {}
        self.readers = {}
        self.lastop = {}
        self.ninst = 0

    def _need(self, eng, p, out):
        if self.waited[eng].get(p.dom, 0) >= p.order:
            return
        cur = out.get(p.dom)
        if cur is None or cur.order < p.order:
            out[p.dom] = p

    def _deps(self, eng, r, w, is_dma=False):
        need = {}
        for b in r:
            p = self.lastw.get(b)
            if p is not None and (is_dma or not (p.eng == eng and eng == "pe" and not p.is_dma)):
                self._need(eng, p, need)
        for b in w:
            p = self.lastw.get(b)
            if p is not None and (is_dma or p.is_dma or p.eng != eng or eng != "pe"):
                self._need(eng, p, need)
            for p in self.readers.get(b, ()):
                if is_dma or p.is_dma or p.eng != eng or eng != "pe":
                    self._need(eng, p, need)
        return need

    def _add(self, op, need, r, w):
        for dom, p in need.items():
            self.waited[op.eng][dom] = p.order
            p.target = True
            op.waits.append(p)
        self.q[op.eng].append(op)
        self.lastop[op.dom] = op
        for b in r:
            self.readers.setdefault(b, []).append(op)
        for b in w:
            self.lastw[b] = op
            self.readers[b] = []
        self.ninst += 1

    def op(self, eng, fn, r=(), w=()):
        need = self._deps(eng, r, w)
        self.order[eng] += 1
        o = _Op(eng, fn, eng, self.order[eng], False)
        self._add(o, need, r, w)

    def dma(self, eng, out, in_, r=(), w=(), stream="d", **kw):
        if eng == "pool":
            key = "swd_%d" % len(self.dsem)
            self.dsem[key] = self.stack.enter_context(self.nc.semaphore(key))
            self.dcnt[key] = 0
            self.dorder[key] = 0
            self.dslot["__swd__" + key] = key
            stream = "__swd__" + key
        if stream not in self.dslot:
            if self.dfree:
                self.dslot[stream] = self.dfree.pop()
            else:
                k = "dma_%d" % len(self.dsem)
                self.dsem[k] = self.stack.enter_context(self.nc.semaphore(k))
                self.dcnt[k] = 0
                self.dorder[k] = 0
                self.dslot[stream] = k
        key = self.dslot[stream]
        need = self._deps(eng, r, w, is_dma=True)
        self.dorder[key] += 1
        fn = lambda e, out=out, in_=in_, kw=kw: e.dma_start(out=out, in_=in_, **kw)
        o = _Op(eng, fn, key, self.dorder[key], True)
        self._add(o, need, r, w)

    def barrier(self):
        lasts = list(self.lastop.values())
        for eng in self.q:
            need = {}
            for p in lasts:
                self._need(eng, p, need)
            if need:
                o = _Op(eng, None, None, 0, False)
                for dom, p in need.items():
                    self.waited[eng][dom] = p.order
                    p.target = True
                    o.waits.append(p)
                self.q[eng].append(o)
        self.lastw = {}
        self.readers = {}

    def flush(self):
        nc = self.nc
        self.barrier()
        q = self.q
        for eng in q:
            for o in q[eng]:
                if o.fn is None:
                    continue
                if o.is_dma:
                    self.dcnt[o.dom] += 16
                    o.val = self.dcnt[o.dom]
                elif o.target:
                    self.cnt[eng] += 1
                    o.val = self.cnt[eng]
        sem, dsem = self.sem, self.dsem

        def run(e, ops):
            for o in ops:
                for p in o.waits:
                    e.wait_ge(dsem[p.dom] if p.is_dma else sem[p.dom], p.val)
                if o.fn is None:
                    continue
                ins = o.fn(e)
                if o.is_dma:
                    ins.then_inc(dsem[o.dom], 16)
                elif o.target:
                    ins.then_inc(sem[o.eng], 1)

        with nc.Block() as block:
            @block.tensor
            def _(e):
                run(e, q["pe"])

            @block.scalar
            def _(e):
                run(e, q["act"])

            @block.vector
            def _(e):
                run(e, q["dve"])

            @block.gpsimd
            def _(e):
                run(e, q["pool"])

            @block.sync
            def _(e):
                run(e, q["sp"])
        for k in q:
            q[k] = []
        self.dfree.extend(v for k_, v in self.dslot.items() if not k_.startswith("__swd__"))
        self.dslot = {}
        self.lastop = {}


C_IDENT, C_TRIU, C_TRIS, C_ONES, C_IOTA, C_BD16, C_RM8, C_IOTA16, C_END = (
    0, 128, 256, 384, 512, 640, 768, 776, 792)


def make_consts():
    c = np.zeros((128, C_END), np.float32)
    k = np.arange(128)
    c[:, C_IDENT:C_IDENT + 128] = np.eye(128)
    c[:, C_TRIU:C_TRIU + 128] = (k[:, None] <= k[None, :])
    c[:, C_TRIS:C_TRIS + 128] = (k[:, None] > k[None, :])
    c[:, C_ONES:C_ONES + 128] = 1.0
    c[:, C_IOTA:C_IOTA + 128] = k[None, :]
    c[:, C_BD16:C_BD16 + 128] = (k[:, None] // 16 == k[None, :] // 16)
    c[:, C_RM8:C_RM8 + 8] = (k[:, None] // 16 == np.arange(8)[None, :])
    c[:, C_IOTA16:C_IOTA16 + 16] = np.arange(16)[None, :]
    return c


def skew_pipeline(stages, n_items):
    ns = len(stages)
    for k in range(n_items + ns - 1):
        for s_ in reversed(range(ns)):
            n = k - s_
            if 0 <= n < n_items:
                stages[s_](n)


IN_SPECS = [
    ("x", [T, D]), ("c", [NB, D]), ("w_ada", [D, 6 * D]), ("b_ada", [1, 6 * D]),
    ("norm1_g", [1, D]), ("w_in", [D, INW]), ("conv_w", [4, 2048]), ("conv_b", [1, 2048]),
    ("dt_bias", [1, 16]), ("a_log", [1, 16]), ("d_ssd", [1, 16]), ("norm_ssd_g", [1, D]),
    ("s5_a_re", [64, 64]), ("s5_a_im", [64, 64]), ("s5_log_dt", [1, 64]),
    ("s5_b_re", [64, 64, 16]), ("s5_b_im", [64, 64, 16]), ("s5_c_re", [64, 16, 64]),
    ("s5_c_im", [64, 16, 64]), ("s5_d", [64, 16]), ("glu_w", [64, 16, 16]), ("glu_b", [64, 16]),
    ("norm_s5_g", [1, D]), ("w_out", [2 * D, D]), ("norm2_g", [1, D]), ("w_query", [D, 2048]),
    ("sub_keys", [16, 128, 128]), ("expert_u", [16384, D]), ("expert_v", [16384, D]),
    ("norm_f_g", [1, D]), ("consts", [128, C_END]),
]


def build(debug=(), stop_after=None):
    nc = bass.Bass("TRN2", target_bir_lowering=False)
    I = {n: nc.dram_tensor(n, sh, F32, kind="ExternalInput").ap() for n, sh in IN_SPECS}
    out = nc.dram_tensor("out", [T, D], F32, kind="ExternalOutput").ap()

    def SCR(name, shape, dt):
        kind = "ExternalOutput" if name in debug else "Internal"
        return nc.dram_tensor(name, shape, dt, kind=kind).ap()

    MODs = SCR("MODs", [NB, 6 * D], F32)
    XCs = SCR("XCs", [2048, T], BF16)
    UTs = SCR("UTs", [1024, T], BF16)
    Zs = SCR("Zs", [T, D], BF16)
    DTs = SCR("DTs", [T, 16], F32)
    YCs = SCR("YCs", [2048, T], BF16)
    Y5s = SCR("Y5s", [1024, T], F32)
    X1s = SCR("X1s", [T, D], F32)
    H2Ts = SCR("H2Ts", [D, T], BF16)
    RTs = SCR("RTs", [128, 3, T], BF16)
    UTb = SCR("UTb", [128, 128, 1024], BF16)
    Vb = SCR("Vb", [16384, D], BF16)

    with ExitStack() as top:
        S = Sched(nc, top)

        def mm(out_, lhsT, rhs, start, stop, r, w):
            S.op("pe", lambda e: e.matmul(out_, lhsT=lhsT, rhs=rhs, start=start, stop=stop), r=r, w=w)

        def tr(out_, in_, ident, r, w):
            S.op("pe", lambda e: e.transpose(out=out_, in_=in_, identity=ident), r=r, w=w)

        def act(out_, in_, func, r, w, eng="act", **kw):
            S.op(eng, lambda e: e.activation(out=out_, in_=in_, func=func, **kw), r=r, w=w)

        def tt(eng, out_, in0, in1, op, r, w):
            S.op(eng, lambda e: e.tensor_tensor(out=out_, in0=in0, in1=in1, op=op), r=r, w=w)

        def ts(eng, out_, in0, s1, s2, op0, op1, r, w):
            if s2 is None:
                S.op(eng, lambda e: e.tensor_scalar(out=out_, in0=in0, scalar1=s1, scalar2=None, op0=op0), r=r, w=w)
            else:
                S.op(eng, lambda e: e.tensor_scalar(out=out_, in0=in0, scalar1=s1, scalar2=s2, op0=op0, op1=op1),
                     r=r, w=w)

        def stt(out_, in0, scalar, in1, op0, op1, r, w):
            S.op("dve", lambda e: e.scalar_tensor_tensor(out=out_, in0=in0, scalar=scalar, in1=in1, op0=op0, op1=op1),
                 r=r, w=w)

        def cp(eng, out_, in_, r, w):
            if eng == "act":
                act(out_, in_, AF.Copy, r, w)
            else:
                S.op(eng, lambda e: e.tensor_copy(out=out_, in_=in_), r=r, w=w)

        def rsqrt(col, n, r, w):
            ts("dve", col, col, 1.0 / n, EPS, ALU.mult, ALU.add, r, w)
            act(col, col, AF.Sqrt, w, w)
            S.op("dve", lambda e: e.reciprocal(out=col, in_=col), r=w, w=w)

        cst = top.enter_context(nc.sbuf_tensor("cst", [128, C_END], F32))
        cstb = top.enter_context(nc.sbuf_tensor("cstb", [128, C_END], BF16))
        S.dma("sp", cst[:], I["consts"][:, :], w=["cst"], stream="cst")
        cp("dve", cstb[:], cst[:], ["cst"], ["cstb"])
        ident_f = cst[:, C_IDENT:C_IDENT + 128]
        ident_b = cstb[:, C_IDENT:C_IDENT + 128]
        triu_f = cst[:, C_TRIU:C_TRIU + 128]
        tris_f = cst[:, C_TRIS:C_TRIS + 128]
        ones_f = cst[:, C_ONES:C_ONES + 128]
        ones_b = cstb[:, C_ONES:C_ONES + 128]

        with ExitStack() as ph:
            A = lambda n, sh, dt=F32: ph.enter_context(nc.sbuf_tensor(n, sh, dt))
            cT = A("cT", [128, 8, NB])
            bada = A("bada", [NB, 6 * D])
            g1r = A("g1r", [NB, D])
            g2r = A("g2r", [NB, D])
            wa = [A("wa%d" % i, [128, 8, 512]) for i in range(3)]
            mod2 = A("mod2", [NB, 6 * D])
            pm = [ph.enter_context(nc.psum_tensor("pm%d" % i, [128, 512], F32)) for i in range(2)]
            for b in range(NB):
                S.dma("sp", cT[:, :, b], I["c"][b:b + 1, :].rearrange("o (kc p) -> p (o kc)", p=128), w=["cT"],
                      stream="p0", allow_slow_non_contiguous=True)
            S.dma("sp", bada[:], I["b_ada"][0:1, :].partition_broadcast(NB), w=["bada"], stream="p0b")
            S.dma("sp", g1r[:], I["norm1_g"][0:1, :].partition_broadcast(NB), w=["g1r"], stream="p0c")
            S.dma("sp", g2r[:], I["norm2_g"][0:1, :].partition_broadcast(NB), w=["g2r"], stream="p0d")
            act(cT[:], cT[:], AF.Silu, ["cT"], ["cT"])
            wav = I["w_ada"].rearrange("(kc p) n -> p kc n", p=128)
            for n in range(12):
                wb = wa[n % 3]
                wk = "wa%d" % (n % 3)
                pk = "pm%d" % (n % 2)
                S.dma("sp", wb[:], wav[:, :, n * 512:(n + 1) * 512], w=[wk], stream=wk)
                for kc in range(8):
                    mm(pm[n % 2][0:NB, :], cT[:, kc, :], wb[:, kc, :], kc == 0, kc == 7, ["cT", wk], [pk])
                tt("dve", mod2[:, n * 512:(n + 1) * 512], pm[n % 2][0:NB, :], bada[:, n * 512:(n + 1) * 512],
                   ALU.add, [pk, "bada"], ["mod2"])
            stt(mod2[:, 1024:2048], mod2[:, 1024:2048], 1.0, g1r[:], ALU.add, ALU.mult, ["mod2", "g1r"], ["mod2"])
            stt(mod2[:, 4096:5120], mod2[:, 4096:5120], 1.0, g2r[:], ALU.add, ALU.mult, ["mod2", "g2r"], ["mod2"])
            S.dma("sp", MODs[:, :], mod2[:], r=["mod2"], w=["MODs"], stream="p0s")
            S.flush()
        if stop_after == 0:
            return nc

        with ExitStack() as ph:
            A = lambda n, sh, dt=F32: ph.enter_context(nc.sbuf_tensor(n, sh, dt))
            P = lambda n, sh, dt=F32: ph.enter_context(nc.psum_tensor(n, sh, dt))
            win = A("win", [128, 8, INW], BF16)
            hT = A("hT", [128, 8, SEQ], BF16)
            G1 = A("G1", [128, D])
            SH1 = A("SH1", [128, D])
            xin = [A("xin%d" % i, [128, D]) for i in range(5)]
            t1 = [A("t1_%d" % i, [128, D]) for i in range(3)]
            junk = A("junk", [128, D], BF16)
            hb = [A("hb%d" % i, [128, D], BF16) for i in range(3)]
            ss = A("ss", [128, 16])
            zst = [A("zst%d" % i, [128, D], BF16) for i in range(2)]
            dts = [A("dts%d" % i, [128, 16]) for i in range(2)]
            xpad = [A("xpad%d" % i, [128, 3 + SEQ]) for i in range(2)]
            acc = [A("acc%d" % i, [128, SEQ]) for i in range(2)]
            xo = [A("xo%d" % i, [128, SEQ], BF16) for i in range(2)]
            cw = A("cw", [128, 16, 4])
            cb = A("cb", [128, 16])
            pT = [P("pT%d" % i, [128, 1024], BF16) for i in range(2)]
            pz = P("pz", [128, 1024])
            pdt = P("pdt", [128, 16])
            pc = [P("pc%d" % i, [128, 512]) for i in range(2)]

            winv = I["w_in"].rearrange("(kc p) n -> p kc n", p=128)
            for kc in range(8):
                S.dma("pool", win[:, kc, :], winv[:, kc, :], w=["win%d" % kc], stream="win")
            for k in range(4):
                S.dma("sp", cw[:, :, k], I["conv_w"][k:k + 1, :].rearrange("o (ct p) -> p (o ct)", p=128), w=["cw"],
                      stream="cw", allow_slow_non_contiguous=True)
            S.dma("sp", cb[:], I["conv_b"].rearrange("o (ct p) -> p (o ct)", p=128), w=["cb"], stream="cb",
                  allow_slow_non_contiguous=True)
            for i in range(2):
                S.op("dve", lambda e, i=i: e.memset(xpad[i][:, 0:3], 0.0), w=["xpad%d" % i])

            def tokgen(b, i):
                tok0 = b * SEQ + i * 128
                r3, r2 = i % 3, i % 2
                xk, tk_, hk, pk = "xin%d" % (i % 5), "t1_%d" % r3, "hb%d" % r3, "pT%d" % r2
                xt = xin[i % 5]
                col = ss[:, i:i + 1]
                S.dma("sp", xt[:], I["x"][tok0:tok0 + 128, :], w=[xk], stream=xk)
                yield
                act(junk[:], xt[:], AF.Square, [xk], ["junk", "ss%d" % i], accum_out=col)
                yield
                ts("dve", col, col, 1.0 / D, EPS, ALU.mult, ALU.add, ["ss%d" % i], ["ss%d" % i])
                yield
                act(col, col, AF.Sqrt, ["ss%d" % i], ["ss%d" % i])
                yield
                S.op("dve", lambda e: e.reciprocal(out=col, in_=col), r=["ss%d" % i], w=["ss%d" % i])
                stt(t1[r3][:], xt[:], col, G1[:], ALU.mult, ALU.mult, [xk, "ss%d" % i, "G1"], [tk_])
                yield
                tt("pool", hb[r3][:], t1[r3][:], SH1[:], ALU.add, [tk_, "SH1"], [hk])
                yield
                for kc in range(8):
                    tr(pT[r2][:, kc * 128:(kc + 1) * 128], hb[r3][:, kc * 128:(kc + 1) * 128], ident_b, [hk, "cstb"], [pk])
                yield
                cp("act", hT[:, :, i * 128:(i + 1) * 128], pT[r2][:, :].rearrange("p (k t) -> p k t", k=8), [pk],
                   ["hT%d" % i])
                yield
                for half in range(2):
                    for kc in range(8):
                        mm(pz[:, half * 512:(half + 1) * 512], hT[:, kc, i * 128:(i + 1) * 128],
                           win[:, kc, half * 512:(half + 1) * 512], kc == 0, kc == 7, ["hT%d" % i, "win%d" % kc], ["pz"])
                for kc in range(8):
                    mm(pdt[:, :], hT[:, kc, i * 128:(i + 1) * 128], win[:, kc, 3072:3088], kc == 0, kc == 7,
                       ["hT%d" % i, "win%d" % kc], ["pdt"])
                yield
                zk, dk = "zst%d" % r2, "dts%d" % r2
                cp("act", zst[r2][:], pz[:, :], ["pz"], [zk])
                cp("dve", dts[r2][:], pdt[:, :], ["pdt"], [dk])
                S.dma("sp", Zs[tok0:tok0 + 128, :], zst[r2][:], r=[zk], w=["Zs"], stream=zk)
                S.dma("sp", DTs[tok0:tok0 + 128, :], dts[r2][:], r=[dk], w=["DTs"], stream=dk)
                yield

            def chgen(b, ct):
                col0 = 1024 + ct * 128 if ct < 16 else 3088 + (ct - 16) * 128
                par = ct % 2
                xpk, xok, ak = "xpad%d" % par, "xo%d" % par, "acc%d" % par
                for blk in range(4):
                    pck = "pc%d" % (blk % 2)
                    for kc in range(8):
                        mm(pc[blk % 2][:, :], win[:, kc, col0:col0 + 128], hT[:, kc, blk * 512:(blk + 1) * 512],
                           kc == 0, kc == 7, ["win%d" % kc] + ["hT%d" % j for j in range(blk * 4, blk * 4 + 4)], [pck])
                    if ct < 16:
                        cp("act", xpad[par][:, 3 + blk * 512:3 + (blk + 1) * 512], pc[blk % 2][:, :], [pck], [xpk])
                    else:
                        cp("act", xo[par][:, blk * 512:(blk + 1) * 512], pc[blk % 2][:, :], [pck], [xok])
                    if blk % 2 == 1:
                        yield
                if ct < 16:
                    xp = xpad[par]
                    ts("dve", acc[par][:], xp[:, 3:3 + SEQ], cw[:, ct, 3:4], cb[:, ct:ct + 1], ALU.mult, ALU.add,
                       [xpk, "cw", "cb"], [ak])
                    yield
                    for k in (2, 1, 0):
                        stt(acc[par][:], xp[:, k:k + SEQ], cw[:, ct, k:k + 1], acc[par][:], ALU.mult, ALU.add,
                            [xpk, "cw", ak], [ak])
                        yield
                    act(xo[par][:], acc[par][:], AF.Silu, [ak], [xok])
                    yield
                    S.dma("sp", XCs[ct * 128:(ct + 1) * 128, b * SEQ:(b + 1) * SEQ], xo[par][:], r=[xok], w=["XCs"],
                          stream=xok)
                else:
                    S.dma("sp", UTs[(ct - 16) * 128:(ct - 15) * 128, b * SEQ:(b + 1) * SEQ], xo[par][:], r=[xok],
                          w=["UTs"], stream=xok)
                yield

            def run_skewed(gens, every):
                active = []
                k = 0
                while gens or active:
                    if gens and k % every == 0:
                        active.append(gens.pop(0))
                    for g_ in list(active):
                        try:
                            next(g_)
                        except StopIteration:
                            active.remove(g_)
                    k += 1

            for b in range(NB):
                S.dma("sp", G1[:], MODs[b:b + 1, 1024:2048].partition_broadcast(128), r=["MODs"], w=["G1"], stream="g1")
                S.dma("sp", SH1[:], MODs[b:b + 1, 0:1024].partition_broadcast(128), r=["MODs"], w=["SH1"], stream="sh1")
                run_skewed([tokgen(b, i) for i in range(16)], 1)
                run_skewed([chgen(b, ct) for ct in range(24)], 4)
            S.flush()
        if stop_after == 1:
            return nc

        with ExitStack() as ph:
            A = lambda n, sh, dt=F32: ph.enter_context(nc.sbuf_tensor("B_" + n, sh, dt))
            P = lambda n, sh, dt=F32: ph.enter_context(nc.psum_tensor("B_" + n, sh, dt))
            dtb = A("dtb", [128, 16]); abc = A("abc", [128, 16]); d16 = A("d16", [128, 16])
            dssd = A("dssd", [128, D]); normg = A("normg", [128, D])
            S32 = A("S32", [128, 16, 64]); Sbf = A("Sbf", [128, 16, 64], BF16)
            RC = 4
            xct = [A("xct%d" % i, [128, 16, 128], BF16) for i in range(RC)]
            zt = [A("zt%d" % i, [128, D], BF16) for i in range(RC)]
            dtr = [A("dtr%d" % i, [128, 16]) for i in range(RC)]
            dtt = [A("dtt%d" % i, [128, 16]) for i in range(RC)]
            da = [A("da%d" % i, [128, 16]) for i in range(RC)]
            c3 = [A("c3_%d" % i, [128, 48]) for i in range(RC)]
            e3 = [A("e3_%d" % i, [128, 48]) for i in range(RC)]
            xs = [A("xs%d" % i, [128, D], BF16) for i in range(RC)]
            Btok = [A("Btok%d" % i, [128, 512], BF16) for i in range(RC)]
            xdt = [A("xdt%d" % i, [128, D], BF16) for i in range(RC)]
            xdd = [A("xdd%d" % i, [128, D], BF16) for i in range(RC)]
            CBm = [A("CBm%d" % i, [128, 4, 128]) for i in range(RC)]
            RH = 3
            lD = [A("lD%d" % i, [128, 128]) for i in range(RH)]
            Lm = [A("Lm%d" % i, [128, 128]) for i in range(RH)]
            Mt = [A("Mt%d" % i, [128, 128], BF16) for i in range(RH)]
            yo = A("yo", [128, D]); y1 = A("y1", [128, D]); xD = A("xD", [128, D]); sz = A("sz", [128, D])
            yg = A("yg", [128, D]); junkb = A("junkb", [128, 256], BF16); ssq = A("ssq", [128, 4])
            yn = A("yn", [128, D]); ynb = A("ynb", [128, D], BF16)
            ynT = [A("ynT%d" % i, [128, 8, 128], BF16) for i in range(2)]
            pTr = P("pTr", [128, 1024], BF16)
            pDs = [P("pD%d" % i, [128, 512]) for i in range(2)]
            pSl = [P("pS%d" % i, [128, 512]) for i in range(2)]
            pPro = P("pPro", [128, 512])
            pY = P("pY", [128, 512])
            pYo = P("pYo", [128, 512])

            S.dma("sp", dtb[:], I["dt_bias"][0:1, :].partition_broadcast(128), w=["dtb"], stream="b0")
            S.dma("sp", abc[:], I["a_log"][0:1, :].partition_broadcast(128), w=["abc"], stream="b1")
            S.dma("sp", d16[:], I["d_ssd"][0:1, :].partition_broadcast(128), w=["d16"], stream="b2")
            S.dma("sp", normg[:], I["norm_ssd_g"][0:1, :].partition_broadcast(128), w=["normg"], stream="b3")
            act(abc[:], abc[:], AF.Exp, ["abc"], ["abc"])
            ts("dve", abc[:], abc[:], -1.0, None, ALU.mult, None, ["abc"], ["abc"])
            cp("dve", dssd[:, :].rearrange("p (h q) -> p h q", q=64), d16[:, :].unsqueeze(2).to_broadcast([128, 16, 64]),
               ["d16"], ["dssd"])
            XCv = XCs.rearrange("(ct p) t -> p ct t", p=128)
            YCv = YCs.rearrange("(ct p) t -> p ct t", p=128)
            v3 = lambda ap: ap.rearrange("p (h q) -> p h q", q=64)
            bc3 = lambda col: col.unsqueeze(2).to_broadcast([128, 16, 64])
            NCH = NB * 16

            def prologue(ci):
                r_ = ci % RC
                tok0 = ci * 128
                K_ = lambda nm: "%s%d" % (nm, r_)
                X = xct[r_]
                S.dma("sp", X[:], XCv[:, :, tok0:tok0 + 128], r=["XCs"], w=[K_("xct")], stream=K_("xct"))
                S.dma("sp", zt[r_][:], Zs[tok0:tok0 + 128, :], r=["Zs"], w=[K_("zt")], stream=K_("zt"))
                S.dma("sp", dtr[r_][:], DTs[tok0:tok0 + 128, :], r=["DTs"], w=[K_("dtr")], stream=K_("dtr"))
                yield
                tt("dve", dtt[r_][:], dtr[r_][:], dtb[:], ALU.add, [K_("dtr"), "dtb"], [K_("dtt")])
                yield
                act(dtt[r_][:], dtt[r_][:], AF.Exp, [K_("dtt")], [K_("dtt")])
                act(dtt[r_][:], dtt[r_][:], AF.Ln, [K_("dtt")], [K_("dtt")], bias=1.0)
                yield
                tt("dve", da[r_][:], dtt[r_][:], abc[:], ALU.mult, [K_("dtt"), "abc"], [K_("da")])
                yield
                mm(pPro[:, 0:16], triu_f, da[r_][:], True, True, ["cst", K_("da")], ["pm_cs"])
                mm(pPro[:, 16:32], ones_f, da[r_][:], True, True, ["cst", K_("da")], ["pm_cs"])
                cp("dve", c3[r_][:, 0:32], pPro[:, 0:32], ["pm_cs"], [K_("c3")])
                tt("dve", c3[r_][:, 32:48], c3[r_][:, 16:32], c3[r_][:, 0:16], ALU.subtract, [K_("c3")], [K_("c3")])
                yield
                act(e3[r_][:], c3[r_][:], AF.Exp, [K_("c3")], [K_("e3")])
                yield
                for j in range(8):
                    tr(pTr[:, j * 128:(j + 1) * 128], X[:, j, :], ident_b, [K_("xct"), "cstb"], ["pTr"])
                cp("act", xs[r_][:], pTr[:, :], ["pTr"], [K_("xs")])
                yield
                for g in range(4):
                    tr(pTr[:, g * 128:(g + 1) * 128], X[:, 8 + g, :], ident_b, [K_("xct"), "cstb"], ["pTr"])
                cp("act", Btok[r_][:], pTr[:, 0:512], ["pTr"], [K_("Btok")])
                yield
                tt("dve", v3(xdt[r_][:, :]), v3(xs[r_][:, :]), bc3(dtt[r_][:, :]), ALU.mult, [K_("xs"), K_("dtt")],
                   [K_("xdt")])
                tt("dve", v3(xdd[r_][:, :]), v3(xdt[r_][:, :]), bc3(e3[r_][:, 32:48]), ALU.mult, [K_("xdt"), K_("e3")],
                   [K_("xdd")])
                yield
                for g in range(4):
                    mm(pPro[:, 128:256], X[:, 8 + g, :], X[:, 12 + g, :], True, True, [K_("xct")], ["pm_cb"])
                    tt("dve", CBm[r_][:, g, :], pPro[:, 128:256], triu_f, ALU.mult, ["pm_cb", "cst"], [K_("CBm") + "_%d" % g])
                    yield

            def epilogue(ci):
                r_ = ci % RC
                tok0 = ci * 128
                c = ci % 16
                K_ = lambda nm: "%s%d" % (nm, r_)
                yield
                tt("pool", xD[:], xs[r_][:], dssd[:], ALU.mult, [K_("xs"), "dssd"], ["xD"])
                yield
                tt("dve", y1[:], y1[:], xD[:], ALU.add, ["y1", "xD"], ["y1"])
                act(sz[:], zt[r_][:], AF.Silu, [K_("zt")], ["sz"])
                yield
                tt("dve", yg[:], y1[:], sz[:], ALU.mult, ["y1", "sz"], ["yg"])
                yield
                for G_ in range(4):
                    act(junkb[:], yg[:, G_ * 256:(G_ + 1) * 256], AF.Square, ["yg"], ["junkb", "ssq"],
                        accum_out=ssq[:, G_:G_ + 1])
                yield
                ts("dve", ssq[:], ssq[:], 1.0 / 256, EPS, ALU.mult, ALU.add, ["ssq"], ["ssq"])
                yield
                act(ssq[:], ssq[:], AF.Sqrt, ["ssq"], ["ssq"])
                yield
                S.op("dve", lambda e: e.reciprocal(out=ssq[:], in_=ssq[:]), r=["ssq"], w=["ssq"])
                tt("dve", yn[:, :].rearrange("p (g q) -> p g q", q=256), yg[:, :].rearrange("p (g q) -> p g q", q=256),
                   ssq[:, :].unsqueeze(2).to_broadcast([128, 4, 256]), ALU.mult, ["yg", "ssq"], ["yn"])
                yield
                tt("pool", ynb[:], yn[:], normg[:], ALU.mult, ["yn", "normg"], ["ynb"])
                yield
                for j in range(8):
                    tr(pTr[:, j * 128:(j + 1) * 128], ynb[:, j * 128:(j + 1) * 128], ident_b, ["ynb", "cstb"], ["pTr"])
                yk = "ynT%d" % (ci % 2)
                cp("act", ynT[ci % 2][:, :, :], pTr[:, :].rearrange("p (k t) -> p k t", k=8), ["pTr"], [yk])
                S.dma("sp", YCv[:, 0:8, tok0:tok0 + 128], ynT[ci % 2][:, :, :], r=[yk], w=["YCs"], stream=yk)
                yield

            def e1_half(ci, hf):
                r_ = ci % RC
                cs_ = slice(hf * 512, (hf + 1) * 512)
                v3h = lambda ap: ap.rearrange("p (h q) -> p h q", q=64)
                if ci % 16 == 0:
                    cp("dve", y1[:, cs_], pY[:, :], ["pY"], ["y1"])
                else:
                    tt("dve", v3h(yo[:, cs_]), v3h(pYo[:, :]),
                       e3[r_][:, hf * 8:hf * 8 + 8].unsqueeze(2).to_broadcast([128, 8, 64]), ALU.mult,
                       ["pYo", "e3_%d" % r_], ["yo"])
                    tt("dve", y1[:, cs_], yo[:, cs_], pY[:, :], ALU.add, ["yo", "pY"], ["y1"])

            def hinfo(n):
                ci, h = n // 16, n % 16
                return ci, h, h // 4, ci % RC, n % RH, ci % 16

            def h_a(n):
                ci, h, g, r_, hr, c = hinfo(n)
                tt("pool", lD[hr][:], tris_f, da[r_][:, h:h + 1].to_broadcast([128, 128]), ALU.mult,
                   ["cst", "da%d" % r_], ["lD%d" % hr])

            def h_b(n):
                ci, h, g, r_, hr, c = hinfo(n)
                mm(pDs[n % 2][:, 0:128], lD[hr][:], triu_f, True, True, ["lD%d" % hr, "cst"], ["pD%d" % (n % 2)])

            def h_c(n):
                ci, h, g, r_, hr, c = hinfo(n)
                act(Lm[hr][:], pDs[n % 2][:, 0:128], AF.Exp, ["pD%d" % (n % 2)], ["Lm%d" % hr])

            def h_d(n):
                ci, h, g, r_, hr, c = hinfo(n)
                tt("dve", Mt[hr][:], Lm[hr][:], CBm[r_][:, g, :], ALU.mult, ["Lm%d" % hr, "CBm%d_%d" % (r_, g)],
                   ["Mt%d" % hr])

            def h_e(n):
                ci, h, g, r_, hr, c = hinfo(n)
                X = xct[r_]
                mm(pY[:, (h % 8) * 64:(h % 8 + 1) * 64], Mt[hr][:], xdt[r_][:, h * 64:(h + 1) * 64], True, True,
                   ["Mt%d" % hr, "xdt%d" % r_], ["pY"])
                if c != 0:
                    mm(pYo[:, (h % 8) * 64:(h % 8 + 1) * 64], X[:, 12 + g, :], Sbf[:, h, :], True, True,
                       ["xct%d" % r_, "Sbf%d" % h], ["pYo"])
                mm(pSl[n % 2][:, 0:64], Btok[r_][:, g * 128:(g + 1) * 128], xdd[r_][:, h * 64:(h + 1) * 64],
                   True, True, ["Btok%d" % r_, "xdd%d" % r_], ["pS%d" % (n % 2)])

            def h_f(n):
                ci, h, g, r_, hr, c = hinfo(n)
                if c == 0:
                    cp("dve", S32[:, h, :], pSl[n % 2][:, 0:64], ["pS%d" % (n % 2)], ["S32_%d" % h])
                else:
                    stt(S32[:, h, :], S32[:, h, :], e3[r_][:, 16 + h:17 + h], pSl[n % 2][:, 0:64],
                        ALU.mult, ALU.add, ["S32_%d" % h, "e3_%d" % r_, "pS%d" % (n % 2)], ["S32_%d" % h])

            def h_g(n):
                ci, h, g, r_, hr, c = hinfo(n)
                cp("pool", Sbf[:, h, :], S32[:, h, :], ["S32_%d" % h], ["Sbf%d" % h])

            stages = [h_a, h_b, h_c, h_d, h_e, h_f, h_g]
            NS = len(stages)
            for _ in prologue(0):
                pass
            for _ in prologue(1):
                pass
            side = []
            NI_ = NCH * 16
            for k in range(NI_ + NS - 1):
                for s_ in reversed(range(NS)):
                    n = k - s_
                    if 0 <= n < NI_:
                        stages[s_](n)
                if k >= 11 and (k - 11) % 16 == 0:
                    e1_half((k - 11) // 16, 0)
                if k >= 19 and (k - 19) % 16 == 0:
                    e1_half((k - 19) // 16, 1)
                    side.append(epilogue((k - 19) // 16))
                if k % 16 == 0 and k // 16 + 2 < NCH:
                    side.append(prologue(k // 16 + 2))
                for g_ in list(side):
                    try:
                        next(g_)
                    except StopIteration:
                        side.remove(g_)
            while side:
                for g_ in list(side):
                    try:
                        next(g_)
                    except StopIteration:
                        side.remove(g_)
            S.flush()
        if stop_after == 2:
            return nc

        with ExitStack() as ph:
            A = lambda n, sh, dt=F32: ph.enter_context(nc.sbuf_tensor(n, sh, dt))
            Blk1 = A("Blk1", [128, 64, 128], BF16)
            Blk2 = A("Blk2", [128, 64, 128], BF16)
            CL1 = A("CL1", [128, 64, 16], BF16)
            CL2 = A("CL2", [128, 64, 16], BF16)
            rcol = A("rcol", [128, 64])
            fcol = A("fcol", [128, 64])
            D5c = A("D5c", [128, 8])
            glub = A("glub", [128, 8])
            g5c = A("g5c", [128, 8])
            Wg = A("Wg", [128, 8, 128], BF16)
            rm8 = cst[:, C_RM8:C_RM8 + 8]
            INV2PI = 1.0 / TWO_PI

            def sin_turns(out_, f_ap, tk, tf, keys_in, kout, e1="dve", e2="dve"):
                ts(e1, tk, f_ap, MAGIC, MAGIC, ALU.add, ALU.subtract, keys_in, ["_tk"])
                tt(e2, tf, f_ap, tk, ALU.subtract, keys_in + ["_tk"], ["_tf"])
                act(out_, tf, AF.Sin, ["_tf"], [kout], scale=TWO_PI)

            with ExitStack() as p0:
                B_ = lambda n, sh, dt=F32: p0.enter_context(nc.sbuf_tensor(n, sh, dt))
                lrT = B_("lrT", [128, 64]); liT = B_("liT", [128, 64]); dtg = B_("dtg", [128, 64])
                ldt = B_("ldt", [128, 64]); f2 = B_("f2", [128, 64]); tk = B_("tk", [128, 64]); tf = B_("tf", [128, 64])
                sn = B_("sn", [128, 64]); cs_ = B_("cs_", [128, 64]); lbr = B_("lbr", [128, 64]); lbi = B_("lbi", [128, 64])
                den = B_("den", [128, 64]); t_a = B_("t_a", [128, 64]); t_b = B_("t_b", [128, 64])
                cre = B_("cre", [128, 64]); cim = B_("cim", [128, 64])
                br = B_("br", [64, 64, 16]); bi = B_("bi", [64, 64, 16])
                bbr = B_("bbr", [64, 64, 16]); bbi = B_("bbi", [64, 64, 16]); t_c = B_("t_c", [64, 64, 16])
                Dre = B_("Dre", [128, 8, 64]); Dim = B_("Dim", [128, 8, 64]); nDre = B_("nDre", [128, 8, 64])
                cc1 = B_("cc1", [128, 128]); cc2 = B_("cc2", [128, 128]); wrow = B_("wrow", [128, 8, 16])
                ptc = p0.enter_context(nc.psum_tensor("ptc", [128, 128], F32))
                for half in range(2):
                    S.dma("sp", lrT[half * 64:(half + 1) * 64, :], I["s5_a_re"].rearrange("g p -> p g"), w=["lrT"],
                          stream="c0a", allow_slow_non_contiguous=True)
                    S.dma("sp", liT[half * 64:(half + 1) * 64, :], I["s5_a_im"].rearrange("g p -> p g"), w=["liT"],
                          stream="c0b", allow_slow_non_contiguous=True)
                S.dma("sp", dtg[:], I["s5_log_dt"][0:1, :].partition_broadcast(128), w=["dtg"], stream="c0c")
                act(dtg[:], dtg[:], AF.Exp, ["dtg"], ["dtg"])
                tt("dve", ldt[:], lrT[:], dtg[:], ALU.mult, ["lrT", "dtg"], ["ldt"])
                act(rcol[:], ldt[:], AF.Exp, ["ldt"], ["rcol"])
                tt("dve", fcol[:], liT[:], dtg[:], ALU.mult, ["liT", "dtg"], ["fcol"])
                ts("dve", fcol[:], fcol[:], INV2PI, None, ALU.mult, None, ["fcol"], ["fcol"])
                sin_turns(sn[:], fcol[:], tk[:], tf[:], ["fcol"], "sn")
                ts("dve", f2[:], fcol[:], 0.25, None, ALU.add, None, ["fcol"], ["f2"])
                sin_turns(cs_[:], f2[:], tk[:], tf[:], ["f2"], "cs_")
                tt("dve", lbr[:], rcol[:], cs_[:], ALU.mult, ["rcol", "cs_"], ["lbr"])
                tt("dve", lbi[:], rcol[:], sn[:], ALU.mult, ["rcol", "sn"], ["lbi"])
                ts("dve", lbr[:], lbr[:], -1.0, None, ALU.add, None, ["lbr"], ["lbr"])
                tt("dve", den[:], lrT[:], lrT[:], ALU.mult, ["lrT"], ["den"])
                tt("dve", t_a[:], liT[:], liT[:], ALU.mult, ["liT"], ["t_a"])
                tt("dve", den[:], den[:], t_a[:], ALU.add, ["den", "t_a"], ["den"])
                S.op("dve", lambda e: e.reciprocal(out=den[:], in_=den[:]), r=["den"], w=["den"])
                tt("dve", t_a[:], lbr[:], lrT[:], ALU.mult, ["lbr", "lrT"], ["t_a"])
                tt("dve", t_b[:], lbi[:], liT[:], ALU.mult, ["lbi", "liT"], ["t_b"])
                tt("dve", t_a[:], t_a[:], t_b[:], ALU.add, ["t_a", "t_b"], ["t_a"])
                tt("dve", cre[:], t_a[:], den[:], ALU.mult, ["t_a", "den"], ["cre"])
                tt("dve", t_a[:], lbi[:], lrT[:], ALU.mult, ["lbi", "lrT"], ["t_a"])
                tt("dve", t_b[:], lbr[:], liT[:], ALU.mult, ["lbr", "liT"], ["t_b"])
                tt("dve", t_a[:], t_a[:], t_b[:], ALU.subtract, ["t_a", "t_b"], ["t_a"])
                tt("dve", cim[:], t_a[:], den[:], ALU.mult, ["t_a", "den"], ["cim"])
                for q4 in range(4):
                    gs = slice(q4 * 16, (q4 + 1) * 16)
                    S.dma("sp", br[:, gs, :], I["s5_b_re"][gs].rearrange("g p h -> p g h"), w=["br"], stream="c0d")
                    S.dma("sp", bi[:, gs, :], I["s5_b_im"][gs].rearrange("g p h -> p g h"), w=["bi"], stream="c0e")
                bcr = cre[0:64, :].unsqueeze(2).to_broadcast([64, 64, 16])
                bci = cim[0:64, :].unsqueeze(2).to_broadcast([64, 64, 16])
                tt("dve", bbr[:], br[:], bcr, ALU.mult, ["br", "cre"], ["bbr"])
                tt("dve", t_c[:], bi[:], bci, ALU.mult, ["bi", "cim"], ["t_c"])
                tt("dve", bbr[:], bbr[:], t_c[:], ALU.subtract, ["bbr", "t_c"], ["bbr"])
                tt("dve", bbi[:], bi[:], bcr, ALU.mult, ["bi", "cre"], ["bbi"])
                tt("dve", t_c[:], br[:], bci, ALU.mult, ["br", "cim"], ["t_c"])
                tt("dve", bbi[:], bbi[:], t_c[:], ALU.add, ["bbi", "t_c"], ["bbi"])
                for j in range(8):
                    tr(ptc[:, 0:64], bbr[:, j * 8:(j + 1) * 8, :].rearrange("p g h -> p (g h)"), ident_f[0:64, 0:64], ["bbr", "cst"], ["ptc"])
                    cp("dve", Dre[:, j, :], ptc[:, 0:64], ["ptc"], ["Dre"])
                    tr(ptc[:, 64:128], bbi[:, j * 8:(j + 1) * 8, :].rearrange("p g h -> p (g h)"), ident_f[0:64, 0:64], ["bbi", "cst"], ["ptc2"])
                    cp("dve", Dim[:, j, :], ptc[:, 64:128], ["ptc2"], ["Dim"])
                ts("dve", nDre[:], Dre[:], -1.0, None, ALU.mult, None, ["Dre"], ["nDre"])
                rmb = rm8.unsqueeze(2).to_broadcast([128, 8, 64])
                for j in range(8):
                    gs = slice(j * 8, (j + 1) * 8)
                    bcD = lambda t: t[:, j, :].unsqueeze(1).to_broadcast([128, 8, 64])
                    tt("dve", Blk1[:, gs, 0:64], bcD(Dre), rmb, ALU.mult, ["Dre", "cst"], ["Blk1"])
                    tt("dve", Blk1[:, gs, 64:128], bcD(Dim), rmb, ALU.mult, ["Dim", "cst"], ["Blk1"])
                    tt("dve", Blk2[:, gs, 0:64], bcD(Dim), rmb, ALU.mult, ["Dim", "cst"], ["Blk2"])
                    tt("dve", Blk2[:, gs, 64:128], bcD(nDre), rmb, ALU.mult, ["nDre", "cst"], ["Blk2"])
                crv = I["s5_c_re"].rearrange("g h p -> (g h) p")
                civ = I["s5_c_im"].rearrange("g h p -> (g h) p")
                for j in range(8):
                    rs_ = slice(j * 128, (j + 1) * 128)
                    S.dma("sp", cc1[:, 0:64], crv[rs_, :], w=["cc1"], stream="c0f")
                    S.dma("sp", cc1[:, 64:128], civ[rs_, :], w=["cc1"], stream="c0f")
                    S.dma("sp", cc2[:, 0:64], civ[rs_, :], w=["cc2"], stream="c0g")
                    S.dma("sp", cc2[:, 64:128], crv[rs_, :], w=["cc2"], stream="c0g")
                    tr(ptc[:, :], cc1[:], ident_f, ["cc1", "cst"], ["ptc", "ptc2"])
                    gs = slice(j * 8, (j + 1) * 8)
                    cp("dve", CL1[0:64, gs, :], ptc[0:64, :].rearrange("p (g h) -> p g h", h=16), ["ptc"], ["CL1"])
                    ts("dve", CL1[64:128, gs, :], ptc[64:128, :].rearrange("p (g h) -> p g h", h=16), -1.0, None,
                       ALU.mult, None, ["ptc"], ["CL1"])
                    tr(ptc[:, :], cc2[:], ident_f, ["cc2", "cst"], ["ptc", "ptc2"])
                    ts("dve", CL2[:, gs, :], ptc[:, :].rearrange("p (g h) -> p g h", h=16), -1.0, None, ALU.mult, None,
                       ["ptc"], ["CL2"])
                S.dma("sp", D5c[:], I["s5_d"].rearrange("(j gl) h -> (gl h) j", gl=8), w=["D5c"], stream="c0h",
                      allow_slow_non_contiguous=True)
                S.dma("sp", glub[:], I["glu_b"].rearrange("(j gl) h -> (gl h) j", gl=8), w=["glub"], stream="c0i",
                      allow_slow_non_contiguous=True)
                S.dma("sp", g5c[:], I["norm_s5_g"].rearrange("o (j p) -> p (o j)", p=128), w=["g5c"], stream="c0j",
                      allow_slow_non_contiguous=True)
                S.dma("sp", wrow[:], I["glu_w"].rearrange("(j gl) h k -> (gl h) j k", gl=8), w=["wrow"], stream="c0k")
                for j in range(8):
                    tt("dve", Wg[:, j, :].rearrange("p (g k) -> p g k", k=16),
                       wrow[:, j, :].unsqueeze(1).to_broadcast([128, 8, 16]),
                       rm8.unsqueeze(2).to_broadcast([128, 8, 16]), ALU.mult, ["wrow", "cst"], ["Wg"])
                S.flush()

            with ExitStack() as p1:
                B_ = lambda n, sh, dt=F32: p1.enter_context(nc.sbuf_tensor(n, sh, dt))
                Pp = lambda n, sh, dt=F32: p1.enter_context(nc.psum_tensor(n, sh, dt))
                iot = B_("iot", [128, SEQ])
                SIN = [B_("SIN%d" % i, [128, SEQ], BF16) for i in range(3)]
                COS = [B_("COS%d" % i, [128, SEQ], BF16) for i in range(3)]
                u1 = B_("u1", [128, SEQ]); k1 = B_("k1", [128, SEQ]); fr = B_("fr", [128, SEQ])
                uT = [B_("uT%d" % i, [128, T], BF16) for i in range(2)]
                R3 = 3
                p1b = [B_("p1b%d" % i, [128, 512], BF16) for i in range(R3)]
                p2b = [B_("p2b%d" % i, [128, 512], BF16) for i in range(R3)]
                w1 = [B_("w1_%d" % i, [128, 512], BF16) for i in range(R3)]
                w2 = [B_("w2_%d" % i, [128, 512], BF16) for i in range(R3)]
                ww = [B_("ww%d" % i, [128, 512]) for i in range(R3)]
                zz = [[B_("zz%d_%d" % (bb, i), [128, 512]) for i in range(R3)] for bb in range(NB)]
                zb = [B_("zb%d" % i, [128, 512], BF16) for i in range(R3)]
                v1 = [B_("v1_%d" % i, [128, 512], BF16) for i in range(R3)]
                v2 = [B_("v2_%d" % i, [128, 512], BF16) for i in range(R3)]
                ysm = [B_("ysm%d" % i, [16, 512]) for i in range(R3)]
                P1 = [Pp("P1_%d" % i, [128, 512]) for i in range(2)]
                P2 = [Pp("P2_%d" % i, [128, 512]) for i in range(2)]
                uf = [B_("uf%d" % i, [128, D]) for i in range(2)]
                vf = [B_("vf%d" % i, [128, D]) for i in range(2)]
                ub = [B_("ub%d" % i, [128, D], BF16) for i in range(2)]
                vbt = [B_("vbt%d" % i, [128, D], BF16) for i in range(2)]
                uts = [B_("uts%d" % i, [128, D], BF16) for i in range(2)]
                pTu = [Pp("pTu%d" % i, [128, 1024], BF16) for i in range(2)]

                def m0_tile(et):
                    pr = et % 2
                    rows = slice(et * 128, (et + 1) * 128)
                    S.dma("sp", uf[pr][:], I["expert_u"][rows, :], w=["uf%d" % pr], stream="uf%d" % pr)
                    S.dma("sp", vf[pr][:], I["expert_v"][rows, :], w=["vf%d" % pr], stream="vf%d" % pr)
                    yield
                    cp("act", ub[pr][:], uf[pr][:], ["uf%d" % pr], ["ub%d" % pr])
                    cp("act", vbt[pr][:], vf[pr][:], ["vf%d" % pr], ["vbt%d" % pr])
                    yield
                    for kc in range(8):
                        tr(pTu[pr][:, kc * 128:(kc + 1) * 128], ub[pr][:, kc * 128:(kc + 1) * 128], ident_b,
                           ["ub%d" % pr, "cstb"], ["pTu%d" % pr])
                    S.dma("sp", Vb[rows, :], vbt[pr][:], r=["vbt%d" % pr], w=["Vb"], stream="vbo%d" % pr)
                    yield
                    cp("act", uts[pr][:], pTu[pr][:, :], ["pTu%d" % pr], ["uts%d" % pr])
                    yield
                    S.dma("sp", UTb[et], uts[pr][:], r=["uts%d" % pr], w=["UTb"], stream="uto%d" % pr)
                    yield
                PY = [Pp("PY%d" % i, [128, 512]) for i in range(2)]
                for k in range(16):
                    ts("dve", iot[:, k * 128:(k + 1) * 128], cst[:, C_IOTA:C_IOTA + 128], float(128 * k), None, ALU.add,
                       None, ["cst"], ["iot"])

                def tables(g):
                    gp = g % 3
                    fg = fcol[:, g:g + 1]
                    for (tab, key, off) in ((SIN[gp], "SIN%d" % gp, 0.0), (COS[gp], "COS%d" % gp, 0.25)):
                        act(u1[:], iot[:], AF.Identity, ["iot", "fcol"], ["u1"], scale=fg, bias=off)
                        yield
                        act(k1[:], u1[:], AF.Identity, ["u1"], ["k1"], scale=1.0, bias=MAGIC)
                        yield
                        stt(fr[:], k1[:], MAGIC, u1[:], ALU.subtract, ALU.subtract, ["u1", "k1"], ["fr"])
                        yield
                        act(tab[:], fr[:], AF.Sin, ["fr"], [key], scale=-TWO_PI)
                        yield

                pieces = [(g, q, bb) for g in range(64) for q in range(4) for bb in range(NB)]

                def info(n):
                    g, q, bb = pieces[n]
                    return g, q, bb, g // 8, g % 3, n % R3, q * 512, bb * SEQ + q * 512

                def st_a(n):
                    g, q, bb, j, gp, pb, t0, tok = info(n)
                    if q == 0 and bb == 0 and g + 1 < 64:
                        side.append(tables(g + 1))
                    if g % 8 == 0 and q == 0 and bb == 0:
                        S.dma("sp", uT[j % 2][:], UTs[j * 128:(j + 1) * 128, :], r=["UTs"], w=["uT%d" % (j % 2)],
                              stream="uT%d" % (j % 2))
                    uk = "uT%d" % (j % 2)
                    mm(P1[n % 2][:, :], Blk1[:, g, :], uT[j % 2][:, tok:tok + 512], True, True, ["Blk1", uk], ["P1_%d" % (n % 2)])
                    mm(P2[n % 2][:, :], Blk2[:, g, :], uT[j % 2][:, tok:tok + 512], True, True, ["Blk2", uk], ["P2_%d" % (n % 2)])

                def st_b(n):
                    g, q, bb, j, gp, pb, t0, tok = info(n)
                    cp("act", p1b[pb][:], P1[n % 2][:, :], ["P1_%d" % (n % 2)], ["p1b%d" % pb])
                    cp("act", p2b[pb][:], P2[n % 2][:, :], ["P2_%d" % (n % 2)], ["p2b%d" % pb])

                def st_c(n):
                    g, q, bb, j, gp, pb, t0, tok = info(n)
                    tt("dve", w1[pb][:], p1b[pb][:], COS[gp][:, t0:t0 + 512], ALU.mult, ["p1b%d" % pb, "COS%d" % gp],
                       ["w1_%d" % pb])
                    tt("dve", w2[pb][:], p2b[pb][:], SIN[gp][:, t0:t0 + 512], ALU.mult, ["p2b%d" % pb, "SIN%d" % gp],
                       ["w2_%d" % pb])

                def st_d(n):
                    g, q, bb, j, gp, pb, t0, tok = info(n)
                    tt("pool", ww[pb][:], w1[pb][:], w2[pb][:], ALU.add, ["w1_%d" % pb, "w2_%d" % pb], ["ww%d" % pb])

                def st_e(n):
                    g, q, bb, j, gp, pb, t0, tok = info(n)
                    zc, zp = zz[bb][q % R3], zz[bb][(q - 1) % R3]
                    zck, zpk = "zz%d_%d" % (bb, q % R3), "zz%d_%d" % (bb, (q - 1) % R3)
                    init = 0.0 if q == 0 else zp[:, 511:512]
                    S.op("dve", lambda e: e.tensor_tensor_scan(
                        out=zc[:], data0=rcol[:, g:g + 1].to_broadcast([128, 512]), data1=ww[pb][:],
                        initial=init, op0=ALU.mult, op1=ALU.add), r=["rcol", "ww%d" % pb, zpk], w=[zck])

                def st_f(n):
                    g, q, bb, j, gp, pb, t0, tok = info(n)
                    cp("act", zb[pb][:], zz[bb][q % R3][:], ["zz%d_%d" % (bb, q % R3)], ["zb%d" % pb])

                def st_g(n):
                    g, q, bb, j, gp, pb, t0, tok = info(n)
                    tt("dve", v1[pb][:], zb[pb][:], COS[gp][:, t0:t0 + 512], ALU.mult, ["zb%d" % pb, "COS%d" % gp],
                       ["v1_%d" % pb])
                    tt("pool", v2[pb][:], zb[pb][:], SIN[gp][:, t0:t0 + 512], ALU.mult, ["zb%d" % pb, "SIN%d" % gp],
                       ["v2_%d" % pb])

                def st_h(n):
                    g, q, bb, j, gp, pb, t0, tok = info(n)
                    pp = n % 2
                    mm(PY[pp][0:16, :], CL1[:, g, :], v1[pb][:], True, False, ["CL1", "v1_%d" % pb], ["PY%d" % pp])
                    mm(PY[pp][0:16, :], CL2[:, g, :], v2[pb][:], False, True, ["CL2", "v2_%d" % pb], ["PY%d" % pp])

                def st_i(n):
                    g, q, bb, j, gp, pb, t0, tok = info(n)
                    pp = n % 2
                    yk = "ysm%d" % pb
                    cp("act", ysm[pb][0:16, :], PY[pp][0:16, :], ["PY%d" % pp], [yk])
                    S.dma("sp", Y5s[g * 16:(g + 1) * 16, tok:tok + 512], ysm[pb][0:16, :], r=[yk], w=["Y5s"], stream=yk)

                for _ in tables(0):
                    pass
                stages_c = [st_a, st_b, st_c, st_d, st_e, st_f, st_g, st_h, st_i]
                side = []
                nxt_et = 0
                for k in range(len(pieces) + len(stages_c) - 1):
                    for s_ in reversed(range(len(stages_c))):
                        n = k - s_
                        if 0 <= n < len(pieces):
                            stages_c[s_](n)
                    if k % 4 == 0 and nxt_et < 128:
                        side.append(m0_tile(nxt_et))
                        nxt_et += 1
                    for g_ in list(side):
                        try:
                            next(g_)
                        except StopIteration:
                            side.remove(g_)
                while side or nxt_et < 128:
                    if nxt_et < 128:
                        side.append(m0_tile(nxt_et))
                        nxt_et += 1
                    for g_ in list(side):
                        try:
                            next(g_)
                        except StopIteration:
                            side.remove(g_)
                S.flush()

            with ExitStack() as p2:
                B_ = lambda n, sh, dt=F32: p2.enter_context(nc.sbuf_tensor("C2_" + n, sh, dt))
                Pp = lambda n, sh, dt=F32: p2.enter_context(nc.psum_tensor("C2_" + n, sh, dt))
                y5 = [B_("y5_%d" % i, [128, 512]) for i in range(3)]
                uu = [B_("uu%d" % i, [128, 512], BF16) for i in range(3)]
                yv = [B_("yv%d" % i, [128, 512]) for i in range(3)]
                vb = [B_("vb%d" % i, [128, 512], BF16) for i in range(3)]
                sg = [B_("sg%d" % i, [128, 512]) for i in range(3)]
                oo = [B_("oo%d" % i, [128, 8, 512]) for i in range(2)]
                sq = [B_("sq%d" % i, [128, 512]) for i in range(3)]
                rs5 = [B_("rs5_%d" % i, [128, 512]) for i in range(2)]
                ycb = [B_("ycb%d" % i, [128, 512], BF16) for i in range(2)]
                PG = [Pp("PG%d" % i, [128, 512]) for i in range(2)]
                PSS = [Pp("PSS%d" % i, [128, 512]) for i in range(2)]
                NBK = T // 512

                def cinfo(n):
                    return n // 8, n % 8, n % 3, (n // 8) * 512

                def c_a(n):
                    blk, j, r3, tok = cinfo(n)
                    S.dma("sp", y5[r3][:], Y5s[j * 128:(j + 1) * 128, tok:tok + 512], r=["Y5s"], w=["y5_%d" % r3],
                          stream="y5_%d" % r3)
                    S.dma("sp", uu[r3][:], UTs[j * 128:(j + 1) * 128, tok:tok + 512], r=["UTs"], w=["uu%d" % r3],
                          stream="uu%d" % r3)

                def c_b(n):
                    blk, j, r3, tok = cinfo(n)
                    stt(yv[r3][:], uu[r3][:], D5c[:, j:j + 1], y5[r3][:], ALU.mult, ALU.add,
                        ["uu%d" % r3, "D5c", "y5_%d" % r3], ["yv%d" % r3])

                def c_c(n):
                    blk, j, r3, tok = cinfo(n)
                    act(vb[r3][:], yv[r3][:], AF.Gelu, ["yv%d" % r3], ["vb%d" % r3])

                def c_d(n):
                    blk, j, r3, tok = cinfo(n)
                    mm(PG[n % 2][:, :], Wg[:, j, :], vb[r3][:], True, True, ["Wg", "vb%d" % r3], ["PG%d" % (n % 2)])

                def c_e(n):
                    blk, j, r3, tok = cinfo(n)
                    act(sg[r3][:], PG[n % 2][:, :], AF.Sigmoid, ["PG%d" % (n % 2), "glub"], ["sg%d" % r3],
                        bias=glub[:, j:j + 1])

                def c_f(n):
                    blk, j, r3, tok = cinfo(n)
                    tt("dve", oo[blk % 2][:, j, :], vb[r3][:], sg[r3][:], ALU.mult, ["vb%d" % r3, "sg%d" % r3],
                       ["oo%d_%d" % (blk % 2, j)])

                def c_g(n):
                    blk, j, r3, tok = cinfo(n)
                    tt("pool", sq[r3][:], oo[blk % 2][:, j, :], oo[blk % 2][:, j, :], ALU.mult,
                       ["oo%d_%d" % (blk % 2, j)], ["sq%d" % r3])

                def c_h(n):
                    blk, j, r3, tok = cinfo(n)
                    mm(PSS[blk % 2][:, :], ones_f, sq[r3][:], j == 0, j == 7, ["cst", "sq%d" % r3], ["PSS%d" % (blk % 2)])

                def c_tail(blk):
                    bp = blk % 2
                    tok = blk * 512
                    rk = "rs5_%d" % bp
                    ts("dve", rs5[bp][:], PSS[bp][:, :], 1.0 / 1024, EPS, ALU.mult, ALU.add, ["PSS%d" % bp], [rk])
                    yield
                    act(rs5[bp][:], rs5[bp][:], AF.Sqrt, [rk], [rk])
                    yield
                    S.op("dve", lambda e: e.reciprocal(out=rs5[bp][:], in_=rs5[bp][:]), r=[rk], w=[rk])
                    yield
                    for j in range(8):
                        jp = j % 2
                        stt(ycb[jp][:], oo[bp][:, j, :], g5c[:, j:j + 1], rs5[bp][:], ALU.mult, ALU.mult,
                            ["oo%d_%d" % (bp, j), "g5c", rk], ["ycb%d" % jp])
                        S.dma("sp", YCs[1024 + j * 128:1024 + (j + 1) * 128, tok:tok + 512], ycb[jp][:],
                              r=["ycb%d" % jp], w=["YCs"], stream="ycb%d" % jp)
                        yield

                st2 = [c_a, c_b, c_c, c_d, c_e, c_f, c_g, c_h]
                NI2 = NBK * 8
                side2 = []
                for k in range(NI2 + len(st2) - 1):
                    for s_ in reversed(range(len(st2))):
                        n = k - s_
                        if 0 <= n < NI2:
                            st2[s_](n)
                    nh = k - (len(st2) - 1)
                    if nh >= 0 and nh % 8 == 7:
                        side2.append(c_tail(nh // 8))
                    for g_ in list(side2):
                        try:
                            next(g_)
                        except StopIteration:
                            side2.remove(g_)
                while side2:
                    for g_ in list(side2):
                        try:
                            next(g_)
                        except StopIteration:
                            side2.remove(g_)
                S.flush()
        if stop_after == 3:
            return nc

        with ExitStack() as ph:
            A = lambda n, sh, dt=F32: ph.enter_context(nc.sbuf_tensor("D_" + n, sh, dt))
            P = lambda n, sh, dt=F32: ph.enter_context(nc.psum_tensor("D_" + n, sh, dt))
            wout = A("wout", [128, 16, D], BF16)
            wq = A("wq", [128, 8, 2048], BF16)
            skf = A("skf", [128, 16, 128])
            skT = A("skT", [128, 16, 128], BF16)
            GT1 = A("GT1", [128, D]); G2 = A("G2", [128, D]); SH2 = A("SH2", [128, D])
            yct = [A("yct%d" % i, [128, 16, 128], BF16) for i in range(2)]
            xin = [A("xin%d" % i, [128, D]) for i in range(2)]
            t1 = A("t1", [128, D]); x1 = [A("x1_%d" % i, [128, D]) for i in range(2)]
            junk = A("junk", [128, D], BF16)
            ss2 = A("ss2", [128, 32])
            hb2 = A("hb2", [128, D], BF16)
            h2T = [A("h2T%d" % i, [128, 8, 128], BF16) for i in range(2)]
            qT = A("qT", [128, 16, 128], BF16)
            scb = [A("sc_%d" % i, [128, 16, 128]) for i in range(2)]; sc2 = A("sc2", [128, 16, 128])
            v8 = A("v8", [128, 16, 16]); i8 = A("i8", [128, 16, 16], U32); i8f = A("i8f", [128, 16, 16])
            cand = A("cand", [128, 8, 256]); cand2 = A("cand2", [128, 8, 256])
            c8 = A("c8", [128, 8, 16]); p8 = A("p8", [128, 8, 16], U32)
            ge = A("ge", [128, 8, 16]); gs = A("gs", [128, 8]); gg = A("gg", [128, 8, 16])
            ra_i = A("ra_i", [128, 128], I32); rb_i = A("rb_i", [128, 128], I32)
            raf = A("raf", [128, 128]); rbf = A("rbf", [128, 128])
            oh = A("oh", [128, 128, 16]); oh2 = A("oh2", [128, 128, 16])
            isel = A("isel", [128, 128]); jsel = A("jsel", [128, 128])
            rstg = [A("rstg%d" % i, [128, 3, 128], BF16) for i in range(2)]
            pT = P("pT", [128, 1024], BF16)
            pM = P("pM", [128, 1024])
            pq = [P("pq%d" % i, [128, 512]) for i in range(2)]
            psc = [P("psc%d" % i, [128, 512]) for i in range(2)]
            pTi = P("pTi", [128, 512])
            iota16 = cst[:, C_IOTA16:C_IOTA16 + 16]

            woutv = I["w_out"].rearrange("(ct p) d -> p ct d", p=128)
            for q4 in range(4):
                S.dma("pool", wout[:, q4 * 4:(q4 + 1) * 4, :], woutv[:, q4 * 4:(q4 + 1) * 4, :], w=["wout"], stream="wout")
            wqv = I["w_query"].rearrange("(kc p) n -> p kc n", p=128)
            for q4 in range(4):
                S.dma("pool", wq[:, q4 * 2:(q4 + 1) * 2, :], wqv[:, q4 * 2:(q4 + 1) * 2, :], w=["wq"], stream="wq")
            S.dma("sp", skf[:], I["sub_keys"].rearrange("m k d -> k m d"), w=["skf"], stream="skf")
            for m in range(16):
                tr(pTi[:, (m % 4) * 128:(m % 4 + 1) * 128], skf[:, m, :], ident_f, ["skf", "cst"], ["pTi"])
                cp("dve", skT[:, m, :], pTi[:, (m % 4) * 128:(m % 4 + 1) * 128], ["pTi"], ["skT"])
            YCv = YCs.rearrange("(ct p) t -> p ct t", p=128)
            H2v = H2Ts.rearrange("(kc p) t -> p kc t", p=128)
            def tile_vars(i):
                return i // 16, i % 2, i * 128

            def front(i):
                b, par, tok0 = tile_vars(i)
                sck = "sc%d" % par
                if i % 16 == 0:
                    S.dma("sp", GT1[:], MODs[b:b + 1, 2048:3072].partition_broadcast(128), r=["MODs"], w=["GT1"], stream="d0")
                    S.dma("sp", G2[:], MODs[b:b + 1, 4096:5120].partition_broadcast(128), r=["MODs"], w=["G2"], stream="d1")
                    S.dma("sp", SH2[:], MODs[b:b + 1, 3072:4096].partition_broadcast(128), r=["MODs"], w=["SH2"], stream="d2")
                yk, xk, x1k, hk = "yct%d" % par, "xin%d" % par, "x1_%d" % par, "h2T%d" % par
                S.dma("sp", yct[par][:], YCv[:, :, tok0:tok0 + 128], r=["YCs"], w=[yk], stream=yk)
                S.dma("sp", xin[par][:], I["x"][tok0:tok0 + 128, :], w=[xk], stream=xk)
                yield
                for half in range(2):
                    for ct in range(16):
                        mm(pM[:, half * 512:(half + 1) * 512], yct[par][:, ct, :], wout[:, ct, half * 512:(half + 1) * 512],
                           ct == 0, ct == 15, [yk, "wout"], ["pM"])
                yield
                tt("dve", t1[:], pM[:, :], GT1[:], ALU.mult, ["pM", "GT1"], ["t1"])
                yield
                tt("pool", x1[par][:], t1[:], xin[par][:], ALU.add, ["t1", xk], [x1k])
                S.dma("sp", X1s[tok0:tok0 + 128, :], x1[par][:], r=[x1k], w=["X1s"], stream=x1k)
                yield
                act(junk[:], x1[par][:], AF.Square, [x1k], ["junk", "ss2"], accum_out=ss2[:, i:i + 1])
                yield
                col = ss2[:, i:i + 1]
                ts("dve", col, col, 1.0 / D, EPS, ALU.mult, ALU.add, ["ss2"], ["ss2"])
                yield
                act(col, col, AF.Sqrt, ["ss2"], ["ss2"])
                yield
                S.op("dve", lambda e: e.reciprocal(out=col, in_=col), r=["ss2"], w=["ss2"])
                stt(t1[:], x1[par][:], ss2[:, i:i + 1], G2[:], ALU.mult, ALU.mult, [x1k, "ss2", "G2"], ["t1"])
                yield
                tt("pool", hb2[:], t1[:], SH2[:], ALU.add, ["t1", "SH2"], ["hb2"])
                yield
                for kc in range(8):
                    tr(pT[:, kc * 128:(kc + 1) * 128], hb2[:, kc * 128:(kc + 1) * 128], ident_b, ["hb2", "cstb"], ["pT"])
                yield
                cp("act", h2T[par][:, :, :], pT[:, :].rearrange("p (k t) -> p k t", k=8), ["pT"], [hk])
                S.dma("sp", H2v[:, :, tok0:tok0 + 128], h2T[par][:, :, :], r=[hk], w=["H2Ts"], stream=hk)
                yield
                for m4 in range(4):
                    pp = m4 % 2
                    for mi in range(4):
                        m = m4 * 4 + mi
                        for kc in range(8):
                            mm(pq[pp][:, mi * 128:(mi + 1) * 128], wq[:, kc, m * 128:(m + 1) * 128], h2T[par][:, kc, :],
                               kc == 0, kc == 7, ["wq", hk], ["pq%d" % pp])
                    cp("act", qT[:, m4 * 4:(m4 + 1) * 4, :], pq[pp][:, :].rearrange("p (m t) -> p m t", m=4),
                       ["pq%d" % pp], ["qT%d" % m4])
                    yield
                for m4 in range(4):
                    pp = m4 % 2
                    for mi in range(4):
                        m = m4 * 4 + mi
                        mm(psc[pp][:, mi * 128:(mi + 1) * 128], qT[:, m, :], skT[:, m, :], True, True,
                           ["qT%d" % m4, "skT"], ["psc%d" % pp])
                    cp("act", scb[par][:, m4 * 4:(m4 + 1) * 4, :], psc[pp][:, :].rearrange("p (m k) -> p m k", m=4),
                       ["psc%d" % pp], [sck])
                    yield

            def back(i):
                b, par, tok0 = tile_vars(i)
                sck = "sc%d" % par
                for m in range(16):
                    S.op("dve", lambda e, m=m: e.max(out=v8[:, m, 0:8], in_=scb[par][:, m, :]), r=[sck], w=["v8a%d" % m])
                yield
                for m in range(16):
                    S.op("dve", lambda e, m=m: e.max_index(out=i8[:, m, 0:8], in_max=v8[:, m, 0:8], in_values=scb[par][:, m, :]),
                         r=[sck, "v8a%d" % m], w=["i8a%d" % m])
                    S.op("dve", lambda e, m=m: e.match_replace(out=sc2[:, m, :], in_to_replace=v8[:, m, 0:8],
                                                               in_values=scb[par][:, m, :], imm_value=-1e30),
                         r=[sck, "v8a%d" % m], w=["sc2_%d" % m])
                    if m % 4 == 3:
                        yield
                for m in range(16):
                    S.op("dve", lambda e, m=m: e.max(out=v8[:, m, 8:16], in_=sc2[:, m, :]), r=["sc2_%d" % m],
                         w=["v8b%d" % m])
                yield
                for m in range(16):
                    S.op("dve", lambda e, m=m: e.max_index(out=i8[:, m, 8:16], in_max=v8[:, m, 8:16],
                                                           in_values=sc2[:, m, :]), r=["sc2_%d" % m, "v8b%d" % m],
                         w=["i8b%d" % m])
                yield
                v8keys = ["v8a%d" % m for m in range(16)] + ["v8b%d" % m for m in range(16)]
                i8keys = ["i8a%d" % m for m in range(16)] + ["i8b%d" % m for m in range(16)]
                cp("dve", i8f[:], i8[:], i8keys, ["i8f"])
                v8v = v8[:, :, :].rearrange("p (h c) r -> p h c r", c=2)
                i8v = i8f[:, :, :].rearrange("p (h c) r -> p h c r", c=2)
                tt("dve", cand[:, :, :].rearrange("p h (r c) -> p h r c", c=16),
                   v8v[:, :, 0, :].unsqueeze(3).to_broadcast([128, 8, 16, 16]),
                   v8v[:, :, 1, :].unsqueeze(2).to_broadcast([128, 8, 16, 16]), ALU.add, v8keys, ["cand"])
                yield
                for h in range(8):
                    S.op("dve", lambda e, h=h: e.max(out=c8[:, h, 0:8], in_=cand[:, h, :]), r=["cand"], w=["c8a%d" % h])
                yield
                for h in range(8):
                    S.op("dve", lambda e, h=h: e.max_index(out=p8[:, h, 0:8], in_max=c8[:, h, 0:8], in_values=cand[:, h, :]),
                         r=["cand", "c8a%d" % h], w=["p8a%d" % h])
                    S.op("dve", lambda e, h=h: e.match_replace(out=cand2[:, h, :], in_to_replace=c8[:, h, 0:8],
                                                               in_values=cand[:, h, :], imm_value=-1e30),
                         r=["cand", "c8a%d" % h], w=["cand2_%d" % h])
                    if h % 4 == 3:
                        yield
                for h in range(8):
                    S.op("dve", lambda e, h=h: e.max(out=c8[:, h, 8:16], in_=cand2[:, h, :]), r=["cand2_%d" % h],
                         w=["c8b%d" % h])
                yield
                for h in range(8):
                    S.op("dve", lambda e, h=h: e.max_index(out=p8[:, h, 8:16], in_max=c8[:, h, 8:16],
                                                           in_values=cand2[:, h, :]), r=["cand2_%d" % h, "c8b%d" % h],
                         w=["p8b%d" % h])
                yield
                c8keys = ["c8a%d" % h for h in range(8)] + ["c8b%d" % h for h in range(8)]
                p8keys = ["p8a%d" % h for h in range(8)] + ["p8b%d" % h for h in range(8)]
                tt("dve", ge[:], c8[:], c8[:, :, 0:1].to_broadcast([128, 8, 16]), ALU.subtract, c8keys, ["ge"])
                yield
                act(ge[:], ge[:], AF.Exp, ["ge"], ["ge"])
                yield
                S.op("dve", lambda e: e.tensor_reduce(out=gs[:], in_=ge[:], axis=AX.X, op=ALU.add), r=["ge"], w=["gs"])
                S.op("dve", lambda e: e.reciprocal(out=gs[:], in_=gs[:]), r=["gs"], w=["gs"])
                tt("dve", gg[:], ge[:], gs[:, :].unsqueeze(2).to_broadcast([128, 8, 16]), ALU.mult, ["ge", "gs"], ["gg"])
                p8i = p8[:, :, :].rearrange("p h k -> p (h k)").bitcast(I32)
                S.op("dve", lambda e: e.tensor_single_scalar(out=ra_i[:], in_=p8i, scalar=4, op=ALU.logical_shift_right),
                     r=p8keys, w=["ra_i"])
                S.op("dve", lambda e: e.tensor_single_scalar(out=rb_i[:], in_=p8i, scalar=15, op=ALU.bitwise_and),
                     r=p8keys, w=["rb_i"])
                cp("dve", raf[:], ra_i[:], ["ra_i"], ["raf"])
                cp("dve", rbf[:], rb_i[:], ["rb_i"], ["rbf"])
                yield
                io3 = iota16.unsqueeze(1).to_broadcast([128, 128, 16])
                for (rf, rk, ci, ohh, ok, sel, sk_) in ((raf, "raf", 0, oh, "oh", isel, "isel"),
                                                        (rbf, "rbf", 1, oh2, "oh2", jsel, "jsel")):
                    eng = "dve" if ci == 0 else "pool"
                    tt("dve", ohh[:], rf[:, :].unsqueeze(2).to_broadcast([128, 128, 16]), io3, ALU.is_equal,
                       [rk, "cst"], [ok])
                    tt(eng, ohh[:, :, :].rearrange("p (h k) r -> p h k r", h=8),
                       ohh[:, :, :].rearrange("p (h k) r -> p h k r", h=8),
                       i8v[:, :, ci, :].unsqueeze(2).to_broadcast([128, 8, 16, 16]), ALU.mult, [ok, "i8f"], [ok])
                    S.op("dve", lambda e, sel=sel, ohh=ohh: e.tensor_reduce(out=sel[:], in_=ohh[:], axis=AX.X, op=ALU.add),
                         r=[ok], w=[sk_])
                    yield
                tr(pTi[:, 0:128], isel[:], ident_f, ["isel", "cst"], ["pTi"])
                tr(pTi[:, 128:256], jsel[:], ident_f, ["jsel", "cst"], ["pTi"])
                tr(pTi[:, 256:384], gg[:, :, :].rearrange("p h k -> p (h k)"), ident_f, ["gg", "cst"], ["pTi"])
                rk_ = "rstg%d" % par
                cp("act", rstg[par][:, :, :], pTi[:, 0:384].rearrange("p (a t) -> p a t", a=3), ["pTi"], [rk_])
                S.dma("sp", RTs[:, :, tok0:tok0 + 128], rstg[par][:, :, :], r=[rk_], w=["RTs"], stream=rk_)

            def interleave(gens):
                gens = [g_ for g_ in gens if g_ is not None]
                while gens:
                    for g_ in list(gens):
                        try:
                            next(g_)
                        except StopIteration:
                            gens.remove(g_)

            interleave([front(0)])
            for i in range(NT):
                interleave([front(i + 1) if i + 1 < NT else None, back(i)])
            S.flush()
        if stop_after == 4:
            return nc

        with ExitStack() as ph:
            A = lambda n, sh, dt=F32: ph.enter_context(nc.sbuf_tensor("M_" + n, sh, dt))
            P = lambda n, sh, dt=F32: ph.enter_context(nc.psum_tensor("M_" + n, sh, dt))
            TB = 256
            G0 = A("G0", [128, 64, TB], BF16)
            G1 = A("G1", [128, 64, TB], BF16)
            utb = [A("utb%d" % i, [128, 2, D], BF16) for i in range(4)]
            vtb = [A("vtb%d" % i, [128, 2, D], BF16) for i in range(4)]
            Pm = [A("Pm%d" % i, [128, 8, 64], BF16) for i in range(4)]
            Q0 = [A("Q0%d" % i, [128, 8, 128], BF16) for i in range(4)]
            Qm = [A("Qm%d" % i, [128, 8, 128], BF16) for i in range(4)]
            h2b = [A("h2b%d" % i, [128, 8, TB], BF16) for i in range(2)]
            rt = [A("rt%d" % i, [128, 3, TB], BF16) for i in range(2)]
            Ag = [A("Ag%d" % i, [128, TB], BF16) for i in range(2)]
            GA = [A("GA%d" % i, [128, TB], BF16) for i in range(2)]
            x1t = [A("x1t%d" % i, [128, D]) for i in range(2)]
            GT2 = A("GT2", [128, D]); nfg = A("nfg", [128, D])
            tm = [A("tm%d" % i, [128, D]) for i in range(2)]; x2 = A("x2", [128, D]); junkm = A("junkm", [128, D], BF16)
            ot = [A("ot%d" % i, [128, D]) for i in range(2)]
            ssf = A("ssf", [128, 32])
            pO = [P("pO%d" % i, [128, 1024]) for i in range(2)]
            pA = [P("pA%d" % i, [128, 512]) for i in range(2)]
            pG = [P("pG%d" % i, [128, 512]) for i in range(2)]
            iota_b = cstb[:, C_IOTA:C_IOTA + 128]
            io32 = iota_b.unsqueeze(1).to_broadcast([128, 32, 128])
            io8 = iota_b.unsqueeze(1).to_broadcast([128, 8, 128])
            H2v = H2Ts.rearrange("(kc p) t -> p kc t", p=128)
            UTv = UTb.rearrange("e p x -> p e x")
            Vv = Vb.rearrange("(e p) d -> p e d", p=128)
            S.dma("sp", nfg[:], I["norm_f_g"][0:1, :].partition_broadcast(128), w=["nfg"], stream="m0")
            Gh = [G0, G1]
            io64 = [iota_b[:, 64 * hf:64 * hf + 64].unsqueeze(1).to_broadcast([128, 8, 64]) for hf in range(2)]
            NBLK = T // TB
            cnts = {"g": 0, "s": 0}
            late = []

            def run_late():
                for f_ in late:
                    f_()
                del late[:]

            def build(blk, hf):
                bp = blk % 2
                tok = blk * TB
                hk, rk = "h2b%d" % bp, "rt%d" % bp
                if hf == 0:
                    S.dma("sp", h2b[bp][:], H2v[:, :, tok:tok + TB], r=["H2Ts"], w=[hk], stream=hk)
                    S.dma("sp", rt[bp][:], RTs[:, :, tok:tok + TB], r=["RTs"], w=[rk], stream=rk)
                    yield
                NG = TB // 8
                base = cnts["s"]
                cnts["s"] += NG

                def dve_part(k):
                    sp_ = (base + k) % 4
                    tsl = slice(k * 8, (k + 1) * 8)
                    bcn = lambda a_, n_: rt[bp][:, a_, tsl].unsqueeze(2).to_broadcast([128, 8, n_])
                    tt("dve", Pm[sp_][:], bcn(0, 64), io64[hf], ALU.is_equal, [rk, "cstb"], ["Pm%d" % sp_])
                    tt("dve", Q0[sp_][:], bcn(1, 128), io8, ALU.is_equal, [rk, "cstb"], ["Q0%d" % sp_])

                def pool_part(k):
                    sp_ = (base + k) % 4
                    tsl = slice(k * 8, (k + 1) * 8)
                    tt("pool", Qm[sp_][:], Q0[sp_][:], rt[bp][:, 2, tsl].unsqueeze(2).to_broadcast([128, 8, 128]),
                       ALU.mult, ["Q0%d" % sp_, rk], ["Qm%d" % sp_])

                def mm_part(k):
                    sp_ = (base + k) % 4
                    for t4 in range(2):
                        gp = cnts["g"] % 2
                        cnts["g"] += 1
                        for ti in range(4):
                            t = t4 * 4 + ti
                            mm(pG[gp][:, ti * 64:(ti + 1) * 64], Qm[sp_][:, t, :], Pm[sp_][:, t, :], True, True,
                               ["Qm%d" % sp_, "Pm%d" % sp_], ["pG%d" % gp])
                        t0 = k * 8 + t4 * 4
                        late.append(lambda gp=gp, t0=t0: cp(
                            "act", Gh[hf][:, :, t0:t0 + 4], pG[gp][:, 0:256].rearrange("p (t i) -> p i t", t=4),
                            ["pG%d" % gp], ["G%d" % hf]))

                dve_part(0)
                dve_part(1)
                dve_part(2)
                yield
                pool_part(0)
                pool_part(1)
                yield
                for k in range(NG):
                    mm_part(k)
                    if k + 3 < NG:
                        late.append(lambda k=k: dve_part(k + 3))
                    if k + 2 < NG:
                        late.append(lambda k=k: pool_part(k + 2))
                    yield

            def final(blk):
                tok = blk * TB
                b_ = tok // SEQ
                if tok % SEQ == 0:
                    S.dma("sp", GT2[:], MODs[b_:b_ + 1, 5120:6144].partition_broadcast(128), r=["MODs"], w=["GT2"],
                          stream="m1")
                for t2 in range(2):
                    tt("dve", tm[t2][:], pO[t2][:, :], GT2[:], ALU.mult, ["pO%d" % t2, "GT2"], ["tm%d" % t2])
                yield
                for t2 in range(2):
                    ti = blk * 2 + t2
                    tk0 = tok + t2 * 128
                    xk, ok_ = "x1t%d" % t2, "ot%d" % t2
                    col = ssf[:, ti % 32:ti % 32 + 1]
                    S.dma("sp", x1t[t2][:], X1s[tk0:tk0 + 128, :], r=["X1s"], w=[xk], stream=xk)
                    yield
                    tt("pool", x2[:], tm[t2][:], x1t[t2][:], ALU.add, ["tm%d" % t2, xk], ["x2"])
                    yield
                    act(junkm[:], x2[:], AF.Square, ["x2"], ["junkm", "ssf"], accum_out=col)
                    yield
                    ts("dve", col, col, 1.0 / D, EPS, ALU.mult, ALU.add, ["ssf"], ["ssf"])
                    yield
                    act(col, col, AF.Sqrt, ["ssf"], ["ssf"])
                    yield
                    S.op("dve", lambda e, col=col: e.reciprocal(out=col, in_=col), r=["ssf"], w=["ssf"])
                    stt(ot[t2][:], x2[:], col, nfg[:], ALU.mult, ALU.mult, ["x2", "ssf", "nfg"], [ok_])
                    yield
                    S.dma("sp", out[tk0:tk0 + 128, :], ot[t2][:], r=[ok_], w=["out"], stream=ok_)
                    yield

            def prefetch(gg):
                if gg >= NBLK * 64:
                    return
                up = gg % 4
                i0 = (gg % 64) * 2
                S.dma("sp", utb[up][:], UTv[:, i0:i0 + 2, :], r=["UTb"], w=["utb%d" % up], stream="utb%d" % up)
                S.dma("sp", vtb[up][:], Vv[:, i0:i0 + 2, :], r=["Vb"], w=["vtb%d" % up], stream="vtb%d" % up)

            def emitA(blk, i):
                bp = blk % 2
                gg = blk * 64 + i // 2
                up = gg % 4
                if gg == 0 and i == 0:
                    prefetch(0)
                    prefetch(1)
                    prefetch(2)
                ap_ = i % 2
                for kc in range(8):
                    mm(pA[ap_][:, 0:256], utb[up][:, i % 2, kc * 128:(kc + 1) * 128], h2b[bp][:, kc, :],
                       kc == 0, kc == 7, ["utb%d" % up, "h2b%d" % bp], ["pA%d" % ap_])

            def emitG(blk, i):
                ap_ = i % 2
                hf = i // 64
                act(Ag[ap_][:], pA[ap_][:, 0:256], AF.Gelu, ["pA%d" % ap_], ["Ag%d" % ap_])
                tt("dve", GA[ap_][:], Ag[ap_][:], Gh[hf][:, i % 64, :], ALU.mult, ["Ag%d" % ap_, "G%d" % hf], ["GA%d" % ap_])

            def emitVm(blk, i):
                gg = blk * 64 + i // 2
                up = gg % 4
                vk = "vtb%d" % up
                ap_ = i % 2
                for t2 in range(2):
                    for half in range(2):
                        mm(pO[t2][:, half * 512:(half + 1) * 512], GA[ap_][:, t2 * 128:(t2 + 1) * 128],
                           vtb[up][:, i % 2, half * 512:(half + 1) * 512], i == 0, i == 127,
                           ["GA%d" % ap_, vk], ["pO%d" % t2])
                if i % 2 == 0:
                    prefetch(gg + 3)

            def step(gens, skip=None):
                for g_ in list(gens):
                    if g_ is skip:
                        continue
                    try:
                        next(g_)
                    except StopIteration:
                        gens.remove(g_)

            for _ in build(0, 0):
                run_late()
            run_late()
            side = []
            for blk in range(NBLK):
                side.append(build(blk, 1))
                emitA(blk, 0)
                emitA(blk, 1)
                emitG(blk, 0)
                bld = side[-1]
                for i in range(128):
                    if i == 63:
                        for _ in bld:
                            run_late()
                        run_late()
                    if i == 64 and blk + 1 < NBLK:
                        bld = build(blk + 1, 0)
                        side.append(bld)
                    ip = i % 64
                    if ((ip + 1) * 37) // 64 > (ip * 37) // 64:
                        step(side)
                    else:
                        step(side, skip=bld)
                    if i + 2 < 128:
                        emitA(blk, i + 2)
                    if i + 1 < 128:
                        emitG(blk, i + 1)
                    run_late()
                    emitVm(blk, i)
                for _ in bld:
                    run_late()
                run_late()
                fg = final(blk)
                next(fg)
                side.append(fg)
            while side:
                step(side)
                run_late()
            S.flush()
        return nc


def prep_inputs(inputs):
    sq = lambda a: np.ascontiguousarray(a[0]) if a.shape[0] == 1 and a.ndim >= 2 else np.ascontiguousarray(a)
    shared = {}
    for n, sh in IN_SPECS:
        if n in ("x", "c", "consts"):
            continue
        a = np.asarray(inputs[n], dtype=np.float32)
        shared[n] = np.ascontiguousarray(a.reshape(sh))
    shared["consts"] = make_consts()
    x = np.asarray(inputs["x"], dtype=np.float32)
    c = np.asarray(inputs["c"], dtype=np.float32)
    maps = []
    for i in range(NCORES):
        m = dict(shared)
        m["x"] = np.ascontiguousarray(x[i * NB:(i + 1) * NB].reshape(T, D))
        m["c"] = np.ascontiguousarray(c[i * NB:(i + 1) * NB])
        maps.append(m)
    return maps


def kernel(**inputs):
    nc = build()
    maps = prep_inputs(inputs)
    res = run_bass_kernel_spmd(nc, maps, core_ids=list(range(NCORES)))
    outs = [np.asarray(r["out"]).reshape(NB, SEQ, D) for r in res.results]
    return np.concatenate(outs, axis=0).astype(np.float32)
```

```python
import os
from contextlib import ExitStack

import numpy as np
import concourse.bass as bass
import concourse.mybir as mybir
from concourse.bass_utils import run_bass_kernel_spmd

F32 = mybir.dt.float32
BF16 = mybir.dt.bfloat16
I32 = mybir.dt.int32
U32 = mybir.dt.uint32
AF = mybir.ActivationFunctionType
ALU = mybir.AluOpType
AX = mybir.AxisListType

NCORES = 8
D = 1024
NB = 2
SEQ = 2048
T = NB * SEQ
NT = T // 128
INW = 4112
EPS = 1e-6
MAGIC = 12582912.0
TWO_PI = 6.283185307179586


class _Op:
    __slots__ = ("eng", "fn", "dom", "order", "waits", "target", "val", "is_dma")

    def __init__(self, eng, fn, dom, order, is_dma):
        self.eng, self.fn, self.dom, self.order, self.is_dma = eng, fn, dom, order, is_dma
        self.waits = []
        self.target = is_dma
        self.val = None


class Sched:
    CE = ("pe", "act", "dve", "pool")

    def __init__(self, nc, stack):
        self.nc = nc
        self.stack = stack
        self.q = {k: [] for k in ("pe", "act", "dve", "pool", "sp")}
        self.sem = {k: stack.enter_context(nc.semaphore("c_" + k)) for k in self.CE}
        self.cnt = {k: 0 for k in self.CE}
        self.order = {k: 0 for k in self.CE}
        self.dsem = {}
        self.dcnt = {}
        self.dslot = {}
        self.dfree = []
        self.dorder = {}
        self.waited = {k: {} for k in self.q}
        self.lastw = {}
        self.readers = {}
        self.lastop = {}
        self.ninst = 0

    def _need(self, eng, p, out):
        if self.waited[eng].get(p.dom, 0) >= p.order:
            return
        cur = out.get(p.dom)
        if cur is None or cur.order < p.order:
            out[p.dom] = p

    def _deps(self, eng, r, w, is_dma=False):
        need = {}
        for b in r:
            p = self.lastw.get(b)
            if p is not None and (is_dma or not (p.eng == eng and eng == "pe" and not p.is_dma)):
                self._need(eng, p, need)
        for b in w:
            p = self.lastw.get(b)
            if p is not None and (is_dma or p.is_dma or p.eng != eng or eng != "pe"):
                self._need(eng, p, need)
            for p in self.readers.get(b, ()):
                if is_dma or p.is_dma or p.eng != eng or eng != "pe":
                    self._need(eng, p, need)
        return need

    def _add(self, op, need, r, w):
        for dom, p in need.items():
            self.waited[op.eng][dom] = p.order
            p.target = True
            op.waits.append(p)
        self.q[op.eng].append(op)
        self.lastop[op.dom] = op
        for b in r:
            self.readers.setdefault(b, []).append(op)
        for b in w:
            self.lastw[b] = op
            self.readers[b] = []
        self.ninst += 1

    def op(self, eng, fn, r=(), w=()):
        need = self._deps(eng, r, w)
        self.order[eng] += 1
        o = _Op(eng, fn, eng, self.order[eng], False)
        self._add(o, need, r, w)

    def dma(self, eng, out, in_, r=(), w=(), stream="d", **kw):
        if eng == "pool":
            key = "swd_%d" % len(self.dsem)
            self.dsem[key] = self.stack.enter_context(self.nc.semaphore(key))
            self.dcnt[key] = 0
            self.dorder[key] = 0
            self.dslot["__swd__" + key] = key
            stream = "__swd__" + key
        if stream not in self.dslot:
            if self.dfree:
                self.dslot[stream] = self.dfree.pop()
            else:
                k = "dma_%d" % len(self.dsem)
                self.dsem[k] = self.stack.enter_context(self.nc.semaphore(k))
                self.dcnt[k] = 0
                self.dorder[k] = 0
                self.dslot[stream] = k
        key = self.dslot[stream]
        need = self._deps(eng, r, w, is_dma=True)
        self.dorder[key] += 1
        fn = lambda e, out=out, in_=in_, kw=kw: e.dma_start(out=out, in_=in_, **kw)
        o = _Op(eng, fn, key, self.dorder[key], True)
        self._add(o, need, r, w)

    def barrier(self):
        lasts = list(self.lastop.values())
        for eng in self.q:
            need = {}
            for p in lasts:
                self._need(eng, p, need)
            if need:
                o = _Op(eng, None, None, 0, False)
                for dom, p in need.items():
                    self.waited[eng][dom] = p.order
                    p.target = True
                    o.waits.append(p)
                self.q[eng].append(o)
        self.lastw = {}
        self.readers = {}

    def flush(self):
        nc = self.nc
        self.barrier()
        q = self.q
        for eng in q:
            for o in q[eng]:
                if o.fn is None:
                    continue
                if o.is_dma:
                    self.dcnt[o.dom] += 16
                    o.val = self.dcnt[o.dom]
                elif o.target:
                    self.cnt[eng] += 1
                    o.val = self.cnt[eng]
        sem, dsem = self.sem, self.dsem

        def run(e, ops):
            for o in ops:
                for p in o.waits:
                    e.wait_ge(dsem[p.dom] if p.is_dma else sem[p.dom], p.val)
                if o.fn is None:
                    continue
                ins = o.fn(e)
                if o.is_dma:
                    ins.then_inc(dsem[o.dom], 16)
                elif o.target:
                    ins.then_inc(sem[o.eng], 1)

        with nc.Block() as block:
            @block.tensor
            def _(e):
                run(e, q["pe"])

            @block.scalar
            def _(e):
                run(e, q["act"])

            @block.vector
            def _(e):
                run(e, q["dve"])

            @block.gpsimd
            def _(e):
                run(e, q["pool"])

            @block.sync
            def _(e):
                run(e, q["sp"])
        for k in q:
            q[k] = []
        self.dfree.extend(v for k_, v in self.dslot.items() if not k_.startswith("__swd__"))
        self.dslot = {}
        self.lastop = {}


C_IDENT, C_TRIU, C_TRIS, C_ONES, C_IOTA, C_BD16, C_RM8, C_IOTA16, C_END = (
    0, 128, 256, 384, 512, 640, 768, 776, 792)


def make_consts():
    c = np.zeros((128, C_END), np.float32)
    k = np.arange(128)
    c[:, C_IDENT:C_IDENT + 128] = np.eye(128)
    c[:, C_TRIU:C_TRIU + 128] = (k[:, None] <= k[None, :])
    c[:, C_TRIS:C_TRIS + 128] = (k[:, None] > k[None, :])
    c[:, C_ONES:C_ONES + 128] = 1.0
    c[:, C_IOTA:C_IOTA + 128] = k[None, :]
    c[:, C_BD16:C_BD16 + 128] = (k[:, None] // 16 == k[None, :] // 16)
    c[:, C_RM8:C_RM8 + 8] = (k[:, None] // 16 == np.arange(8)[None, :])
    c[:, C_IOTA16:C_IOTA16 + 16] = np.arange(16)[None, :]
    return c


def skew_pipeline(stages, n_items):
    ns = len(stages)
    for k in range(n_items + ns - 1):
        for s_ in reversed(range(ns)):
            n = k - s_
            if 0 <= n < n_items:
                stages[s_](n)


IN_SPECS = [
    ("x", [T, D]), ("c", [NB, D]), ("w_ada", [D, 6 * D]), ("b_ada", [1, 6 * D]),
    ("norm1_g", [1, D]), ("w_in", [D, INW]), ("conv_w", [4, 2048]), ("conv_b", [1, 2048]),
    ("dt_bias", [1, 16]), ("a_log", [1, 16]), ("d_ssd", [1, 16]), ("norm_ssd_g", [1, D]),
    ("s5_a_re", [64, 64]), ("s5_a_im", [64, 64]), ("s5_log_dt", [1, 64]),
    ("s5_b_re", [64, 64, 16]), ("s5_b_im", [64, 64, 16]), ("s5_c_re", [64, 16, 64]),
    ("s5_c_im", [64, 16, 64]), ("s5_d", [64, 16]), ("glu_w", [64, 16, 16]), ("glu_b", [64, 16]),
    ("norm_s5_g", [1, D]), ("w_out", [2 * D, D]), ("norm2_g", [1, D]), ("w_query", [D, 2048]),
    ("sub_keys", [16, 128, 128]), ("expert_u", [16384, D]), ("expert_v", [16384, D]),
    ("norm_f_g", [1, D]), ("consts", [128, C_END]),
]


def build(debug=(), stop_after=None):
    nc = bass.Bass("TRN2", target_bir_lowering=False)
    I = {n: nc.dram_tensor(n, sh, F32, kind="ExternalInput").ap() for n, sh in IN_SPECS}
    out = nc.dram_tensor("out", [T, D], F32, kind="ExternalOutput").ap()

    def SCR(name, shape, dt):
        kind = "ExternalOutput" if name in debug else "Internal"
        return nc.dram_tensor(name, shape, dt, kind=kind).ap()

    MODs = SCR("MODs", [NB, 6 * D], F32)
    XCs = SCR("XCs", [2048, T], BF16)
    UTs = SCR("UTs", [1024, T], BF16)
    Zs = SCR("Zs", [T, D], BF16)
    DTs = SCR("DTs", [T, 16], F32)
    YCs = SCR("YCs", [2048, T], BF16)
    Y5s = SCR("Y5s", [1024, T], F32)
    X1s = SCR("X1s", [T, D], F32)
    H2Ts = SCR("H2Ts", [D, T], BF16)
    RTs = SCR("RTs", [128, 3, T], BF16)
    UTb = SCR("UTb", [128, 128, 1024], BF16)
    Vb = SCR("Vb", [16384, D], BF16)

    with ExitStack() as top:
        S = Sched(nc, top)

        def mm(out_, lhsT, rhs, start, stop, r, w):
            S.op("pe", lambda e: e.matmul(out_, lhsT=lhsT, rhs=rhs, start=start, stop=stop), r=r, w=w)

        def tr(out_, in_, ident, r, w):
            S.op("pe", lambda e: e.transpose(out=out_, in_=in_, identity=ident), r=r, w=w)

        def act(out_, in_, func, r, w, eng="act", **kw):
            S.op(eng, lambda e: e.activation(out=out_, in_=in_, func=func, **kw), r=r, w=w)

        def tt(eng, out_, in0, in1, op, r, w):
            S.op(eng, lambda e: e.tensor_tensor(out=out_, in0=in0, in1=in1, op=op), r=r, w=w)

        def ts(eng, out_, in0, s1, s2, op0, op1, r, w):
            if s2 is None:
                S.op(eng, lambda e: e.tensor_scalar(out=out_, in0=in0, scalar1=s1, scalar2=None, op0=op0), r=r, w=w)
            else:
                S.op(eng, lambda e: e.tensor_scalar(out=out_, in0=in0, scalar1=s1, scalar2=s2, op0=op0, op1=op1),
                     r=r, w=w)

        def stt(out_, in0, scalar, in1, op0, op1, r, w):
            S.op("dve", lambda e: e.scalar_tensor_tensor(out=out_, in0=in0, scalar=scalar, in1=in1, op0=op0, op1=op1),
                 r=r, w=w)

        def cp(eng, out_, in_, r, w):
            if eng == "act":
                act(out_, in_, AF.Copy, r, w)
            else:
                S.op(eng, lambda e: e.tensor_copy(out=out_, in_=in_), r=r, w=w)

        def rsqrt(col, n, r, w):
            ts("dve", col, col, 1.0 / n, EPS, ALU.mult, ALU.add, r, w)
            act(col, col, AF.Sqrt, w, w)
            S.op("dve", lambda e: e.reciprocal(out=col, in_=col), r=w, w=w)

        cst = top.enter_context(nc.sbuf_tensor("cst", [128, C_END], F32))
        cstb = top.enter_context(nc.sbuf_tensor("cstb", [128, C_END], BF16))
        S.dma("sp", cst[:], I["consts"][:, :], w=["cst"], stream="cst")
        cp("dve", cstb[:], cst[:], ["cst"], ["cstb"])
        ident_f = cst[:, C_IDENT:C_IDENT + 128]
        ident_b = cstb[:, C_IDENT:C_IDENT + 128]
        triu_f = cst[:, C_TRIU:C_TRIU + 128]
        tris_f = cst[:, C_TRIS:C_TRIS + 128]
        ones_f = cst[:, C_ONES:C_ONES + 128]
        ones_b = cstb[:, C_ONES:C_ONES + 128]

        with ExitStack() as ph:
            A = lambda n, sh, dt=F32: ph.enter_context(nc.sbuf_tensor(n, sh, dt))
            cT = A("cT", [128, 8, NB])
            bada = A("bada", [NB, 6 * D])
            g1r = A("g1r", [NB, D])
            g2r = A("g2r", [NB, D])
            wa = [A("wa%d" % i, [128, 8, 512]) for i in range(3)]
            mod2 = A("mod2", [NB, 6 * D])
            pm = [ph.enter_context(nc.psum_tensor("pm%d" % i, [128, 512], F32)) for i in range(2)]
            for b in range(NB):
                S.dma("sp", cT[:, :, b], I["c"][b:b + 1, :].rearrange("o (kc p) -> p (o kc)", p=128), w=["cT"],
                      stream="p0", allow_slow_non_contiguous=True)
            S.dma("sp", bada[:], I["b_ada"][0:1, :].partition_broadcast(NB), w=["bada"], stream="p0b")
            S.dma("sp", g1r[:], I["norm1_g"][0:1, :].partition_broadcast(NB), w=["g1r"], stream="p0c")
            S.dma("sp", g2r[:], I["norm2_g"][0:1, :].partition_broadcast(NB), w=["g2r"], stream="p0d")
            act(cT[:], cT[:], AF.Silu, ["cT"], ["cT"])
            wav = I["w_ada"].rearrange("(kc p) n -> p kc n", p=128)
            for n in range(12):
                wb = wa[n % 3]
                wk = "wa%d" % (n % 3)
                pk = "pm%d" % (n % 2)
                S.dma("sp", wb[:], wav[:, :, n * 512:(n + 1) * 512], w=[wk], stream=wk)
                for kc in range(8):
                    mm(pm[n % 2][0:NB, :], cT[:, kc, :], wb[:, kc, :], kc == 0, kc == 7, ["cT", wk], [pk])
                tt("dve", mod2[:, n * 512:(n + 1) * 512], pm[n % 2][0:NB, :], bada[:, n * 512:(n + 1) * 512],
                   ALU.add, [pk, "bada"], ["mod2"])
            stt(mod2[:, 1024:2048], mod2[:, 1024:2048], 1.0, g1r[:], ALU.add, ALU.mult, ["mod2", "g1r"], ["mod2"])
            stt(mod2[:, 4096:5120], mod2[:, 4096:5120], 1.0, g2r[:], ALU.add, ALU.mult, ["mod2", "g2r"], ["mod2"])
            S.dma("sp", MODs[:, :], mod2[:], r=["mod2"], w=["MODs"], stream="p0s")
            S.flush()
        if stop_after == 0:
            return nc

        with ExitStack() as ph:
            A = lambda n, sh, dt=F32: ph.enter_context(nc.sbuf_tensor(n, sh, dt))
            P = lambda n, sh, dt=F32: ph.enter_context(nc.psum_tensor(n, sh, dt))
            win = A("win", [128, 8, INW], BF16)
            hT = A("hT", [128, 8, SEQ], BF16)
            G1 = A("G1", [128, D])
            SH1 = A("SH1", [128, D])
            xin = [A("xin%d" % i, [128, D]) for i in range(5)]
            t1 = [A("t1_%d" % i, [128, D]) for i in range(3)]
            junk = A("junk", [128, D], BF16)
            hb = [A("hb%d" % i, [128, D], BF16) for i in range(3)]
            ss = A("ss", [128, 16])
            zst = [A("zst%d" % i, [128, D], BF16) for i in range(2)]
            dts = [A("dts%d" % i, [128, 16]) for i in range(2)]
            xpad = [A("xpad%d" % i, [128, 3 + SEQ]) for i in range(2)]
            acc = [A("acc%d" % i, [128, SEQ]) for i in range(2)]
            xo = [A("xo%d" % i, [128, SEQ], BF16) for i in range(2)]
            cw = A("cw", [128, 16, 4])
            cb = A("cb", [128, 16])
            pT = [P("pT%d" % i, [128, 1024], BF16) for i in range(2)]
            pz = P("pz", [128, 1024])
            pdt = P("pdt", [128, 16])
            pc = [P("pc%d" % i, [128, 512]) for i in range(2)]

            winv = I["w_in"].rearrange("(kc p) n -> p kc n", p=128)
            for kc in range(8):
                S.dma("pool", win[:, kc, :], winv[:, kc, :], w=["win%d" % kc], stream="win")
            for k in range(4):
                S.dma("sp", cw[:, :, k], I["conv_w"][k:k + 1, :].rearrange("o (ct p) -> p (o ct)", p=128), w=["cw"],
                      stream="cw", allow_slow_non_contiguous=True)
            S.dma("sp", cb[:], I["conv_b"].rearrange("o (ct p) -> p (o ct)", p=128), w=["cb"], stream="cb",
                  allow_slow_non_contiguous=True)
            for i in range(2):
                S.op("dve", lambda e, i=i: e.memset(xpad[i][:, 0:3], 0.0), w=["xpad%d" % i])

            def tokgen(b, i):
                tok0 = b * SEQ + i * 128
                r3, r2 = i % 3, i % 2
                xk, tk_, hk, pk = "xin%d" % (i % 5), "t1_%d" % r3, "hb%d" % r3, "pT%d" % r2
                xt = xin[i % 5]
                col = ss[:, i:i + 1]
                S.dma("sp", xt[:], I["x"][tok0:tok0 + 128, :], w=[xk], stream=xk)
                yield
                act(junk[:], xt[:], AF.Square, [xk], ["junk", "ss%d" % i], accum_out=col)
                yield
                ts("dve", col, col, 1.0 / D, EPS, ALU.mult, ALU.add, ["ss%d" % i], ["ss%d" % i])
                yield
                act(col, col, AF.Sqrt, ["ss%d" % i], ["ss%d" % i])
                yield
                S.op("dve", lambda e: e.reciprocal(out=col, in_=col), r=["ss%d" % i], w=["ss%d" % i])
                stt(t1[r3][:], xt[:], col, G1[:], ALU.mult, ALU.mult, [xk, "ss%d" % i, "G1"], [tk_])
                yield
                tt("pool", hb[r3][:], t1[r3][:], SH1[:], ALU.add, [tk_, "SH1"], [hk])
                yield
                for kc in range(8):
                    tr(pT[r2][:, kc * 128:(kc + 1) * 128], hb[r3][:, kc * 128:(kc + 1) * 128], ident_b, [hk, "cstb"], [pk])
                yield
                cp("act", hT[:, :, i * 128:(i + 1) * 128], pT[r2][:, :].rearrange("p (k t) -> p k t", k=8), [pk],
                   ["hT%d" % i])
                yield
                for half in range(2):
                    for kc in range(8):
                        mm(pz[:, half * 512:(half + 1) * 512], hT[:, kc, i * 128:(i + 1) * 128],
                           win[:, kc, half * 512:(half + 1) * 512], kc == 0, kc == 7, ["hT%d" % i, "win%d" % kc], ["pz"])
                for kc in range(8):
                    mm(pdt[:, :], hT[:, kc, i * 128:(i + 1) * 128], win[:, kc, 3072:3088], kc == 0, kc == 7,
                       ["hT%d" % i, "win%d" % kc], ["pdt"])
                yield
                zk, dk = "zst%d" % r2, "dts%d" % r2
                cp("act", zst[r2][:], pz[:, :], ["pz"], [zk])
                cp("dve", dts[r2][:], pdt[:, :], ["pdt"], [dk])
                S.dma("sp", Zs[tok0:tok0 + 128, :], zst[r2][:], r=[zk], w=["Zs"], stream=zk)
                S.dma("sp", DTs[tok0:tok0 + 128, :], dts[r2][:], r=[dk], w=["DTs"], stream=dk)
                yield

            def chgen(b, ct):
                col0 = 1024 + ct * 128 if ct < 16 else 3088 + (ct - 16) * 128
                par = ct % 2
                xpk, xok, ak = "xpad%d" % par, "xo%d" % par, "acc%d" % par
                for blk in range(4):
                    pck = "pc%d" % (blk % 2)
                    for kc in range(8):
                        mm(pc[blk % 2][:, :], win[:, kc, col0:col0 + 128], hT[:, kc, blk * 512:(blk + 1) * 512],
                           kc == 0, kc == 7, ["win%d" % kc] + ["hT%d" % j for j in range(blk * 4, blk * 4 + 4)], [pck])
                    if ct < 16:
                        cp("act", xpad[par][:, 3 + blk * 512:3 + (blk + 1) * 512], pc[blk % 2][:, :], [pck], [xpk])
                    else:
                        cp("act", xo[par][:, blk * 512:(blk + 1) * 512], pc[blk % 2][:, :], [pck], [xok])
                    if blk % 2 == 1:
                        yield
                if ct < 16:
                    xp = xpad[par]
                    ts("dve", acc[par][:], xp[:, 3:3 + SEQ], cw[:, ct, 3:4], cb[:, ct:ct + 1], ALU.mult, ALU.add,
                       [xpk, "cw", "cb"], [ak])
                    yield
                    for k in (2, 1, 0):
                        stt(acc[par][:], xp[:, k:k + SEQ], cw[:, ct, k:k + 1], acc[par][:], ALU.mult, ALU.add,
                            [xpk, "cw", ak], [ak])
                        yield
                    act(xo[par][:], acc[par][:], AF.Silu, [ak], [xok])
                    yield
                    S.dma("sp", XCs[ct * 128:(ct + 1) * 128, b * SEQ:(b + 1) * SEQ], xo[par][:], r=[xok], w=["XCs"],
                          stream=xok)
                else:
                    S.dma("sp", UTs[(ct - 16) * 128:(ct - 15) * 128, b * SEQ:(b + 1) * SEQ], xo[par][:], r=[xok],
                          w=["UTs"], stream=xok)
                yield

            def run_skewed(gens, every):
                active = []
                k = 0
                while gens or active:
                    if gens and k % every == 0:
                        active.append(gens.pop(0))
                    for g_ in list(active):
                        try:
                            next(g_)
                        except StopIteration:
                            active.remove(g_)
                    k += 1

            for b in range(NB):
                S.dma("sp", G1[:], MODs[b:b + 1, 1024:2048].partition_broadcast(128), r=["MODs"], w=["G1"], stream="g1")
                S.dma("sp", SH1[:], MODs[b:b + 1, 0:1024].partition_broadcast(128), r=["MODs"], w=["SH1"], stream="sh1")
                run_skewed([tokgen(b, i) for i in range(16)], 1)
                run_skewed([chgen(b, ct) for ct in range(24)], 4)
            S.flush()
        if stop_after == 1:
            return nc

        with ExitStack() as ph:
            A = lambda n, sh, dt=F32: ph.enter_context(nc.sbuf_tensor("B_" + n, sh, dt))
            P = lambda n, sh, dt=F32: ph.enter_context(nc.psum_tensor("B_" + n, sh, dt))
            dtb = A("dtb", [128, 16]); abc = A("abc", [128, 16]); d16 = A("d16", [128, 16])
            dssd = A("dssd", [128, D]); normg = A("normg", [128, D])
            S32 = A("S32", [128, 16, 64]); Sbf = A("Sbf", [128, 16, 64], BF16)
            RC = 4
            xct = [A("xct%d" % i, [128, 16, 128], BF16) for i in range(RC)]
            zt = [A("zt%d" % i, [128, D], BF16) for i in range(RC)]
            dtr = [A("dtr%d" % i, [128, 16]) for i in range(RC)]
            dtt = [A("dtt%d" % i, [128, 16]) for i in range(RC)]
            da = [A("da%d" % i, [128, 16]) for i in range(RC)]
            c3 = [A("c3_%d" % i, [128, 48]) for i in range(RC)]
            e3 = [A("e3_%d" % i, [128, 48]) for i in range(RC)]
            xs = [A("xs%d" % i, [128, D], BF16) for i in range(RC)]
            Btok = [A("Btok%d" % i, [128, 512], BF16) for i in range(RC)]
            xdt = [A("xdt%d" % i, [128, D], BF16) for i in range(RC)]
            xdd = [A("xdd%d" % i, [128, D], BF16) for i in range(RC)]
            CBm = [A("CBm%d" % i, [128, 4, 128]) for i in range(RC)]
            RH = 3
            lD = [A("lD%d" % i, [128, 128]) for i in range(RH)]
            Lm = [A("Lm%d" % i, [128, 128]) for i in range(RH)]
            Mt = [A("Mt%d" % i, [128, 128], BF16) for i in range(RH)]
            yo = A("yo", [128, D]); y1 = A("y1", [128, D]); xD = A("xD", [128, D]); sz = A("sz", [128, D])
            yg = A("yg", [128, D]); junkb = A("junkb", [128, 256], BF16); ssq = A("ssq", [128, 4])
            yn = A("yn", [128, D]); ynb = A("ynb", [128, D], BF16)
            ynT = [A("ynT%d" % i, [128, 8, 128], BF16) for i in range(2)]
            pTr = P("pTr", [128, 1024], BF16)
            pDs = [P("pD%d" % i, [128, 512]) for i in range(2)]
            pSl = [P("pS%d" % i, [128, 512]) for i in range(2)]
            pPro = P("pPro", [128, 512])
            pY = P("pY", [128, 512])
            pYo = P("pYo", [128, 512])

            S.dma("sp", dtb[:], I["dt_bias"][0:1, :].partition_broadcast(128), w=["dtb"], stream="b0")
            S.dma("sp", abc[:], I["a_log"][0:1, :].partition_broadcast(128), w=["abc"], stream="b1")
            S.dma("sp", d16[:], I["d_ssd"][0:1, :].partition_broadcast(128), w=["d16"], stream="b2")
            S.dma("sp", normg[:], I["norm_ssd_g"][0:1, :].partition_broadcast(128), w=["normg"], stream="b3")
            act(abc[:], abc[:], AF.Exp, ["abc"], ["abc"])
            ts("dve", abc[:], abc[:], -1.0, None, ALU.mult, None, ["abc"], ["abc"])
            cp("dve", dssd[:, :].rearrange("p (h q) -> p h q", q=64), d16[:, :].unsqueeze(2).to_broadcast([128, 16, 64]),
               ["d16"], ["dssd"])
            XCv = XCs.rearrange("(ct p) t -> p ct t", p=128)
            YCv = YCs.rearrange("(ct p) t -> p ct t", p=128)
            v3 = lambda ap: ap.rearrange("p (h q) -> p h q", q=64)
            bc3 = lambda col: col.unsqueeze(2).to_broadcast([128, 16, 64])
            NCH = NB * 16

            def prologue(ci):
                r_ = ci % RC
                tok0 = ci * 128
                K_ = lambda nm: "%s%d" % (nm, r_)
                X = xct[r_]
                S.dma("sp", X[:], XCv[:, :, tok0:tok0 + 128], r=["XCs"], w=[K_("xct")], stream=K_("xct"))
                S.dma("sp", zt[r_][:], Zs[tok0:tok0 + 128, :], r=["Zs"], w=[K_("zt")], stream=K_("zt"))
                S.dma("sp", dtr[r_][:], DTs[tok0:tok0 + 128, :], r=["DTs"], w=[K_("dtr")], stream=K_("dtr"))
                yield
                tt("dve", dtt[r_][:], dtr[r_][:], dtb[:], ALU.add, [K_("dtr"), "dtb"], [K_("dtt")])
                yield
                act(dtt[r_][:], dtt[r_][:], AF.Exp, [K_("dtt")], [K_("dtt")])
                act(dtt[r_][:], dtt[r_][:], AF.Ln, [K_("dtt")], [K_("dtt")], bias=1.0)
                yield
                tt("dve", da[r_][:], dtt[r_][:], abc[:], ALU.mult, [K_("dtt"), "abc"], [K_("da")])
                yield
                mm(pPro[:, 0:16], triu_f, da[r_][:], True, True, ["cst", K_("da")], ["pm_cs"])
                mm(pPro[:, 16:32], ones_f, da[r_][:], True, True, ["cst", K_("da")], ["pm_cs"])
                cp("dve", c3[r_][:, 0:32], pPro[:, 0:32], ["pm_cs"], [K_("c3")])
                tt("dve", c3[r_][:, 32:48], c3[r_][:, 16:32], c3[r_][:, 0:16], ALU.subtract, [K_("c3")], [K_("c3")])
                yield
                act(e3[r_][:], c3[r_][:], AF.Exp, [K_("c3")], [K_("e3")])
                yield
                for j in range(8):
                    tr(pTr[:, j * 128:(j + 1) * 128], X[:, j, :], ident_b, [K_("xct"), "cstb"], ["pTr"])
                cp("act", xs[r_][:], pTr[:, :], ["pTr"], [K_("xs")])
                yield
                for g in range(4):
                    tr(pTr[:, g * 128:(g + 1) * 128], X[:, 8 + g, :], ident_b, [K_("xct"), "cstb"], ["pTr"])
                cp("act", Btok[r_][:], pTr[:, 0:512], ["pTr"], [K_("Btok")])
                yield
                tt("dve", v3(xdt[r_][:, :]), v3(xs[r_][:, :]), bc3(dtt[r_][:, :]), ALU.mult, [K_("xs"), K_("dtt")],
                   [K_("xdt")])
                tt("dve", v3(xdd[r_][:, :]), v3(xdt[r_][:, :]), bc3(e3[r_][:, 32:48]), ALU.mult, [K_("xdt"), K_("e3")],
                   [K_("xdd")])
                yield
                for g in range(4):
                    mm(pPro[:, 128:256], X[:, 8 + g, :], X[:, 12 + g, :], True, True, [K_("xct")], ["pm_cb"])
                    tt("dve", CBm[r_][:, g, :], pPro[:, 128:256], triu_f, ALU.mult, ["pm_cb", "cst"], [K_("CBm") + "_%d" % g])
                    yield

            def epilogue(ci):
                r_ = ci % RC
                tok0 = ci * 128
                c = ci % 16
                K_ = lambda nm: "%s%d" % (nm, r_)
                yield
                tt("pool", xD[:], xs[r_][:], dssd[:], ALU.mult, [K_("xs"), "dssd"], ["xD"])
                yield
                tt("dve", y1[:], y1[:], xD[:], ALU.add, ["y1", "xD"], ["y1"])
                act(sz[:], zt[r_][:], AF.Silu, [K_("zt")], ["sz"])
                yield
                tt("dve", yg[:], y1[:], sz[:], ALU.mult, ["y1", "sz"], ["yg"])
                yield
                for G_ in range(4):
                    act(junkb[:], yg[:, G_ * 256:(G_ + 1) * 256], AF.Square, ["yg"], ["junkb", "ssq"],
                        accum_out=ssq[:, G_:G_ + 1])
                yield
                ts("dve", ssq[:], ssq[:], 1.0 / 256, EPS, ALU.mult, ALU.add, ["ssq"], ["ssq"])
                yield
                act(ssq[:], ssq[:], AF.Sqrt, ["ssq"], ["ssq"])
                yield
                S.op("dve", lambda e: e.reciprocal(out=ssq[:], in_=ssq[:]), r=["ssq"], w=["ssq"])
                tt("dve", yn[:, :].rearrange("p (g q) -> p g q", q=256), yg[:, :].rearrange("p (g q) -> p g q", q=256),
                   ssq[:, :].unsqueeze(2).to_broadcast([128, 4, 256]), ALU.mult, ["yg", "ssq"], ["yn"])
                yield
                tt("pool", ynb[:], yn[:], normg[:], ALU.mult, ["yn", "normg"], ["ynb"])
                yield
                for j in range(8):
                    tr(pTr[:, j * 128:(j + 1) * 128], ynb[:, j * 128:(j + 1) * 128], ident_b, ["ynb", "cstb"], ["pTr"])
                yk = "ynT%d" % (ci % 2)
                cp("act", ynT[ci % 2][:, :, :], pTr[:, :].rearrange("p (k t) -> p k t", k=8), ["pTr"], [yk])
                S.dma("sp", YCv[:, 0:8, tok0:tok0 + 128], ynT[ci % 2][:, :, :], r=[yk], w=["YCs"], stream=yk)
                yield

            def e1_half(ci, hf):
                r_ = ci % RC
                cs_ = slice(hf * 512, (hf + 1) * 512)
                v3h = lambda ap: ap.rearrange("p (h q) -> p h q", q=64)
                if ci % 16 == 0:
                    cp("dve", y1[:, cs_], pY[:, :], ["pY"], ["y1"])
                else:
                    tt("dve", v3h(yo[:, cs_]), v3h(pYo[:, :]),
                       e3[r_][:, hf * 8:hf * 8 + 8].unsqueeze(2).to_broadcast([128, 8, 64]), ALU.mult,
                       ["pYo", "e3_%d" % r_], ["yo"])
                    tt("dve", y1[:, cs_], yo[:, cs_], pY[:, :], ALU.add, ["yo", "pY"], ["y1"])

            def hinfo(n):
                ci, h = n // 16, n % 16
                return ci, h, h // 4, ci % RC, n % RH, ci % 16

            def h_a(n):
                ci, h, g, r_, hr, c = hinfo(n)
                tt("pool", lD[hr][:], tris_f, da[r_][:, h:h + 1].to_broadcast([128, 128]), ALU.mult,
                   ["cst", "da%d" % r_], ["lD%d" % hr])

            def h_b(n):
                ci, h, g, r_, hr, c = hinfo(n)
                mm(pDs[n % 2][:, 0:128], lD[hr][:], triu_f, True, True, ["lD%d" % hr, "cst"], ["pD%d" % (n % 2)])

            def h_c(n):
                ci, h, g, r_, hr, c = hinfo(n)
                act(Lm[hr][:], pDs[n % 2][:, 0:128], AF.Exp, ["pD%d" % (n % 2)], ["Lm%d" % hr])

            def h_d(n):
                ci, h, g, r_, hr, c = hinfo(n)
                tt("dve", Mt[hr][:], Lm[hr][:], CBm[r_][:, g, :], ALU.mult, ["Lm%d" % hr, "CBm%d_%d" % (r_, g)],
                   ["Mt%d" % hr])

            def h_e(n):
                ci, h, g, r_, hr, c = hinfo(n)
                X = xct[r_]
                mm(pY[:, (h % 8) * 64:(h % 8 + 1) * 64], Mt[hr][:], xdt[r_][:, h * 64:(h + 1) * 64], True, True,
                   ["Mt%d" % hr, "xdt%d" % r_], ["pY"])
                if c != 0:
                    mm(pYo[:, (h % 8) * 64:(h % 8 + 1) * 64], X[:, 12 + g, :], Sbf[:, h, :], True, True,
                       ["xct%d" % r_, "Sbf%d" % h], ["pYo"])
                mm(pSl[n % 2][:, 0:64], Btok[r_][:, g * 128:(g + 1) * 128], xdd[r_][:, h * 64:(h + 1) * 64],
                   True, True, ["Btok%d" % r_, "xdd%d" % r_], ["pS%d" % (n % 2)])

            def h_f(n):
                ci, h, g, r_, hr, c = hinfo(n)
                if c == 0:
                    cp("dve", S32[:, h, :], pSl[n % 2][:, 0:64], ["pS%d" % (n % 2)], ["S32_%d" % h])
                else:
                    stt(S32[:, h, :], S32[:, h, :], e3[r_][:, 16 + h:17 + h], pSl[n % 2][:, 0:64],
                        ALU.mult, ALU.add, ["S32_%d" % h, "e3_%d" % r_, "pS%d" % (n % 2)], ["S32_%d" % h])

            def h_g(n):
                ci, h, g, r_, hr, c = hinfo(n)
                cp("pool", Sbf[:, h, :], S32[:, h, :], ["S32_%d" % h], ["Sbf%d" % h])

            stages = [h_a, h_b, h_c, h_d, h_e, h_f, h_g]
            NS = len(stages)
            for _ in prologue(0):
                pass
            for _ in prologue(1):
                pass
            side = []
            NI_ = NCH * 16
            for k in range(NI_ + NS - 1):
                for s_ in reversed(range(NS)):
                    n = k - s_
                    if 0 <= n < NI_:
                        stages[s_](n)
                if k >= 11 and (k - 11) % 16 == 0:
                    e1_half((k - 11) // 16, 0)
                if k >= 19 and (k - 19) % 16 == 0:
                    e1_half((k - 19) // 16, 1)
                    side.append(epilogue((k - 19) // 16))
                if k % 16 == 0 and k // 16 + 2 < NCH:
                    side.append(prologue(k // 16 + 2))
                for g_ in list(side):
                    try:
                        next(g_)
                    except StopIteration:
                        side.remove(g_)
            while side:
                for g_ in list(side):
                    try:
                        next(g_)
                    except StopIteration:
                        side.remove(g_)
            S.flush()
        if stop_after == 2:
            return nc

        with ExitStack() as ph:
            A = lambda n, sh, dt=F32: ph.enter_context(nc.sbuf_tensor(n, sh, dt))
            Blk1 = A("Blk1", [128, 64, 128], BF16)
            Blk2 = A("Blk2", [128, 64, 128], BF16)
            CL1 = A("CL1", [128, 64, 16], BF16)
            CL2 = A("CL2", [128, 64, 16], BF16)
            rcol = A("rcol", [128, 64])
            fcol = A("fcol", [128, 64])
            D5c = A("D5c", [128, 8])
            glub = A("glub", [128, 8])
            g5c = A("g5c", [128, 8])
            Wg = A("Wg", [128, 8, 128], BF16)
            rm8 = cst[:, C_RM8:C_RM8 + 8]
            INV2PI = 1.0 / TWO_PI

            def sin_turns(out_, f_ap, tk, tf, keys_in, kout, e1="dve", e2="dve"):
                ts(e1, tk, f_ap, MAGIC, MAGIC, ALU.add, ALU.subtract, keys_in, ["_tk"])
                tt(e2, tf, f_ap, tk, ALU.subtract, keys_in + ["_tk"], ["_tf"])
                act(out_, tf, AF.Sin, ["_tf"], [kout], scale=TWO_PI)

            with ExitStack() as p0:
                B_ = lambda n, sh, dt=F32: p0.enter_context(nc.sbuf_tensor(n, sh, dt))
                lrT = B_("lrT", [128, 64]); liT = B_("liT", [128, 64]); dtg = B_("dtg", [128, 64])
                ldt = B_("ldt", [128, 64]); f2 = B_("f2", [128, 64]); tk = B_("tk", [128, 64]); tf = B_("tf", [128, 64])
                sn = B_("sn", [128, 64]); cs_ = B_("cs_", [128, 64]); lbr = B_("lbr", [128, 64]); lbi = B_("lbi", [128, 64])
                den = B_("den", [128, 64]); t_a = B_("t_a", [128, 64]); t_b = B_("t_b", [128, 64])
                cre = B_("cre", [128, 64]); cim = B_("cim", [128, 64])
                br = B_("br", [64, 64, 16]); bi = B_("bi", [64, 64, 16])
                bbr = B_("bbr", [64, 64, 16]); bbi = B_("bbi", [64, 64, 16]); t_c = B_("t_c", [64, 64, 16])
                Dre = B_("Dre", [128, 8, 64]); Dim = B_("Dim", [128, 8, 64]); nDre = B_("nDre", [128, 8, 64])
                cc1 = B_("cc1", [128, 128]); cc2 = B_("cc2", [128, 128]); wrow = B_("wrow", [128, 8, 16])
                ptc = p0.enter_context(nc.psum_tensor("ptc", [128, 128], F32))
                for half in range(2):
                    S.dma("sp", lrT[half * 64:(half + 1) * 64, :], I["s5_a_re"].rearrange("g p -> p g"), w=["lrT"],
                          stream="c0a", allow_slow_non_contiguous=True)
                    S.dma("sp", liT[half * 64:(half + 1) * 64, :], I["s5_a_im"].rearrange("g p -> p g"), w=["liT"],
                          stream="c0b", allow_slow_non_contiguous=True)
                S.dma("sp", dtg[:], I["s5_log_dt"][0:1, :].partition_broadcast(128), w=["dtg"], stream="c0c")
                act(dtg[:], dtg[:], AF.Exp, ["dtg"], ["dtg"])
                tt("dve", ldt[:], lrT[:], dtg[:], ALU.mult, ["lrT", "dtg"], ["ldt"])
                act(rcol[:], ldt[:], AF.Exp, ["ldt"], ["rcol"])
                tt("dve", fcol[:], liT[:], dtg[:], ALU.mult, ["liT", "dtg"], ["fcol"])
                ts("dve", fcol[:], fcol[:], INV2PI, None, ALU.mult, None, ["fcol"], ["fcol"])
                sin_turns(sn[:], fcol[:], tk[:], tf[:], ["fcol"], "sn")
                ts("dve", f2[:], fcol[:], 0.25, None, ALU.add, None, ["fcol"], ["f2"])
                sin_turns(cs_[:], f2[:], tk[:], tf[:], ["f2"], "cs_")
                tt("dve", lbr[:], rcol[:], cs_[:], ALU.mult, ["rcol", "cs_"], ["lbr"])
                tt("dve", lbi[:], rcol[:], sn[:], ALU.mult, ["rcol", "sn"], ["lbi"])
                ts("dve", lbr[:], lbr[:], -1.0, None, ALU.add, None, ["lbr"], ["lbr"])
                tt("dve", den[:], lrT[:], lrT[:], ALU.mult, ["lrT"], ["den"])
                tt("dve", t_a[:], liT[:], liT[:], ALU.mult, ["liT"], ["t_a"])
                tt("dve", den[:], den[:], t_a[:], ALU.add, ["den", "t_a"], ["den"])
                S.op("dve", lambda e: e.reciprocal(out=den[:], in_=den[:]), r=["den"], w=["den"])
                tt("dve", t_a[:], lbr[:], lrT[:], ALU.mult, ["lbr", "lrT"], ["t_a"])
                tt("dve", t_b[:], lbi[:], liT[:], ALU.mult, ["lbi", "liT"], ["t_b"])
                tt("dve", t_a[:], t_a[:], t_b[:], ALU.add, ["t_a", "t_b"], ["t_a"])
                tt("dve", cre[:], t_a[:], den[:], ALU.mult, ["t_a", "den"], ["cre"])
                tt("dve", t_a[:], lbi[:], lrT[:], ALU.mult, ["lbi", "lrT"], ["t_a"])
                tt("dve", t_b[:], lbr[:], liT[:], ALU.mult, ["lbr", "liT"], ["t_b"])
                tt("dve", t_a[:], t_a[:], t_b[:], ALU.subtract, ["t_a", "t_b"], ["t_a"])
                tt("dve", cim[:], t_a[:], den[:], ALU.mult, ["t_a", "den"], ["cim"])
                for q4 in range(4):
                    gs = slice(q4 * 16, (q4 + 1) * 16)
                    S.dma("sp", br[:, gs, :], I["s5_b_re"][gs].rearrange("g p h -> p g h"), w=["br"], stream="c0d")
                    S.dma("sp", bi[:, gs, :], I["s5_b_im"][gs].rearrange("g p h -> p g h"), w=["bi"], stream="c0e")
                bcr = cre[0:64, :].unsqueeze(2).to_broadcast([64, 64, 16])
                bci = cim[0:64, :].unsqueeze(2).to_broadcast([64, 64, 16])
                tt("dve", bbr[:], br[:], bcr, ALU.mult, ["br", "cre"], ["bbr"])
                tt("dve", t_c[:], bi[:], bci, ALU.mult, ["bi", "cim"], ["t_c"])
                tt("dve", bbr[:], bbr[:], t_c[:], ALU.subtract, ["bbr", "t_c"], ["bbr"])
                tt("dve", bbi[:], bi[:], bcr, ALU.mult, ["bi", "cre"], ["bbi"])
                tt("dve", t_c[:], br[:], bci, ALU.mult, ["br", "cim"], ["t_c"])
                tt("dve", bbi[:], bbi[:], t_c[:], ALU.add, ["bbi", "t_c"], ["bbi"])
                for j in range(8):
                    tr(ptc[:, 0:64], bbr[:, j * 8:(j + 1) * 8, :].rearrange("p g h -> p (g h)"), ident_f[0:64, 0:64], ["bbr", "cst"], ["ptc"])
                    cp("dve", Dre[:, j, :], ptc[:, 0:64], ["ptc"], ["Dre"])
                    tr(ptc[:, 64:128], bbi[:, j * 8:(j + 1) * 8, :].rearrange("p g h -> p (g h)"), ident_f[0:64, 0:64], ["bbi", "cst"], ["ptc2"])
                    cp("dve", Dim[:, j, :], ptc[:, 64:128], ["ptc2"], ["Dim"])
                ts("dve", nDre[:], Dre[:], -1.0, None, ALU.mult, None, ["Dre"], ["nDre"])
                rmb = rm8.unsqueeze(2).to_broadcast([128, 8, 64])
                for j in range(8):
                    gs = slice(j * 8, (j + 1) * 8)
                    bcD = lambda t: t[:, j, :].unsqueeze(1).to_broadcast([128, 8, 64])
                    tt("dve", Blk1[:, gs, 0:64], bcD(Dre), rmb, ALU.mult, ["Dre", "cst"], ["Blk1"])
                    tt("dve", Blk1[:, gs, 64:128], bcD(Dim), rmb, ALU.mult, ["Dim", "cst"], ["Blk1"])
                    tt("dve", Blk2[:, gs, 0:64], bcD(Dim), rmb, ALU.mult, ["Dim", "cst"], ["Blk2"])
                    tt("dve", Blk2[:, gs, 64:128], bcD(nDre), rmb, ALU.mult, ["nDre", "cst"], ["Blk2"])
                crv = I["s5_c_re"].rearrange("g h p -> (g h) p")
                civ = I["s5_c_im"].rearrange("g h p -> (g h) p")
                for j in range(8):
                    rs_ = slice(j * 128, (j + 1) * 128)
                    S.dma("sp", cc1[:, 0:64], crv[rs_, :], w=["cc1"], stream="c0f")
                    S.dma("sp", cc1[:, 64:128], civ[rs_, :], w=["cc1"], stream="c0f")
                    S.dma("sp", cc2[:, 0:64], civ[rs_, :], w=["cc2"], stream="c0g")
                    S.dma("sp", cc2[:, 64:128], crv[rs_, :], w=["cc2"], stream="c0g")
                    tr(ptc[:, :], cc1[:], ident_f, ["cc1", "cst"], ["ptc", "ptc2"])
                    gs = slice(j * 8, (j + 1) * 8)
                    cp("dve", CL1[0:64, gs, :], ptc[0:64, :].rearrange("p (g h) -> p g h", h=16), ["ptc"], ["CL1"])
                    ts("dve", CL1[64:128, gs, :], ptc[64:128, :].rearrange("p (g h) -> p g h", h=16), -1.0, None,
                       ALU.mult, None, ["ptc"], ["CL1"])
                    tr(ptc[:, :], cc2[:], ident_f, ["cc2", "cst"], ["ptc", "ptc2"])
                    ts("dve", CL2[:, gs, :], ptc[:, :].rearrange("p (g h) -> p g h", h=16), -1.0, None, ALU.mult, None,
                       ["ptc"], ["CL2"])
                S.dma("sp", D5c[:], I["s5_d"].rearrange("(j gl) h -> (gl h) j", gl=8), w=["D5c"], stream="c0h",
                      allow_slow_non_contiguous=True)
                S.dma("sp", glub[:], I["glu_b"].rearrange("(j gl) h -> (gl h) j", gl=8), w=["glub"], stream="c0i",
                      allow_slow_non_contiguous=True)
                S.dma("sp", g5c[:], I["norm_s5_g"].rearrange("o (j p) -> p (o j)", p=128), w=["g5c"], stream="c0j",
                      allow_slow_non_contiguous=True)
                S.dma("sp", wrow[:], I["glu_w"].rearrange("(j gl) h k -> (gl h) j k", gl=8), w=["wrow"], stream="c0k")
                for j in range(8):
                    tt("dve", Wg[:, j, :].rearrange("p (g k) -> p g k", k=16),
                       wrow[:, j, :].unsqueeze(1).to_broadcast([128, 8, 16]),
                       rm8.unsqueeze(2).to_broadcast([128, 8, 16]), ALU.mult, ["wrow", "cst"], ["Wg"])
                S.flush()

            with ExitStack() as p1:
                B_ = lambda n, sh, dt=F32: p1.enter_context(nc.sbuf_tensor(n, sh, dt))
                Pp = lambda n, sh, dt=F32: p1.enter_context(nc.psum_tensor(n, sh, dt))
                iot = B_("iot", [128, SEQ])
                SIN = [B_("SIN%d" % i, [128, SEQ], BF16) for i in range(3)]
                COS = [B_("COS%d" % i, [128, SEQ], BF16) for i in range(3)]
                u1 = B_("u1", [128, SEQ]); k1 = B_("k1", [128, SEQ]); fr = B_("fr", [128, SEQ])
                uT = [B_("uT%d" % i, [128, T], BF16) for i in range(2)]
                R3 = 3
                p1b = [B_("p1b%d" % i, [128, 512], BF16) for i in range(R3)]
                p2b = [B_("p2b%d" % i, [128, 512], BF16) for i in range(R3)]
                w1 = [B_("w1_%d" % i, [128, 512], BF16) for i in range(R3)]
                w2 = [B_("w2_%d" % i, [128, 512], BF16) for i in range(R3)]
                ww = [B_("ww%d" % i, [128, 512]) for i in range(R3)]
                zz = [[B_("zz%d_%d" % (bb, i), [128, 512]) for i in range(R3)] for bb in range(NB)]
                zb = [B_("zb%d" % i, [128, 512], BF16) for i in range(R3)]
                v1 = [B_("v1_%d" % i, [128, 512], BF16) for i in range(R3)]
                v2 = [B_("v2_%d" % i, [128, 512], BF16) for i in range(R3)]
                ysm = [B_("ysm%d" % i, [16, 512]) for i in range(R3)]
                P1 = [Pp("P1_%d" % i, [128, 512]) for i in range(2)]
                P2 = [Pp("P2_%d" % i, [128, 512]) for i in range(2)]
                uf = [B_("uf%d" % i, [128, D]) for i in range(2)]
                vf = [B_("vf%d" % i, [128, D]) for i in range(2)]
                ub = [B_("ub%d" % i, [128, D], BF16) for i in range(2)]
                vbt = [B_("vbt%d" % i, [128, D], BF16) for i in range(2)]
                uts = [B_("uts%d" % i, [128, D], BF16) for i in range(2)]
                pTu = [Pp("pTu%d" % i, [128, 1024], BF16) for i in range(2)]

                def m0_tile(et):
                    pr = et % 2
                    rows = slice(et * 128, (et + 1) * 128)
                    S.dma("sp", uf[pr][:], I["expert_u"][rows, :], w=["uf%d" % pr], stream="uf%d" % pr)
                    S.dma("sp", vf[pr][:], I["expert_v"][rows, :], w=["vf%d" % pr], stream="vf%d" % pr)
                    yield
                    cp("act", ub[pr][:], uf[pr][:], ["uf%d" % pr], ["ub%d" % pr])
                    cp("act", vbt[pr][:], vf[pr][:], ["vf%d" % pr], ["vbt%d" % pr])
                    yield
                    for kc in range(8):
                        tr(pTu[pr][:, kc * 128:(kc + 1) * 128], ub[pr][:, kc * 128:(kc + 1) * 128], ident_b,
                           ["ub%d" % pr, "cstb"], ["pTu%d" % pr])
                    S.dma("sp", Vb[rows, :], vbt[pr][:], r=["vbt%d" % pr], w=["Vb"], stream="vbo%d" % pr)
                    yield
                    cp("act", uts[pr][:], pTu[pr][:, :], ["pTu%d" % pr], ["uts%d" % pr])
                    yield
                    S.dma("sp", UTb[et], uts[pr][:], r=["uts%d" % pr], w=["UTb"], stream="uto%d" % pr)
                    yield
                PY = [Pp("PY%d" % i, [128, 512]) for i in range(2)]
                for k in range(16):
                    ts("dve", iot[:, k * 128:(k + 1) * 128], cst[:, C_IOTA:C_IOTA + 128], float(128 * k), None, ALU.add,
                       None, ["cst"], ["iot"])

                def tables(g):
                    gp = g % 3
                    fg = fcol[:, g:g + 1]
                    for (tab, key, off) in ((SIN[gp], "SIN%d" % gp, 0.0), (COS[gp], "COS%d" % gp, 0.25)):
                        act(u1[:], iot[:], AF.Identity, ["iot", "fcol"], ["u1"], scale=fg, bias=off)
                        yield
                        act(k1[:], u1[:], AF.Identity, ["u1"], ["k1"], scale=1.0, bias=MAGIC)
                        yield
                        stt(fr[:], k1[:], MAGIC, u1[:], ALU.subtract, ALU.subtract, ["u1", "k1"], ["fr"])
                        yield
                        act(tab[:], fr[:], AF.Sin, ["fr"], [key], scale=-TWO_PI)
                        yield

                pieces = [(g, q, bb) for g in range(64) for q in range(4) for bb in range(NB)]

                def info(n):
                    g, q, bb = pieces[n]
                    return g, q, bb, g // 8, g % 3, n % R3, q * 512, bb * SEQ + q * 512

                def st_a(n):
                    g, q, bb, j, gp, pb, t0, tok = info(n)
                    if q == 0 and bb == 0 and g + 1 < 64:
                        side.append(tables(g + 1))
                    if g % 8 == 0 and q == 0 and bb == 0:
                        S.dma("sp", uT[j % 2][:], UTs[j * 128:(j + 1) * 128, :], r=["UTs"], w=["uT%d" % (j % 2)],
                              stream="uT%d" % (j % 2))
                    uk = "uT%d" % (j % 2)
                    mm(P1[n % 2][:, :], Blk1[:, g, :], uT[j % 2][:, tok:tok + 512], True, True, ["Blk1", uk], ["P1_%d" % (n % 2)])
                    mm(P2[n % 2][:, :], Blk2[:, g, :], uT[j % 2][:, tok:tok + 512], True, True, ["Blk2", uk], ["P2_%d" % (n % 2)])

                def st_b(n):
                    g, q, bb, j, gp, pb, t0, tok = info(n)
                    cp("act", p1b[pb][:], P1[n % 2][:, :], ["P1_%d" % (n % 2)], ["p1b%d" % pb])
                    cp("act", p2b[pb][:], P2[n % 2][:, :], ["P2_%d" % (n % 2)], ["p2b%d" % pb])

                def st_c(n):
                    g, q, bb, j, gp, pb, t0, tok = info(n)
                    tt("dve", w1[pb][:], p1b[pb][:], COS[gp][:, t0:t0 + 512], ALU.mult, ["p1b%d" % pb, "COS%d" % gp],
                       ["w1_%d" % pb])
                    tt("dve", w2[pb][:], p2b[pb][:], SIN[gp][:, t0:t0 + 512], ALU.mult, ["p2b%d" % pb, "SIN%d" % gp],
                       ["w2_%d" % pb])

                def st_d(n):
                    g, q, bb, j, gp, pb, t0, tok = info(n)
                    tt("pool", ww[pb][:], w1[pb][:], w2[pb][:], ALU.add, ["w1_%d" % pb, "w2_%d" % pb], ["ww%d" % pb])

                def st_e(n):
                    g, q, bb, j, gp, pb, t0, tok = info(n)
                    zc, zp = zz[bb][q % R3], zz[bb][(q - 1) % R3]
                    zck, zpk = "zz%d_%d" % (bb, q % R3), "zz%d_%d" % (bb, (q - 1) % R3)
                    init = 0.0 if q == 0 else zp[:, 511:512]
                    S.op("dve", lambda e: e.tensor_tensor_scan(
                        out=zc[:], data0=rcol[:, g:g + 1].to_broadcast([128, 512]), data1=ww[pb][:],
                        initial=init, op0=ALU.mult, op1=ALU.add), r=["rcol", "ww%d" % pb, zpk], w=[zck])

                def st_f(n):
                    g, q, bb, j, gp, pb, t0, tok = info(n)
                    cp("act", zb[pb][:], zz[bb][q % R3][:], ["zz%d_%d" % (bb, q % R3)], ["zb%d" % pb])

                def st_g(n):
                    g, q, bb, j, gp, pb, t0, tok = info(n)
                    tt("dve", v1[pb][:], zb[pb][:], COS[gp][:, t0:t0 + 512], ALU.mult, ["zb%d" % pb, "COS%d" % gp],
                       ["v1_%d" % pb])
                    tt("pool", v2[pb][:], zb[pb][:], SIN[gp][:, t0:t0 + 512], ALU.mult, ["zb%d" % pb, "SIN%d" % gp],
                       ["v2_%d" % pb])

                def st_h(n):
                    g, q, bb, j, gp, pb, t0, tok = info(n)
                    pp = n % 2
                    mm(PY[pp][0:16, :], CL1[:, g, :], v1[pb][:], True, False, ["CL1", "v1_%d" % pb], ["PY%d" % pp])
                    mm(PY[pp][0:16, :], CL2[:, g, :], v2[pb][:], False, True, ["CL2", "v2_%d" % pb], ["PY%d" % pp])

                def st_i(n):
                    g, q, bb, j, gp, pb, t0, tok = info(n)
                    pp = n % 2
                    yk = "ysm%d" % pb
                    cp("act", ysm[pb][0:16, :], PY[pp][0:16, :], ["PY%d" % pp], [yk])
                    S.dma("sp", Y5s[g * 16:(g + 1) * 16, tok:tok + 512], ysm[pb][0:16, :], r=[yk], w=["Y5s"], stream=yk)

                for _ in tables(0):
                    pass
                stages_c = [st_a, st_b, st_c, st_d, st_e, st_f, st_g, st_h, st_i]
                side = []
                nxt_et = 0
                for k in range(len(pieces) + len(stages_c) - 1):
                    for s_ in reversed(range(len(stages_c))):
                        n = k - s_
                        if 0 <= n < len(pieces):
                            stages_c[s_](n)
                    if k % 4 == 0 and nxt_et < 128:
                        side.append(m0_tile(nxt_et))
                        nxt_et += 1
                    for g_ in list(side):
                        try:
                            next(g_)
                        except StopIteration:
                            side.remove(g_)
                while side or nxt_et < 128:
                    if nxt_et < 128:
                        side.append(m0_tile(nxt_et))
                        nxt_et += 1
                    for g_ in list(side):
                        try:
                            next(g_)
                        except StopIteration:
                            side.remove(g_)
                S.flush()

            with ExitStack() as p2:
                B_ = lambda n, sh, dt=F32: p2.enter_context(nc.sbuf_tensor("C2_" + n, sh, dt))
                Pp = lambda n, sh, dt=F32: p2.enter_context(nc.psum_tensor("C2_" + n, sh, dt))
                y5 = [B_("y5_%d" % i, [128, 512]) for i in range(3)]
                uu = [B_("uu%d" % i, [128, 512], BF16) for i in range(3)]
                yv = [B_("yv%d" % i, [128, 512]) for i in range(3)]
                vb = [B_("vb%d" % i, [128, 512], BF16) for i in range(3)]
                sg = [B_("sg%d" % i, [128, 512]) for i in range(3)]
                oo = [B_("oo%d" % i, [128, 8, 512]) for i in range(2)]
                sq = [B_("sq%d" % i, [128, 512]) for i in range(3)]
                rs5 = [B_("rs5_%d" % i, [128, 512]) for i in range(2)]
                ycb = [B_("ycb%d" % i, [128, 512], BF16) for i in range(2)]
                PG = [Pp("PG%d" % i, [128, 512]) for i in range(2)]
                PSS = [Pp("PSS%d" % i, [128, 512]) for i in range(2)]
                NBK = T // 512

                def cinfo(n):
                    return n // 8, n % 8, n % 3, (n // 8) * 512

                def c_a(n):
                    blk, j, r3, tok = cinfo(n)
                    S.dma("sp", y5[r3][:], Y5s[j * 128:(j + 1) * 128, tok:tok + 512], r=["Y5s"], w=["y5_%d" % r3],
                          stream="y5_%d" % r3)
                    S.dma("sp", uu[r3][:], UTs[j * 128:(j + 1) * 128, tok:tok + 512], r=["UTs"], w=["uu%d" % r3],
                          stream="uu%d" % r3)

                def c_b(n):
                    blk, j, r3, tok = cinfo(n)
                    stt(yv[r3][:], uu[r3][:], D5c[:, j:j + 1], y5[r3][:], ALU.mult, ALU.add,
                        ["uu%d" % r3, "D5c", "y5_%d" % r3], ["yv%d" % r3])

                def c_c(n):
                    blk, j, r3, tok = cinfo(n)
                    act(vb[r3][:], yv[r3][:], AF.Gelu, ["yv%d" % r3], ["vb%d" % r3])

                def c_d(n):
                    blk, j, r3, tok = cinfo(n)
                    mm(PG[n % 2][:, :], Wg[:, j, :], vb[r3][:], True, True, ["Wg", "vb%d" % r3], ["PG%d" % (n % 2)])

                def c_e(n):
                    blk, j, r3, tok = cinfo(n)
                    act(sg[r3][:], PG[n % 2][:, :], AF.Sigmoid, ["PG%d" % (n % 2), "glub"], ["sg%d" % r3],
                        bias=glub[:, j:j + 1])

                def c_f(n):
                    blk, j, r3, tok = cinfo(n)
                    tt("dve", oo[blk % 2][:, j, :], vb[r3][:], sg[r3][:], ALU.mult, ["vb%d" % r3, "sg%d" % r3],
                       ["oo%d_%d" % (blk % 2, j)])

                def c_g(n):
                    blk, j, r3, tok = cinfo(n)
                    tt("pool", sq[r3][:], oo[blk % 2][:, j, :], oo[blk % 2][:, j, :], ALU.mult,
                       ["oo%d_%d" % (blk % 2, j)], ["sq%d" % r3])

                def c_h(n):
                    blk, j, r3, tok = cinfo(n)
                    mm(PSS[blk % 2][:, :], ones_f, sq[r3][:], j == 0, j == 7, ["cst", "sq%d" % r3], ["PSS%d" % (blk % 2)])

                def c_tail(blk):
                    bp = blk % 2
                    tok = blk * 512
                    rk = "rs5_%d" % bp
                    ts("dve", rs5[bp][:], PSS[bp][:, :], 1.0 / 1024, EPS, ALU.mult, ALU.add, ["PSS%d" % bp], [rk])
                    yield
                    act(rs5[bp][:], rs5[bp][:], AF.Sqrt, [rk], [rk])
                    yield
                    S.op("dve", lambda e: e.reciprocal(out=rs5[bp][:], in_=rs5[bp][:]), r=[rk], w=[rk])
                    yield
                    for j in range(8):
                        jp = j % 2
                        stt(ycb[jp][:], oo[bp][:, j, :], g5c[:, j:j + 1], rs5[bp][:], ALU.mult, ALU.mult,
                            ["oo%d_%d" % (bp, j), "g5c", rk], ["ycb%d" % jp])
                        S.dma("sp", YCs[1024 + j * 128:1024 + (j + 1) * 128, tok:tok + 512], ycb[jp][:],
                              r=["ycb%d" % jp], w=["YCs"], stream="ycb%d" % jp)
                        yield

                st2 = [c_a, c_b, c_c, c_d, c_e, c_f, c_g, c_h]
                NI2 = NBK * 8
                side2 = []
                for k in range(NI2 + len(st2) - 1):
                    for s_ in reversed(range(len(st2))):
                        n = k - s_
                        if 0 <= n < NI2:
                            st2[s_](n)
                    nh = k - (len(st2) - 1)
                    if nh >= 0 and nh % 8 == 7:
                        side2.append(c_tail(nh // 8))
                    for g_ in list(side2):
                        try:
                            next(g_)
                        except StopIteration:
                            side2.remove(g_)
                while side2:
                    for g_ in list(side2):
                        try:
                            next(g_)
                        except StopIteration:
                            side2.remove(g_)
                S.flush()
        if stop_after == 3:
            return nc

        with ExitStack() as ph:
            A = lambda n, sh, dt=F32: ph.enter_context(nc.sbuf_tensor("D_" + n, sh, dt))
            P = lambda n, sh, dt=F32: ph.enter_context(nc.psum_tensor("D_" + n, sh, dt))
            wout = A("wout", [128, 16, D], BF16)
            wq = A("wq", [128, 8, 2048], BF16)
            skf = A("skf", [128, 16, 128])
            skT = A("skT", [128, 16, 128], BF16)
            GT1 = A("GT1", [128, D]); G2 = A("G2", [128, D]); SH2 = A("SH2", [128, D])
            yct = [A("yct%d" % i, [128, 16, 128], BF16) for i in range(2)]
            xin = [A("xin%d" % i, [128, D]) for i in range(2)]
            t1 = A("t1", [128, D]); x1 = [A("x1_%d" % i, [128, D]) for i in range(2)]
            junk = A("junk", [128, D], BF16)
            ss2 = A("ss2", [128, 32])
            hb2 = A("hb2", [128, D], BF16)
            h2T = [A("h2T%d" % i, [128, 8, 128], BF16) for i in range(2)]
            qT = A("qT", [128, 16, 128], BF16)
            scb = [A("sc_%d" % i, [128, 16, 128]) for i in range(2)]; sc2 = A("sc2", [128, 16, 128])
            v8 = A("v8", [128, 16, 16]); i8 = A("i8", [128, 16, 16], U32); i8f = A("i8f", [128, 16, 16])
            cand = A("cand", [128, 8, 256]); cand2 = A("cand2", [128, 8, 256])
            c8 = A("c8", [128, 8, 16]); p8 = A("p8", [128, 8, 16], U32)
            ge = A("ge", [128, 8, 16]); gs = A("gs", [128, 8]); gg = A("gg", [128, 8, 16])
            ra_i = A("ra_i", [128, 128], I32); rb_i = A("rb_i", [128, 128], I32)
            raf = A("raf", [128, 128]); rbf = A("rbf", [128, 128])
            oh = A("oh", [128, 128, 16]); oh2 = A("oh2", [128, 128, 16])
            isel = A("isel", [128, 128]); jsel = A("jsel", [128, 128])
            rstg = [A("rstg%d" % i, [128, 3, 128], BF16) for i in range(2)]
            pT = P("pT", [128, 1024], BF16)
            pM = P("pM", [128, 1024])
            pq = [P("pq%d" % i, [128, 512]) for i in range(2)]
            psc = [P("psc%d" % i, [128, 512]) for i in range(2)]
            pTi = P("pTi", [128, 512])
            iota16 = cst[:, C_IOTA16:C_IOTA16 + 16]

            woutv = I["w_out"].rearrange("(ct p) d -> p ct d", p=128)
            for q4 in range(4):
                S.dma("pool", wout[:, q4 * 4:(q4 + 1) * 4, :], woutv[:, q4 * 4:(q4 + 1) * 4, :], w=["wout"], stream="wout")
            wqv = I["w_query"].rearrange("(kc p) n -> p kc n", p=128)
            for q4 in range(4):
                S.dma("pool", wq[:, q4 * 2:(q4 + 1) * 2, :], wqv[:, q4 * 2:(q4 + 1) * 2, :], w=["wq"], stream="wq")
            S.dma("sp", skf[:], I["sub_keys"].rearrange("m k d -> k m d"), w=["skf"], stream="skf")
            for m in range(16):
                tr(pTi[:, (m % 4) * 128:(m % 4 + 1) * 128], skf[:, m, :], ident_f, ["skf", "cst"], ["pTi"])
                cp("dve", skT[:, m, :], pTi[:, (m % 4) * 128:(m % 4 + 1) * 128], ["pTi"], ["skT"])
            YCv = YCs.rearrange("(ct p) t -> p ct t", p=128)
            H2v = H2Ts.rearrange("(kc p) t -> p kc t", p=128)
            def tile_vars(i):
                return i // 16, i % 2, i * 128

            def front(i):
                b, par, tok0 = tile_vars(i)
                sck = "sc%d" % par
                if i % 16 == 0:
                    S.dma("sp", GT1[:], MODs[b:b + 1, 2048:3072].partition_broadcast(128), r=["MODs"], w=["GT1"], stream="d0")
                    S.dma("sp", G2[:], MODs[b:b + 1, 4096:5120].partition_broadcast(128), r=["MODs"], w=["G2"], stream="d1")
                    S.dma("sp", SH2[:], MODs[b:b + 1, 3072:4096].partition_broadcast(128), r=["MODs"], w=["SH2"], stream="d2")
                yk, xk, x1k, hk = "yct%d" % par, "xin%d" % par, "x1_%d" % par, "h2T%d" % par
                S.dma("sp", yct[par][:], YCv[:, :, tok0:tok0 + 128], r=["YCs"], w=[yk], stream=yk)
                S.dma("sp", xin[par][:], I["x"][tok0:tok0 + 128, :], w=[xk], stream=xk)
                yield
                for half in range(2):
                    for ct in range(16):
                        mm(pM[:, half * 512:(half + 1) * 512], yct[par][:, ct, :], wout[:, ct, half * 512:(half + 1) * 512],
                           ct == 0, ct == 15, [yk, "wout"], ["pM"])
                yield
                yield
                tt("dve", t1[:], pM[:, :], GT1[:], ALU.mult, ["pM", "GT1"], ["t1"])
                yield
                tt("pool", x1[par][:], t1[:], xin[par][:], ALU.add, ["t1", xk], [x1k])
                S.dma("sp", X1s[tok0:tok0 + 128, :], x1[par][:], r=[x1k], w=["X1s"], stream=x1k)
                yield
                act(junk[:], x1[par][:], AF.Square, [x1k], ["junk", "ss2"], accum_out=ss2[:, i:i + 1])
                yield
                col = ss2[:, i:i + 1]
                ts("dve", col, col, 1.0 / D, EPS, ALU.mult, ALU.add, ["ss2"], ["ss2"])
                yield
                act(col, col, AF.Sqrt, ["ss2"], ["ss2"])
                yield
                S.op("dve", lambda e: e.reciprocal(out=col, in_=col), r=["ss2"], w=["ss2"])
                stt(t1[:], x1[par][:], ss2[:, i:i + 1], G2[:], ALU.mult, ALU.mult, [x1k, "ss2", "G2"], ["t1"])
                yield
                tt("pool", hb2[:], t1[:], SH2[:], ALU.add, ["t1", "SH2"], ["hb2"])
                yield
                for kc in range(8):
                    tr(pT[:, kc * 128:(kc + 1) * 128], hb2[:, kc * 128:(kc + 1) * 128], ident_b, ["hb2", "cstb"], ["pT"])
                yield
                cp("act", h2T[par][:, :, :], pT[:, :].rearrange("p (k t) -> p k t", k=8), ["pT"], [hk])
                S.dma("sp", H2v[:, :, tok0:tok0 + 128], h2T[par][:, :, :], r=[hk], w=["H2Ts"], stream=hk)
                yield
                for m4 in range(4):
                    pp = m4 % 2
                    for mi in range(4):
                        m = m4 * 4 + mi
                        for kc in range(8):
                            mm(pq[pp][:, mi * 128:(mi + 1) * 128], wq[:, kc, m * 128:(m + 1) * 128], h2T[par][:, kc, :],
                               kc == 0, kc == 7, ["wq", hk], ["pq%d" % pp])
                    cp("act", qT[:, m4 * 4:(m4 + 1) * 4, :], pq[pp][:, :].rearrange("p (m t) -> p m t", m=4),
                       ["pq%d" % pp], ["qT%d" % m4])
                    yield
                for m4 in range(4):
                    pp = m4 % 2
                    for mi in range(4):
                        m = m4 * 4 + mi
                        mm(psc[pp][:, mi * 128:(mi + 1) * 128], qT[:, m, :], skT[:, m, :], True, True,
                           ["qT%d" % m4, "skT"], ["psc%d" % pp])
                    cp("act", scb[par][:, m4 * 4:(m4 + 1) * 4, :], psc[pp][:, :].rearrange("p (m k) -> p m k", m=4),
                       ["psc%d" % pp], [sck])
                    yield

            def back(i):
                b, par, tok0 = tile_vars(i)
                sck = "sc%d" % par
                for m in range(16):
                    S.op("dve", lambda e, m=m: e.max(out=v8[:, m, 0:8], in_=scb[par][:, m, :]), r=[sck], w=["v8a%d" % m])
                yield
                for m in range(16):
                    S.op("dve", lambda e, m=m: e.max_index(out=i8[:, m, 0:8], in_max=v8[:, m, 0:8], in_values=scb[par][:, m, :]),
                         r=[sck, "v8a%d" % m], w=["i8a%d" % m])
                    S.op("dve", lambda e, m=m: e.match_replace(out=sc2[:, m, :], in_to_replace=v8[:, m, 0:8],
                                                               in_values=scb[par][:, m, :], imm_value=-1e30),
                         r=[sck, "v8a%d" % m], w=["sc2_%d" % m])
                    if m % 4 == 3:
                        yield
                for m in range(16):
                    S.op("dve", lambda e, m=m: e.max(out=v8[:, m, 8:16], in_=sc2[:, m, :]), r=["sc2_%d" % m],
                         w=["v8b%d" % m])
                yield
                for m in range(16):
                    S.op("dve", lambda e, m=m: e.max_index(out=i8[:, m, 8:16], in_max=v8[:, m, 8:16],
                                                           in_values=sc2[:, m, :]), r=["sc2_%d" % m, "v8b%d" % m],
                         w=["i8b%d" % m])
                yield
                v8keys = ["v8a%d" % m for m in range(16)] + ["v8b%d" % m for m in range(16)]
                i8keys = ["i8a%d" % m for m in range(16)] + ["i8b%d" % m for m in range(16)]
                cp("dve", i8f[:], i8[:], i8keys, ["i8f"])
                v8v = v8[:, :, :].rearrange("p (h c) r -> p h c r", c=2)
                i8v = i8f[:, :, :].rearrange("p (h c) r -> p h c r", c=2)
                tt("dve", cand[:, :, :].rearrange("p h (r c) -> p h r c", c=16),
                   v8v[:, :, 0, :].unsqueeze(3).to_broadcast([128, 8, 16, 16]),
                   v8v[:, :, 1, :].unsqueeze(2).to_broadcast([128, 8, 16, 16]), ALU.add, v8keys, ["cand"])
                yield
                for h in range(8):
                    S.op("dve", lambda e, h=h: e.max(out=c8[:, h, 0:8], in_=cand[:, h, :]), r=["cand"], w=["c8a%d" % h])
                yield
                for h in range(8):
                    S.op("dve", lambda e, h=h: e.max_index(out=p8[:, h, 0:8], in_max=c8[:, h, 0:8], in_values=cand[:, h, :]),
                         r=["cand", "c8a%d" % h], w=["p8a%d" % h])
                    S.op("dve", lambda e, h=h: e.match_replace(out=cand2[:, h, :], in_to_replace=c8[:, h, 0:8],
                                                               in_values=cand[:, h, :], imm_value=-1e30),
                         r=["cand", "c8a%d" % h], w=["cand2_%d" % h])
                    if h % 4 == 3:
                        yield
                for h in range(8):
                    S.op("dve", lambda e, h=h: e.max(out=c8[:, h, 8:16], in_=cand2[:, h, :]), r=["cand2_%d" % h],
                         w=["c8b%d" % h])
                yield
                for h in range(8):
                    S.op("dve", lambda e, h=h: e.max_index(out=p8[:, h, 8:16], in_max=c8[:, h, 8:16],
                                                           in_values=cand2[:, h, :]), r=["cand2_%d" % h, "c8b%d" % h],
                         w=["p8b%d" % h])
                yield
                c8keys = ["c8a%d" % h for h in range(8)] + ["c8b%d" % h for h in range(8)]
                p8keys = ["p8a%d" % h for h in range(8)] + ["p8b%d" % h for h in range(8)]
                tt("dve", ge[:], c8[:], c8[:, :, 0:1].to_broadcast([128, 8, 16]), ALU.subtract, c8keys, ["ge"])
                yield
                act(ge[:], ge[:], AF.Exp, ["ge"], ["ge"])
                yield
                S.op("dve", lambda e: e.tensor_reduce(out=gs[:], in_=ge[:], axis=AX.X, op=ALU.add), r=["ge"], w=["gs"])
                S.op("dve", lambda e: e.reciprocal(out=gs[:], in_=gs[:]), r=["gs"], w=["gs"])
                tt("dve", gg[:], ge[:], gs[:, :].unsqueeze(2).to_broadcast([128, 8, 16]), ALU.mult, ["ge", "gs"], ["gg"])
                p8i = p8[:, :, :].rearrange("p h k -> p (h k)").bitcast(I32)
                S.op("dve", lambda e: e.tensor_single_scalar(out=ra_i[:], in_=p8i, scalar=4, op=ALU.logical_shift_right),
                     r=p8keys, w=["ra_i"])
                S.op("dve", lambda e: e.tensor_single_scalar(out=rb_i[:], in_=p8i, scalar=15, op=ALU.bitwise_and),
                     r=p8keys, w=["rb_i"])
                cp("dve", raf[:], ra_i[:], ["ra_i"], ["raf"])
                cp("dve", rbf[:], rb_i[:], ["rb_i"], ["rbf"])
                yield
                io3 = iota16.unsqueeze(1).to_broadcast([128, 128, 16])
                for (rf, rk, ci, ohh, ok, sel, sk_) in ((raf, "raf", 0, oh, "oh", isel, "isel"),
                                                        (rbf, "rbf", 1, oh2, "oh2", jsel, "jsel")):
                    eng = "dve" if ci == 0 else "pool"
                    tt("dve", ohh[:], rf[:, :].unsqueeze(2).to_broadcast([128, 128, 16]), io3, ALU.is_equal,
                       [rk, "cst"], [ok])
                    tt(eng, ohh[:, :, :].rearrange("p (h k) r -> p h k r", h=8),
                       ohh[:, :, :].rearrange("p (h k) r -> p h k r", h=8),
                       i8v[:, :, ci, :].unsqueeze(2).to_broadcast([128, 8, 16, 16]), ALU.mult, [ok, "i8f"], [ok])
                    S.op("dve", lambda e, sel=sel, ohh=ohh: e.tensor_reduce(out=sel[:], in_=ohh[:], axis=AX.X, op=ALU.add),
                         r=[ok], w=[sk_])
                    yield
                tr(pTi[:, 0:128], isel[:], ident_f, ["isel", "cst"], ["pTi"])
                tr(pTi[:, 128:256], jsel[:], ident_f, ["jsel", "cst"], ["pTi"])
                tr(pTi[:, 256:384], gg[:, :, :].rearrange("p h k -> p (h k)"), ident_f, ["gg", "cst"], ["pTi"])
                rk_ = "rstg%d" % par
                cp("act", rstg[par][:, :, :], pTi[:, 0:384].rearrange("p (a t) -> p a t", a=3), ["pTi"], [rk_])
                S.dma("sp", RTs[:, :, tok0:tok0 + 128], rstg[par][:, :, :], r=[rk_], w=["RTs"], stream=rk_)

            def interleave(gens):
                gens = [g_ for g_ in gens if g_ is not None]
                while gens:
                    for g_ in list(gens):
                        try:
                            next(g_)
                        except StopIteration:
                            gens.remove(g_)

            interleave([front(0)])
            for i in range(NT):
                interleave([front(i + 1) if i + 1 < NT else None, back(i)])
            S.flush()
        if stop_after == 4:
            return nc

        with ExitStack() as ph:
            A = lambda n, sh, dt=F32: ph.enter_context(nc.sbuf_tensor("M_" + n, sh, dt))
            P = lambda n, sh, dt=F32: ph.enter_context(nc.psum_tensor("M_" + n, sh, dt))
            TB = 256
            G0 = A("G0", [128, 64, TB], BF16)
            G1 = A("G1", [128, 64, TB], BF16)
            utb = [A("utb%d" % i, [128, 2, D], BF16) for i in range(4)]
            vtb = [A("vtb%d" % i, [128, 2, D], BF16) for i in range(4)]
            Pm = [A("Pm%d" % i, [128, 8, 64], BF16) for i in range(4)]
            Q0 = [A("Q0%d" % i, [128, 8, 128], BF16) for i in range(4)]
            Qm = [A("Qm%d" % i, [128, 8, 128], BF16) for i in range(4)]
            h2b = [A("h2b%d" % i, [128, 8, TB], BF16) for i in range(2)]
            rt = [A("rt%d" % i, [128, 3, TB], BF16) for i in range(2)]
            Ag = [A("Ag%d" % i, [128, TB], BF16) for i in range(2)]
            GA = [A("GA%d" % i, [128, TB], BF16) for i in range(2)]
            x1t = [A("x1t%d" % i, [128, D]) for i in range(2)]
            GT2 = A("GT2", [128, D]); nfg = A("nfg", [128, D])
            tm = [A("tm%d" % i, [128, D]) for i in range(2)]; x2 = A("x2", [128, D]); junkm = A("junkm", [128, D], BF16)
            ot = [A("ot%d" % i, [128, D]) for i in range(2)]
            ssf = A("ssf", [128, 32])
            pO = [P("pO%d" % i, [128, 1024]) for i in range(2)]
            pA = [P("pA%d" % i, [128, 512]) for i in range(2)]
            pG = [P("pG%d" % i, [128, 512]) for i in range(2)]
            iota_b = cstb[:, C_IOTA:C_IOTA + 128]
            io32 = iota_b.unsqueeze(1).to_broadcast([128, 32, 128])
            io8 = iota_b.unsqueeze(1).to_broadcast([128, 8, 128])
            H2v = H2Ts.rearrange("(kc p) t -> p kc t", p=128)
            UTv = UTb.rearrange("e p x -> p e x")
            Vv = Vb.rearrange("(e p) d -> p e d", p=128)
            S.dma("sp", nfg[:], I["norm_f_g"][0:1, :].partition_broadcast(128), w=["nfg"], stream="m0")
            Gh = [G0, G1]
            io64 = [iota_b[:, 64 * hf:64 * hf + 64].unsqueeze(1).to_broadcast([128, 8, 64]) for hf in range(2)]
            NBLK = T // TB
            cnts = {"g": 0, "s": 0}
            late = []

            def run_late():
                for f_ in late:
                    f_()
                del late[:]

            def build(blk, hf):
                bp = blk % 2
                tok = blk * TB
                hk, rk = "h2b%d" % bp, "rt%d" % bp
                if hf == 0:
                    S.dma("sp", h2b[bp][:], H2v[:, :, tok:tok + TB], r=["H2Ts"], w=[hk], stream=hk)
                    S.dma("sp", rt[bp][:], RTs[:, :, tok:tok + TB], r=["RTs"], w=[rk], stream=rk)
                    yield
                NG = TB // 8
                base = cnts["s"]
                cnts["s"] += NG

                def dve_part(k):
                    sp_ = (base + k) % 4
                    tsl = slice(k * 8, (k + 1) * 8)
                    bcn = lambda a_, n_: rt[bp][:, a_, tsl].unsqueeze(2).to_broadcast([128, 8, n_])
                    tt("dve", Pm[sp_][:], bcn(0, 64), io64[hf], ALU.is_equal, [rk, "cstb"], ["Pm%d" % sp_])
                    tt("dve", Q0[sp_][:], bcn(1, 128), io8, ALU.is_equal, [rk, "cstb"], ["Q0%d" % sp_])

                def pool_part(k):
                    sp_ = (base + k) % 4
                    tsl = slice(k * 8, (k + 1) * 8)
                    tt("pool", Qm[sp_][:], Q0[sp_][:], rt[bp][:, 2, tsl].unsqueeze(2).to_broadcast([128, 8, 128]),
                       ALU.mult, ["Q0%d" % sp_, rk], ["Qm%d" % sp_])

                def mm_part(k):
                    sp_ = (base + k) % 4
                    for t4 in range(2):
                        gp = cnts["g"] % 2
                        cnts["g"] += 1
                        for ti in range(4):
                            t = t4 * 4 + ti
                            mm(pG[gp][:, ti * 64:(ti + 1) * 64], Qm[sp_][:, t, :], Pm[sp_][:, t, :], True, True,
                               ["Qm%d" % sp_, "Pm%d" % sp_], ["pG%d" % gp])
                        t0 = k * 8 + t4 * 4
                        late.append(lambda gp=gp, t0=t0: cp(
                            "act", Gh[hf][:, :, t0:t0 + 4], pG[gp][:, 0:256].rearrange("p (t i) -> p i t", t=4),
                            ["pG%d" % gp], ["G%d" % hf]))

                dve_part(0)
                dve_part(1)
                dve_part(2)
                yield
                pool_part(0)
                pool_part(1)
                yield
                for k in range(NG):
                    mm_part(k)
                    if k + 3 < NG:
                        late.append(lambda k=k: dve_part(k + 3))
                    if k + 2 < NG:
                        late.append(lambda k=k: pool_part(k + 2))
                    yield

            def final(blk):
                tok = blk * TB
                b_ = tok // SEQ
                if tok % SEQ == 0:
                    S.dma("sp", GT2[:], MODs[b_:b_ + 1, 5120:6144].partition_broadcast(128), r=["MODs"], w=["GT2"],
                          stream="m1")
                for t2 in range(2):
                    tt("dve", tm[t2][:], pO[t2][:, :], GT2[:], ALU.mult, ["pO%d" % t2, "GT2"], ["tm%d" % t2])
                yield
                for t2 in range(2):
                    ti = blk * 2 + t2
                    tk0 = tok + t2 * 128
                    xk, ok_ = "x1t%d" % t2, "ot%d" % t2
                    col = ssf[:, ti % 32:ti % 32 + 1]
                    S.dma("sp", x1t[t2][:], X1s[tk0:tk0 + 128, :], r=["X1s"], w=[xk], stream=xk)
                    yield
                    tt("pool", x2[:], tm[t2][:], x1t[t2][:], ALU.add, ["tm%d" % t2, xk], ["x2"])
                    yield
                    act(junkm[:], x2[:], AF.Square, ["x2"], ["junkm", "ssf"], accum_out=col)
                    yield
                    ts("dve", col, col, 1.0 / D, EPS, ALU.mult, ALU.add, ["ssf"], ["ssf"])
                    yield
                    act(col, col, AF.Sqrt, ["ssf"], ["ssf"])
                    yield
                    S.op("dve", lambda e, col=col: e.reciprocal(out=col, in_=col), r=["ssf"], w=["ssf"])
                    stt(ot[t2][:], x2[:], col, nfg[:], ALU.mult, ALU.mult, ["x2", "ssf", "nfg"], [ok_])
                    yield
                    S.dma("sp", out[tk0:tk0 + 128, :], ot[t2][:], r=[ok_], w=["out"], stream=ok_)
                    yield

            def prefetch(gg):
                if gg >= NBLK * 64:
                    return
                up = gg % 4
                i0 = (gg % 64) * 2
                S.dma("sp", utb[up][:], UTv[:, i0:i0 + 2, :], r=["UTb"], w=["utb%d" % up], stream="utb%d" % up)
                S.dma("sp", vtb[up][:], Vv[:, i0:i0 + 2, :], r=["Vb"], w=["vtb%d" % up], stream="vtb%d" % up)

            def emitA(blk, i):
                bp = blk % 2
                gg = blk * 64 + i // 2
                up = gg % 4
                if gg == 0 and i == 0:
                    prefetch(0)
                    prefetch(1)
                    prefetch(2)
                ap_ = i % 2
                for kc in range(8):
                    mm(pA[ap_][:, 0:256], utb[up][:, i % 2, kc * 128:(kc + 1) * 128], h2b[bp][:, kc, :],
                       kc == 0, kc == 7, ["utb%d" % up, "h2b%d" % bp], ["pA%d" % ap_])

            def emitG(blk, i):
                ap_ = i % 2
                hf = i // 64
                act(Ag[ap_][:], pA[ap_][:, 0:256], AF.Gelu, ["pA%d" % ap_], ["Ag%d" % ap_])
                tt("dve", GA[ap_][:], Ag[ap_][:], Gh[hf][:, i % 64, :], ALU.mult, ["Ag%d" % ap_, "G%d" % hf], ["GA%d" % ap_])

            def emitVm(blk, i):
                gg = blk * 64 + i // 2
                up = gg % 4
                vk = "vtb%d" % up
                ap_ = i % 2
                for t2 in range(2):
                    for half in range(2):
                        mm(pO[t2][:, half * 512:(half + 1) * 512], GA[ap_][:, t2 * 128:(t2 + 1) * 128],
                           vtb[up][:, i % 2, half * 512:(half + 1) * 512], i == 0, i == 127,
                           ["GA%d" % ap_, vk], ["pO%d" % t2])
                if i % 2 == 0:
                    prefetch(gg + 3)

            def step(gens, skip=None):
                for g_ in list(gens):
                    if g_ is skip:
                        continue
                    try:
                        next(g_)
                    except StopIteration:
                        gens.remove(g_)

            for _ in build(0, 0):
                run_late()
            run_late()
            side = []
            for blk in range(NBLK):
                side.append(build(blk, 1))
                emitA(blk, 0)
                emitA(blk, 1)
                emitG(blk, 0)
                bld = side[-1]
                for i in range(128):
                    if i == 63:
                        for _ in bld:
                            run_late()
                        run_late()
                    if i == 64 and blk + 1 < NBLK:
                        bld = build(blk + 1, 0)
                        side.append(bld)
                    ip = i % 64
                    if ((ip + 1) * 37) // 64 > (ip * 37) // 64:
                        step(side)
                    else:
                        step(side, skip=bld)
                    if i + 2 < 128:
                        emitA(blk, i + 2)
                    if i + 1 < 128:
                        emitG(blk, i + 1)
                    run_late()
                    emitVm(blk, i)
                for _ in bld:
                    run_late()
                run_late()
                fg = final(blk)
                next(fg)
                side.append(fg)
            while side:
                step(side)
                run_late()
            S.flush()
        return nc


def prep_inputs(inputs):
    sq = lambda a: np.ascontiguousarray(a[0]) if a.shape[0] == 1 and a.ndim >= 2 else np.ascontiguousarray(a)
    shared = {}
    for n, sh in IN_SPECS:
        if n in ("x", "c", "consts"):
            continue
        a = np.asarray(inputs[n], dtype=np.float32)
        shared[n] = np.ascontiguousarray(a.reshape(sh))
    shared["consts"] = make_consts()
    x = np.asarray(inputs["x"], dtype=np.float32)
    c = np.asarray(inputs["c"], dtype=np.float32)
    maps = []
    for i in range(NCORES):
        m = dict(shared)
        m["x"] = np.ascontiguousarray(x[i * NB:(i + 1) * NB].reshape(T, D))
        m["c"] = np.ascontiguousarray(c[i * NB:(i + 1) * NB])
        maps.append(m)
    return maps


def kernel(**inputs):
    nc = build()
    maps = prep_inputs(inputs)
    res = run_bass_kernel_spmd(nc, maps, core_ids=list(range(NCORES)))
    outs = [np.asarray(r["out"]).reshape(NB, SEQ, D) for r in res.results]
    return np.concatenate(outs, axis=0).astype(np.float32)
```
